# Optimizing a Trainium2 kernel written in Bass

```python
import math
import jax, jax.numpy as jnp
from jax import lax
import numpy as np

D_MODEL = 1024
BATCH = 2
SEQ = 8192
DEPTH = 1

NSA_HEADS = 8
NSA_KV_GROUPS = 2
NSA_HPG = NSA_HEADS // NSA_KV_GROUPS
HEAD_DIM = 64
NSA_WIDTH = NSA_HEADS * HEAD_DIM
KV_WIDTH = NSA_KV_GROUPS * HEAD_DIM
CMP_BLOCK = 32
CMP_STRIDE = 16
CMP_HIDDEN = 128
SEL_BLOCK = 64
N_SEL = 16
WINDOW = 512
Q_BLOCK = 128
NSA_N_BRANCH = 3
ROPE_THETA = 500000.0
ROT_DIM = HEAD_DIM // 4
S5_WIDTH = 512
S5_GROUP_DIM = 16
S5_GROUPS = S5_WIDTH // S5_GROUP_DIM
S5_STATE = 64
N_MEM = 256
MEM_HEADS = 4
MEM_HEAD_DIM = 128
MEM_WIDTH = MEM_HEADS * MEM_HEAD_DIM
N_BRANCH = 3
N_EXPERTS = 32
TOP_K = 4
D_FF = 1024
SWIGLU_LIMIT = 7.0
SWIGLU_ALPHA = 1.702
MOE_BLOCK = 256
LN_EPS = 1e-5
DEEPNORM_ALPHA = (2 * DEPTH) ** 0.25
DEEPNORM_BETA = (8 * DEPTH) ** -0.25

SPLIT_WIDTHS = (NSA_WIDTH, KV_WIDTH, KV_WIDTH, KV_WIDTH, KV_WIDTH, KV_WIDTH, KV_WIDTH,
                NSA_HEADS * NSA_N_BRANCH, S5_WIDTH, MEM_WIDTH, N_BRANCH * D_MODEL)
IN_WIDTH = sum(SPLIT_WIDTHS)

kernel_name = 'hybrid_nsa_s5_memory_moe_deepnorm'

F32 = jnp.float32


def _split_points():
    pts, acc = [], 0
    for w in SPLIT_WIDTHS[:-1]:
        acc += w
        pts.append(acc)
    return pts


def _layer_norm(x, g, b):
    xf = x.astype(F32)
    mu = jnp.mean(xf, axis=-1, keepdims=True)
    var = jnp.mean(jnp.square(xf - mu), axis=-1, keepdims=True)
    return ((xf - mu) * lax.rsqrt(var + LN_EPS) * g + b).astype(x.dtype)


def _masked_softmax(s, mask):
    s = jnp.where(mask, s.astype(F32), -jnp.inf)
    m = jnp.max(s, axis=-1, keepdims=True)
    m = jnp.where(jnp.isfinite(m), m, 0.0)
    e = jnp.exp(s - m)
    return e / jnp.maximum(jnp.sum(e, axis=-1, keepdims=True), jnp.finfo(F32).tiny)


def _rope_tables(positions):
    inv = ROPE_THETA ** (-jnp.arange(0, ROT_DIM, 2, dtype=F32) / ROT_DIM)
    ang = positions.astype(F32)[..., None] * inv
    return jnp.cos(ang)[:, :, None, :], jnp.sin(ang)[:, :, None, :]


def _apply_rope(t, cos, sin):
    half = ROT_DIM // 2
    t1 = t[..., :half].astype(F32)
    t2 = t[..., half:ROT_DIM].astype(F32)
    rot = jnp.concatenate([t1 * cos - t2 * sin, t2 * cos + t1 * sin], axis=-1).astype(t.dtype)
    return jnp.concatenate([rot, t[..., ROT_DIM:]], axis=-1)


def _compress(tok, pe, w1, w2):
    B_, L, G, HD = tok.shape
    ch = tok.reshape(B_, L // CMP_STRIDE, CMP_STRIDE, G, HD)
    blk = jnp.concatenate([ch[:, :-1], ch[:, 1:]], axis=2)
    blk = blk + pe[None, None, :, None, :]
    hid = jax.nn.gelu(jnp.einsum('bcsgd,sdf->bcgf', blk, w1))
    return jnp.einsum('bcgf,fd->bcgd', hid, w2)


def _nsa(q, kc, vc, ks, vs, kw, vw, gates, pe_k, pe_v, wk1, wk2, wv1, wv2):
    B_, L = q.shape[0], q.shape[1]
    G, HPG, HD = NSA_KV_GROUPS, NSA_HPG, HEAD_DIM
    dt = q.dtype
    ck = _compress(kc, pe_k, wk1, wk2)
    cv = _compress(vc, pe_v, wv1, wv2)
    n_cmp = ck.shape[1]
    n_sb = L // SEL_BLOCK
    n_sel = min(N_SEL, n_sb)
    ksb = ks.reshape(B_, n_sb, SEL_BLOCK, G, HD).transpose(0, 3, 1, 2, 4)
    vsb = vs.reshape(B_, n_sb, SEL_BLOCK, G, HD).transpose(0, 3, 1, 2, 4)
    kw_pad = jnp.pad(kw, ((0, 0), (WINDOW, 0), (0, 0), (0, 0)))
    vw_pad = jnp.pad(vw, ((0, 0), (WINDOW, 0), (0, 0), (0, 0)))
    b_ix = jnp.arange(B_)[:, None, None, None]
    g_ix = jnp.arange(G)[None, None, :, None]
    c_end = jnp.arange(n_cmp) * CMP_STRIDE + CMP_BLOCK - 1
    jb = jnp.arange(n_sb)
    scale = HEAD_DIM ** -0.5

    def block(qi):
        q0 = qi * Q_BLOCK
        t = q0 + jnp.arange(Q_BLOCK)
        qb = lax.dynamic_slice_in_dim(q, q0, Q_BLOCK, axis=1).reshape(B_, Q_BLOCK, G, HPG, HD)
        gb = lax.dynamic_slice_in_dim(gates, q0, Q_BLOCK, axis=1).reshape(B_, Q_BLOCK, G, HPG, NSA_N_BRANCH)
        s = jnp.einsum('bqghd,bcgd->bqghc', qb, ck) * scale
        p_cmp = _masked_softmax(s, (c_end[None, :] <= t[:, None])[None, :, None, None, :])
        o_cmp = jnp.einsum('bqghc,bcgd->bqghd', p_cmp.astype(dt), cv)
        imp = jnp.sum(p_cmp, axis=3)
        chunk = (jnp.pad(imp, ((0, 0), (0, 0), (0, 0), (0, 1)))
                 + jnp.pad(imp, ((0, 0), (0, 0), (0, 0), (1, 0))))
        score = jnp.sum(chunk.reshape(B_, Q_BLOCK, G, n_sb, SEL_BLOCK // CMP_STRIDE), axis=-1)
        tb = (t // SEL_BLOCK)[None, :, None, None]
        forced = (jb == 0) | (jb == tb) | (jb == tb - 1)
        score = jnp.where(forced, jnp.inf, jnp.where(jb > tb, -jnp.inf, score))
        _, idx = lax.top_k(score, n_sel)
        k_g = ksb[b_ix, g_ix, idx]
        v_g = vsb[b_ix, g_ix, idx]
        kpos = idx[..., None] * SEL_BLOCK + jnp.arange(SEL_BLOCK)
        m_sel = (kpos <= t[None, :, None, None, None]).reshape(B_, Q_BLOCK, G, 1, n_sel * SEL_BLOCK)
        s = jnp.einsum('bqghd,bqgnsd->bqghns', qb, k_g).reshape(B_, Q_BLOCK, G, HPG, n_sel * SEL_BLOCK) * scale
        p = _masked_softmax(s, m_sel)
        o_sel = jnp.einsum('bqghk,bqgkd->bqghd', p.astype(dt),
                           v_g.reshape(B_, Q_BLOCK, G, n_sel * SEL_BLOCK, HD))
        kwb = lax.dynamic_slice_in_dim(kw_pad, q0, WINDOW + Q_BLOCK, axis=1)
        vwb = lax.dynamic_slice_in_dim(vw_pad, q0, WINDOW + Q_BLOCK, axis=1)
        spos = q0 - WINDOW + jnp.arange(WINDOW + Q_BLOCK)
        diff = t[:, None] - spos[None, :]
        m_win = (spos[None, :] >= 0) & (diff >= 0) & (diff < WINDOW)
        s = jnp.einsum('bqghd,bsgd->bqghs', qb, kwb) * scale
        p = _masked_softmax(s, m_win[None, :, None, None, :])
        o_win = jnp.einsum('bqghs,bsgd->bqghd', p.astype(dt), vwb)
        o = o_cmp * gb[..., 0:1] + o_sel * gb[..., 1:2] + o_win * gb[..., 2:3]
        return o.reshape(B_, Q_BLOCK, NSA_HEADS, HD)

    out = lax.map(block, jnp.arange(L // Q_BLOCK))
    return out.transpose(1, 0, 2, 3, 4).reshape(B_, L, NSA_WIDTH)


def _s5(u, a_re, a_im, log_dt, b_re, b_im, c_re, c_im, d_skip):
    B_, L, _ = u.shape
    uf = u.astype(F32).reshape(B_, L, S5_GROUPS, S5_GROUP_DIM)
    lr, li = a_re.astype(F32), a_im.astype(F32)
    step = jnp.exp(log_dt.astype(F32))[:, None]
    mag = jnp.exp(lr * step)
    ab_re, ab_im = mag * jnp.cos(li * step), mag * jnp.sin(li * step)
    den = lr * lr + li * li
    nr = ab_re - 1.0
    coef_re = (nr * lr + ab_im * li) / den
    coef_im = (ab_im * lr - nr * li) / den
    br, bi = b_re.astype(F32), b_im.astype(F32)
    bb_re = coef_re[..., None] * br - coef_im[..., None] * bi
    bb_im = coef_re[..., None] * bi + coef_im[..., None] * br
    bu_re = jnp.einsum('blgp,gnp->blgn', uf, bb_re)
    bu_im = jnp.einsum('blgp,gnp->blgn', uf, bb_im)
    ar = jnp.broadcast_to(ab_re, bu_re.shape)
    ai = jnp.broadcast_to(ab_im, bu_re.shape)

    def combine(e1, e2):
        a1r, a1i, b1r, b1i = e1
        a2r, a2i, b2r, b2i = e2
        return (a2r * a1r - a2i * a1i, a2r * a1i + a2i * a1r,
                a2r * b1r - a2i * b1i + b2r, a2r * b1i + a2i * b1r + b2i)

    _, _, sr, si = lax.associative_scan(combine, (ar, ai, bu_re, bu_im), axis=1)
    y = (jnp.einsum('blgn,gpn->blgp', sr, c_re.astype(F32))
         - jnp.einsum('blgn,gpn->blgp', si, c_im.astype(F32)))
    y = y.reshape(B_, L, S5_WIDTH) + d_skip.astype(F32) * u.astype(F32)
    return y.astype(u.dtype)


def _memory_attention(qm, mem, w_mem_kv):
    B_, L, _ = qm.shape
    kv = jnp.einsum('bmd,de->bme', mem, w_mem_kv).reshape(B_, mem.shape[1], 2, MEM_HEADS, MEM_HEAD_DIM)
    k, v = kv[:, :, 0], kv[:, :, 1]
    q = qm.reshape(B_, L, MEM_HEADS, MEM_HEAD_DIM)
    s = jnp.einsum('blhd,bmhd->blhm', q, k).astype(F32) * (MEM_HEAD_DIM ** -0.5)
    p = jax.nn.softmax(s, axis=-1).astype(qm.dtype)
    return jnp.einsum('blhm,bmhd->blhd', p, v).reshape(B_, L, MEM_WIDTH)


def _hybrid_mixer(h, mem, cos, sin, w_in, pe_k_cmp, pe_v_cmp, w_kcmp1, w_kcmp2, w_vcmp1, w_vcmp2,
                  s5_a_re, s5_a_im, s5_log_dt, s5_b_re, s5_b_im, s5_c_re, s5_c_im, s5_d,
                  w_s5_glu, w_mem_kv, w_nsa_out, w_mem_out, w_o):
    B_, L, _ = h.shape
    z = jnp.einsum('bld,de->ble', h, w_in)
    q, kc, vc, ks, vs, kw, vw, g_nsa, u, qm, g_mrg = jnp.split(z, _split_points(), axis=-1)
    G = NSA_KV_GROUPS
    q = _apply_rope(q.reshape(B_, L, NSA_HEADS, HEAD_DIM), cos, sin)
    kc = _apply_rope(kc.reshape(B_, L, G, HEAD_DIM), cos, sin)
    ks = _apply_rope(ks.reshape(B_, L, G, HEAD_DIM), cos, sin)
    kw = _apply_rope(kw.reshape(B_, L, G, HEAD_DIM), cos, sin)
    vc = vc.reshape(B_, L, G, HEAD_DIM)
    vs = vs.reshape(B_, L, G, HEAD_DIM)
    vw = vw.reshape(B_, L, G, HEAD_DIM)
    g_nsa = jax.nn.sigmoid(g_nsa.astype(F32)).astype(h.dtype).reshape(B_, L, NSA_HEADS, NSA_N_BRANCH)
    y_nsa = _nsa(q, kc, vc, ks, vs, kw, vw, g_nsa, pe_k_cmp, pe_v_cmp,
                 w_kcmp1, w_kcmp2, w_vcmp1, w_vcmp2) @ w_nsa_out
    y = _s5(u, s5_a_re, s5_a_im, s5_log_dt, s5_b_re, s5_b_im, s5_c_re, s5_c_im, s5_d)
    ga, gb = jnp.split(jax.nn.gelu(y) @ w_s5_glu, 2, axis=-1)
    y_s5 = ga * jax.nn.sigmoid(gb)
    y_mem = _memory_attention(qm, mem, w_mem_kv) @ w_mem_out
    g = jax.nn.sigmoid(g_mrg.astype(F32)).astype(h.dtype).reshape(B_, L, N_BRANCH, D_MODEL)
    merged = g[:, :, 0] * y_nsa + g[:, :, 1] * y_s5 + g[:, :, 2] * y_mem
    return merged @ w_o


def _moe(h, w_router, b_router, w_gate_up, b_gate_up, w_down, b_down):
    B_, L, D = h.shape
    T = B_ * L
    xt = h.reshape(T, D)
    logits = (xt @ w_router + b_router).astype(F32)
    top_val, top_idx = lax.top_k(logits, TOP_K)
    gate = jax.nn.softmax(top_val, axis=-1)
    e_flat = top_idx.reshape(-1)
    tok_flat = jnp.arange(T * TOP_K, dtype=jnp.int32) // TOP_K
    w_flat = gate.reshape(-1)
    order = jnp.argsort(e_flat)
    e_s, tok_s, w_s = e_flat[order], tok_flat[order], w_flat[order]
    counts = jnp.bincount(e_flat, length=N_EXPERTS)
    padded = (counts + MOE_BLOCK - 1) // MOE_BLOCK * MOE_BLOCK
    start = jnp.cumsum(counts) - counts
    pend = jnp.cumsum(padded)
    pstart = pend - padded
    dest = pstart[e_s] + (jnp.arange(T * TOP_K, dtype=jnp.int32) - start[e_s])
    cap = (T * TOP_K + MOE_BLOCK - 1) // MOE_BLOCK * MOE_BLOCK + N_EXPERTS * MOE_BLOCK
    n_blk = cap // MOE_BLOCK
    slot_tok = jnp.full((cap,), T, jnp.int32).at[dest].set(tok_s)
    slot_w = jnp.zeros((cap,), F32).at[dest].set(w_s)
    blk_expert = jnp.minimum(jnp.searchsorted(pend, jnp.arange(n_blk) * MOE_BLOCK, side='right'),
                             N_EXPERTS - 1)
    x_pad = jnp.concatenate([xt, jnp.zeros((1, D), xt.dtype)], axis=0)
    xs = x_pad[slot_tok].reshape(n_blk, MOE_BLOCK, D)

    def expert_block(args):
        xb, e = args
        gu = xb @ w_gate_up[e] + b_gate_up[e]
        g, lin = jnp.split(gu, 2, axis=-1)
        g = jnp.minimum(g, SWIGLU_LIMIT)
        lin = jnp.clip(lin, -SWIGLU_LIMIT, SWIGLU_LIMIT)
        act = g * jax.nn.sigmoid(SWIGLU_ALPHA * g) * (lin + 1.0)
        return act @ w_down[e] + b_down[e]

    ys = lax.map(expert_block, (xs, blk_expert)).reshape(cap, D)
    ys = ys * slot_w[:, None].astype(ys.dtype)
    out = jnp.zeros((T + 1, D), ys.dtype).at[slot_tok].add(ys)[:T]
    return out.reshape(B_, L, D)


def setup_inputs(seed: int = 0) -> dict:
    key = jax.random.key(seed)
    ks = jax.random.split(key, 40)
    beta = DEEPNORM_BETA

    def nrm(k, shape, scale):
        return jax.random.normal(k, shape, F32) * scale

    x = nrm(ks[0], (BATCH, SEQ, D_MODEL), 1.0)
    mem = nrm(ks[1], (BATCH, N_MEM, D_MODEL), 1.0)
    positions = (jnp.arange(SEQ, dtype=jnp.int32)[None, :]
                 + jax.random.randint(ks[2], (BATCH, 1), 0, 1024, dtype=jnp.int32))
    ones = lambda n: jnp.ones((n,), F32)
    betas = lambda n: jnp.full((n,), beta, F32)
    col_scale = jnp.concatenate([
        ones(NSA_WIDTH),
        ones(KV_WIDTH), betas(KV_WIDTH),
        ones(KV_WIDTH), betas(KV_WIDTH),
        ones(KV_WIDTH), betas(KV_WIDTH),
        ones(NSA_HEADS * NSA_N_BRANCH),
        betas(S5_WIDTH),
        ones(MEM_WIDTH),
        ones(N_BRANCH * D_MODEL)])
    w_in = nrm(ks[3], (DEPTH, D_MODEL, IN_WIDTH), D_MODEL ** -0.5) * col_scale
    s5_a_re = -0.5 * jnp.exp(nrm(ks[14], (DEPTH, S5_GROUPS, S5_STATE), 0.05))
    s5_a_im = jnp.broadcast_to(jnp.pi * jnp.arange(S5_STATE, dtype=F32), (DEPTH, S5_GROUPS, S5_STATE))
    mem_kv_scale = jnp.concatenate([ones(MEM_WIDTH), betas(MEM_WIDTH)])
    return {
        'x': x,
        'mem': mem,
        'positions': positions,
        'ln_emb_g': 1.0 + nrm(ks[4], (D_MODEL,), 0.02),
        'ln_emb_b': nrm(ks[5], (D_MODEL,), 0.02),
        'w_in': w_in,
        'pe_k_cmp': nrm(ks[6], (DEPTH, CMP_BLOCK, HEAD_DIM), 0.1),
        'pe_v_cmp': nrm(ks[7], (DEPTH, CMP_BLOCK, HEAD_DIM), 0.1),
        'w_kcmp1': nrm(ks[8], (DEPTH, CMP_BLOCK, HEAD_DIM, CMP_HIDDEN), (CMP_BLOCK * HEAD_DIM) ** -0.5),
        'w_kcmp2': nrm(ks[9], (DEPTH, CMP_HIDDEN, HEAD_DIM), CMP_HIDDEN ** -0.5),
        'w_vcmp1': nrm(ks[10], (DEPTH, CMP_BLOCK, HEAD_DIM, CMP_HIDDEN), (CMP_BLOCK * HEAD_DIM) ** -0.5),
        'w_vcmp2': nrm(ks[11], (DEPTH, CMP_HIDDEN, HEAD_DIM), CMP_HIDDEN ** -0.5),
        's5_a_re': s5_a_re,
        's5_a_im': s5_a_im,
        's5_log_dt': jax.random.uniform(ks[12], (DEPTH, S5_GROUPS), F32, math.log(1e-3), math.log(1e-1)),
        's5_b_re': nrm(ks[13], (DEPTH, S5_GROUPS, S5_STATE, S5_GROUP_DIM), (2 * S5_GROUP_DIM) ** -0.5),
        's5_b_im': nrm(ks[15], (DEPTH, S5_GROUPS, S5_STATE, S5_GROUP_DIM), (2 * S5_GROUP_DIM) ** -0.5),
        's5_c_re': nrm(ks[16], (DEPTH, S5_GROUPS, S5_GROUP_DIM, S5_STATE), (2 * S5_STATE) ** -0.5),
        's5_c_im': nrm(ks[17], (DEPTH, S5_GROUPS, S5_GROUP_DIM, S5_STATE), (2 * S5_STATE) ** -0.5),
        's5_d': nrm(ks[18], (DEPTH, S5_WIDTH), 1.0),
        'w_s5_glu': nrm(ks[19], (DEPTH, S5_WIDTH, 2 * D_MODEL), S5_WIDTH ** -0.5 * beta),
        'w_mem_kv': nrm(ks[20], (DEPTH, D_MODEL, 2 * MEM_WIDTH), D_MODEL ** -0.5) * mem_kv_scale,
        'w_nsa_out': nrm(ks[21], (DEPTH, NSA_WIDTH, D_MODEL), NSA_WIDTH ** -0.5 * beta),
        'w_mem_out': nrm(ks[22], (DEPTH, MEM_WIDTH, D_MODEL), MEM_WIDTH ** -0.5 * beta),
        'w_o': nrm(ks[23], (DEPTH, D_MODEL, D_MODEL), D_MODEL ** -0.5 * beta),
        'ln1_g': 1.0 + nrm(ks[24], (DEPTH, D_MODEL), 0.02),
        'ln1_b': nrm(ks[25], (DEPTH, D_MODEL), 0.02),
        'w_router': nrm(ks[26], (DEPTH, D_MODEL, N_EXPERTS), D_MODEL ** -0.5),
        'b_router': nrm(ks[27], (DEPTH, N_EXPERTS), 0.01),
        'w_gate_up': nrm(ks[28], (DEPTH, N_EXPERTS, D_MODEL, 2 * D_FF), D_MODEL ** -0.5 * beta),
        'b_gate_up': nrm(ks[29], (DEPTH, N_EXPERTS, 2 * D_FF), 0.02),
        'w_down': nrm(ks[30], (DEPTH, N_EXPERTS, D_FF, D_MODEL), D_FF ** -0.5 * beta),
        'b_down': nrm(ks[31], (DEPTH, N_EXPERTS, D_MODEL), 0.02),
        'ln2_g': 1.0 + nrm(ks[32], (DEPTH, D_MODEL), 0.02),
        'ln2_b': nrm(ks[33], (DEPTH, D_MODEL), 0.02),
    }


def reference(x, mem, positions, ln_emb_g, ln_emb_b, w_in, pe_k_cmp, pe_v_cmp, w_kcmp1, w_kcmp2,
              w_vcmp1, w_vcmp2, s5_a_re, s5_a_im, s5_log_dt, s5_b_re, s5_b_im, s5_c_re, s5_c_im,
              s5_d, w_s5_glu, w_mem_kv, w_nsa_out, w_mem_out, w_o, ln1_g, ln1_b, w_router, b_router,
              w_gate_up, b_gate_up, w_down, b_down, ln2_g, ln2_b):
    h = _layer_norm(x, ln_emb_g, ln_emb_b)
    cos, sin = _rope_tables(positions)
    for l in range(DEPTH):
        mix = _hybrid_mixer(h, mem, cos, sin, w_in[l], pe_k_cmp[l], pe_v_cmp[l], w_kcmp1[l], w_kcmp2[l],
                            w_vcmp1[l], w_vcmp2[l], s5_a_re[l], s5_a_im[l], s5_log_dt[l], s5_b_re[l],
                            s5_b_im[l], s5_c_re[l], s5_c_im[l], s5_d[l], w_s5_glu[l], w_mem_kv[l],
                            w_nsa_out[l], w_mem_out[l], w_o[l])
        h = _layer_norm(DEEPNORM_ALPHA * h + mix, ln1_g[l], ln1_b[l])
        ffn = _moe(h, w_router[l], b_router[l], w_gate_up[l], b_gate_up[l], w_down[l], b_down[l])
        h = _layer_norm(DEEPNORM_ALPHA * h + ffn, ln2_g[l], ln2_b[l])
    return h
```

```python
import contextlib
import math
import numpy as np
import ml_dtypes
import concourse.bass as bass
import concourse.mybir as mybir
from concourse.bass_utils import run_bass_kernel_spmd

F32 = mybir.dt.float32
BF16 = mybir.dt.bfloat16
I32 = mybir.dt.int32
ALU = mybir.AluOpType
AF = mybir.ActivationFunctionType
AX = mybir.AxisListType

EPOCH = 20000
NDMA_SLOTS = 6
NEG = -30000.0
LN_EPS = 1e-5
ALPHA = 2.0 ** 0.25
TWO_PI = 2.0 * math.pi
CW1 = 6.28125
CW2 = TWO_PI - 6.28125
PI_LO = 3.1415925


class _Eng:
    def __init__(self, fw, name, handle):
        self.fw = fw
        self.name = name
        self.h = handle
        self.sems = []
        self.epoch = -1
        self.count = 0
        self.pending = False
        self.seen = {}
        self.dma_slots = None
        self.dma_next = 0
        self.new_epoch()

    def new_epoch(self):
        self.epoch += 1
        self.count = 0
        s = self.fw.new_sem(f"{self.name}_e{self.epoch}")
        self.sems.append(s)
        self.fw.semobjs[(self.name, self.epoch)] = s

    def cur_key(self):
        return (self.name, self.epoch)


class FW:
    def __init__(self, nc, stack):
        self.nc = nc
        self.stack = stack
        self.semobjs = {}
        self.eng = {}
        for name, h in (("pe", nc.tensor), ("dve", nc.vector), ("act", nc.scalar),
                        ("pool", nc.gpsimd), ("sp", nc.sync)):
            self.eng[name] = _Eng(self, name, h)
        self.last_w = {}
        self.readers = {}
        self.ninstr = 0
        self.nwait = 0

    def new_sem(self, name):
        return self.stack.enter_context(self.nc.semaphore(name))

    def _wait(self, e, tok):
        semkey, val = tok
        if e.seen.get(semkey, 0) >= val:
            return
        if not semkey[0].endswith("_dma"):
            for (n2, ep2), v2 in e.seen.items():
                if n2 == semkey[0] and ep2 > semkey[1]:
                    return
        e.h.wait_ge(self.semobjs[semkey], val)
        self.nwait += 1
        e.seen[semkey] = val

    def _deps(self, reads, writes):
        deps = []
        for r in reads:
            t = self.last_w.get(r)
            if t is not None:
                deps.append(t)
        for w in writes:
            t = self.last_w.get(w)
            if t is not None:
                deps.append(t)
            deps.extend(self.readers.get(w, ()))
        return deps

    def _record(self, tok, reads, writes):
        for r in reads:
            lst = self.readers.setdefault(r, [])
            lst.append(tok)
            if len(lst) > 64:
                best = {}
                for sk, v in lst:
                    if best.get(sk, 0) < v:
                        best[sk] = v
                self.readers[r] = list(best.items())
        for w in writes:
            self.last_w[w] = tok
            self.readers[w] = []

    def op(self, engname, fn, reads=(), writes=(), signal=True):
        e = self.eng[engname]
        if e.count >= EPOCH and not e.pending:
            e.new_epoch()
        semkey = e.cur_key()
        for tok in self._deps(reads, writes):
            if engname == "pe" and tok[0][0] == "pe":
                continue
            self._wait(e, tok)
        ins = fn()
        self.ninstr += 1
        if signal:
            ins.then_inc(e.sems[e.epoch], 1)
            e.count += 1
            e.pending = False
            tok = (semkey, e.count)
        else:
            e.pending = True
            tok = (semkey, e.count + 1)
        self._record(tok, reads, writes)
        return ins

    def dma(self, qname, out, in_, reads=(), writes=(), **kw):
        e = self.eng[qname]
        if e.dma_slots is None:
            e.dma_slots = []
            for i in range(NDMA_SLOTS):
                key = (qname + "_dma", i)
                self.semobjs[key] = self.new_sem(f"{qname}_d{i}")
                e.dma_slots.append([key, 0])
        slot = e.dma_slots[e.dma_next % NDMA_SLOTS]
        e.dma_next += 1
        key, uses = slot
        if uses > 0:
            self._wait(e, (key, 16 * uses))
        for tok in self._deps(reads, writes):
            self._wait(e, tok)
        ins = e.h.dma_start(out=out, in_=in_, **kw)
        ins.then_inc(self.semobjs[key], 16)
        self.ninstr += 1
        slot[1] = uses + 1
        tok = (key, 16 * (uses + 1))
        self._record(tok, reads, writes)
        return tok

    def barrier(self):
        toks = []
        for e in self.eng.values():
            assert not e.pending, e.name
            if e.count > 0:
                toks.append((e.cur_key(), e.count))
            if e.dma_slots:
                for key, uses in e.dma_slots:
                    if uses > 0:
                        toks.append((key, 16 * uses))
        for e in self.eng.values():
            for t in toks:
                self._wait(e, t)
        self.last_w = {}
        self.readers = {}

    def finish(self, qname="sp"):
        e = self.eng[qname]
        for k, tok in list(self.last_w.items()):
            self._wait(e, tok)


C_Q, C_KC, C_VC, C_KS, C_VS, C_KW, C_VW, C_GN, C_U, C_QM, C_GM = 0, 512, 640, 768, 896, 1024, 1152, 1280, 1304, 1816, 2328
NT = 64
OWN0 = 48


def build_program(upto=99, dbg=()):
    nc = bass.Bass("TRN2", target_bir_lowering=False)
    V, A, G, T = nc.vector, nc.scalar, nc.gpsimd, nc.tensor

    def din(name, shape, dt=F32):
        return nc.dram_tensor(name, list(shape), dt, kind="ExternalInput").ap()

    def dscr(name, shape, dt):
        return nc.dram_tensor(name, list(shape), dt, kind="Internal").ap()

    dbg_out = {}

    def ddbg(name, shape, dt):
        dbg_out[name] = nc.dram_tensor(name, list(shape), dt, kind="ExternalOutput").ap()
        return dbg_out[name]

    xr = din("xr", [8192, 1024])
    posr = din("posr", [1, 8192], I32)
    vrow_d = din("vrow", [1, 8192])
    vcol_d = din("vcol", [128, 64])
    ln_emb = din("ln_emb", [2, 1024])
    w_in = din("w_in", [1024, 5400])
    invf_d = din("invf", [128, 1])
    identf_d = din("identf", [128, 128])
    w_kcmp1 = din("w_kcmp1", [32, 64, 128]); w_kcmp2 = din("w_kcmp2", [128, 64])
    w_vcmp1 = din("w_vcmp1", [32, 64, 128]); w_vcmp2 = din("w_vcmp2", [128, 64])
    pe_k = din("pe_k", [32, 64]); pe_v = din("pe_v", [32, 64])
    vcc_d = din("vcc", [128, 4])
    s5_a_re = din("s5_a_re", [32, 64]); s5_a_im = din("s5_a_im", [32, 64]); s5_log_dt = din("s5_log_dt", [1, 32])
    s5_b_re = din("s5_b_re", [32, 64, 16]); s5_b_im = din("s5_b_im", [32, 64, 16])
    s5_c_re = din("s5_c_re", [32, 16, 64]); s5_c_im = din("s5_c_im", [32, 16, 64]); s5_d = din("s5_d", [512])
    sgn_d = din("sgn", [128, 1]); psw_d = din("psw", [128, 128])
    mem_d = din("mem", [256, 1024]); w_mem_kv = din("w_mem_kv", [1024, 1024])
    w_nsa_out = din("w_nsa_out", [512, 1024]); w_mem_out = din("w_mem_out", [512, 1024])
    w_s5_glu = din("w_s5_glu", [512, 2048]); w_o = din("w_o", [1024, 1024]); ln1_d = din("ln1", [2, 1024])
    epat_d = din("epat", [64, 8192]); tri_d = din("tri", [128, 512]); atri_d = din("atri", [128, 512])
    cba_d = din("cba", [16, 128, 512]); cbb_d = din("cbb", [16, 2, 128, 512])
    addb_d = din("addb", [16, 128, 128]); futb_d = din("futb", [16, 128, 128])
    h1scr = dscr("h1scr", [2048, 1024], F32)
    wscr = dscr("wscr", [60, 128, 1024], BF16)
    wqscr = dscr("wqscr", [3, 128, 4096], BF16)
    wgnscr = dscr("wgnscr", [128, 192], BF16)
    ln2_d = din("ln2", [2, 1024]); w_router = din("w_router", [1024, 32]); b_router = din("b_router", [1, 32])
    w_gate_up = din("w_gate_up", [32, 1024, 2048]); b_gate_up = din("b_gate_up", [512, 128])
    w_down = din("w_down", [32, 1024, 1024]); b_down = din("b_down", [32, 1024])
    out_d = nc.dram_tensor("out", [2048, 1024], F32, kind="ExternalOutput").ap()

    uscr = dscr("uscr", [4, 128, 8192], BF16)

    with contextlib.ExitStack() as S0:
        fw = FW(nc, S0)

        _names = {}

        def _uniq(n):
            c = _names.get(n, 0)
            _names[n] = c + 1
            return n if c == 0 else f"{n}__{c}"

        def sb(st, name, shape, dt):
            return st.enter_context(nc.sbuf_tensor(_uniq("s_" + name), list(shape), dt))

        def ps(st, name, shape, dt=F32):
            return st.enter_context(nc.psum_tensor(_uniq("p_" + name), list(shape), dt))

        def dve(fn, r, w):
            return fw.op("dve", fn, r, w)

        def act(fn, r, w):
            return fw.op("act", fn, r, w)

        def pool(fn, r, w):
            return fw.op("pool", fn, r, w)

        def mm(out, lhsT, rhs, start, stop, r, w):
            return fw.op("pe", lambda: T.matmul(out, lhsT=lhsT, rhs=rhs, start=start, stop=stop), r, w, signal=stop)

        def tr(out, in_, ident, r, w, signal=True):
            return fw.op("pe", lambda: T.transpose(out=out, in_=in_, identity=ident), r, w, signal=signal)

        identf = sb(S0, "identf", [128, 128], F32)
        identb = sb(S0, "identb", [128, 128], BF16)
        zerob = sb(S0, "zerob", [128, 128], BF16)
        invf = sb(S0, "invf", [128, 1], F32)
        vcol = sb(S0, "vcol", [128, 64], F32)
        gcol = sb(S0, "gcol", [128, 8], F32)
        bcol = sb(S0, "bcol", [128, 8], F32)
        fw.dma("sp", identf[:], identf_d[:, :], writes=["identf"])
        fw.dma("pool", identb[:], identf_d[:, :], writes=["identb"])
        fw.dma("sp", invf[:], invf_d[:, :], writes=["invf"])
        fw.op("pool", lambda: G.memset(zerob[:], 0.0), [], ["zerob"])
        fw.dma("sp", vcol[:], vcol_d[:, :], writes=["vcol"])
        fw.dma("sp", gcol[:], ln_emb[0, :].rearrange("(dc p) -> p dc", p=128), writes=["gcol"], allow_slow_non_contiguous=True)
        fw.dma("sp", bcol[:], ln_emb[1, :].rearrange("(dc p) -> p dc", p=128), writes=["bcol"], allow_slow_non_contiguous=True)

        SKV = S0.enter_context(contextlib.ExitStack())
        KA = [sb(SKV, f"KA{i}", [128, 8192], BF16) for i in range(2)]
        VsA = sb(SKV, "VsA", [128, NT, 2, 65], BF16)
        KwT = sb(SKV, "KwT", [128, 20 * 128], BF16)
        VwA = sb(SKV, "VwA", [128, 20, 2, 65], BF16)
        CkT = sb(SKV, "CkT", [128, 512], BF16)
        CvA = sb(SKV, "CvA", [128, 4, 2, 65], BF16)
        gyT = sb(SKV, "gyT", [128, 4, 2048], BF16)
        vcc = sb(SKV, "vcc", [128, 4], F32)
        fw.dma("sp", vcc[:], vcc_d[:, :], writes=["vcc"])
        for q4 in range(4):
            fw.dma("pool", KA[0][64:128, q4 * 2048:(q4 + 1) * 2048], epat_d[:, q4 * 2048:(q4 + 1) * 2048], writes=["KA0e"])
            fw.dma("pool", KA[1][0:64, q4 * 2048:(q4 + 1) * 2048], epat_d[:, q4 * 2048:(q4 + 1) * 2048], writes=["KA1e"])

        def layer_norm_T(st_keys, xt_ap, hT_dst, PT, xn, stt, mv, rs, tmpf, gexp, bexp, tag):
            kx = st_keys
            dve(lambda: V.bn_stats(out=stt[:, 0:6], in_=xt_ap[:, 0:512]), [kx], ["stt" + tag])
            dve(lambda: V.bn_stats(out=stt[:, 6:12], in_=xt_ap[:, 512:1024]), [kx], ["stt" + tag])
            dve(lambda: V.bn_aggr(out=mv[:, 0:2], in_=stt[:, 0:12]), ["stt" + tag], ["mv" + tag])
            act(lambda: A.activation(out=rs[:], in_=mv[:, 1:2], func=AF.Sqrt, bias=LN_EPS, scale=1.0), ["mv" + tag], ["rs" + tag])
            dve(lambda: V.reciprocal(out=rs[:], in_=rs[:]), ["rs" + tag], ["rs" + tag])
            dve(lambda: V.tensor_scalar(out=xn[:], in0=xt_ap, scalar1=mv[:, 0:1], scalar2=rs[:, 0:1],
                                        op0=ALU.subtract, op1=ALU.mult), [kx, "mv" + tag, "rs" + tag], ["xn" + tag])
            for dc in range(8):
                tr(PT[:, dc * 128:(dc + 1) * 128], xn[:, dc * 128:(dc + 1) * 128], identb[:],
                   ["xn" + tag, "identb"], ["PT" + tag], signal=(dc == 7))
            dve(lambda: V.tensor_tensor(out=tmpf[:], in0=PT[:], in1=gexp[:], op=ALU.mult), ["PT" + tag, "gexp"], ["tmpf"])
            return tmpf

        with contextlib.ExitStack() as S1:
            kcT = sb(S1, "kcT", [128, 8192], BF16)
            vcT = sb(S1, "vcT", [128, 8192], BF16)
            S1t = S1.enter_context(contextlib.ExitStack())
            Wk = sb(S1t, "Wk", [128, 8, 1280], BF16)
            Wsw = sb(S1t, "Wsw", [128, 8, 3, 128], BF16)
            wv = w_in.rearrange("(dc p) c -> p dc c", p=128)
            for i, (c0, n) in enumerate(((C_KC, 128), (C_KS, 128), (C_KW, 128), (C_VC, 128), (C_VS, 128), (C_VW, 128), (C_U, 512))):
                dst0 = i * 128
                for dc in range(0, 8, 4):
                    fw.dma("pool", Wk[:, dc:dc + 4, dst0:dst0 + n], wv[:, dc:dc + 4, c0:c0 + n], writes=["Wk"])
            pool(lambda: G.memset(Wsw[:], 0.0), [], ["Wsw"])
            for i in range(3):
                for g in range(2):
                    b0 = i * 128 + g * 64
                    dve(lambda: V.tensor_scalar(out=Wsw[:, :, i, g * 64:g * 64 + 8], in0=Wk[:, :, b0 + 8:b0 + 16], scalar1=-1.0,
                                                scalar2=None, op0=ALU.mult), ["Wk", "Wsw"], ["Wsw"])
                    dve(lambda: V.tensor_copy(out=Wsw[:, :, i, g * 64 + 8:g * 64 + 16], in_=Wk[:, :, b0:b0 + 8]), ["Wk", "Wsw"], ["Wsw"])
            gexp = sb(S1t, "gexp", [128, 1024], F32)
            bexp = sb(S1t, "bexp", [128, 1024], F32)
            pool(lambda: G.memset(gexp[:], 1.0), [], ["gexp"])
            pool(lambda: G.memset(bexp[:], 1.0), [], ["bexp"])
            for dc in range(8):
                dve(lambda: V.tensor_scalar(out=gexp[:, dc * 128:(dc + 1) * 128], in0=gexp[:, dc * 128:(dc + 1) * 128],
                                            scalar1=gcol[:, dc:dc + 1], scalar2=None, op0=ALU.mult), ["gexp", "gcol"], ["gexp"])
                dve(lambda: V.tensor_scalar(out=bexp[:, dc * 128:(dc + 1) * 128], in0=bexp[:, dc * 128:(dc + 1) * 128],
                                            scalar1=bcol[:, dc:dc + 1], scalar2=None, op0=ALU.mult), ["bexp", "bcol"], ["bexp"])
            xbuf = [sb(S1t, f"xb{i}", [128, 1024], F32) for i in range(2)]
            xn = [sb(S1t, f"xn{i}", [128, 1024], BF16) for i in range(2)]
            tmpf = [sb(S1t, f"tmpf{i}", [128, 1024], F32) for i in range(1)] * 2
            stt = [sb(S1t, f"stt{i}", [128, 12], F32) for i in range(2)]
            mv = [sb(S1t, f"mv{i}", [128, 2], F32) for i in range(2)]
            rs = [sb(S1t, f"rs{i}", [128, 1], F32) for i in range(2)]
            hTb = [sb(S1t, f"hTb{i}", [128, 8, 512], BF16) for i in range(2)]
            posi = sb(S1t, "posi", [128, 512], I32)
            posf = sb(S1t, "posf", [128, 512], F32)
            angs = sb(S1t, "angs", [128, 512], F32)
            ki = sb(S1t, "ki", [128, 512], I32)
            kf = sb(S1t, "kf", [128, 512], F32)
            r1 = sb(S1t, "r1", [128, 512], F32)
            cosT = [sb(S1t, f"cosT{i}", [128, 512], F32) for i in range(1)] * 2
            sinT = [sb(S1t, f"sinT{i}", [128, 512], F32) for i in range(1)] * 2
            vrow = [sb(S1t, f"vrow{i}", [128, 512], F32) for i in range(2)]
            t1 = [sb(S1t, f"t1_{i}", [128, 512], F32) for i in range(1)] * 2
            t2 = [sb(S1t, f"t2_{i}", [128, 512], F32) for i in range(1)] * 2
            ust = [sb(S1t, f"ust{i}", [128, 512], BF16) for i in range(2)]
            S1p = S1.enter_context(contextlib.ExitStack())
            PT = [ps(S1p, f"PT{i}", [128, 1024], BF16) for i in range(2)]
            PF = [ps(S1p, f"PF{i}", [128, 512], F32) for i in range(4)]
            PV = [ps(S1p, f"PV{i}", [128, 256], F32) for i in range(2)]
            pfi = [0]
            ropei = [0]

            def next_pf():
                i = pfi[0] % 4
                pfi[0] += 1
                return PF[i], f"PF{i}"

            for bi in range(16):
                p2 = bi % 2
                hT = hTb[p2]
                hk = f"hTb{p2}"
                fw.dma("sp", posi[:], posr[:, bi * 512:(bi + 1) * 512].partition_broadcast(128), writes=["posi"])
                fw.dma("sp", vrow[p2][:], vrow_d[:, bi * 512:(bi + 1) * 512].partition_broadcast(128), writes=[f"vrow{p2}"])
                dve(lambda: V.tensor_copy(out=posf[:], in_=posi[:]), ["posi"], ["posf"])
                for which, shift, dst, dk in (("s", 0.0, sinT[p2], "sinT"), ("c", math.pi / 2, cosT[p2], "cosT")):
                    dve(lambda: V.tensor_scalar(out=angs[:], in0=posf[:], scalar1=invf[:, 0:1], scalar2=shift, op0=ALU.mult, op1=ALU.add),
                        ["posf", "invf"], ["angs"])
                    dve(lambda: V.tensor_scalar(out=ki[:], in0=angs[:], scalar1=1.0 / TWO_PI, scalar2=None, op0=ALU.mult), ["angs"], ["ki"])
                    dve(lambda: V.tensor_copy(out=kf[:], in_=ki[:]), ["ki"], ["kf"])
                    dve(lambda: V.scalar_tensor_tensor(out=r1[:], in0=kf[:], scalar=-CW1, in1=angs[:], op0=ALU.mult, op1=ALU.add),
                        ["kf", "angs"], ["r1"])
                    dve(lambda: V.scalar_tensor_tensor(out=r1[:], in0=kf[:], scalar=-CW2, in1=r1[:], op0=ALU.mult, op1=ALU.add),
                        ["kf", "r1"], ["r1"])
                    dve(lambda: V.tensor_scalar(out=r1[:], in0=r1[:], scalar1=-PI_LO, scalar2=PI_LO, op0=ALU.max, op1=ALU.min), ["r1"], ["r1"])
                    act(lambda: A.activation(out=dst[:], in_=r1[:], func=AF.Sin), ["r1"], [dk])
                for tl in range(4):
                    Tt = bi * 4 + tl
                    q2 = Tt % 2
                    tag = str(q2)
                    fw.dma("sp", xbuf[q2][:], xr[Tt * 128:(Tt + 1) * 128, :], writes=[f"xb{q2}"])
                    tf = layer_norm_T(f"xb{q2}", xbuf[q2][:], None, PT[q2], xn[q2], stt[q2], mv[q2], rs[q2], tmpf[q2], gexp, bexp, tag)
                    pool(lambda: G.tensor_tensor(out=hT[:, :, tl * 128:(tl + 1) * 128], in0=tf[:].rearrange("p (a b) -> p a b", a=8),
                                                 in1=bexp[:].rearrange("p (a b) -> p a b", a=8), op=ALU.add),
                         ["tmpf", "bexp"], [hk])
                for i, dstT, dkey, col0 in ((0, kcT, "kcT", bi * 512), (1, None, "KA", bi * 512), (2, KwT, "KwT", (bi - 11) * 512)):
                    if i == 2 and bi < 11:
                        continue
                    pa, pak = next_pf()
                    for dc in range(8):
                        mm(pa[:], Wk[:, dc, i * 128:(i + 1) * 128], hT[:, dc, :], dc == 0, dc == 7, ["Wk", hk], [pak])
                    pb, pbk = next_pf()
                    for dc in range(8):
                        mm(pb[:], Wsw[:, dc, i, :], hT[:, dc, :], dc == 0, dc == 7, ["Wsw", hk], [pbk])
                    ri = ropei[0] % 2
                    ropei[0] += 1
                    dve(lambda: V.tensor_tensor(out=t1[ri][:], in0=pa[:], in1=cosT[p2][:], op=ALU.mult), [pak, "cosT"], ["t1_"])
                    dve(lambda: V.tensor_tensor(out=t2[ri][:], in0=pb[:], in1=sinT[p2][:], op=ALU.mult), [pbk, "sinT"], ["t2_"])
                    if dstT is None:
                        pool(lambda: G.tensor_tensor(out=KA[0][0:64, col0:col0 + 512], in0=t1[ri][0:64, :], in1=t2[ri][0:64, :], op=ALU.add),
                             ["t1_", "t2_"], ["KA0"])
                        pool(lambda: G.tensor_tensor(out=KA[1][64:128, col0:col0 + 512], in0=t1[ri][64:128, :], in1=t2[ri][64:128, :], op=ALU.add),
                             ["t1_", "t2_"], ["KA1"])
                    else:
                        pool(lambda: G.tensor_tensor(out=dstT[:, col0:col0 + 512], in0=t1[ri][:], in1=t2[ri][:], op=ALU.add),
                             ["t1_", "t2_"], [dkey])
                pa, pak = next_pf()
                for dc in range(8):
                    mm(pa[:], Wk[:, dc, 384:512], hT[:, dc, :], dc == 0, dc == 7, ["Wk", hk], [pak])
                act(lambda: A.copy(out=vcT[:, bi * 512:(bi + 1) * 512], in_=pa[:]), [pak], ["vcT"])
                for ct in range(4):
                    pa, pak = next_pf()
                    for dc in range(8):
                        mm(pa[:], Wk[:, dc, 768 + ct * 128:768 + (ct + 1) * 128], hT[:, dc, :], dc == 0, dc == 7, ["Wk", hk], [pak])
                    u2 = ct % 2
                    dve(lambda: V.tensor_tensor(out=ust[u2][:], in0=pa[:], in1=vrow[p2][:], op=ALU.mult), [pak, f"vrow{p2}"], [f"ust{u2}"])
                    fw.dma("sp", uscr[ct, :, bi * 512:(bi + 1) * 512], ust[u2][:], reads=[f"ust{u2}"], writes=["uscr"])
                for tl in range(4):
                    Tt = bi * 4 + tl
                    pv = PV[tl % 2]
                    pvk = f"PV{tl % 2}"
                    for dc in range(8):
                        mm(pv[:], hT[:, dc, tl * 128:(tl + 1) * 128], Wk[:, dc, 512:768], dc == 0, dc == 7, ["Wk", hk], [pvk])
                    act(lambda: A.activation(out=VsA[:, Tt, :, 0:64], in_=pv[:, 0:128].rearrange("p (g d) -> p g d", g=2),
                                             func=AF.Copy, scale=vcol[:, Tt:Tt + 1]), [pvk, "vcol"], ["VsA"])
                    if Tt >= 44:
                        act(lambda: A.activation(out=VwA[:, Tt - 44, :, 0:64], in_=pv[:, 128:256].rearrange("p (g d) -> p g d", g=2),
                                                 func=AF.Copy, scale=vcol[:, Tt:Tt + 1]), [pvk, "vcol"], ["VwA"])
            for g in range(2):
                dve(lambda: V.tensor_copy(out=VsA[:, :, g, 64], in_=vcol[:, :]), ["vcol", "VsA"], ["VsA"])
                dve(lambda: V.tensor_copy(out=VwA[:, :, g, 64], in_=vcol[:, 44:64]), ["vcol", "VwA"], ["VwA"])

            if "p1" in dbg:
                fw.dma("sp", ddbg("d_KA0", [128, 8192], BF16)[:, :], KA[0][:], reads=["KA0", "KA0e"], writes=["d_KA0"])
                fw.dma("sp", ddbg("d_KA1", [128, 8192], BF16)[:, :], KA[1][:], reads=["KA1", "KA1e"], writes=["d_KA1"])
                fw.dma("sp", ddbg("d_kcT", [128, 8192], BF16)[:, :], kcT[:], reads=["kcT"], writes=["d_kcT"])
                fw.dma("sp", ddbg("d_vcT", [128, 8192], BF16)[:, :], vcT[:], reads=["vcT"], writes=["d_vcT"])
                fw.dma("sp", ddbg("d_KwT", [128, 2560], BF16)[:, :], KwT[:], reads=["KwT"], writes=["d_KwT"])
                fw.dma("sp", ddbg("d_VsA", [128, NT * 130], BF16)[:, :], VsA[:].rearrange("p a b c -> p (a b c)"), reads=["VsA"], writes=["d_VsA"])
                fw.dma("sp", ddbg("d_VwA", [128, 20 * 130], BF16)[:, :], VwA[:].rearrange("p a b c -> p (a b c)"), reads=["VwA"], writes=["d_VwA"])
                fw.dma("sp", ddbg("d_u", [4, 128, 8192], BF16)[:, :, :], uscr[:, :, :], reads=["uscr"], writes=["d_u"])
            if upto <= 1:
                fw.finish("sp")
                print("instr", fw.ninstr, "waits", fw.nwait)
                return nc, dbg_out
            fw.barrier()
            S1p.close()
            S1t.close()

            with contextlib.ExitStack() as S2:
                W1 = {}
                for kind, wd in (("k", w_kcmp1), ("v", w_vcmp1)):
                    W1[kind] = sb(S2, "W1" + kind, [128, 32, 128], BF16)
                    src = wd.rearrange("s d f -> d s f")
                    for half in (0, 64):
                        for s0 in range(0, 32, 8):
                            fw.dma("pool", W1[kind][half:half + 64, s0:s0 + 8, :], src[:, s0:s0 + 8, :], writes=["W1" + kind])
                W2p = sb(S2, "W2p", [128, 2, 128], BF16)
                W2v = sb(S2, "W2v", [128, 64], BF16)
                pool(lambda: G.memset(W2p[:], 0.0), [], ["W2p"])
                for g in range(2):
                    fw.dma("pool", W2p[:, g, g * 64:(g + 1) * 64], w_kcmp2[:, :], writes=["W2p"])
                fw.dma("pool", W2v[:], w_vcmp2[:, :], writes=["W2v"])
                peT = {}
                constc = {}
                PSc = ps(S2, "PSc", [128, 8], F32)
                PSh = [ps(S2, f"PSh{i}", [128, 512], F32) for i in range(2)]
                PSck = ps(S2, "PSck", [128, 512], F32)
                PScv = [ps(S2, f"PScv{i}", [128, 64], F32) for i in range(2)]
                for kind, ped in (("k", pe_k), ("v", pe_v)):
                    peT[kind] = sb(S2, "peT" + kind, [64, 32], BF16)
                    fw.dma("pool", peT[kind][:], ped.rearrange("s d -> d s"), writes=["peT" + kind], allow_slow_non_contiguous=True)
                    constc[kind] = sb(S2, "constc" + kind, [128, 1], F32)
                    for s_ in range(32):
                        mm(PSc[:, 0:1], W1[kind][0:64, s_, :], peT[kind][:, s_:s_ + 1], s_ == 0, s_ == 31, ["W1" + kind, "peT" + kind], ["PSc"])
                    act(lambda: A.copy(out=constc[kind][:], in_=PSc[:, 0:1]), ["PSc"], ["constc" + kind])
                hid = {}
                hi = 0
                for kind, srcT, sk in (("k", kcT, "kcT"), ("v", vcT, "vcT")):
                    for g in range(2):
                        hd = sb(S2, f"hid{kind}{g}", [128, 512], BF16)
                        hid[(kind, g)] = hd
                        hk2 = f"hid{kind}{g}"
                        pool(lambda: G.memset(hd[:], 0.0), [], [hk2])
                        ph = PSh[hi % 2]
                        phk = f"PSh{hi % 2}"
                        hi += 1
                        for s_ in range(32):
                            mm(ph[:, 0:511], W1[kind][g * 64:(g + 1) * 64, s_, :], srcT[g * 64:(g + 1) * 64, s_:s_ + 16 * 510 + 1:16],
                               s_ == 0, s_ == 31, ["W1" + kind, sk], [phk])
                        act(lambda: A.activation(out=hd[:, 0:511], in_=ph[:, 0:511], func=AF.Gelu_apprx_tanh, bias=constc[kind][:, 0:1]),
                            [phk, "constc" + kind], [hk2])
                for g in range(2):
                    mm(PSck[:], W2p[:, g, :], hid[("k", g)][:], g == 0, g == 1, ["W2p", f"hidk{g}"], ["PSck"])
                act(lambda: A.copy(out=CkT[:], in_=PSck[:]), ["PSck"], ["CkT"])
                ci = 0
                for g in range(2):
                    for cc in range(4):
                        pc = PScv[ci % 2]
                        pck = f"PScv{ci % 2}"
                        ci += 1
                        mm(pc[:], hid[("v", g)][:, cc * 128:(cc + 1) * 128], W2v[:], True, True, [f"hidv{g}", "W2v"], [pck])
                        act(lambda: A.activation(out=CvA[:, cc, g, 0:64], in_=pc[:], func=AF.Copy, scale=vcc[:, cc:cc + 1]), [pck, "vcc"], ["CvA"])
                    dve(lambda: V.tensor_copy(out=CvA[:, :, g, 64], in_=vcc[:, :]), ["vcc", "CvA"], ["CvA"])
                if "p2" in dbg:
                    fw.dma("sp", ddbg("d_CkT", [128, 512], BF16)[:, :], CkT[:], reads=["CkT"], writes=["d_CkT"])
                    fw.dma("sp", ddbg("d_CvA", [128, 4 * 130], BF16)[:, :], CvA[:].rearrange("p a b c -> p (a b c)"), reads=["CvA"], writes=["d_CvA"])
                fw.barrier()
            if upto <= 2:
                fw.finish("sp")
                print("instr", fw.ninstr, "waits", fw.nwait)
                return nc, dbg_out
        with contextlib.ExitStack() as S3:
            def sincos(st, angt, n, sdst, cdst, tagk):
                a2 = sb(st, "sc_a2" + tagk, [128, n], F32)
                ki_ = sb(st, "sc_ki" + tagk, [128, n], I32)
                kf_ = sb(st, "sc_kf" + tagk, [128, n], F32)
                r_ = sb(st, "sc_r" + tagk, [128, n], F32)
                for shift, dst, dk in ((0.0, sdst, "sc_s" + tagk), (math.pi / 2, cdst, "sc_c" + tagk)):
                    dve(lambda: V.tensor_scalar(out=a2[:], in0=angt[:], scalar1=shift, scalar2=None, op0=ALU.add), ["sc_ang" + tagk], ["sc_a2" + tagk])
                    dve(lambda: V.tensor_scalar(out=ki_[:], in0=a2[:], scalar1=1.0 / TWO_PI, scalar2=None, op0=ALU.mult), ["sc_a2" + tagk], ["sc_ki" + tagk])
                    dve(lambda: V.tensor_copy(out=kf_[:], in_=ki_[:]), ["sc_ki" + tagk], ["sc_kf" + tagk])
                    dve(lambda: V.scalar_tensor_tensor(out=r_[:], in0=kf_[:], scalar=-CW1, in1=a2[:], op0=ALU.mult, op1=ALU.add),
                        ["sc_kf" + tagk, "sc_a2" + tagk], ["sc_r" + tagk])
                    dve(lambda: V.scalar_tensor_tensor(out=r_[:], in0=kf_[:], scalar=-CW2, in1=r_[:], op0=ALU.mult, op1=ALU.add),
                        ["sc_kf" + tagk, "sc_r" + tagk], ["sc_r" + tagk])
                    dve(lambda: V.tensor_scalar(out=r_[:], in0=r_[:], scalar1=-PI_LO, scalar2=PI_LO, op0=ALU.max, op1=ALU.min), ["sc_r" + tagk], ["sc_r" + tagk])
                    act(lambda: A.activation(out=dst[:], in_=r_[:], func=AF.Sin), ["sc_r" + tagk], [dk])

            def tt(out, a, b, op, r, w):
                dve(lambda: V.tensor_tensor(out=out, in0=a, in1=b, op=op), r, w)

            LR = sb(S3, "LR", [128, 32], F32)
            LI = sb(S3, "LI", [128, 32], F32)
            LDT = sb(S3, "LDT", [128, 32], F32)
            for half in (0, 64):
                fw.dma("sp", LR[half:half + 64, :], s5_a_re.rearrange("g n -> n g"), writes=["LR"], allow_slow_non_contiguous=True)
                fw.dma("sp", LI[half:half + 64, :], s5_a_im.rearrange("g n -> n g"), writes=["LI"], allow_slow_non_contiguous=True)
            fw.dma("sp", LDT[:], s5_log_dt[0:1, :].partition_broadcast(128), writes=["LDT"])
            sgn = sb(S3, "sgn", [128, 1], F32)
            Psw = sb(S3, "Psw", [128, 128], F32)
            dcol = sb(S3, "dcol", [128, 4], F32)
            fw.dma("sp", sgn[:], sgn_d[:, :], writes=["sgn"])
            fw.dma("sp", Psw[:], psw_d[:, :], writes=["Psw"])
            fw.dma("sp", dcol[:], s5_d.rearrange("(ct p) -> p ct", p=128), writes=["dcol"], allow_slow_non_contiguous=True)
            stp = sb(S3, "stp", [128, 32], F32)
            mag = sb(S3, "mag", [128, 32], F32)
            ang = sb(S3, "ang", [128, 32], F32)
            sn = sb(S3, "sn", [128, 32], F32)
            cs = sb(S3, "cs", [128, 32], F32)
            abr = sb(S3, "abr", [128, 32], F32)
            abi = sb(S3, "abi", [128, 32], F32)
            den = sb(S3, "den", [128, 32], F32)
            tq = sb(S3, "tq", [128, 32], F32)
            nr = sb(S3, "nr", [128, 32], F32)
            cre = sb(S3, "cre", [128, 32], F32)
            cim = sb(S3, "cim", [128, 32], F32)
            act(lambda: A.activation(out=stp[:], in_=LDT[:], func=AF.Exp), ["LDT"], ["stp"])
            tt(mag[:], LR[:], stp[:], ALU.mult, ["LR", "stp"], ["mag"])
            act(lambda: A.activation(out=mag[:], in_=mag[:], func=AF.Exp), ["mag"], ["mag"])
            tt(ang[:], LI[:], stp[:], ALU.mult, ["LI", "stp"], ["sc_angp"])
            sincos(S3, ang, 32, sn, cs, "p")
            tt(abr[:], mag[:], cs[:], ALU.mult, ["mag", "sc_cp"], ["abr"])
            tt(abi[:], mag[:], sn[:], ALU.mult, ["mag", "sc_sp"], ["abi"])
            tt(den[:], LR[:], LR[:], ALU.mult, ["LR"], ["den"])
            tt(tq[:], LI[:], LI[:], ALU.mult, ["LI"], ["tq"])
            tt(den[:], den[:], tq[:], ALU.add, ["den", "tq"], ["den"])
            dve(lambda: V.reciprocal(out=den[:], in_=den[:]), ["den"], ["den"])
            dve(lambda: V.tensor_scalar(out=nr[:], in0=abr[:], scalar1=-1.0, scalar2=None, op0=ALU.add), ["abr"], ["nr"])
            tt(cre[:], nr[:], LR[:], ALU.mult, ["nr", "LR"], ["cre"])
            tt(tq[:], abi[:], LI[:], ALU.mult, ["abi", "LI"], ["tq"])
            tt(cre[:], cre[:], tq[:], ALU.add, ["cre", "tq"], ["cre"])
            tt(cre[:], cre[:], den[:], ALU.mult, ["cre", "den"], ["cre"])
            tt(cim[:], abi[:], LR[:], ALU.mult, ["abi", "LR"], ["cim"])
            tt(tq[:], nr[:], LI[:], ALU.mult, ["nr", "LI"], ["tq"])
            tt(cim[:], cim[:], tq[:], ALU.subtract, ["cim", "tq"], ["cim"])
            tt(cim[:], cim[:], den[:], ALU.mult, ["cim", "den"], ["cim"])
            NL = 12
            WR = sb(S3, "WR", [128, NL, 32], F32)
            WIS = sb(S3, "WIS", [128, NL, 32], F32)
            dve(lambda: V.tensor_copy(out=WR[:, 0, :], in_=abr[:]), ["abr"], ["WR"])
            dve(lambda: V.tensor_scalar(out=WIS[:, 0, :], in0=abi[:], scalar1=sgn[:, 0:1], scalar2=None, op0=ALU.mult), ["abi", "sgn"], ["WIS"])
            for l in range(1, NL):
                tt(tq[:], WR[:, l - 1, :], WR[:, l - 1, :], ALU.mult, ["WR"], ["tq"])
                tt(nr[:], WIS[:, l - 1, :], WIS[:, l - 1, :], ALU.mult, ["WIS"], ["nr"])
                tt(WR[:, l, :], tq[:], nr[:], ALU.subtract, ["tq", "nr", "WR"], ["WR"])
                dve(lambda: V.scalar_tensor_tensor(out=WIS[:, l, :], in0=WR[:, l - 1, :], scalar=2.0, in1=WIS[:, l - 1, :], op0=ALU.mult, op1=ALU.mult),
                    ["WR", "WIS"], ["WIS"])
            Bpad = sb(S3, "Bpad", [128, 32, 128], BF16)
            Cpad = sb(S3, "Cpad", [128, 32, 128], BF16)
            with contextlib.ExitStack() as S3a:
                PSs = [ps(S3a, f"PSs{i}", [128, 128], F32) for i in range(2)]
                BR = sb(S3a, "BR", [64, 32, 16], F32)
                BI = sb(S3a, "BI", [64, 32, 16], F32)
                fw.dma("sp", BR[:], s5_b_re.rearrange("g n p -> n g p"), writes=["BR"])
                fw.dma("sp", BI[:], s5_b_im.rearrange("g n p -> n g p"), writes=["BI"])
                bbr = sb(S3a, "bbr", [64, 32, 16], F32)
                bbi = sb(S3a, "bbi", [64, 32, 16], F32)
                tb_ = sb(S3a, "tb_", [64, 32, 16], F32)
                creb = cre[0:64, :].unsqueeze(2).broadcast_to([64, 32, 16])
                cimb = cim[0:64, :].unsqueeze(2).broadcast_to([64, 32, 16])
                tt(bbr[:], BR[:], creb, ALU.mult, ["BR", "cre"], ["bbr"])
                tt(tb_[:], BI[:], cimb, ALU.mult, ["BI", "cim"], ["tb_"])
                tt(bbr[:], bbr[:], tb_[:], ALU.subtract, ["bbr", "tb_"], ["bbr"])
                tt(bbi[:], BI[:], creb, ALU.mult, ["BI", "cre"], ["bbi"])
                tt(tb_[:], BR[:], cimb, ALU.mult, ["BR", "cim", "bbr"], ["tb_"])
                tt(bbi[:], bbi[:], tb_[:], ALU.add, ["bbi", "tb_"], ["bbi"])
                ZpR = sb(S3a, "ZpR", [64, 32, 128], F32)
                ZpI = sb(S3a, "ZpI", [64, 32, 128], F32)
                pool(lambda: G.memset(ZpR[:], 0.0), [], ["ZpR"])
                pool(lambda: G.memset(ZpI[:], 0.0), [], ["ZpI"])
                for gl in range(8):
                    for Z, bb, zk, bk in ((ZpR, bbr, "ZpR", "bbr"), (ZpI, bbi, "ZpI", "bbi")):
                        dve(lambda: V.tensor_copy(out=Z[:].rearrange("n (ct gl) c -> n ct gl c", gl=8)[:, :, gl, 16 * gl:16 * gl + 16],
                                                  in_=bb[:].rearrange("n (ct gl) p -> n ct gl p", gl=8)[:, :, gl, :]), [bk, zk], [zk])
                for g in range(32):
                    pz = PSs[g % 2]
                    pzk = f"PSs{g % 2}"
                    tr(pz[:, 0:64], ZpR[:, g, :], identf[0:64, 0:64], ["ZpR", "identf"], [pzk], signal=False)
                    tr(pz[:, 64:128], ZpI[:, g, :], identf[0:64, 0:64], ["ZpI", "identf"], [pzk])
                    act(lambda: A.copy(out=Bpad[:, g, :], in_=pz[:]), [pzk], ["Bpad"])
                Cnat = sb(S3a, "Cnat", [128, 4, 128], F32)
                fw.dma("sp", Cnat[:, :, 0:64], s5_c_re.rearrange("(ct gl) p n -> (gl p) ct n", gl=8), writes=["Cnat"])
                fw.dma("sp", Cnat[:, :, 64:128], s5_c_im.rearrange("(ct gl) p n -> (gl p) ct n", gl=8), writes=["Cnat"])
                dve(lambda: V.tensor_scalar(out=Cnat[:, :, 64:128], in0=Cnat[:, :, 64:128], scalar1=-1.0, scalar2=None, op0=ALU.mult), ["Cnat"], ["Cnat"])
                pool(lambda: G.memset(Cpad[:], 0.0), [], ["Cpad"])
                for ct in range(4):
                    pz = PSs[ct % 2]
                    pzk = f"PSs{ct % 2}"
                    tr(pz[:, :], Cnat[:, ct, :], identf[:], ["Cnat", "identf"], [pzk])
                    for gl in range(8):
                        act(lambda: A.copy(out=Cpad[:, ct * 8 + gl, 16 * gl:16 * gl + 16], in_=pz[:, 16 * gl:16 * gl + 16]), [pzk, "Cpad"], ["Cpad"])
                fw.barrier()
            uT = [sb(S3, f"uT{i}", [128, 8192], BF16) for i in range(2)]
            X0 = [sb(S3, f"X0_{i}", [128, 8192], BF16) for i in range(2)]
            Ya = [sb(S3, f"Ya{i}", [128, 3072], BF16) for i in range(2)]
            Yb = [sb(S3, f"Yb{i}", [128, 1536], BF16) for i in range(2)]
            Wa = [sb(S3, f"Wa{i}", [128, 2048], BF16) for i in range(2)]
            Wb = [sb(S3, f"Wb{i}", [128, 2048], BF16) for i in range(2)]
            Rm = [sb(S3, f"Rm{i}", [128, NL, 128], BF16) for i in range(2)]
            rtmp = sb(S3, "rtmp", [128, 128], F32)
            xin = [sb(S3, f"xin{i}", [128, 2], BF16) for i in range(2)]
            ytmp = sb(S3, "ytmp", [128, 512], F32)
            yps = [ps(S3, f"yps{i}", [128, 512], F32) for i in range(4)]
            PB = [ps(S3, f"PB{i}", [128, 512], F32) for i in range(4)]
            evi = [0]
            pbi = [0, 0]

            def evac(out, in_, r, w):
                evi[0] += 1
                if evi[0] % 2:
                    act(lambda: A.copy(out=out, in_=in_), r, w)
                else:
                    dve(lambda: V.tensor_copy(out=out, in_=in_), r, w)

            def next_pb(gp):
                i = 2 * gp + pbi[gp] % 2
                pbi[gp] += 1
                return PB[i], f"PB{i}"

            for ct in range(4):
                u = uT[ct % 2]
                uk = f"uT{ct % 2}"
                for q4 in range(4):
                    fw.dma("sp", u[:, q4 * 2048:(q4 + 1) * 2048], uscr[ct, :, q4 * 2048:(q4 + 1) * 2048], reads=["uscr"], writes=[uk])
                def s5_group(ct, gl, u, uk):
                    g = ct * 8 + gl
                    gp = g % 2
                    R = Rm[gp]
                    rk = f"Rm{gp}"
                    for l in range(NL):
                        dve(lambda: V.tensor_scalar(out=rtmp[:], in0=identf[:], scalar1=WR[:, l, g:g + 1], scalar2=None, op0=ALU.mult),
                            ["identf", "WR"], ["rtmp"])
                        dve(lambda: V.scalar_tensor_tensor(out=R[:, l, :], in0=Psw[:], scalar=WIS[:, l, g:g + 1], in1=rtmp[:], op0=ALU.mult, op1=ALU.add),
                            ["Psw", "WIS", "rtmp"], [rk])
                    x0 = X0[gp]
                    x0k = f"X0_{gp}"
                    for tb in range(16):
                        pb, pbk = next_pb(gp)
                        mm(pb[:], Bpad[:, g, :], u[:, tb * 512:(tb + 1) * 512], True, True, ["Bpad", uk], [pbk])
                        evac(x0[:, tb * 512:(tb + 1) * 512], pb[:], [pbk], [x0k])
                        yield
                    src, srck = x0, x0k
                    bufs = [(Ya[gp], f"Ya{gp}"), (Yb[gp], f"Yb{gp}")]
                    n = 6144
                    l = 0
                    bsel = 0
                    while n > 3:
                        half = n // 2
                        dst, dstk = bufs[bsel]
                        for c0 in range(0, half, 512):
                            w_ = min(512, half - c0)
                            pb, pbk = next_pb(gp)
                            mm(pb[:, 0:w_], R[:, l, :], src[:, 2 * c0:2 * c0 + 2 * w_:2], True, False, [rk, srck], [pbk])
                            mm(pb[:, 0:w_], identb[:], src[:, 2 * c0 + 1:2 * c0 + 2 * w_:2], False, True, ["identb", srck], [pbk])
                            evac(dst[:, c0:c0 + w_], pb[:, 0:w_], [pbk], [dstk])
                            yield
                        src, srck = dst, dstk
                        bsel ^= 1
                        n = half
                        l += 1
                    assert l == 11 and n == 3
                    xi = xin[gp]
                    xik = f"xin{gp}"
                    pb, pbk = next_pb(gp)
                    mm(pb[:, 0:1], R[:, 11, :], src[:, 0:1], True, False, [rk, srck], [pbk])
                    mm(pb[:, 0:1], identb[:], src[:, 1:2], False, True, ["identb", srck], [pbk])
                    evac(xi[:, 0:1], pb[:, 0:1], [pbk], [xik])
                    yield
                    pb, pbk = next_pb(gp)
                    mm(pb[:, 0:1], R[:, 11, :], xi[:, 0:1], True, False, [rk, xik], [pbk])
                    mm(pb[:, 0:1], identb[:], src[:, 2:3], False, True, ["identb", srck], [pbk])
                    evac(xi[:, 1:2], pb[:, 0:1], [pbk], [xik])
                    yield
                    pb, pbk = next_pb(gp)
                    mm(pb[:, 0:1], R[:, 0, :], xi[:, 1:2], True, False, [rk, xik], [pbk])
                    mm(pb[:, 0:1], identb[:], x0[:, 6144:6145], False, True, ["identb", x0k], [pbk])
                    evac(x0[:, 6144:6145], pb[:, 0:1], [pbk], [x0k])
                    yield
                    cur, curk, off = x0, x0k, 6144
                    wb = [(Wa[gp], f"Wa{gp}"), (Wb[gp], f"Wb{gp}")]
                    for l in range(11):
                        k_ = 1 << l
                        dst, dstk = wb[l % 2]
                        for blk in range(4):
                            c0 = blk * 512
                            pb, pbk = next_pb(gp)
                            has2 = c0 + 512 > k_
                            mm(pb[:], identb[:], cur[:, off + c0:off + c0 + 512], True, not has2, ["identb", curk], [pbk])
                            if has2:
                                lo = max(c0, k_)
                                mm(pb[:, lo - c0:512], R[:, l, :], cur[:, off + lo - k_:off + c0 + 512 - k_], False, True, [rk, curk], [pbk])
                            evac(dst[:, c0:c0 + 512], pb[:], [pbk], [dstk])
                            yield
                        cur, curk, off = dst, dstk, 0
                    for blk in range(4):
                        mm(yps[blk][:], Cpad[:, g, :], cur[:, blk * 512:(blk + 1) * 512], gl == 0, gl == 7, ["Cpad", curk], [f"yps{blk}"])

                for gpair in range(4):
                    gens = [s5_group(ct, 2 * gpair, u, uk), s5_group(ct, 2 * gpair + 1, u, uk)]
                    alive = [True, True]
                    while any(alive):
                        for gi_ in range(2):
                            if alive[gi_]:
                                try:
                                    next(gens[gi_])
                                except StopIteration:
                                    alive[gi_] = False
                for blk in range(4):
                    dve(lambda: V.scalar_tensor_tensor(out=ytmp[:], in0=u[:, 6144 + blk * 512:6144 + (blk + 1) * 512], scalar=dcol[:, ct:ct + 1],
                                                       in1=yps[blk][:], op0=ALU.mult, op1=ALU.add), [uk, "dcol", f"yps{blk}"], ["ytmp"])
                    act(lambda: A.activation(out=gyT[:, ct, blk * 512:(blk + 1) * 512], in_=ytmp[:], func=AF.Gelu_apprx_tanh), ["ytmp"], ["gyT"])
            if "p3" in dbg:
                fw.dma("sp", ddbg("d_gyT", [128, 4 * 2048], BF16)[:, :], gyT[:].rearrange("p a b -> p (a b)"), reads=["gyT"], writes=["d_gyT"])
            fw.barrier()
        if upto <= 3:
            fw.finish("sp")
            print("instr", fw.ninstr, "waits", fw.nwait)
            return nc, dbg_out

        with contextlib.ExitStack() as S4:
            tri = sb(S4, "tri", [128, 512], BF16)
            atri = sb(S4, "atri", [128, 512], BF16)
            fw.dma("pool", tri[:], tri_d[:, :], writes=["tri"])
            fw.dma("pool", atri[:], atri_d[:, :], writes=["atri"])
            grow = sb(S4, "grow", [128, 1024], F32)
            brow = sb(S4, "brow", [128, 1024], F32)
            g1row = sb(S4, "g1row", [128, 1024], F32)
            b1row = sb(S4, "b1row", [128, 1024], F32)
            fw.dma("sp", grow[:], ln_emb[0:1, :].partition_broadcast(128), writes=["grow"])
            fw.dma("sp", brow[:], ln_emb[1:2, :].partition_broadcast(128), writes=["brow"])
            fw.dma("sp", g1row[:], ln1_d[0:1, :].partition_broadcast(128), writes=["g1row"])
            fw.dma("sp", b1row[:], ln1_d[1:2, :].partition_broadcast(128), writes=["b1row"])
            Wo = sb(S4, "Wo", [128, 8, 1024], BF16)
            wov = w_o.rearrange("(kc p) c -> p kc c", p=128)
            for kc in range(0, 8, 2):
                fw.dma("pool", Wo[:, kc:kc + 2, :], wov[:, kc:kc + 2, :], writes=["Wo"])
            KmT = sb(S4, "KmT", [128, 4, 256], BF16)
            VmA = sb(S4, "VmA", [128, 2, 4, 129], BF16)
            wv = w_in.rearrange("(dc p) c -> p dc c", p=128)
            PS_S = [ps(S4, f"PS_S{i}", [128, 512], F32) for i in range(2)]
            PO = [ps(S4, f"PO{i}", [128, 512], F32) for i in range(2)]
            PF = [ps(S4, f"PF4_{i}", [128, 512], F32) for i in range(2)]
            PT4 = ps(S4, "PT4", [128, 1024], BF16)
            PX = ps(S4, "PX", [128, 512], F32)
            cnt = {"s": 0, "o": 0, "f": 0, "pt": 0, "wp": 0}

            def nxt(lst, key, name):
                i = cnt[key] % len(lst)
                cnt[key] += 1
                return lst[i], f"{name}{i}"

            with contextlib.ExitStack() as S4m:
                memf = sb(S4m, "memf", [128, 2, 1024], F32)
                memb = sb(S4m, "memb", [128, 2, 1024], BF16)
                memT = sb(S4m, "memT", [128, 8, 256], BF16)
                Wmk = sb(S4m, "Wmk", [128, 8, 512], BF16)
                Wmv = sb(S4m, "Wmv", [128, 8, 512], BF16)
                fw.dma("sp", memf[:], mem_d.rearrange("(t p) c -> p t c", p=128), writes=["memf"])
                wmv_ = w_mem_kv.rearrange("(dc p) c -> p dc c", p=128)
                for dc in range(0, 8, 4):
                    fw.dma("pool", Wmk[:, dc:dc + 4, :], wmv_[:, dc:dc + 4, 0:512], writes=["Wmk"])
                    fw.dma("pool", Wmv[:, dc:dc + 4, :], wmv_[:, dc:dc + 4, 512:1024], writes=["Wmv"])
                dve(lambda: V.tensor_copy(out=memb[:], in_=memf[:]), ["memf"], ["memb"])
                for mt in range(2):
                    for dc in range(8):
                        tr(PT4[:, dc * 128:(dc + 1) * 128], memb[:, mt, dc * 128:(dc + 1) * 128], identb[:], ["memb", "identb"], ["PT4"], signal=(dc == 7))
                    act(lambda: A.copy(out=memT[:, :, mt * 128:(mt + 1) * 128], in_=PT4[:].rearrange("p (a b) -> p a b", a=8)), ["PT4"], ["memT"])
                for h in range(4):
                    pf, pfk = nxt(PF, "f", "PF4_")
                    for dc in range(8):
                        mm(pf[:, 0:256], Wmk[:, dc, h * 128:(h + 1) * 128], memT[:, dc, :], dc == 0, dc == 7, ["Wmk", "memT"], [pfk])
                    act(lambda: A.copy(out=KmT[:, h, :], in_=pf[:, 0:256]), [pfk], ["KmT"])
                for mc in range(2):
                    pf, pfk = nxt(PF, "f", "PF4_")
                    for dc in range(8):
                        mm(pf[:], memT[:, dc, mc * 128:(mc + 1) * 128], Wmv[:, dc, :], dc == 0, dc == 7, ["Wmv", "memT"], [pfk])
                    act(lambda: A.copy(out=VmA[:, mc, :, 0:128], in_=pf[:].rearrange("p (h d) -> p h d", h=4)), [pfk], ["VmA"])
                pool(lambda: G.memset(VmA[:, :, :, 128:129], 1.0), ["VmA"], ["VmA"])
                fw.barrier()

            hTq = sb(S4, "hTq", [128, 8, 512], BF16)
            QT = sb(S4, "QT", [128, 4, 4, 128], BF16)
            qmT = sb(S4, "qmT", [128, 4, 4, 128], BF16)
            gn = sb(S4, "gn", [128, 4, 24], F32)
            oT = sb(S4, "oT", [128, 4, 512], BF16)
            omT = sb(S4, "omT", [128, 4, 512], BF16)
            mrgT = sb(S4, "mrgT", [128, 8, 512], BF16)
            st4 = sb(S4, "st4", [128, 12], F32)
            mv4 = sb(S4, "mv4", [128, 2], F32)
            rs4 = sb(S4, "rs4", [128, 1], F32)
            def ln_tok(xt, xk, dst, dstk, grow_, brow_, gk, bk):
                dve(lambda: V.bn_stats(out=st4[:, 0:6], in_=xt[:, 0:512]), [xk], ["st4"])
                dve(lambda: V.bn_stats(out=st4[:, 6:12], in_=xt[:, 512:1024]), [xk], ["st4"])
                dve(lambda: V.bn_aggr(out=mv4[:, 0:2], in_=st4[:, 0:12]), ["st4"], ["mv4"])
                act(lambda: A.activation(out=rs4[:], in_=mv4[:, 1:2], func=AF.Sqrt, bias=LN_EPS, scale=1.0), ["mv4"], ["rs4"])
                dve(lambda: V.reciprocal(out=rs4[:], in_=rs4[:]), ["rs4"], ["rs4"])
                dve(lambda: V.tensor_scalar(out=dst[:], in0=xt[:], scalar1=mv4[:, 0:1], scalar2=rs4[:, 0:1], op0=ALU.subtract, op1=ALU.mult),
                    [xk, "mv4", "rs4"], [dstk])
                pool(lambda: G.tensor_tensor(out=dst[:], in0=dst[:], in1=grow_[:], op=ALU.mult), [dstk, gk], [dstk])
                pool(lambda: G.tensor_tensor(out=dst[:], in0=dst[:], in1=brow_[:], op=ALU.add), [dstk, bk], [dstk])

            wpid = [0]
            curB = [0]

            def wpiece(src_ap):
                w_, wk_ = nxt(wp, "wp", "wp")
                nk_ = src_ap.shape[1]
                pid = wpid[0]
                wpid[0] += 1
                sc_ = wscr[pid, :, 0:nk_ * 128].rearrange("p (k c) -> p k c", k=nk_)
                if curB[0] == 0:
                    fw.dma("pool", w_[:, 0:nk_, :], src_ap, writes=[wk_])
                    fw.dma("sp", sc_, w_[:, 0:nk_, :], reads=[wk_], writes=[f"wscr{pid}"])
                else:
                    fw.dma("sp", w_[:, 0:nk_, :], sc_, reads=[f"wscr{pid}"], writes=[wk_])
                return w_, wk_

            wno_v = w_nsa_out.rearrange("(kc p) c -> p kc c", p=128)
            wmo_v = w_mem_out.rearrange("(kc p) c -> p kc c", p=128)
            wgl_v = w_s5_glu.rearrange("(kc p) c -> p kc c", p=128)

            for B in range(4):
                wpid[0] = 0
                curB[0] = B
                with contextlib.ExitStack() as SA:
                    xq = [sb(SA, f"xq{i}", [128, 1024], F32) for i in range(1)]
                    h0t = sb(SA, "h0t", [128, 1024], F32)
                    h0b = sb(SA, "h0b", [128, 1024], BF16)
                    posi4 = sb(SA, "posi4", [128, 512], I32)
                    posf4 = sb(SA, "posf4", [128, 512], F32)
                    angs4 = sb(SA, "sc_angq", [128, 512], F32)
                    cos4 = sb(SA, "cos4", [128, 512], F32)
                    sin4 = sb(SA, "sin4", [128, 512], F32)
                    t14 = sb(SA, "t14", [128, 512], F32)
                    t24 = sb(SA, "t24", [128, 512], F32)
                    Wq4 = sb(SA, "Wq4", [128, 8, 4, 128], BF16)
                    Wqsw = sb(SA, "Wqsw", [128, 8, 4, 128], BF16)
                    Wgn = sb(SA, "Wgn", [128, 8, 24], BF16)
                    if B == 0:
                        for half in range(2):
                            for dc in range(8):
                                fw.dma("pool", Wq4[:, dc, :, half * 64:(half + 1) * 64],
                                       wv[:, dc, half * 256:(half + 1) * 256].rearrange("p (h d) -> p h d", h=4), writes=["Wq4"])
                        fw.dma("pool", Wgn[:], wv[:, :, C_GN:C_GN + 24], writes=["Wgn"])
                        pool(lambda: G.memset(Wqsw[:], 0.0), [], ["Wqsw"])
                        for half in range(2):
                            b0 = half * 64
                            dve(lambda: V.tensor_scalar(out=Wqsw[:, :, :, b0:b0 + 8], in0=Wq4[:, :, :, b0 + 8:b0 + 16], scalar1=-1.0, scalar2=None, op0=ALU.mult),
                                ["Wq4", "Wqsw"], ["Wqsw"])
                            dve(lambda: V.tensor_copy(out=Wqsw[:, :, :, b0 + 8:b0 + 16], in_=Wq4[:, :, :, b0:b0 + 8]), ["Wq4", "Wqsw"], ["Wqsw"])
                        fw.dma("sp", wqscr[0, :, :], Wq4[:].rearrange("p a b c -> p (a b c)"), reads=["Wq4"], writes=["wqscr0"])
                        fw.dma("sp", wqscr[1, :, :], Wqsw[:].rearrange("p a b c -> p (a b c)"), reads=["Wqsw"], writes=["wqscr1"])
                        fw.dma("sp", wgnscr[:, :], Wgn[:].rearrange("p a b -> p (a b)"), reads=["Wgn"], writes=["wgnscr"])
                    else:
                        fw.dma("sp", Wq4[:].rearrange("p a b c -> p (a b c)"), wqscr[0, :, :], reads=["wqscr0"], writes=["Wq4"])
                        fw.dma("sp", Wqsw[:].rearrange("p a b c -> p (a b c)"), wqscr[1, :, :], reads=["wqscr1"], writes=["Wqsw"])
                        fw.dma("sp", Wgn[:].rearrange("p a b -> p (a b)"), wgnscr[:, :], reads=["wgnscr"], writes=["Wgn"])

                    fw.dma("sp", posi4[:], posr[:, 6144 + B * 512:6144 + (B + 1) * 512].partition_broadcast(128), writes=["posi4"])
                    dve(lambda: V.tensor_copy(out=posf4[:], in_=posi4[:]), ["posi4"], ["posf4"])
                    dve(lambda: V.tensor_scalar(out=angs4[:], in0=posf4[:], scalar1=invf[:, 0:1], scalar2=None, op0=ALU.mult), ["posf4", "invf"], ["sc_angq"])
                    with contextlib.ExitStack() as Ssc:
                        sincos(Ssc, angs4, 512, sin4, cos4, "q")
                        fw.barrier()
                    Wqm = sb(SA, "Wqm", [128, 8, 512], BF16)
                    if B == 0:
                        for dc in range(0, 8, 4):
                            fw.dma("pool", Wqm[:, dc:dc + 4, :], wv[:, dc:dc + 4, C_QM:C_QM + 512], writes=["Wqm"])
                        fw.dma("sp", wqscr[2, :, :], Wqm[:].rearrange("p a b -> p (a b)"), reads=["Wqm"], writes=["wqscr2"])
                    else:
                        fw.dma("sp", Wqm[:].rearrange("p a b -> p (a b)"), wqscr[2, :, :], reads=["wqscr2"], writes=["Wqm"])
                    for tl in range(4):
                        Tt = OWN0 + B * 4 + tl
                        xt, xk = nxt(xq, "pt", "xq")
                        fw.dma("sp", xt[:], xr[Tt * 128:(Tt + 1) * 128, :], writes=[xk])
                        ln_tok(xt, xk, h0t, "h0t", grow, brow, "grow", "brow")
                        dve(lambda: V.tensor_copy(out=h0b[:], in_=h0t[:]), ["h0t"], ["h0b"])
                        for dc in range(8):
                            tr(PT4[:, dc * 128:(dc + 1) * 128], h0b[:, dc * 128:(dc + 1) * 128], identb[:], ["h0b", "identb"], ["PT4"], signal=(dc == 7))
                        act(lambda: A.copy(out=hTq[:, :, tl * 128:(tl + 1) * 128], in_=PT4[:].rearrange("p (a b) -> p a b", a=8)), ["PT4"], ["hTq"])
                    if "p4o" in dbg and B == 0:
                        fw.dma("sp", ddbg("d_Wq4", [128, 4096], BF16)[:, :], Wq4[:].rearrange("p a b c -> p (a b c)"), reads=["Wq4"], writes=["d_Wq4"])
                        fw.dma("sp", ddbg("d_Wqsw", [128, 4096], BF16)[:, :], Wqsw[:].rearrange("p a b c -> p (a b c)"), reads=["Wqsw"], writes=["d_Wqsw"])
                        fw.dma("sp", ddbg("d_hTq", [128, 4096], BF16)[:, :], hTq[:].rearrange("p a b -> p (a b)"), reads=["hTq"], writes=["d_hTq"])
                        fw.dma("sp", ddbg("d_cos4", [128, 512], F32)[:, :], cos4[:], reads=["sc_cq"], writes=["d_cos4"])
                        fw.dma("sp", ddbg("d_sin4", [128, 512], F32)[:, :], sin4[:], reads=["sc_sq"], writes=["d_sin4"])
                    for hh in range(4):
                        pa, pak = nxt(PF, "f", "PF4_")
                        for dc in range(8):
                            mm(pa[:], Wq4[:, dc, hh, :], hTq[:, dc, :], dc == 0, dc == 7, ["Wq4", "hTq"], [pak])
                        pb, pbk = nxt(PF, "f", "PF4_")
                        for dc in range(8):
                            mm(pb[:], Wqsw[:, dc, hh, :], hTq[:, dc, :], dc == 0, dc == 7, ["Wqsw", "hTq"], [pbk])
                        dve(lambda: V.tensor_tensor(out=t14[:], in0=pa[:], in1=cos4[:], op=ALU.mult), [pak, "sc_cq"], ["t14"])
                        dve(lambda: V.tensor_tensor(out=t24[:], in0=pb[:], in1=sin4[:], op=ALU.mult), [pbk, "sc_sq"], ["t24"])
                        pool(lambda: G.tensor_tensor(out=QT[:, :, hh, :], in0=t14[:].rearrange("p (t q) -> p t q", t=4),
                                                     in1=t24[:].rearrange("p (t q) -> p t q", t=4), op=ALU.add), ["t14", "t24"], ["QT"])
                    for h in range(4):
                        pa, pak = nxt(PF, "f", "PF4_")
                        for dc in range(8):
                            mm(pa[:], Wqm[:, dc, h * 128:(h + 1) * 128], hTq[:, dc, :], dc == 0, dc == 7, ["Wqm", "hTq"], [pak])
                        act(lambda: A.copy(out=qmT[:, :, h, :], in_=pa[:].rearrange("p (t q) -> p t q", t=4)), [pak], ["qmT"])
                    for tl in range(4):
                        pa, pak = nxt(PF, "f", "PF4_")
                        for dc in range(8):
                            mm(pa[:, 0:24], hTq[:, dc, tl * 128:(tl + 1) * 128], Wgn[:, dc, :], dc == 0, dc == 7, ["Wgn", "hTq"], [pak])
                        act(lambda: A.activation(out=gn[:, tl, :], in_=pa[:, 0:24], func=AF.Sigmoid), [pak], ["gn"])

                    fw.barrier()
                with contextlib.ExitStack() as SB:
                    cbA = [sb(SB, f"cbA{i}", [128, 512], BF16) for i in range(2)]
                    cbB = [sb(SB, f"cbB{i}", [128, 2, 512], BF16) for i in range(2)]
                    addb = [sb(SB, f"addb{i}", [128, 128], F32) for i in range(2)]
                    futb = [sb(SB, f"futb{i}", [128, 128], F32) for i in range(2)]
                    Pa = [sb(SB, f"Pa{i}", [128, 512], F32) for i in range(2)]
                    PTb = [sb(SB, f"PTb{i}", [128, 512], BF16) for i in range(3)]
                    imp = sb(SB, "imp", [128, 512], F32)
                    chk = sb(SB, "chk", [128, 512], F32)
                    sc2 = sb(SB, "sc2", [128, 128], F32)
                    sc3 = sb(SB, "sc3", [128, 128], F32)
                    m8a = sb(SB, "m8a", [128, 8], F32)
                    m8b = sb(SB, "m8b", [128, 8], F32)
                    selb = sb(SB, "selb", [128, 128], F32)
                    selsw = sb(SB, "selsw", [128, 128], F32)
                    QaA = [sb(SB, f"QaA{i}", [128, 4, 128], BF16) for i in range(2)]
                    QaB = [sb(SB, f"QaB{i}", [128, 4, 128], BF16) for i in range(2)]
                    rsum = sb(SB, "rsum", [128, 4], F32)
                    rinv = sb(SB, "rinv", [128, 4], F32)
                    coef = sb(SB, "coef", [128, 4], F32)
                    oacc = sb(SB, "oacc", [128, 512], F32)
                    otmp = sb(SB, "otmp", [128, 256], F32)
                    oaccb = sb(SB, "oaccb", [128, 512], BF16)
                    omacc = sb(SB, "omacc", [128, 512], F32)
                    omb = sb(SB, "omb", [128, 512], BF16)
                    for tl in range(4):
                        j = B * 4 + tl
                        qt = OWN0 + j
                        c2 = j % 2
                        fw.dma("pool", cbA[c2][:], cba_d[j, :, :], writes=[f"cbA{c2}"])
                        fw.dma("pool", cbB[c2][:], cbb_d[j, :, :, :].rearrange("a p c -> p a c"), writes=[f"cbB{c2}"])
                        fw.dma("sp", addb[c2][:], addb_d[j, :, :], writes=[f"addb{c2}"])
                        fw.dma("sp", futb[c2][:], futb_d[j, :, :], writes=[f"futb{c2}"])
                        for g in range(2):
                            r0 = g * 64
                            Qg = QT[r0:r0 + 64, tl, :, :].rearrange("p h q -> p (h q)")
                            for hh in range(4):
                                pS, pSk = nxt(PS_S, "s", "PS_S")
                                mm(pS[:], QT[r0:r0 + 64, tl, hh, :], CkT[r0:r0 + 64, :], True, False, ["QT", "CkT"], [pSk])
                                mm(pS[:], identb[:], cbA[c2][:], False, True, ["identb", f"cbA{c2}"], [pSk])
                                pa_ = Pa[hh % 2]
                                pak_ = f"Pa{hh % 2}"
                                act(lambda: A.activation(out=pa_[:], in_=pS[:], func=AF.Exp, scale=0.125, accum_out=rsum[:, hh:hh + 1]), [pSk], [pak_, "rsum"])
                                dve(lambda: V.tensor_scalar(out=rinv[:, hh:hh + 1], in0=rsum[:, hh:hh + 1], scalar1=1e-30, scalar2=None, op0=ALU.max), ["rsum"], ["rinv"])
                                dve(lambda: V.reciprocal(out=rinv[:, hh:hh + 1], in_=rinv[:, hh:hh + 1]), ["rinv"], ["rinv"])
                                if hh == 0:
                                    dve(lambda: V.tensor_scalar(out=imp[:], in0=pa_[:], scalar1=rinv[:, 0:1], scalar2=None, op0=ALU.mult), [pak_, "rinv"], ["imp"])
                                else:
                                    dve(lambda: V.scalar_tensor_tensor(out=imp[:], in0=pa_[:], scalar=rinv[:, hh:hh + 1], in1=imp[:], op0=ALU.mult, op1=ALU.add),
                                        [pak_, "rinv", "imp"], ["imp"])
                            dve(lambda: V.tensor_tensor(out=chk[:, 1:512], in0=imp[:, 1:512], in1=imp[:, 0:511], op=ALU.add), ["imp"], ["chk"])
                            dve(lambda: V.tensor_copy(out=chk[:, 0:1], in_=imp[:, 0:1]), ["imp", "chk"], ["chk"])
                            dve(lambda: V.tensor_reduce(out=sc2[:], in_=chk[:].rearrange("p (a b) -> p a b", b=4), axis=AX.X, op=ALU.add), ["chk"], ["sc2"])
                            dve(lambda: V.tensor_tensor(out=sc2[:], in0=sc2[:], in1=addb[c2][:], op=ALU.add), ["sc2", f"addb{c2}"], ["sc2"])
                            dve(lambda: V.max(out=m8a[:], in_=sc2[:]), ["sc2"], ["m8a"])
                            dve(lambda: V.match_replace(out=sc3[:], in_to_replace=m8a[:], in_values=sc2[:], imm_value=-3e9), ["sc2", "m8a"], ["sc3"])
                            dve(lambda: V.max(out=m8b[:], in_=sc3[:]), ["sc3"], ["m8b"])
                            dve(lambda: V.tensor_scalar(out=selb[:], in0=sc2[:], scalar1=m8b[:, 7:8], scalar2=NEG, op0=ALU.is_lt, op1=ALU.mult), ["sc2", "m8b"], ["selb"])
                            dve(lambda: V.tensor_tensor(out=selb[:], in0=selb[:], in1=futb[c2][:], op=ALU.min), ["selb", f"futb{c2}"], ["selb"])
                            dve(lambda: V.tensor_copy(out=selsw[:, 0:64], in_=selb[:, 64:128]), ["selb"], ["selsw"])
                            dve(lambda: V.tensor_copy(out=selsw[:, 64:128], in_=selb[:, 0:64]), ["selb", "selsw"], ["selsw"])
                            tr(PX[:, 0:128], selb[:], identf[:], ["selb", "identf"], ["PX"], signal=False)
                            tr(PX[:, 128:256], selsw[:], identf[:], ["selsw", "identf"], ["PX"])
                            qa, qb = QaA[g], QaB[g]
                            qr = slice(0, 64) if g == 0 else slice(64, 128)
                            br_ = slice(64, 128) if g == 0 else slice(0, 64)
                            srcA = PX[br_, 128:256] if g == 0 else PX[br_, 0:128]
                            srcB = PX[br_, 0:128] if g == 0 else PX[br_, 128:256]
                            act(lambda: A.copy(out=qa[br_, :, :], in_=srcA.unsqueeze(1).broadcast_to([64, 4, 128])), ["PX"], [f"QaA{g}"])
                            act(lambda: A.copy(out=qb[br_, :, :], in_=srcB.unsqueeze(1).broadcast_to([64, 4, 128])), ["PX"], [f"QaB{g}"])
                            pool(lambda: G.tensor_copy(out=qa[qr, :, :], in_=QT[qr, tl, :, :]), ["QT", f"QaA{g}"], [f"QaA{g}"])
                            pool(lambda: G.tensor_copy(out=qb[qr, :, :], in_=QT[qr, tl, :, :]), ["QT", f"QaB{g}"], [f"QaB{g}"])

                            def branch(klist, gate_idx, first):
                                po, pok = nxt(PO, "o", "PO")
                                n = len(klist)
                                scores = []

                                def emit_score(i):
                                    kap, kkey, extra, vap, vkey = klist[i][:5]
                                    rhs_, rhsk_ = (klist[i][5], klist[i][6]) if len(klist[i]) > 5 else (Qg, "QT")
                                    pS, pSk = nxt(PS_S, "s", "PS_S")
                                    mm(pS[:], kap, rhs_, True, len(extra) == 0, kkey if isinstance(kkey, list) else [kkey, rhsk_], [pSk])
                                    for ei, (el, er, ek) in enumerate(extra):
                                        mm(pS[:], el, er, False, ei == len(extra) - 1, ek, [pSk])
                                    return pS, pSk

                                mm(po[:, 0:260], zerob[:], VsA[:, 0:2, :, :].rearrange("p a b c -> p (a b c)"), True, False, ["zerob", "VsA"], [pok])
                                nxt_s = emit_score(0)
                                for i in range(n):
                                    pS, pSk = nxt_s
                                    if i + 1 < n:
                                        nxt_s = emit_score(i + 1)
                                    pt_, ptk = nxt(PTb, "pt", "PTb")
                                    act(lambda: A.activation(out=pt_[:], in_=pS[:], func=AF.Exp, scale=0.125), [pSk], [ptk])
                                    vap, vkey = klist[i][3], klist[i][4]
                                    for hh in range(4):
                                        mm(po[:, hh * 65:(hh + 1) * 65], pt_[:, hh * 128:(hh + 1) * 128], vap, False, (i == n - 1 and hh == 3), [ptk, vkey], [pok])
                                pov = po[:, 0:260].rearrange("p (h e) -> p h e", h=4)
                                dve(lambda: V.tensor_scalar(out=coef[:], in0=pov[:, :, 64], scalar1=1e-30, scalar2=None, op0=ALU.max), [pok], ["coef"])
                                dve(lambda: V.reciprocal(out=coef[:], in_=coef[:]), ["coef"], ["coef"])
                                dve(lambda: V.tensor_tensor(out=coef[:], in0=coef[:], in1=gn[:, tl, 12 * g + gate_idx:12 * g + 12:3], op=ALU.mult), ["coef", "gn"], ["coef"])
                                oslice = oacc[:, g * 256:(g + 1) * 256].rearrange("p (h d) -> p h d", h=4)
                                cb_ = coef[:].unsqueeze(2).broadcast_to([128, 4, 64])
                                if first:
                                    dve(lambda: V.tensor_tensor(out=oslice, in0=pov[:, :, 0:64], in1=cb_, op=ALU.mult), [pok, "coef", "oacc"], ["oacc"])
                                else:
                                    dve(lambda: V.tensor_tensor(out=otmp[:].rearrange("p (h d) -> p h d", h=4), in0=pov[:, :, 0:64], in1=cb_, op=ALU.mult),
                                        [pok, "coef"], ["otmp"])
                                    dve(lambda: V.tensor_tensor(out=oslice, in0=oslice, in1=otmp[:].rearrange("p (h d) -> p h d", h=4), op=ALU.add),
                                        ["oacc", "otmp"], ["oacc"])

                            kl = []
                            for cc in range(4):
                                extra = []
                                if cc >= 2:
                                    extra = [(identb[:], cbB[c2][:, cc - 2, :], ["identb", f"cbB{c2}"])]
                                kl.append((CkT[r0:r0 + 64, cc * 128:(cc + 1) * 128], "CkT", extra, CvA[:, cc, g, :], "CvA"))
                            branch(kl, 0, True)
                            kl = []
                            for kt in range(qt + 1):
                                extra = []
                                if kt == qt:
                                    extra.append((identb[:], tri[:], ["identb", "tri"]))
                                qx, qxk = (QaA[g], f"QaA{g}") if kt < 32 else (QaB[g], f"QaB{g}")
                                kl.append((KA[g][:, kt * 128:(kt + 1) * 128], [f"KA{g}", f"KA{g}e", qxk], extra, VsA[:, kt, g, :], "VsA",
                                           qx[:].rearrange("p h q -> p (h q)"), qxk))
                            branch(kl, 1, False)
                            kl = []
                            for kt in range(qt - 4, qt + 1):
                                extra = []
                                if kt == qt:
                                    extra = [(identb[:], tri[:], ["identb", "tri"])]
                                elif kt == qt - 4:
                                    extra = [(identb[:], atri[:], ["identb", "atri"])]
                                kl.append((KwT[r0:r0 + 64, (kt - 44) * 128:(kt - 43) * 128], "KwT", extra, VwA[:, kt - 44, g, :], "VwA"))
                            branch(kl, 2, False)
                        pos_ = [nxt(PO, "o", "PO"), nxt(PO, "o", "PO")]
                        for po, pok in pos_:
                            mm(po[:, 0:258], zerob[:], VsA[:, 0:2, :, :].rearrange("p a b c -> p (a b c)")[:, 0:258], True, False, ["zerob", "VsA"], [pok])
                        for mc in range(2):
                            pS, pSk = nxt(PS_S, "s", "PS_S")
                            for h in range(4):
                                mm(pS[:, h * 128:(h + 1) * 128], KmT[:, h, mc * 128:(mc + 1) * 128], qmT[:, tl, h, :], True, True, ["KmT", "qmT"], [pSk])
                            pt_, ptk = nxt(PTb, "pt", "PTb")
                            act(lambda: A.activation(out=pt_[:], in_=pS[:], func=AF.Exp, scale=128.0 ** -0.5), [pSk], [ptk])
                            for h in range(4):
                                po, pok = pos_[h // 2]
                                mm(po[:, (h % 2) * 129:(h % 2 + 1) * 129], pt_[:, h * 128:(h + 1) * 128], VmA[:, mc, h, :], False, (mc == 1 and h % 2 == 1), [ptk, "VmA"], [pok])
                        for hp in range(2):
                            po, pok = pos_[hp]
                            pov = po[:, 0:258].rearrange("p (h e) -> p h e", h=2)
                            dve(lambda: V.reciprocal(out=coef[:, 0:2], in_=pov[:, :, 128]), [pok], ["coef"])
                            dve(lambda: V.tensor_tensor(out=omacc[:, hp * 256:(hp + 1) * 256].rearrange("p (h d) -> p h d", h=2), in0=pov[:, :, 0:128],
                                                        in1=coef[:, 0:2].unsqueeze(2).broadcast_to([128, 2, 128]), op=ALU.mult), [pok, "coef"], ["omacc"])
                        dve(lambda: V.tensor_copy(out=oaccb[:], in_=oacc[:]), ["oacc"], ["oaccb"])
                        dve(lambda: V.tensor_copy(out=omb[:], in_=omacc[:]), ["omacc"], ["omb"])
                        for src_, sk_, dstT, dk_ in ((oaccb, "oaccb", oT, "oT"), (omb, "omb", omT, "omT")):
                            for kc in range(4):
                                tr(PT4[:, kc * 128:(kc + 1) * 128], src_[:, kc * 128:(kc + 1) * 128], identb[:], [sk_, "identb"], ["PT4"], signal=(kc == 3))
                            act(lambda: A.copy(out=dstT[:, :, tl * 128:(tl + 1) * 128], in_=PT4[:, 0:512].rearrange("p (a b) -> p a b", a=4)), ["PT4"], [dk_])

                    fw.barrier()
                if "p4o" in dbg and B == 0:
                    fw.dma("sp", ddbg("d_QT", [128, 2048], BF16)[:, :], QT[:].rearrange("p a b c -> p (a b c)"), reads=["QT"], writes=["d_QT"])
                    fw.dma("sp", ddbg("d_qmT", [128, 2048], BF16)[:, :], qmT[:].rearrange("p a b c -> p (a b c)"), reads=["qmT"], writes=["d_qmT"])
                    fw.dma("sp", ddbg("d_gn", [128, 96], F32)[:, :], gn[:].rearrange("p a b -> p (a b)"), reads=["gn"], writes=["d_gn"])
                    fw.dma("sp", ddbg("d_oT", [128, 4 * 512], BF16)[:, :], oT[:].rearrange("p a b -> p (a b)"), reads=["oT"], writes=["d_oT"])
                    fw.dma("sp", ddbg("d_omT", [128, 4 * 512], BF16)[:, :], omT[:].rearrange("p a b -> p (a b)"), reads=["omT"], writes=["d_omT"])

                with contextlib.ExitStack() as SC:
                    xq = [sb(SC, f"xq{i}", [128, 1024], F32) for i in range(2)]
                    h0t = sb(SC, "h0t", [128, 1024], F32)
                    sgt = [sb(SC, f"sgt{i}", [128, 512], F32) for i in range(2)]
                    mrg = sb(SC, "mrg", [128, 512], F32)
                    ytm = sb(SC, "ytm", [128, 512], F32)
                    vres = sb(SC, "vres", [128, 1024], F32)
                    wp = [sb(SC, f"wp{i}", [128, 8, 128], BF16) for i in range(6)]
                    for ft in range(8):
                        fs = slice(ft * 128, (ft + 1) * 128)

                        def proj(wsrc, nk, rhsT, rk):
                            w_, wk_ = wpiece(wsrc)
                            pf, pfk = nxt(PF, "f", "PF4_")
                            for kc in range(nk):
                                mm(pf[:], w_[:, kc, :], (rhsT[:, kc, B * 512:(B + 1) * 512] if rhsT is gyT else rhsT[:, kc, :]), kc == 0, kc == nk - 1, [wk_, rk], [pfk])
                            return pf, pfk

                        def gate(i):
                            pf, pfk = proj(wv[:, :, C_GM + i * 1024 + ft * 128:C_GM + i * 1024 + (ft + 1) * 128], 8, hTq, "hTq")
                            sg, sgk = nxt(sgt, "wp", "sgt")
                            act(lambda: A.activation(out=sg[:], in_=pf[:], func=AF.Sigmoid), [pfk], [sgk])
                            return sg, sgk

                        py, pyk = proj(wno_v[:, :, fs], 4, oT, "oT")
                        sg, sgk = gate(0)
                        dve(lambda: V.tensor_tensor(out=mrg[:], in0=py[:], in1=sg[:], op=ALU.mult), [pyk, sgk], ["mrg"])
                        pga, pgak = proj(wgl_v[:, :, fs], 4, gyT, "gyT")
                        pgb, pgbk = proj(wgl_v[:, :, 1024 + ft * 128:1024 + (ft + 1) * 128], 4, gyT, "gyT")
                        sg2, sg2k = nxt(sgt, "wp", "sgt")
                        act(lambda: A.activation(out=sg2[:], in_=pgb[:], func=AF.Sigmoid), [pgbk], [sg2k])
                        dve(lambda: V.tensor_tensor(out=ytm[:], in0=pga[:], in1=sg2[:], op=ALU.mult), [pgak, sg2k], ["ytm"])
                        sg, sgk = gate(1)
                        dve(lambda: V.tensor_tensor(out=ytm[:], in0=ytm[:], in1=sg[:], op=ALU.mult), ["ytm", sgk], ["ytm"])
                        dve(lambda: V.tensor_tensor(out=mrg[:], in0=mrg[:], in1=ytm[:], op=ALU.add), ["ytm", "mrg"], ["mrg"])
                        py, pyk = proj(wmo_v[:, :, fs], 4, omT, "omT")
                        sg, sgk = gate(2)
                        dve(lambda: V.tensor_tensor(out=ytm[:], in0=py[:], in1=sg[:], op=ALU.mult), [pyk, sgk], ["ytm"])
                        dve(lambda: V.tensor_tensor(out=mrgT[:, ft, :], in0=mrg[:], in1=ytm[:], op=ALU.add), ["ytm", "mrg"], ["mrgT"])

                    if "p4o" in dbg and B == 0:
                        fw.dma("sp", ddbg("d_mrgT", [128, 4096], BF16)[:, :], mrgT[:].rearrange("p a b -> p (a b)"), reads=["mrgT"], writes=["d_mrgT"])
                    for tl in range(4):
                        j = B * 4 + tl
                        Tt = OWN0 + j
                        xt, xk = nxt(xq, "pt", "xq")
                        fw.dma("sp", xt[:], xr[Tt * 128:(Tt + 1) * 128, :], writes=[xk])
                        ln_tok(xt, xk, h0t, "h0t", grow, brow, "grow", "brow")
                        for hf in range(2):
                            pf, pfk = nxt(PF, "f", "PF4_")
                            for ft in range(8):
                                mm(pf[:], mrgT[:, ft, tl * 128:(tl + 1) * 128], Wo[:, ft, hf * 512:(hf + 1) * 512], ft == 0, ft == 7, ["mrgT", "Wo"], [pfk])
                            dve(lambda: V.scalar_tensor_tensor(out=vres[:, hf * 512:(hf + 1) * 512], in0=h0t[:, hf * 512:(hf + 1) * 512], scalar=ALPHA,
                                                               in1=pf[:], op0=ALU.mult, op1=ALU.add), ["h0t", pfk], ["vres"])
                        ln_tok(vres, "vres", h0t, "h0t", g1row, b1row, "g1row", "b1row")
                        fw.dma("sp", h1scr[j * 128:(j + 1) * 128, :], h0t[:], reads=["h0t"], writes=["h1scr"])
                    fw.barrier()
            if "p4" in dbg:
                fw.dma("sp", ddbg("d_h1", [2048, 1024], F32)[:, :], h1scr[:, :], reads=["h1scr"], writes=["d_h1"])
            fw.barrier()
        if upto <= 4:
            fw.finish("sp")
            print("instr", fw.ninstr, "waits", fw.nwait)
            return nc, dbg_out
        SKV.close()

        with contextlib.ExitStack() as S5:
            acc = sb(S5, "acc", [128, 16, 1024], F32)
            h1T = sb(S5, "h1T", [128, 8, 2048], BF16)
            gates = sb(S5, "gates", [128, 16, 32], F32)
            bguT = sb(S5, "bguT", [128, 512], F32)
            g2row = sb(S5, "g2row", [128, 1024], F32)
            b2row = sb(S5, "b2row", [128, 1024], F32)
            st5 = sb(S5, "st5", [128, 12], F32)
            mv5 = sb(S5, "mv5", [128, 2], F32)
            rs5 = sb(S5, "rs5", [128, 1], F32)
            fw.dma("sp", g2row[:], ln2_d[0:1, :].partition_broadcast(128), writes=["g2row"])
            fw.dma("sp", b2row[:], ln2_d[1:2, :].partition_broadcast(128), writes=["b2row"])
            with contextlib.ExitStack() as S5i:
                Wr = sb(S5i, "Wr", [128, 8, 32], F32)
                brr = sb(S5i, "brr", [128, 32], F32)
                bdn = sb(S5i, "bdn", [32, 1024], F32)
                bgn = sb(S5i, "bgn", [128, 4, 128], F32)
                fw.dma("sp", Wr[:], w_router.rearrange("(dc p) e -> p dc e", p=128), writes=["Wr"])
                fw.dma("sp", brr[:], b_router[0:1, :].partition_broadcast(128), writes=["brr"])
                fw.dma("sp", bdn[:], b_down[:, :], writes=["bdn"])
                fw.dma("sp", bgn[:], b_gate_up.rearrange("(a p) c -> p a c", p=128), writes=["bgn"])
                h1t = [sb(S5i, f"h1t{i}", [128, 1024], F32) for i in range(2)]
                h1b = sb(S5i, "h1b", [128, 1024], BF16)
                h1Tf = sb(S5i, "h1Tf", [128, 8, 128], F32)
                lg = sb(S5i, "lg", [128, 32], F32)
                ex = sb(S5i, "ex", [128, 32], F32)
                msk = sb(S5i, "msk", [128, 32], F32)
                m85 = sb(S5i, "m85", [128, 8], F32)
                nmx = sb(S5i, "nmx", [128, 1], F32)
                ssm = sb(S5i, "ssm", [128, 1], F32)
                gT = sb(S5i, "gT", [32, 128], F32)
                PT5 = ps(S5i, "PT5", [128, 1024], BF16)
                PTf = [ps(S5i, f"PTf{i}", [128, 512], F32) for i in range(2)]
                PLg = ps(S5i, "PLg", [128, 128], F32)
                PBd = [ps(S5i, f"PBd{i}", [128, 512], F32) for i in range(2)]
                for a in range(4):
                    tr(PTf[a % 2][:, 0:128], bgn[:, a, :], identf[:], ["bgn", "identf"], [f"PTf{a % 2}"])
                    act(lambda: A.copy(out=bguT[:, a * 128:(a + 1) * 128], in_=PTf[a % 2][:, 0:128]), [f"PTf{a % 2}"], ["bguT"])
                for j in range(16):
                    ht = h1t[j % 2]
                    hk_ = f"h1t{j % 2}"
                    fw.dma("sp", ht[:], h1scr[j * 128:(j + 1) * 128, :], reads=["h1scr"], writes=[hk_])
                    act(lambda: A.mul(out=acc[:, j, :], in_=ht[:], mul=ALPHA), [hk_], [f"acc{j}"])
                    dve(lambda: V.tensor_copy(out=h1b[:], in_=ht[:]), [hk_], ["h1b"])
                    for dc in range(8):
                        tr(PT5[:, dc * 128:(dc + 1) * 128], h1b[:, dc * 128:(dc + 1) * 128], identb[:], ["h1b", "identb"], ["PT5"], signal=(dc == 7))
                    act(lambda: A.copy(out=h1T[:, :, j * 128:(j + 1) * 128], in_=PT5[:].rearrange("p (a b) -> p a b", a=8)), ["PT5"], ["h1T"])
                    for hf in range(2):
                        for d4 in range(4):
                            dc = hf * 4 + d4
                            tr(PTf[hf][:, d4 * 128:(d4 + 1) * 128], ht[:, dc * 128:(dc + 1) * 128], identf[:], [hk_, "identf"], [f"PTf{hf}"], signal=(d4 == 3))
                        dve(lambda: V.tensor_copy(out=h1Tf[:, hf * 4:(hf + 1) * 4, :], in_=PTf[hf][:].rearrange("p (a b) -> p a b", a=4)), [f"PTf{hf}"], ["h1Tf"])
                    for dc in range(8):
                        mm(PLg[:, 0:32], h1Tf[:, dc, :], Wr[:, dc, :], dc == 0, dc == 7, ["h1Tf", "Wr"], ["PLg"])
                    dve(lambda: V.tensor_tensor(out=lg[:], in0=PLg[:, 0:32], in1=brr[:], op=ALU.add), ["PLg", "brr"], ["lg"])
                    dve(lambda: V.max(out=m85[:], in_=lg[:]), ["lg"], ["m85"])
                    dve(lambda: V.tensor_scalar(out=nmx[:], in0=m85[:, 0:1], scalar1=-1.0, scalar2=None, op0=ALU.mult), ["m85"], ["nmx"])
                    act(lambda: A.activation(out=ex[:], in_=lg[:], func=AF.Exp, bias=nmx[:, 0:1], scale=1.0), ["lg", "nmx"], ["ex"])
                    dve(lambda: V.tensor_scalar(out=msk[:], in0=lg[:], scalar1=m85[:, 3:4], scalar2=None, op0=ALU.is_ge), ["lg", "m85"], ["msk"])
                    dve(lambda: V.tensor_tensor(out=ex[:], in0=ex[:], in1=msk[:], op=ALU.mult), ["ex", "msk"], ["ex"])
                    dve(lambda: V.reduce_sum(out=ssm[:], in_=ex[:], axis=AX.X), ["ex"], ["ssm"])
                    dve(lambda: V.reciprocal(out=ssm[:], in_=ssm[:]), ["ssm"], ["ssm"])
                    dve(lambda: V.tensor_scalar(out=gates[:, j, :], in0=ex[:], scalar1=ssm[:, 0:1], scalar2=None, op0=ALU.mult), ["ex", "ssm"], ["gates"])
                    tr(PLg[0:32, :], gates[:, j, :], identf[:], ["gates", "identf"], ["PLg"])
                    act(lambda: A.copy(out=gT[:], in_=PLg[0:32, :]), ["PLg"], ["gT"])
                    for hf in range(2):
                        mm(PBd[hf][:], gT[:], bdn[:, hf * 512:(hf + 1) * 512], True, True, ["gT", "bdn"], [f"PBd{hf}"])
                        dve(lambda: V.tensor_tensor(out=acc[:, j, hf * 512:(hf + 1) * 512], in0=PBd[hf][:], in1=acc[:, j, hf * 512:(hf + 1) * 512], op=ALU.add),
                            [f"PBd{hf}", f"acc{j}"], [f"acc{j}"])
                if "p5g" in dbg:
                    fw.dma("sp", ddbg("d_gates", [128, 512], F32)[:, :], gates[:].rearrange("p a b -> p (a b)"), reads=["gates"], writes=["d_gates"])
                fw.barrier()
            actT = sb(S5, "actT", [128, 8, 2048], BF16)
            Wgu = [sb(S5, f"Wgu{i}", [128, 8, 2, 256], BF16) for i in range(2)]
            Wd = [sb(S5, f"Wd{i}", [128, 8, 512], BF16) for i in range(2)]
            gs = [sb(S5, f"gs{i}", [128, 512], F32) for i in range(3)]
            sg = [sb(S5, f"sg{i}", [128, 512], F32) for i in range(3)]
            ls = [sb(S5, f"ls{i}", [128, 512], F32) for i in range(3)]
            PG = [ps(S5, f"PG{i}", [128, 512], F32) for i in range(3)]
            PL = [ps(S5, f"PL{i}", [128, 512], F32) for i in range(3)]
            PD = [ps(S5, f"PD{i}", [128, 512], F32) for i in range(2)]
            ui = 0
            di = 0
            NE = 32 if "moe_e" not in dbg else 2
            NU = 3

            def load_wgu(e, pg):
                par = (e * 4 + pg) % 2
                wgu_v = w_gate_up[e].rearrange("(dc p) c -> p dc c", p=128)
                for gl_ in range(2):
                    c0 = gl_ * 1024 + pg * 256
                    for d0 in range(0, 8, 4):
                        fw.dma("pool", Wgu[par][:, d0:d0 + 4, gl_, :], wgu_v[:, d0:d0 + 4, c0:c0 + 256], writes=[f"Wgu{par}_{gl_}_{d0 // 4}"])

            def load_wd(e, h2):
                wd_v = w_down[e].rearrange("(fc p) c -> p fc c", p=128)
                for d0 in range(0, 8, 4):
                    fw.dma("pool", Wd[h2][:, d0:d0 + 4, :], wd_v[:, d0:d0 + 4, h2 * 512:(h2 + 1) * 512], writes=[f"Wd{h2}_{d0 // 4}"])

            load_wgu(0, 0)
            load_wd(0, 0)
            load_wd(0, 1)
            for e in range(NE):
                for pg in range(4):
                    par = (e * 4 + pg) % 2
                    wb = Wgu[par]
                    if pg < 3:
                        load_wgu(e, pg + 1)
                    elif e + 1 < NE:
                        load_wgu(e + 1, 0)
                    for pi in range(2):
                        i = pg * 2 + pi
                        bg_ = bguT[:, e * 16 + i:e * 16 + i + 1]
                        bl_ = bguT[:, e * 16 + 8 + i:e * 16 + 8 + i + 1]
                        for tb in range(4):
                            u2 = ui % NU
                            ui += 1
                            for dc in range(8):
                                mm(PG[u2][:], wb[:, dc, 0, pi * 128:(pi + 1) * 128], h1T[:, dc, tb * 512:(tb + 1) * 512], dc == 0, dc == 7,
                                   [f"Wgu{par}_0_{dc // 4}", "h1T"], [f"PG{u2}"])
                            for dc in range(8):
                                mm(PL[u2][:], wb[:, dc, 1, pi * 128:(pi + 1) * 128], h1T[:, dc, tb * 512:(tb + 1) * 512], dc == 0, dc == 7,
                                   [f"Wgu{par}_1_{dc // 4}", "h1T"], [f"PL{u2}"])
                            dve(lambda: V.tensor_scalar(out=gs[u2][:], in0=PG[u2][:], scalar1=bg_, scalar2=7.0, op0=ALU.add, op1=ALU.min), [f"PG{u2}", "bguT"], [f"gs{u2}"])
                            act(lambda: A.activation(out=sg[u2][:], in_=gs[u2][:], func=AF.Sigmoid, scale=1.702), [f"gs{u2}"], [f"sg{u2}"])
                            dve(lambda: V.tensor_scalar(out=ls[u2][:], in0=PL[u2][:], scalar1=bl_, scalar2=7.0, op0=ALU.add, op1=ALU.min), [f"PL{u2}", "bguT"], [f"ls{u2}"])
                            dve(lambda: V.tensor_scalar(out=ls[u2][:], in0=ls[u2][:], scalar1=-7.0, scalar2=1.0, op0=ALU.max, op1=ALU.add), [f"ls{u2}"], [f"ls{u2}"])
                            pool(lambda: G.tensor_tensor(out=gs[u2][:], in0=gs[u2][:], in1=sg[u2][:], op=ALU.mult), [f"gs{u2}", f"sg{u2}"], [f"gs{u2}"])
                            pool(lambda: G.tensor_tensor(out=actT[:, i, tb * 512:(tb + 1) * 512], in0=gs[u2][:], in1=ls[u2][:], op=ALU.mult),
                                 [f"gs{u2}", f"ls{u2}"], [f"actT{tb}"])
                for h2 in range(2):
                    wdb = Wd[h2]
                    for j in range(16):
                        d2 = di % 2
                        di += 1
                        for fc in range(8):
                            mm(PD[d2][:], actT[:, fc, j * 128:(j + 1) * 128], wdb[:, fc, :], fc == 0, fc == 7, [f"actT{j // 4}", f"Wd{h2}_{fc // 4}"], [f"PD{d2}"])
                        dve(lambda: V.scalar_tensor_tensor(out=acc[:, j, h2 * 512:(h2 + 1) * 512], in0=PD[d2][:], scalar=gates[:, j, e:e + 1],
                                                           in1=acc[:, j, h2 * 512:(h2 + 1) * 512], op0=ALU.mult, op1=ALU.add),
                            [f"PD{d2}", "gates", f"acc{j}"], [f"acc{j}"])
                    if e + 1 < NE:
                        load_wd(e + 1, h2)
            ot = [sb(S5, f"ot{i}", [128, 1024], F32) for i in range(2)]
            for j in range(16):
                o_ = ot[j % 2]
                ok_ = f"ot{j % 2}"
                xt = acc[:, j, :]
                xk = f"acc{j}"
                dve(lambda: V.bn_stats(out=st5[:, 0:6], in_=xt[:, 0:512]), [xk], ["st5"])
                dve(lambda: V.bn_stats(out=st5[:, 6:12], in_=xt[:, 512:1024]), [xk], ["st5"])
                dve(lambda: V.bn_aggr(out=mv5[:, 0:2], in_=st5[:, 0:12]), ["st5"], ["mv5"])
                act(lambda: A.activation(out=rs5[:], in_=mv5[:, 1:2], func=AF.Sqrt, bias=LN_EPS, scale=1.0), ["mv5"], ["rs5"])
                dve(lambda: V.reciprocal(out=rs5[:], in_=rs5[:]), ["rs5"], ["rs5"])
                dve(lambda: V.tensor_scalar(out=o_[:], in0=xt, scalar1=mv5[:, 0:1], scalar2=rs5[:, 0:1], op0=ALU.subtract, op1=ALU.mult),
                    [xk, "mv5", "rs5"], [ok_])
                pool(lambda: G.tensor_tensor(out=o_[:], in0=o_[:], in1=g2row[:], op=ALU.mult), [ok_, "g2row"], [ok_])
                pool(lambda: G.tensor_tensor(out=o_[:], in0=o_[:], in1=b2row[:], op=ALU.add), [ok_, "b2row"], [ok_])
                fw.dma("sp", out_d[j * 128:(j + 1) * 128, :], o_[:], reads=[ok_], writes=[f"out{j}"])
            fw.finish("sp")
            fw.barrier()
        print("instr", fw.ninstr, "waits", fw.nwait)
        return nc, dbg_out

        fw.finish("sp")
    return nc, dbg_out


def host_prep(inputs):
    x = np.asarray(inputs["x"], np.float32)
    pos = np.asarray(inputs["positions"], np.int32)
    maps = []
    inv = (np.float32(500000.0) ** (-(np.arange(0, 16, 2, dtype=np.float32)) / np.float32(16))).astype(np.float32)
    invf = np.zeros((128, 1), np.float32)
    for base in (0, 64):
        invf[base:base + 8, 0] = inv
        invf[base + 8:base + 16, 0] = inv
    identf = np.eye(128, dtype=np.float32)
    ln_emb = np.stack([inputs["ln_emb_g"], inputs["ln_emb_b"]]).astype(np.float32)
    w_in = np.ascontiguousarray(np.asarray(inputs["w_in"], np.float32)[0])
    g0 = lambda n: np.ascontiguousarray(np.asarray(inputs[n], np.float32)[0])
    sgn = np.ones((128, 1), np.float32); sgn[64:] = -1.0
    psw = np.zeros((128, 128), np.float32)
    psw[np.arange(128), (np.arange(128) + 64) % 128] = 1.0
    shared = {
        "w_kcmp1": g0("w_kcmp1"), "w_kcmp2": g0("w_kcmp2"), "w_vcmp1": g0("w_vcmp1"), "w_vcmp2": g0("w_vcmp2"),
        "pe_k": g0("pe_k_cmp"), "pe_v": g0("pe_v_cmp"),
        "s5_a_re": g0("s5_a_re"), "s5_a_im": g0("s5_a_im"), "s5_log_dt": np.asarray(inputs["s5_log_dt"], np.float32).reshape(1, 32),
        "s5_b_re": g0("s5_b_re"), "s5_b_im": g0("s5_b_im"), "s5_c_re": g0("s5_c_re"), "s5_c_im": g0("s5_c_im"), "s5_d": g0("s5_d"),
        "sgn": sgn, "psw": psw,
        "w_mem_kv": g0("w_mem_kv"), "w_nsa_out": g0("w_nsa_out"), "w_mem_out": g0("w_mem_out"), "w_s5_glu": g0("w_s5_glu"),
        "w_o": g0("w_o"), "ln1": np.stack([g0("ln1_g"), g0("ln1_b")]),
        "ln2": np.stack([g0("ln2_g"), g0("ln2_b")]), "w_router": g0("w_router"), "b_router": g0("b_router").reshape(1, 32),
        "w_gate_up": g0("w_gate_up"), "b_gate_up": g0("b_gate_up").reshape(512, 128), "w_down": g0("w_down"), "b_down": g0("b_down"),
    }
    kk = np.arange(8192)
    shared["epat"] = (((kk[None, :] // 64) % 64) == np.arange(64)[:, None]).astype(np.float32)
    ii = np.arange(128)
    tri = np.where(ii[:, None] <= ii[None, :], 0.0, NEG).astype(np.float32)
    atri = np.where(ii[:, None] > ii[None, :], 0.0, NEG).astype(np.float32)
    shared["tri"] = np.tile(tri, (1, 4))
    shared["atri"] = np.tile(atri, (1, 4))
    for k in range(8):
        b, r = k // 4, k % 4
        pad = 2048 * (3 - r)
        xr = np.zeros((8192, 1024), np.float32)
        xr[pad:] = x[b, :8192 - pad]
        posr = np.zeros((1, 8192), np.int32)
        posr[0, pad:] = pos[b, :8192 - pad]
        valid = (np.arange(8192) >= pad).astype(np.float32)
        cidx = np.arange(512)
        vc = ((cidx <= 510) & (16 * cidx >= pad)).astype(np.float32)
        rho = 6144 + np.arange(2048)
        cend = 16 * cidx + 31
        okc = (vc[None, :] > 0) & (cend[None, :] <= rho[:, None])
        cba = np.where(okc, 0.0, NEG).astype(np.float32).reshape(16, 128, 512)
        cbb = np.where(okc, 0.0, NEG).astype(np.float32).reshape(16, 128, 512)[:, :, 256:512]
        cbb = cbb.reshape(16, 128, 2, 128).transpose(0, 2, 3, 1)
        cbb = np.ascontiguousarray(np.tile(cbb, (1, 1, 1, 4)))
        blk = np.arange(128)
        tb = rho // 64
        blk0 = pad // 64
        forced = (blk[None, :] == tb[:, None]) | (blk[None, :] == tb[:, None] - 1) | (blk[None, :] == blk0)
        invalid = (blk[None, :] > tb[:, None]) | (blk[None, :] < blk0)
        addb = (forced * 1e4 + invalid * (-1e9)).astype(np.float32).reshape(16, 128, 128)
        futb = np.where(invalid, NEG, 0.0).astype(np.float32).reshape(16, 128, 128)
        m = {
            "cba": cba, "cbb": cbb, "addb": addb, "futb": futb, "mem": np.ascontiguousarray(np.asarray(inputs["mem"], np.float32)[b]),
            "vcc": np.ascontiguousarray(vc.reshape(4, 128).T),
            "xr": xr, "posr": posr, "vrow": valid[None, :].copy(),
            "vcol": np.ascontiguousarray(valid.reshape(64, 128).T),
            "ln_emb": ln_emb, "w_in": w_in, "invf": invf, "identf": identf,
        }
        m.update(shared)
        maps.append(m)
    return maps


def kernel(**inputs):
    nc, _ = build_program()
    maps = host_prep(inputs)
    res = run_bass_kernel_spmd(nc, maps, core_ids=list(range(8)))
    out = np.zeros((2, 8192, 1024), np.float32)
    for k in range(8):
        b, r = k // 4, k % 4
        out[b, 2048 * r:2048 * (r + 1)] = res.results[k]["out"]
    return out
```

```python
import contextlib
import math
import numpy as np
import ml_dtypes
import concourse.bass as bass
import concourse.mybir as mybir
from concourse.bass_utils import run_bass_kernel_spmd

F32 = mybir.dt.float32
BF16 = mybir.dt.bfloat16
I32 = mybir.dt.int32
ALU = mybir.AluOpType
AF = mybir.ActivationFunctionType
AX = mybir.AxisListType

EPOCH = 20000
NDMA_SLOTS = 6
NEG = -30000.0
LN_EPS = 1e-5
ALPHA = 2.0 ** 0.25
TWO_PI = 2.0 * math.pi
CW1 = 6.28125
CW2 = TWO_PI - 6.28125
PI_LO = 3.1415925


class _Eng:
    def __init__(self, fw, name, handle):
        self.fw = fw
        self.name = name
        self.h = handle
        self.sems = []
        self.epoch = -1
        self.count = 0
        self.pending = False
        self.seen = {}
        self.dma_slots = None
        self.dma_next = 0
        self.new_epoch()

    def new_epoch(self):
        self.epoch += 1
        self.count = 0
        s = self.fw.new_sem(f"{self.name}_e{self.epoch}")
        self.sems.append(s)
        self.fw.semobjs[(self.name, self.epoch)] = s

    def cur_key(self):
        return (self.name, self.epoch)


class FW:
    def __init__(self, nc, stack):
        self.nc = nc
        self.stack = stack
        self.semobjs = {}
        self.eng = {}
        for name, h in (("pe", nc.tensor), ("dve", nc.vector), ("act", nc.scalar),
                        ("pool", nc.gpsimd), ("sp", nc.sync)):
            self.eng[name] = _Eng(self, name, h)
        self.last_w = {}
        self.readers = {}
        self.ninstr = 0
        self.nwait = 0

    def new_sem(self, name):
        return self.stack.enter_context(self.nc.semaphore(name))

    def _wait(self, e, tok):
        semkey, val = tok
        if e.seen.get(semkey, 0) >= val:
            return
        if not semkey[0].endswith("_dma"):
            for (n2, ep2), v2 in e.seen.items():
                if n2 == semkey[0] and ep2 > semkey[1]:
                    return
        e.h.wait_ge(self.semobjs[semkey], val)
        self.nwait += 1
        e.seen[semkey] = val

    def _deps(self, reads, writes):
        deps = []
        for r in reads:
            t = self.last_w.get(r)
            if t is not None:
                deps.append(t)
        for w in writes:
            t = self.last_w.get(w)
            if t is not None:
                deps.append(t)
            deps.extend(self.readers.get(w, ()))
        return deps

    def _record(self, tok, reads, writes):
        for r in reads:
            lst = self.readers.setdefault(r, [])
            lst.append(tok)
            if len(lst) > 64:
                best = {}
                for sk, v in lst:
                    if best.get(sk, 0) < v:
                        best[sk] = v
                self.readers[r] = list(best.items())
        for w in writes:
            self.last_w[w] = tok
            self.readers[w] = []

    def op(self, engname, fn, reads=(), writes=(), signal=True):
        e = self.eng[engname]
        if e.count >= EPOCH and not e.pending:
            e.new_epoch()
        semkey = e.cur_key()
        for tok in self._deps(reads, writes):
            if engname == "pe" and tok[0][0] == "pe":
                continue
            self._wait(e, tok)
        ins = fn()
        self.ninstr += 1
        if signal:
            ins.then_inc(e.sems[e.epoch], 1)
            e.count += 1
            e.pending = False
            tok = (semkey, e.count)
        else:
            e.pending = True
            tok = (semkey, e.count + 1)
        self._record(tok, reads, writes)
        return ins

    def dma(self, qname, out, in_, reads=(), writes=(), **kw):
        e = self.eng[qname]
        if e.dma_slots is None:
            e.dma_slots = []
            for i in range(NDMA_SLOTS):
                key = (qname + "_dma", i)
                self.semobjs[key] = self.new_sem(f"{qname}_d{i}")
                e.dma_slots.append([key, 0])
        slot = e.dma_slots[e.dma_next % NDMA_SLOTS]
        e.dma_next += 1
        key, uses = slot
        if uses > 0:
            self._wait(e, (key, 16 * uses))
        for tok in self._deps(reads, writes):
            self._wait(e, tok)
        ins = e.h.dma_start(out=out, in_=in_, **kw)
        ins.then_inc(self.semobjs[key], 16)
        self.ninstr += 1
        slot[1] = uses + 1
        tok = (key, 16 * (uses + 1))
        self._record(tok, reads, writes)
        return tok

    def barrier(self):
        toks = []
        for e in self.eng.values():
            assert not e.pending, e.name
            if e.count > 0:
                toks.append((e.cur_key(), e.count))
            if e.dma_slots:
                for key, uses in e.dma_slots:
                    if uses > 0:
                        toks.append((key, 16 * uses))
        for e in self.eng.values():
            for t in toks:
                self._wait(e, t)
        self.last_w = {}
        self.readers = {}

    def finish(self, qname="sp"):
        e = self.eng[qname]
        for k, tok in list(self.last_w.items()):
            self._wait(e, tok)


C_Q, C_KC, C_VC, C_KS, C_VS, C_KW, C_VW, C_GN, C_U, C_QM, C_GM = 0, 512, 640, 768, 896, 1024, 1152, 1280, 1304, 1816, 2328
NT = 64
OWN0 = 48


def build_program(upto=99, dbg=()):
    nc = bass.Bass("TRN2", target_bir_lowering=False)
    V, A, G, T = nc.vector, nc.scalar, nc.gpsimd, nc.tensor

    def din(name, shape, dt=F32):
        return nc.dram_tensor(name, list(shape), dt, kind="ExternalInput").ap()

    def dscr(name, shape, dt):
        return nc.dram_tensor(name, list(shape), dt, kind="Internal").ap()

    dbg_out = {}

    def ddbg(name, shape, dt):
        dbg_out[name] = nc.dram_tensor(name, list(shape), dt, kind="ExternalOutput").ap()
        return dbg_out[name]

    xr = din("xr", [8192, 1024])
    posr = din("posr", [1, 8192], I32)
    vrow_d = din("vrow", [1, 8192])
    vcol_d = din("vcol", [128, 64])
    ln_emb = din("ln_emb", [2, 1024])
    w_in = din("w_in", [1024, 5400])
    invf_d = din("invf", [128, 1])
    identf_d = din("identf", [128, 128])
    w_kcmp1 = din("w_kcmp1", [32, 64, 128]); w_kcmp2 = din("w_kcmp2", [128, 64])
    w_vcmp1 = din("w_vcmp1", [32, 64, 128]); w_vcmp2 = din("w_vcmp2", [128, 64])
    pe_k = din("pe_k", [32, 64]); pe_v = din("pe_v", [32, 64])
    vcc_d = din("vcc", [128, 4])
    s5_a_re = din("s5_a_re", [32, 64]); s5_a_im = din("s5_a_im", [32, 64]); s5_log_dt = din("s5_log_dt", [1, 32])
    s5_b_re = din("s5_b_re", [32, 64, 16]); s5_b_im = din("s5_b_im", [32, 64, 16])
    s5_c_re = din("s5_c_re", [32, 16, 64]); s5_c_im = din("s5_c_im", [32, 16, 64]); s5_d = din("s5_d", [512])
    sgn_d = din("sgn", [128, 1]); psw_d = din("psw", [128, 128])
    mem_d = din("mem", [256, 1024]); w_mem_kv = din("w_mem_kv", [1024, 1024])
    w_nsa_out = din("w_nsa_out", [512, 1024]); w_mem_out = din("w_mem_out", [512, 1024])
    w_s5_glu = din("w_s5_glu", [512, 2048]); w_o = din("w_o", [1024, 1024]); ln1_d = din("ln1", [2, 1024])
    epat_d = din("epat", [64, 8192]); tri_d = din("tri", [128, 512]); atri_d = din("atri", [128, 512])
    cba_d = din("cba", [16, 128, 512]); cbb_d = din("cbb", [16, 2, 128, 512])
    addb_d = din("addb", [16, 128, 128]); futb_d = din("futb", [16, 128, 128])
    h1scr = dscr("h1scr", [2048, 1024], F32)
    wscr = dscr("wscr", [60, 128, 1024], BF16)
    wqscr = dscr("wqscr", [3, 128, 4096], BF16)
    wgnscr = dscr("wgnscr", [128, 192], BF16)
    ln2_d = din("ln2", [2, 1024]); w_router = din("w_router", [1024, 32]); b_router = din("b_router", [1, 32])
    w_gate_up = din("w_gate_up", [32, 1024, 2048]); b_gate_up = din("b_gate_up", [512, 128])
    w_down = din("w_down", [32, 1024, 1024]); b_down = din("b_down", [32, 1024])
    out_d = nc.dram_tensor("out", [2048, 1024], F32, kind="ExternalOutput").ap()

    uscr = dscr("uscr", [4, 128, 8192], BF16)

    with contextlib.ExitStack() as S0:
        fw = FW(nc, S0)

        _names = {}

        def _uniq(n):
            c = _names.get(n, 0)
            _names[n] = c + 1
            return n if c == 0 else f"{n}__{c}"

        def sb(st, name, shape, dt):
            return st.enter_context(nc.sbuf_tensor(_uniq("s_" + name), list(shape), dt))

        def ps(st, name, shape, dt=F32):
            return st.enter_context(nc.psum_tensor(_uniq("p_" + name), list(shape), dt))

        def dve(fn, r, w):
            return fw.op("dve", fn, r, w)

        def act(fn, r, w):
            return fw.op("act", fn, r, w)

        def pool(fn, r, w):
            return fw.op("pool", fn, r, w)

        def mm(out, lhsT, rhs, start, stop, r, w):
            return fw.op("pe", lambda: T.matmul(out, lhsT=lhsT, rhs=rhs, start=start, stop=stop), r, w, signal=stop)

        def tr(out, in_, ident, r, w, signal=True):
            return fw.op("pe", lambda: T.transpose(out=out, in_=in_, identity=ident), r, w, signal=signal)

        identf = sb(S0, "identf", [128, 128], F32)
        identb = sb(S0, "identb", [128, 128], BF16)
        zerob = sb(S0, "zerob", [128, 128], BF16)
        invf = sb(S0, "invf", [128, 1], F32)
        vcol = sb(S0, "vcol", [128, 64], F32)
        gcol = sb(S0, "gcol", [128, 8], F32)
        bcol = sb(S0, "bcol", [128, 8], F32)
        fw.dma("sp", identf[:], identf_d[:, :], writes=["identf"])
        fw.dma("pool", identb[:], identf_d[:, :], writes=["identb"])
        fw.dma("sp", invf[:], invf_d[:, :], writes=["invf"])
        fw.op("pool", lambda: G.memset(zerob[:], 0.0), [], ["zerob"])
        fw.dma("sp", vcol[:], vcol_d[:, :], writes=["vcol"])
        fw.dma("sp", gcol[:], ln_emb[0, :].rearrange("(dc p) -> p dc", p=128), writes=["gcol"], allow_slow_non_contiguous=True)
        fw.dma("sp", bcol[:], ln_emb[1, :].rearrange("(dc p) -> p dc", p=128), writes=["bcol"], allow_slow_non_contiguous=True)

        SKV = S0.enter_context(contextlib.ExitStack())
        KA = [sb(SKV, f"KA{i}", [128, 8192], BF16) for i in range(2)]
        VsA = sb(SKV, "VsA", [128, NT, 2, 65], BF16)
        KwT = sb(SKV, "KwT", [128, 20 * 128], BF16)
        VwA = sb(SKV, "VwA", [128, 20, 2, 65], BF16)
        CkT = sb(SKV, "CkT", [128, 512], BF16)
        CvA = sb(SKV, "CvA", [128, 4, 2, 65], BF16)
        gyT = sb(SKV, "gyT", [128, 4, 2048], BF16)
        vcc = sb(SKV, "vcc", [128, 4], F32)
        fw.dma("sp", vcc[:], vcc_d[:, :], writes=["vcc"])
        for q4 in range(4):
            fw.dma("pool", KA[0][64:128, q4 * 2048:(q4 + 1) * 2048], epat_d[:, q4 * 2048:(q4 + 1) * 2048], writes=["KA0e"])
            fw.dma("pool", KA[1][0:64, q4 * 2048:(q4 + 1) * 2048], epat_d[:, q4 * 2048:(q4 + 1) * 2048], writes=["KA1e"])

        def layer_norm_T(st_keys, xt_ap, hT_dst, PT, xn, stt, mv, rs, tmpf, gexp, bexp, tag):
            kx = st_keys
            dve(lambda: V.bn_stats(out=stt[:, 0:6], in_=xt_ap[:, 0:512]), [kx], ["stt" + tag])
            dve(lambda: V.bn_stats(out=stt[:, 6:12], in_=xt_ap[:, 512:1024]), [kx], ["stt" + tag])
            dve(lambda: V.bn_aggr(out=mv[:, 0:2], in_=stt[:, 0:12]), ["stt" + tag], ["mv" + tag])
            act(lambda: A.activation(out=rs[:], in_=mv[:, 1:2], func=AF.Sqrt, bias=LN_EPS, scale=1.0), ["mv" + tag], ["rs" + tag])
            dve(lambda: V.reciprocal(out=rs[:], in_=rs[:]), ["rs" + tag], ["rs" + tag])
            dve(lambda: V.tensor_scalar(out=xn[:], in0=xt_ap, scalar1=mv[:, 0:1], scalar2=rs[:, 0:1],
                                        op0=ALU.subtract, op1=ALU.mult), [kx, "mv" + tag, "rs" + tag], ["xn" + tag])
            for dc in range(8):
                tr(PT[:, dc * 128:(dc + 1) * 128], xn[:, dc * 128:(dc + 1) * 128], identb[:],
                   ["xn" + tag, "identb"], ["PT" + tag], signal=(dc == 7))
            dve(lambda: V.tensor_tensor(out=tmpf[:], in0=PT[:], in1=gexp[:], op=ALU.mult), ["PT" + tag, "gexp"], ["tmpf"])
            return tmpf

        with contextlib.ExitStack() as S1:
            kcT = sb(S1, "kcT", [128, 8192], BF16)
            vcT = sb(S1, "vcT", [128, 8192], BF16)
            S1t = S1.enter_context(contextlib.ExitStack())
            Wk = sb(S1t, "Wk", [128, 8, 1280], BF16)
            Wsw = sb(S1t, "Wsw", [128, 8, 3, 128], BF16)
            wv = w_in.rearrange("(dc p) c -> p dc c", p=128)
            for i, (c0, n) in enumerate(((C_KC, 128), (C_KS, 128), (C_KW, 128), (C_VC, 128), (C_VS, 128), (C_VW, 128), (C_U, 512))):
                dst0 = i * 128
                for dc in range(0, 8, 4):
                    fw.dma("pool", Wk[:, dc:dc + 4, dst0:dst0 + n], wv[:, dc:dc + 4, c0:c0 + n], writes=["Wk"])
            pool(lambda: G.memset(Wsw[:], 0.0), [], ["Wsw"])
            for i in range(3):
                for g in range(2):
                    b0 = i * 128 + g * 64
                    dve(lambda: V.tensor_scalar(out=Wsw[:, :, i, g * 64:g * 64 + 8], in0=Wk[:, :, b0 + 8:b0 + 16], scalar1=-1.0,
                                                scalar2=None, op0=ALU.mult), ["Wk", "Wsw"], ["Wsw"])
                    dve(lambda: V.tensor_copy(out=Wsw[:, :, i, g * 64 + 8:g * 64 + 16], in_=Wk[:, :, b0:b0 + 8]), ["Wk", "Wsw"], ["Wsw"])
            gexp = sb(S1t, "gexp", [128, 1024], F32)
            bexp = sb(S1t, "bexp", [128, 1024], F32)
            pool(lambda: G.memset(gexp[:], 1.0), [], ["gexp"])
            pool(lambda: G.memset(bexp[:], 1.0), [], ["bexp"])
            for dc in range(8):
                dve(lambda: V.tensor_scalar(out=gexp[:, dc * 128:(dc + 1) * 128], in0=gexp[:, dc * 128:(dc + 1) * 128],
                                            scalar1=gcol[:, dc:dc + 1], scalar2=None, op0=ALU.mult), ["gexp", "gcol"], ["gexp"])
                dve(lambda: V.tensor_scalar(out=bexp[:, dc * 128:(dc + 1) * 128], in0=bexp[:, dc * 128:(dc + 1) * 128],
                                            scalar1=bcol[:, dc:dc + 1], scalar2=None, op0=ALU.mult), ["bexp", "bcol"], ["bexp"])
            xbuf = [sb(S1t, f"xb{i}", [128, 1024], F32) for i in range(2)]
            xn = [sb(S1t, f"xn{i}", [128, 1024], BF16) for i in range(2)]
            tmpf = [sb(S1t, f"tmpf{i}", [128, 1024], F32) for i in range(1)] * 2
            stt = [sb(S1t, f"stt{i}", [128, 12], F32) for i in range(2)]
            mv = [sb(S1t, f"mv{i}", [128, 2], F32) for i in range(2)]
            rs = [sb(S1t, f"rs{i}", [128, 1], F32) for i in range(2)]
            hTb = [sb(S1t, f"hTb{i}", [128, 8, 512], BF16) for i in range(2)]
            posi = sb(S1t, "posi", [128, 512], I32)
            posf = sb(S1t, "posf", [128, 512], F32)
            angs = sb(S1t, "angs", [128, 512], F32)
            ki = sb(S1t, "ki", [128, 512], I32)
            kf = sb(S1t, "kf", [128, 512], F32)
            r1 = sb(S1t, "r1", [128, 512], F32)
            cosT = [sb(S1t, f"cosT{i}", [128, 512], F32) for i in range(1)] * 2
            sinT = [sb(S1t, f"sinT{i}", [128, 512], F32) for i in range(1)] * 2
            vrow = [sb(S1t, f"vrow{i}", [128, 512], F32) for i in range(2)]
            t1 = [sb(S1t, f"t1_{i}", [128, 512], F32) for i in range(1)] * 2
            t2 = [sb(S1t, f"t2_{i}", [128, 512], F32) for i in range(1)] * 2
            ust = [sb(S1t, f"ust{i}", [128, 512], BF16) for i in range(2)]
            S1p = S1.enter_context(contextlib.ExitStack())
            PT = [ps(S1p, f"PT{i}", [128, 1024], BF16) for i in range(2)]
            PF = [ps(S1p, f"PF{i}", [128, 512], F32) for i in range(4)]
            PV = [ps(S1p, f"PV{i}", [128, 256], F32) for i in range(2)]
            pfi = [0]
            ropei = [0]

            def next_pf():
                i = pfi[0] % 4
                pfi[0] += 1
                return PF[i], f"PF{i}"

            for bi in range(16):
                p2 = bi % 2
                hT = hTb[p2]
                hk = f"hTb{p2}"
                fw.dma("sp", posi[:], posr[:, bi * 512:(bi + 1) * 512].partition_broadcast(128), writes=["posi"])
                fw.dma("sp", vrow[p2][:], vrow_d[:, bi * 512:(bi + 1) * 512].partition_broadcast(128), writes=[f"vrow{p2}"])
                dve(lambda: V.tensor_copy(out=posf[:], in_=posi[:]), ["posi"], ["posf"])
                for which, shift, dst, dk in (("s", 0.0, sinT[p2], "sinT"), ("c", math.pi / 2, cosT[p2], "cosT")):
                    dve(lambda: V.tensor_scalar(out=angs[:], in0=posf[:], scalar1=invf[:, 0:1], scalar2=shift, op0=ALU.mult, op1=ALU.add),
                        ["posf", "invf"], ["angs"])
                    dve(lambda: V.tensor_scalar(out=ki[:], in0=angs[:], scalar1=1.0 / TWO_PI, scalar2=None, op0=ALU.mult), ["angs"], ["ki"])
                    dve(lambda: V.tensor_copy(out=kf[:], in_=ki[:]), ["ki"], ["kf"])
                    dve(lambda: V.scalar_tensor_tensor(out=r1[:], in0=kf[:], scalar=-CW1, in1=angs[:], op0=ALU.mult, op1=ALU.add),
                        ["kf", "angs"], ["r1"])
                    dve(lambda: V.scalar_tensor_tensor(out=r1[:], in0=kf[:], scalar=-CW2, in1=r1[:], op0=ALU.mult, op1=ALU.add),
                        ["kf", "r1"], ["r1"])
                    dve(lambda: V.tensor_scalar(out=r1[:], in0=r1[:], scalar1=-PI_LO, scalar2=PI_LO, op0=ALU.max, op1=ALU.min), ["r1"], ["r1"])
                    act(lambda: A.activation(out=dst[:], in_=r1[:], func=AF.Sin), ["r1"], [dk])
                for tl in range(4):
                    Tt = bi * 4 + tl
                    q2 = Tt % 2
                    tag = str(q2)
                    fw.dma("sp", xbuf[q2][:], xr[Tt * 128:(Tt + 1) * 128, :], writes=[f"xb{q2}"])
                    tf = layer_norm_T(f"xb{q2}", xbuf[q2][:], None, PT[q2], xn[q2], stt[q2], mv[q2], rs[q2], tmpf[q2], gexp, bexp, tag)
                    pool(lambda: G.tensor_tensor(out=hT[:, :, tl * 128:(tl + 1) * 128], in0=tf[:].rearrange("p (a b) -> p a b", a=8),
                                                 in1=bexp[:].rearrange("p (a b) -> p a b", a=8), op=ALU.add),
                         ["tmpf", "bexp"], [hk])
                for i, dstT, dkey, col0 in ((0, kcT, "kcT", bi * 512), (1, None, "KA", bi * 512), (2, KwT, "KwT", (bi - 11) * 512)):
                    if i == 2 and bi < 11:
                        continue
                    pa, pak = next_pf()
                    for dc in range(8):
                        mm(pa[:], Wk[:, dc, i * 128:(i + 1) * 128], hT[:, dc, :], dc == 0, dc == 7, ["Wk", hk], [pak])
                    pb, pbk = next_pf()
                    for dc in range(8):
                        mm(pb[:], Wsw[:, dc, i, :], hT[:, dc, :], dc == 0, dc == 7, ["Wsw", hk], [pbk])
                    ri = ropei[0] % 2
                    ropei[0] += 1
                    dve(lambda: V.tensor_tensor(out=t1[ri][:], in0=pa[:], in1=cosT[p2][:], op=ALU.mult), [pak, "cosT"], ["t1_"])
                    dve(lambda: V.tensor_tensor(out=t2[ri][:], in0=pb[:], in1=sinT[p2][:], op=ALU.mult), [pbk, "sinT"], ["t2_"])
                    if dstT is None:
                        pool(lambda: G.tensor_tensor(out=KA[0][0:64, col0:col0 + 512], in0=t1[ri][0:64, :], in1=t2[ri][0:64, :], op=ALU.add),
                             ["t1_", "t2_"], ["KA0"])
                        pool(lambda: G.tensor_tensor(out=KA[1][64:128, col0:col0 + 512], in0=t1[ri][64:128, :], in1=t2[ri][64:128, :], op=ALU.add),
                             ["t1_", "t2_"], ["KA1"])
                    else:
                        pool(lambda: G.tensor_tensor(out=dstT[:, col0:col0 + 512], in0=t1[ri][:], in1=t2[ri][:], op=ALU.add),
                             ["t1_", "t2_"], [dkey])
                pa, pak = next_pf()
                for dc in range(8):
                    mm(pa[:], Wk[:, dc, 384:512], hT[:, dc, :], dc == 0, dc == 7, ["Wk", hk], [pak])
                act(lambda: A.copy(out=vcT[:, bi * 512:(bi + 1) * 512], in_=pa[:]), [pak], ["vcT"])
                for ct in range(4):
                    pa, pak = next_pf()
                    for dc in range(8):
                        mm(pa[:], Wk[:, dc, 768 + ct * 128:768 + (ct + 1) * 128], hT[:, dc, :], dc == 0, dc == 7, ["Wk", hk], [pak])
                    u2 = ct % 2
                    dve(lambda: V.tensor_tensor(out=ust[u2][:], in0=pa[:], in1=vrow[p2][:], op=ALU.mult), [pak, f"vrow{p2}"], [f"ust{u2}"])
                    fw.dma("sp", uscr[ct, :, bi * 512:(bi + 1) * 512], ust[u2][:], reads=[f"ust{u2}"], writes=["uscr"])
                for tl in range(4):
                    Tt = bi * 4 + tl
                    pv = PV[tl % 2]
                    pvk = f"PV{tl % 2}"
                    for dc in range(8):
                        mm(pv[:], hT[:, dc, tl * 128:(tl + 1) * 128], Wk[:, dc, 512:768], dc == 0, dc == 7, ["Wk", hk], [pvk])
                    act(lambda: A.activation(out=VsA[:, Tt, :, 0:64], in_=pv[:, 0:128].rearrange("p (g d) -> p g d", g=2),
                                             func=AF.Copy, scale=vcol[:, Tt:Tt + 1]), [pvk, "vcol"], ["VsA"])
                    if Tt >= 44:
                        act(lambda: A.activation(out=VwA[:, Tt - 44, :, 0:64], in_=pv[:, 128:256].rearrange("p (g d) -> p g d", g=2),
                                                 func=AF.Copy, scale=vcol[:, Tt:Tt + 1]), [pvk, "vcol"], ["VwA"])
            for g in range(2):
                dve(lambda: V.tensor_copy(out=VsA[:, :, g, 64], in_=vcol[:, :]), ["vcol", "VsA"], ["VsA"])
                dve(lambda: V.tensor_copy(out=VwA[:, :, g, 64], in_=vcol[:, 44:64]), ["vcol", "VwA"], ["VwA"])

            if "p1" in dbg:
                fw.dma("sp", ddbg("d_KA0", [128, 8192], BF16)[:, :], KA[0][:], reads=["KA0", "KA0e"], writes=["d_KA0"])
                fw.dma("sp", ddbg("d_KA1", [128, 8192], BF16)[:, :], KA[1][:], reads=["KA1", "KA1e"], writes=["d_KA1"])
                fw.dma("sp", ddbg("d_kcT", [128, 8192], BF16)[:, :], kcT[:], reads=["kcT"], writes=["d_kcT"])
                fw.dma("sp", ddbg("d_vcT", [128, 8192], BF16)[:, :], vcT[:], reads=["vcT"], writes=["d_vcT"])
                fw.dma("sp", ddbg("d_KwT", [128, 2560], BF16)[:, :], KwT[:], reads=["KwT"], writes=["d_KwT"])
                fw.dma("sp", ddbg("d_VsA", [128, NT * 130], BF16)[:, :], VsA[:].rearrange("p a b c -> p (a b c)"), reads=["VsA"], writes=["d_VsA"])
                fw.dma("sp", ddbg("d_VwA", [128, 20 * 130], BF16)[:, :], VwA[:].rearrange("p a b c -> p (a b c)"), reads=["VwA"], writes=["d_VwA"])
                fw.dma("sp", ddbg("d_u", [4, 128, 8192], BF16)[:, :, :], uscr[:, :, :], reads=["uscr"], writes=["d_u"])
            if upto <= 1:
                fw.finish("sp")
                print("instr", fw.ninstr, "waits", fw.nwait)
                return nc, dbg_out
            fw.barrier()
            S1p.close()
            S1t.close()

            with contextlib.ExitStack() as S2:
                W1 = {}
                for kind, wd in (("k", w_kcmp1), ("v", w_vcmp1)):
                    W1[kind] = sb(S2, "W1" + kind, [128, 32, 128], BF16)
                    src = wd.rearrange("s d f -> d s f")
                    for half in (0, 64):
                        for s0 in range(0, 32, 8):
                            fw.dma("pool", W1[kind][half:half + 64, s0:s0 + 8, :], src[:, s0:s0 + 8, :], writes=["W1" + kind])
                W2p = sb(S2, "W2p", [128, 2, 128], BF16)
                W2v = sb(S2, "W2v", [128, 64], BF16)
                pool(lambda: G.memset(W2p[:], 0.0), [], ["W2p"])
                for g in range(2):
                    fw.dma("pool", W2p[:, g, g * 64:(g + 1) * 64], w_kcmp2[:, :], writes=["W2p"])
                fw.dma("pool", W2v[:], w_vcmp2[:, :], writes=["W2v"])
                peT = {}
                constc = {}
                PSc = ps(S2, "PSc", [128, 8], F32)
                PSh = [ps(S2, f"PSh{i}", [128, 512], F32) for i in range(2)]
                PSck = ps(S2, "PSck", [128, 512], F32)
                PScv = [ps(S2, f"PScv{i}", [128, 64], F32) for i in range(2)]
                for kind, ped in (("k", pe_k), ("v", pe_v)):
                    peT[kind] = sb(S2, "peT" + kind, [64, 32], BF16)
                    fw.dma("pool", peT[kind][:], ped.rearrange("s d -> d s"), writes=["peT" + kind], allow_slow_non_contiguous=True)
                    constc[kind] = sb(S2, "constc" + kind, [128, 1], F32)
                    for s_ in range(32):
                        mm(PSc[:, 0:1], W1[kind][0:64, s_, :], peT[kind][:, s_:s_ + 1], s_ == 0, s_ == 31, ["W1" + kind, "peT" + kind], ["PSc"])
                    act(lambda: A.copy(out=constc[kind][:], in_=PSc[:, 0:1]), ["PSc"], ["constc" + kind])
                hid = {}
                hi = 0
                for kind, srcT, sk in (("k", kcT, "kcT"), ("v", vcT, "vcT")):
                    for g in range(2):
                        hd = sb(S2, f"hid{kind}{g}", [128, 512], BF16)
                        hid[(kind, g)] = hd
                        hk2 = f"hid{kind}{g}"
                        pool(lambda: G.memset(hd[:], 0.0), [], [hk2])
                        ph = PSh[hi % 2]
                        phk = f"PSh{hi % 2}"
                        hi += 1
                        for s_ in range(32):
                            mm(ph[:, 0:511], W1[kind][g * 64:(g + 1) * 64, s_, :], srcT[g * 64:(g + 1) * 64, s_:s_ + 16 * 510 + 1:16],
                               s_ == 0, s_ == 31, ["W1" + kind, sk], [phk])
                        act(lambda: A.activation(out=hd[:, 0:511], in_=ph[:, 0:511], func=AF.Gelu_apprx_tanh, bias=constc[kind][:, 0:1]),
                            [phk, "constc" + kind], [hk2])
                for g in range(2):
                    mm(PSck[:], W2p[:, g, :], hid[("k", g)][:], g == 0, g == 1, ["W2p", f"hidk{g}"], ["PSck"])
                act(lambda: A.copy(out=CkT[:], in_=PSck[:]), ["PSck"], ["CkT"])
                ci = 0
                for g in range(2):
                    for cc in range(4):
                        pc = PScv[ci % 2]
                        pck = f"PScv{ci % 2}"
                        ci += 1
                        mm(pc[:], hid[("v", g)][:, cc * 128:(cc + 1) * 128], W2v[:], True, True, [f"hidv{g}", "W2v"], [pck])
                        act(lambda: A.activation(out=CvA[:, cc, g, 0:64], in_=pc[:], func=AF.Copy, scale=vcc[:, cc:cc + 1]), [pck, "vcc"], ["CvA"])
                    dve(lambda: V.tensor_copy(out=CvA[:, :, g, 64], in_=vcc[:, :]), ["vcc", "CvA"], ["CvA"])
                if "p2" in dbg:
                    fw.dma("sp", ddbg("d_CkT", [128, 512], BF16)[:, :], CkT[:], reads=["CkT"], writes=["d_CkT"])
                    fw.dma("sp", ddbg("d_CvA", [128, 4 * 130], BF16)[:, :], CvA[:].rearrange("p a b c -> p (a b c)"), reads=["CvA"], writes=["d_CvA"])
                fw.barrier()
            if upto <= 2:
                fw.finish("sp")
                print("instr", fw.ninstr, "waits", fw.nwait)
                return nc, dbg_out
        with contextlib.ExitStack() as S3:
            def sincos(st, angt, n, sdst, cdst, tagk):
                a2 = sb(st, "sc_a2" + tagk, [128, n], F32)
                ki_ = sb(st, "sc_ki" + tagk, [128, n], I32)
                kf_ = sb(st, "sc_kf" + tagk, [128, n], F32)
                r_ = sb(st, "sc_r" + tagk, [128, n], F32)
                for shift, dst, dk in ((0.0, sdst, "sc_s" + tagk), (math.pi / 2, cdst, "sc_c" + tagk)):
                    dve(lambda: V.tensor_scalar(out=a2[:], in0=angt[:], scalar1=shift, scalar2=None, op0=ALU.add), ["sc_ang" + tagk], ["sc_a2" + tagk])
                    dve(lambda: V.tensor_scalar(out=ki_[:], in0=a2[:], scalar1=1.0 / TWO_PI, scalar2=None, op0=ALU.mult), ["sc_a2" + tagk], ["sc_ki" + tagk])
                    dve(lambda: V.tensor_copy(out=kf_[:], in_=ki_[:]), ["sc_ki" + tagk], ["sc_kf" + tagk])
                    dve(lambda: V.scalar_tensor_tensor(out=r_[:], in0=kf_[:], scalar=-CW1, in1=a2[:], op0=ALU.mult, op1=ALU.add),
                        ["sc_kf" + tagk, "sc_a2" + tagk], ["sc_r" + tagk])
                    dve(lambda: V.scalar_tensor_tensor(out=r_[:], in0=kf_[:], scalar=-CW2, in1=r_[:], op0=ALU.mult, op1=ALU.add),
                        ["sc_kf" + tagk, "sc_r" + tagk], ["sc_r" + tagk])
                    dve(lambda: V.tensor_scalar(out=r_[:], in0=r_[:], scalar1=-PI_LO, scalar2=PI_LO, op0=ALU.max, op1=ALU.min), ["sc_r" + tagk], ["sc_r" + tagk])
                    act(lambda: A.activation(out=dst[:], in_=r_[:], func=AF.Sin), ["sc_r" + tagk], [dk])

            def tt(out, a, b, op, r, w):
                dve(lambda: V.tensor_tensor(out=out, in0=a, in1=b, op=op), r, w)

            LR = sb(S3, "LR", [128, 32], F32)
            LI = sb(S3, "LI", [128, 32], F32)
            LDT = sb(S3, "LDT", [128, 32], F32)
            for half in (0, 64):
                fw.dma("sp", LR[half:half + 64, :], s5_a_re.rearrange("g n -> n g"), writes=["LR"], allow_slow_non_contiguous=True)
                fw.dma("sp", LI[half:half + 64, :], s5_a_im.rearrange("g n -> n g"), writes=["LI"], allow_slow_non_contiguous=True)
            fw.dma("sp", LDT[:], s5_log_dt[0:1, :].partition_broadcast(128), writes=["LDT"])
            sgn = sb(S3, "sgn", [128, 1], F32)
            Psw = sb(S3, "Psw", [128, 128], F32)
            dcol = sb(S3, "dcol", [128, 4], F32)
            fw.dma("sp", sgn[:], sgn_d[:, :], writes=["sgn"])
            fw.dma("sp", Psw[:], psw_d[:, :], writes=["Psw"])
            fw.dma("sp", dcol[:], s5_d.rearrange("(ct p) -> p ct", p=128), writes=["dcol"], allow_slow_non_contiguous=True)
            stp = sb(S3, "stp", [128, 32], F32)
            mag = sb(S3, "mag", [128, 32], F32)
            ang = sb(S3, "ang", [128, 32], F32)
            sn = sb(S3, "sn", [128, 32], F32)
            cs = sb(S3, "cs", [128, 32], F32)
            abr = sb(S3, "abr", [128, 32], F32)
            abi = sb(S3, "abi", [128, 32], F32)
            den = sb(S3, "den", [128, 32], F32)
            tq = sb(S3, "tq", [128, 32], F32)
            nr = sb(S3, "nr", [128, 32], F32)
            cre = sb(S3, "cre", [128, 32], F32)
            cim = sb(S3, "cim", [128, 32], F32)
            act(lambda: A.activation(out=stp[:], in_=LDT[:], func=AF.Exp), ["LDT"], ["stp"])
            tt(mag[:], LR[:], stp[:], ALU.mult, ["LR", "stp"], ["mag"])
            act(lambda: A.activation(out=mag[:], in_=mag[:], func=AF.Exp), ["mag"], ["mag"])
            tt(ang[:], LI[:], stp[:], ALU.mult, ["LI", "stp"], ["sc_angp"])
            sincos(S3, ang, 32, sn, cs, "p")
            tt(abr[:], mag[:], cs[:], ALU.mult, ["mag", "sc_cp"], ["abr"])
            tt(abi[:], mag[:], sn[:], ALU.mult, ["mag", "sc_sp"], ["abi"])
            tt(den[:], LR[:], LR[:], ALU.mult, ["LR"], ["den"])
            tt(tq[:], LI[:], LI[:], ALU.mult, ["LI"], ["tq"])
            tt(den[:], den[:], tq[:], ALU.add, ["den", "tq"], ["den"])
            dve(lambda: V.reciprocal(out=den[:], in_=den[:]), ["den"], ["den"])
            dve(lambda: V.tensor_scalar(out=nr[:], in0=abr[:], scalar1=-1.0, scalar2=None, op0=ALU.add), ["abr"], ["nr"])
            tt(cre[:], nr[:], LR[:], ALU.mult, ["nr", "LR"], ["cre"])
            tt(tq[:], abi[:], LI[:], ALU.mult, ["abi", "LI"], ["tq"])
            tt(cre[:], cre[:], tq[:], ALU.add, ["cre", "tq"], ["cre"])
            tt(cre[:], cre[:], den[:], ALU.mult, ["cre", "den"], ["cre"])
            tt(cim[:], abi[:], LR[:], ALU.mult, ["abi", "LR"], ["cim"])
            tt(tq[:], nr[:], LI[:], ALU.mult, ["nr", "LI"], ["tq"])
            tt(cim[:], cim[:], tq[:], ALU.subtract, ["cim", "tq"], ["cim"])
            tt(cim[:], cim[:], den[:], ALU.mult, ["cim", "den"], ["cim"])
            NL = 12
            WR = sb(S3, "WR", [128, NL, 32], F32)
            WIS = sb(S3, "WIS", [128, NL, 32], F32)
            dve(lambda: V.tensor_copy(out=WR[:, 0, :], in_=abr[:]), ["abr"], ["WR"])
            dve(lambda: V.tensor_scalar(out=WIS[:, 0, :], in0=abi[:], scalar1=sgn[:, 0:1], scalar2=None, op0=ALU.mult), ["abi", "sgn"], ["WIS"])
            for l in range(1, NL):
                tt(tq[:], WR[:, l - 1, :], WR[:, l - 1, :], ALU.mult, ["WR"], ["tq"])
                tt(nr[:], WIS[:, l - 1, :], WIS[:, l - 1, :], ALU.mult, ["WIS"], ["nr"])
                tt(WR[:, l, :], tq[:], nr[:], ALU.subtract, ["tq", "nr", "WR"], ["WR"])
                dve(lambda: V.scalar_tensor_tensor(out=WIS[:, l, :], in0=WR[:, l - 1, :], scalar=2.0, in1=WIS[:, l - 1, :], op0=ALU.mult, op1=ALU.mult),
                    ["WR", "WIS"], ["WIS"])
            Bpad = sb(S3, "Bpad", [128, 32, 128], BF16)
            Cpad = sb(S3, "Cpad", [128, 32, 128], BF16)
            with contextlib.ExitStack() as S3a:
                PSs = [ps(S3a, f"PSs{i}", [128, 128], F32) for i in range(2)]
                BR = sb(S3a, "BR", [64, 32, 16], F32)
                BI = sb(S3a, "BI", [64, 32, 16], F32)
                fw.dma("sp", BR[:], s5_b_re.rearrange("g n p -> n g p"), writes=["BR"])
                fw.dma("sp", BI[:], s5_b_im.rearrange("g n p -> n g p"), writes=["BI"])
                bbr = sb(S3a, "bbr", [64, 32, 16], F32)
                bbi = sb(S3a, "bbi", [64, 32, 16], F32)
                tb_ = sb(S3a, "tb_", [64, 32, 16], F32)
                creb = cre[0:64, :].unsqueeze(2).broadcast_to([64, 32, 16])
                cimb = cim[0:64, :].unsqueeze(2).broadcast_to([64, 32, 16])
                tt(bbr[:], BR[:], creb, ALU.mult, ["BR", "cre"], ["bbr"])
                tt(tb_[:], BI[:], cimb, ALU.mult, ["BI", "cim"], ["tb_"])
                tt(bbr[:], bbr[:], tb_[:], ALU.subtract, ["bbr", "tb_"], ["bbr"])
                tt(bbi[:], BI[:], creb, ALU.mult, ["BI", "cre"], ["bbi"])
                tt(tb_[:], BR[:], cimb, ALU.mult, ["BR", "cim", "bbr"], ["tb_"])
                tt(bbi[:], bbi[:], tb_[:], ALU.add, ["bbi", "tb_"], ["bbi"])
                ZpR = sb(S3a, "ZpR", [64, 32, 128], F32)
                ZpI = sb(S3a, "ZpI", [64, 32, 128], F32)
                pool(lambda: G.memset(ZpR[:], 0.0), [], ["ZpR"])
                pool(lambda: G.memset(ZpI[:], 0.0), [], ["ZpI"])
                for gl in range(8):
                    for Z, bb, zk, bk in ((ZpR, bbr, "ZpR", "bbr"), (ZpI, bbi, "ZpI", "bbi")):
                        dve(lambda: V.tensor_copy(out=Z[:].rearrange("n (ct gl) c -> n ct gl c", gl=8)[:, :, gl, 16 * gl:16 * gl + 16],
                                                  in_=bb[:].rearrange("n (ct gl) p -> n ct gl p", gl=8)[:, :, gl, :]), [bk, zk], [zk])
                for g in range(32):
                    pz = PSs[g % 2]
                    pzk = f"PSs{g % 2}"
                    tr(pz[:, 0:64], ZpR[:, g, :], identf[0:64, 0:64], ["ZpR", "identf"], [pzk], signal=False)
                    tr(pz[:, 64:128], ZpI[:, g, :], identf[0:64, 0:64], ["ZpI", "identf"], [pzk])
                    act(lambda: A.copy(out=Bpad[:, g, :], in_=pz[:]), [pzk], ["Bpad"])
                Cnat = sb(S3a, "Cnat", [128, 4, 128], F32)
                fw.dma("sp", Cnat[:, :, 0:64], s5_c_re.rearrange("(ct gl) p n -> (gl p) ct n", gl=8), writes=["Cnat"])
                fw.dma("sp", Cnat[:, :, 64:128], s5_c_im.rearrange("(ct gl) p n -> (gl p) ct n", gl=8), writes=["Cnat"])
                dve(lambda: V.tensor_scalar(out=Cnat[:, :, 64:128], in0=Cnat[:, :, 64:128], scalar1=-1.0, scalar2=None, op0=ALU.mult), ["Cnat"], ["Cnat"])
                pool(lambda: G.memset(Cpad[:], 0.0), [], ["Cpad"])
                for ct in range(4):
                    pz = PSs[ct % 2]
                    pzk = f"PSs{ct % 2}"
                    tr(pz[:, :], Cnat[:, ct, :], identf[:], ["Cnat", "identf"], [pzk])
                    for gl in range(8):
                        act(lambda: A.copy(out=Cpad[:, ct * 8 + gl, 16 * gl:16 * gl + 16], in_=pz[:, 16 * gl:16 * gl + 16]), [pzk, "Cpad"], ["Cpad"])
                fw.barrier()
            uT = [sb(S3, f"uT{i}", [128, 8192], BF16) for i in range(2)]
            X0 = [sb(S3, f"X0_{i}", [128, 8192], BF16) for i in range(2)]
            Ya = [sb(S3, f"Ya{i}", [128, 3072], BF16) for i in range(2)]
            Yb = [sb(S3, f"Yb{i}", [128, 1536], BF16) for i in range(2)]
            Wa = [sb(S3, f"Wa{i}", [128, 2048], BF16) for i in range(2)]
            Wb = [sb(S3, f"Wb{i}", [128, 2048], BF16) for i in range(2)]
            Rm = [sb(S3, f"Rm{i}", [128, NL, 128], BF16) for i in range(2)]
            rtmp = sb(S3, "rtmp", [128, 128], F32)
            xin = [sb(S3, f"xin{i}", [128, 2], BF16) for i in range(2)]
            ytmp = sb(S3, "ytmp", [128, 512], F32)
            yps = [ps(S3, f"yps{i}", [128, 512], F32) for i in range(4)]
            PB = [ps(S3, f"PB{i}", [128, 512], F32) for i in range(4)]
            evi = [0]
            pbi = [0, 0]

            def evac(out, in_, r, w):
                evi[0] += 1
                if evi[0] % 2:
                    act(lambda: A.copy(out=out, in_=in_), r, w)
                else:
                    dve(lambda: V.tensor_copy(out=out, in_=in_), r, w)

            def next_pb(gp):
                i = 2 * gp + pbi[gp] % 2
                pbi[gp] += 1
                return PB[i], f"PB{i}"

            for ct in range(4):
                u = uT[ct % 2]
                uk = f"uT{ct % 2}"
                for q4 in range(4):
                    fw.dma("sp", u[:, q4 * 2048:(q4 + 1) * 2048], uscr[ct, :, q4 * 2048:(q4 + 1) * 2048], reads=["uscr"], writes=[uk])
                def s5_group(ct, gl, u, uk):
                    g = ct * 8 + gl
                    gp = g % 2
                    R = Rm[gp]
                    rk = f"Rm{gp}"
                    for l in range(NL):
                        dve(lambda: V.tensor_scalar(out=rtmp[:], in0=identf[:], scalar1=WR[:, l, g:g + 1], scalar2=None, op0=ALU.mult),
                            ["identf", "WR"], ["rtmp"])
                        dve(lambda: V.scalar_tensor_tensor(out=R[:, l, :], in0=Psw[:], scalar=WIS[:, l, g:g + 1], in1=rtmp[:], op0=ALU.mult, op1=ALU.add),
                            ["Psw", "WIS", "rtmp"], [rk])
                    x0 = X0[gp]
                    x0k = f"X0_{gp}"
                    for tb in range(16):
                        pb, pbk = next_pb(gp)
                        mm(pb[:], Bpad[:, g, :], u[:, tb * 512:(tb + 1) * 512], True, True, ["Bpad", uk], [pbk])
                        evac(x0[:, tb * 512:(tb + 1) * 512], pb[:], [pbk], [x0k])
                        yield
                    src, srck = x0, x0k
                    bufs = [(Ya[gp], f"Ya{gp}"), (Yb[gp], f"Yb{gp}")]
                    n = 6144
                    l = 0
                    bsel = 0
                    while n > 3:
                        half = n // 2
                        dst, dstk = bufs[bsel]
                        for c0 in range(0, half, 512):
                            w_ = min(512, half - c0)
                            pb, pbk = next_pb(gp)
                            mm(pb[:, 0:w_], R[:, l, :], src[:, 2 * c0:2 * c0 + 2 * w_:2], True, False, [rk, srck], [pbk])
                            mm(pb[:, 0:w_], identb[:], src[:, 2 * c0 + 1:2 * c0 + 2 * w_:2], False, True, ["identb", srck], [pbk])
                            evac(dst[:, c0:c0 + w_], pb[:, 0:w_], [pbk], [dstk])
                            yield
                        src, srck = dst, dstk
                        bsel ^= 1
                        n = half
                        l += 1
                    assert l == 11 and n == 3
                    xi = xin[gp]
                    xik = f"xin{gp}"
                    pb, pbk = next_pb(gp)
                    mm(pb[:, 0:1], R[:, 11, :], src[:, 0:1], True, False, [rk, srck], [pbk])
                    mm(pb[:, 0:1], identb[:], src[:, 1:2], False, True, ["identb", srck], [pbk])
                    evac(xi[:, 0:1], pb[:, 0:1], [pbk], [xik])
                    yield
                    pb, pbk = next_pb(gp)
                    mm(pb[:, 0:1], R[:, 11, :], xi[:, 0:1], True, False, [rk, xik], [pbk])
                    mm(pb[:, 0:1], identb[:], src[:, 2:3], False, True, ["identb", srck], [pbk])
                    evac(xi[:, 1:2], pb[:, 0:1], [pbk], [xik])
                    yield
                    pb, pbk = next_pb(gp)
                    mm(pb[:, 0:1], R[:, 0, :], xi[:, 1:2], True, False, [rk, xik], [pbk])
                    mm(pb[:, 0:1], identb[:], x0[:, 6144:6145], False, True, ["identb", x0k], [pbk])
                    evac(x0[:, 6144:6145], pb[:, 0:1], [pbk], [x0k])
                    yield
                    cur, curk, off = x0, x0k, 6144
                    wb = [(Wa[gp], f"Wa{gp}"), (Wb[gp], f"Wb{gp}")]
                    for l in range(11):
                        k_ = 1 << l
                        dst, dstk = wb[l % 2]
                        for blk in range(4):
                            c0 = blk * 512
                            pb, pbk = next_pb(gp)
                            has2 = c0 + 512 > k_
                            mm(pb[:], identb[:], cur[:, off + c0:off + c0 + 512], True, not has2, ["identb", curk], [pbk])
                            if has2:
                                lo = max(c0, k_)
                                mm(pb[:, lo - c0:512], R[:, l, :], cur[:, off + lo - k_:off + c0 + 512 - k_], False, True, [rk, curk], [pbk])
                            evac(dst[:, c0:c0 + 512], pb[:], [pbk], [dstk])
                            yield
                        cur, curk, off = dst, dstk, 0
                    for blk in range(4):
                        mm(yps[blk][:], Cpad[:, g, :], cur[:, blk * 512:(blk + 1) * 512], gl == 0, gl == 7, ["Cpad", curk], [f"yps{blk}"])

                for gpair in range(4):
                    gens = [s5_group(ct, 2 * gpair, u, uk), s5_group(ct, 2 * gpair + 1, u, uk)]
                    alive = [True, True]
                    while any(alive):
                        for gi_ in range(2):
                            if alive[gi_]:
                                try:
                                    next(gens[gi_])
                                except StopIteration:
                                    alive[gi_] = False
                for blk in range(4):
                    dve(lambda: V.scalar_tensor_tensor(out=ytmp[:], in0=u[:, 6144 + blk * 512:6144 + (blk + 1) * 512], scalar=dcol[:, ct:ct + 1],
                                                       in1=yps[blk][:], op0=ALU.mult, op1=ALU.add), [uk, "dcol", f"yps{blk}"], ["ytmp"])
                    act(lambda: A.activation(out=gyT[:, ct, blk * 512:(blk + 1) * 512], in_=ytmp[:], func=AF.Gelu_apprx_tanh), ["ytmp"], ["gyT"])
            if "p3" in dbg:
                fw.dma("sp", ddbg("d_gyT", [128, 4 * 2048], BF16)[:, :], gyT[:].rearrange("p a b -> p (a b)"), reads=["gyT"], writes=["d_gyT"])
            fw.barrier()
        if upto <= 3:
            fw.finish("sp")
            print("instr", fw.ninstr, "waits", fw.nwait)
            return nc, dbg_out

        with contextlib.ExitStack() as S4:
            tri = sb(S4, "tri", [128, 512], BF16)
            atri = sb(S4, "atri", [128, 512], BF16)
            fw.dma("pool", tri[:], tri_d[:, :], writes=["tri"])
            fw.dma("pool", atri[:], atri_d[:, :], writes=["atri"])
            grow = sb(S4, "grow", [128, 1024], F32)
            brow = sb(S4, "brow", [128, 1024], F32)
            g1row = sb(S4, "g1row", [128, 1024], F32)
            b1row = sb(S4, "b1row", [128, 1024], F32)
            fw.dma("sp", grow[:], ln_emb[0:1, :].partition_broadcast(128), writes=["grow"])
            fw.dma("sp", brow[:], ln_emb[1:2, :].partition_broadcast(128), writes=["brow"])
            fw.dma("sp", g1row[:], ln1_d[0:1, :].partition_broadcast(128), writes=["g1row"])
            fw.dma("sp", b1row[:], ln1_d[1:2, :].partition_broadcast(128), writes=["b1row"])
            Wo = sb(S4, "Wo", [128, 8, 1024], BF16)
            wov = w_o.rearrange("(kc p) c -> p kc c", p=128)
            for kc in range(0, 8, 2):
                fw.dma("pool", Wo[:, kc:kc + 2, :], wov[:, kc:kc + 2, :], writes=["Wo"])
            KmT = sb(S4, "KmT", [128, 4, 256], BF16)
            VmA = sb(S4, "VmA", [128, 2, 4, 129], BF16)
            wv = w_in.rearrange("(dc p) c -> p dc c", p=128)
            PS_S = [ps(S4, f"PS_S{i}", [128, 512], F32) for i in range(2)]
            PO = [ps(S4, f"PO{i}", [128, 512], F32) for i in range(2)]
            PF = [ps(S4, f"PF4_{i}", [128, 512], F32) for i in range(2)]
            PT4 = ps(S4, "PT4", [128, 1024], BF16)
            PX = ps(S4, "PX", [128, 512], F32)
            PS_S = PS_S + [PF[1]]
            cnt = {"s": 0, "o": 0, "f": 0, "pt": 0, "wp": 0}

            def nxt(lst, key, name):
                i = cnt[key] % len(lst)
                cnt[key] += 1
                if name == "PS_S" and i == 2:
                    return lst[i], "PF4_1"
                return lst[i], f"{name}{i}"

            with contextlib.ExitStack() as S4m:
                memf = sb(S4m, "memf", [128, 2, 1024], F32)
                memb = sb(S4m, "memb", [128, 2, 1024], BF16)
                memT = sb(S4m, "memT", [128, 8, 256], BF16)
                Wmk = sb(S4m, "Wmk", [128, 8, 512], BF16)
                Wmv = sb(S4m, "Wmv", [128, 8, 512], BF16)
                fw.dma("sp", memf[:], mem_d.rearrange("(t p) c -> p t c", p=128), writes=["memf"])
                wmv_ = w_mem_kv.rearrange("(dc p) c -> p dc c", p=128)
                for dc in range(0, 8, 4):
                    fw.dma("pool", Wmk[:, dc:dc + 4, :], wmv_[:, dc:dc + 4, 0:512], writes=["Wmk"])
                    fw.dma("pool", Wmv[:, dc:dc + 4, :], wmv_[:, dc:dc + 4, 512:1024], writes=["Wmv"])
                dve(lambda: V.tensor_copy(out=memb[:], in_=memf[:]), ["memf"], ["memb"])
                for mt in range(2):
                    for dc in range(8):
                        tr(PT4[:, dc * 128:(dc + 1) * 128], memb[:, mt, dc * 128:(dc + 1) * 128], identb[:], ["memb", "identb"], ["PT4"], signal=(dc == 7))
                    act(lambda: A.copy(out=memT[:, :, mt * 128:(mt + 1) * 128], in_=PT4[:].rearrange("p (a b) -> p a b", a=8)), ["PT4"], ["memT"])
                for h in range(4):
                    pf, pfk = nxt(PF, "f", "PF4_")
                    for dc in range(8):
                        mm(pf[:, 0:256], Wmk[:, dc, h * 128:(h + 1) * 128], memT[:, dc, :], dc == 0, dc == 7, ["Wmk", "memT"], [pfk])
                    act(lambda: A.copy(out=KmT[:, h, :], in_=pf[:, 0:256]), [pfk], ["KmT"])
                for mc in range(2):
                    pf, pfk = nxt(PF, "f", "PF4_")
                    for dc in range(8):
                        mm(pf[:], memT[:, dc, mc * 128:(mc + 1) * 128], Wmv[:, dc, :], dc == 0, dc == 7, ["Wmv", "memT"], [pfk])
                    act(lambda: A.copy(out=VmA[:, mc, :, 0:128], in_=pf[:].rearrange("p (h d) -> p h d", h=4)), [pfk], ["VmA"])
                pool(lambda: G.memset(VmA[:, :, :, 128:129], 1.0), ["VmA"], ["VmA"])
                fw.barrier()

            hTq = sb(S4, "hTq", [128, 8, 512], BF16)
            QT = sb(S4, "QT", [128, 4, 4, 128], BF16)
            qmT = sb(S4, "qmT", [128, 4, 4, 128], BF16)
            gn = sb(S4, "gn", [128, 4, 24], F32)
            oT = sb(S4, "oT", [128, 4, 512], BF16)
            omT = sb(S4, "omT", [128, 4, 512], BF16)
            mrgT = sb(S4, "mrgT", [128, 8, 512], BF16)
            st4 = sb(S4, "st4", [128, 12], F32)
            mv4 = sb(S4, "mv4", [128, 2], F32)
            rs4 = sb(S4, "rs4", [128, 1], F32)
            def ln_tok(xt, xk, dst, dstk, grow_, brow_, gk, bk):
                dve(lambda: V.bn_stats(out=st4[:, 0:6], in_=xt[:, 0:512]), [xk], ["st4"])
                dve(lambda: V.bn_stats(out=st4[:, 6:12], in_=xt[:, 512:1024]), [xk], ["st4"])
                dve(lambda: V.bn_aggr(out=mv4[:, 0:2], in_=st4[:, 0:12]), ["st4"], ["mv4"])
                act(lambda: A.activation(out=rs4[:], in_=mv4[:, 1:2], func=AF.Sqrt, bias=LN_EPS, scale=1.0), ["mv4"], ["rs4"])
                dve(lambda: V.reciprocal(out=rs4[:], in_=rs4[:]), ["rs4"], ["rs4"])
                dve(lambda: V.tensor_scalar(out=dst[:], in0=xt[:], scalar1=mv4[:, 0:1], scalar2=rs4[:, 0:1], op0=ALU.subtract, op1=ALU.mult),
                    [xk, "mv4", "rs4"], [dstk])
                pool(lambda: G.tensor_tensor(out=dst[:], in0=dst[:], in1=grow_[:], op=ALU.mult), [dstk, gk], [dstk])
                pool(lambda: G.tensor_tensor(out=dst[:], in0=dst[:], in1=brow_[:], op=ALU.add), [dstk, bk], [dstk])

            wpid = [0]
            curB = [0]

            def wpiece(src_ap):
                w_, wk_ = nxt(wp, "wp", "wp")
                nk_ = src_ap.shape[1]
                pid = wpid[0]
                wpid[0] += 1
                sc_ = wscr[pid, :, 0:nk_ * 128].rearrange("p (k c) -> p k c", k=nk_)
                if curB[0] == 0:
                    fw.dma("pool", w_[:, 0:nk_, :], src_ap, writes=[wk_])
                    fw.dma("sp", sc_, w_[:, 0:nk_, :], reads=[wk_], writes=[f"wscr{pid}"])
                else:
                    fw.dma("sp", w_[:, 0:nk_, :], sc_, reads=[f"wscr{pid}"], writes=[wk_])
                return w_, wk_

            wno_v = w_nsa_out.rearrange("(kc p) c -> p kc c", p=128)
            wmo_v = w_mem_out.rearrange("(kc p) c -> p kc c", p=128)
            wgl_v = w_s5_glu.rearrange("(kc p) c -> p kc c", p=128)

            for B in range(4):
                wpid[0] = 0
                curB[0] = B
                with contextlib.ExitStack() as SA:
                    xq = [sb(SA, f"xq{i}", [128, 1024], F32) for i in range(1)]
                    h0t = sb(SA, "h0t", [128, 1024], F32)
                    h0b = sb(SA, "h0b", [128, 1024], BF16)
                    posi4 = sb(SA, "posi4", [128, 512], I32)
                    posf4 = sb(SA, "posf4", [128, 512], F32)
                    angs4 = sb(SA, "sc_angq", [128, 512], F32)
                    cos4 = sb(SA, "cos4", [128, 512], F32)
                    sin4 = sb(SA, "sin4", [128, 512], F32)
                    t14 = sb(SA, "t14", [128, 512], F32)
                    t24 = sb(SA, "t24", [128, 512], F32)
                    Wq4 = sb(SA, "Wq4", [128, 8, 4, 128], BF16)
                    Wqsw = sb(SA, "Wqsw", [128, 8, 4, 128], BF16)
                    Wgn = sb(SA, "Wgn", [128, 8, 24], BF16)
                    if B == 0:
                        for half in range(2):
                            for dc in range(8):
                                fw.dma("pool", Wq4[:, dc, :, half * 64:(half + 1) * 64],
                                       wv[:, dc, half * 256:(half + 1) * 256].rearrange("p (h d) -> p h d", h=4), writes=["Wq4"])
                        fw.dma("pool", Wgn[:], wv[:, :, C_GN:C_GN + 24], writes=["Wgn"])
                        pool(lambda: G.memset(Wqsw[:], 0.0), [], ["Wqsw"])
                        for half in range(2):
                            b0 = half * 64
                            dve(lambda: V.tensor_scalar(out=Wqsw[:, :, :, b0:b0 + 8], in0=Wq4[:, :, :, b0 + 8:b0 + 16], scalar1=-1.0, scalar2=None, op0=ALU.mult),
                                ["Wq4", "Wqsw"], ["Wqsw"])
                            dve(lambda: V.tensor_copy(out=Wqsw[:, :, :, b0 + 8:b0 + 16], in_=Wq4[:, :, :, b0:b0 + 8]), ["Wq4", "Wqsw"], ["Wqsw"])
                        fw.dma("sp", wqscr[0, :, :], Wq4[:].rearrange("p a b c -> p (a b c)"), reads=["Wq4"], writes=["wqscr0"])
                        fw.dma("sp", wqscr[1, :, :], Wqsw[:].rearrange("p a b c -> p (a b c)"), reads=["Wqsw"], writes=["wqscr1"])
                        fw.dma("sp", wgnscr[:, :], Wgn[:].rearrange("p a b -> p (a b)"), reads=["Wgn"], writes=["wgnscr"])
                    else:
                        fw.dma("sp", Wq4[:].rearrange("p a b c -> p (a b c)"), wqscr[0, :, :], reads=["wqscr0"], writes=["Wq4"])
                        fw.dma("sp", Wqsw[:].rearrange("p a b c -> p (a b c)"), wqscr[1, :, :], reads=["wqscr1"], writes=["Wqsw"])
                        fw.dma("sp", Wgn[:].rearrange("p a b -> p (a b)"), wgnscr[:, :], reads=["wgnscr"], writes=["Wgn"])

                    fw.dma("sp", posi4[:], posr[:, 6144 + B * 512:6144 + (B + 1) * 512].partition_broadcast(128), writes=["posi4"])
                    dve(lambda: V.tensor_copy(out=posf4[:], in_=posi4[:]), ["posi4"], ["posf4"])
                    dve(lambda: V.tensor_scalar(out=angs4[:], in0=posf4[:], scalar1=invf[:, 0:1], scalar2=None, op0=ALU.mult), ["posf4", "invf"], ["sc_angq"])
                    with contextlib.ExitStack() as Ssc:
                        sincos(Ssc, angs4, 512, sin4, cos4, "q")
                        fw.barrier()
                    Wqm = sb(SA, "Wqm", [128, 8, 512], BF16)
                    if B == 0:
                        for dc in range(0, 8, 4):
                            fw.dma("pool", Wqm[:, dc:dc + 4, :], wv[:, dc:dc + 4, C_QM:C_QM + 512], writes=["Wqm"])
                        fw.dma("sp", wqscr[2, :, :], Wqm[:].rearrange("p a b -> p (a b)"), reads=["Wqm"], writes=["wqscr2"])
                    else:
                        fw.dma("sp", Wqm[:].rearrange("p a b -> p (a b)"), wqscr[2, :, :], reads=["wqscr2"], writes=["Wqm"])
                    for tl in range(4):
                        Tt = OWN0 + B * 4 + tl
                        xt, xk = nxt(xq, "pt", "xq")
                        fw.dma("sp", xt[:], xr[Tt * 128:(Tt + 1) * 128, :], writes=[xk])
                        ln_tok(xt, xk, h0t, "h0t", grow, brow, "grow", "brow")
                        dve(lambda: V.tensor_copy(out=h0b[:], in_=h0t[:]), ["h0t"], ["h0b"])
                        for dc in range(8):
                            tr(PT4[:, dc * 128:(dc + 1) * 128], h0b[:, dc * 128:(dc + 1) * 128], identb[:], ["h0b", "identb"], ["PT4"], signal=(dc == 7))
                        act(lambda: A.copy(out=hTq[:, :, tl * 128:(tl + 1) * 128], in_=PT4[:].rearrange("p (a b) -> p a b", a=8)), ["PT4"], ["hTq"])
                    if "p4o" in dbg and B == 0:
                        fw.dma("sp", ddbg("d_Wq4", [128, 4096], BF16)[:, :], Wq4[:].rearrange("p a b c -> p (a b c)"), reads=["Wq4"], writes=["d_Wq4"])
                        fw.dma("sp", ddbg("d_Wqsw", [128, 4096], BF16)[:, :], Wqsw[:].rearrange("p a b c -> p (a b c)"), reads=["Wqsw"], writes=["d_Wqsw"])
                        fw.dma("sp", ddbg("d_hTq", [128, 4096], BF16)[:, :], hTq[:].rearrange("p a b -> p (a b)"), reads=["hTq"], writes=["d_hTq"])
                        fw.dma("sp", ddbg("d_cos4", [128, 512], F32)[:, :], cos4[:], reads=["sc_cq"], writes=["d_cos4"])
                        fw.dma("sp", ddbg("d_sin4", [128, 512], F32)[:, :], sin4[:], reads=["sc_sq"], writes=["d_sin4"])
                    for hh in range(4):
                        pa, pak = nxt(PF, "f", "PF4_")
                        for dc in range(8):
                            mm(pa[:], Wq4[:, dc, hh, :], hTq[:, dc, :], dc == 0, dc == 7, ["Wq4", "hTq"], [pak])
                        pb, pbk = nxt(PF, "f", "PF4_")
                        for dc in range(8):
                            mm(pb[:], Wqsw[:, dc, hh, :], hTq[:, dc, :], dc == 0, dc == 7, ["Wqsw", "hTq"], [pbk])
                        dve(lambda: V.tensor_tensor(out=t14[:], in0=pa[:], in1=cos4[:], op=ALU.mult), [pak, "sc_cq"], ["t14"])
                        dve(lambda: V.tensor_tensor(out=t24[:], in0=pb[:], in1=sin4[:], op=ALU.mult), [pbk, "sc_sq"], ["t24"])
                        pool(lambda: G.tensor_tensor(out=QT[:, :, hh, :], in0=t14[:].rearrange("p (t q) -> p t q", t=4),
                                                     in1=t24[:].rearrange("p (t q) -> p t q", t=4), op=ALU.add), ["t14", "t24"], ["QT"])
                    for h in range(4):
                        pa, pak = nxt(PF, "f", "PF4_")
                        for dc in range(8):
                            mm(pa[:], Wqm[:, dc, h * 128:(h + 1) * 128], hTq[:, dc, :], dc == 0, dc == 7, ["Wqm", "hTq"], [pak])
                        act(lambda: A.copy(out=qmT[:, :, h, :], in_=pa[:].rearrange("p (t q) -> p t q", t=4)), [pak], ["qmT"])
                    for tl in range(4):
                        pa, pak = nxt(PF, "f", "PF4_")
                        for dc in range(8):
                            mm(pa[:, 0:24], hTq[:, dc, tl * 128:(tl + 1) * 128], Wgn[:, dc, :], dc == 0, dc == 7, ["Wgn", "hTq"], [pak])
                        act(lambda: A.activation(out=gn[:, tl, :], in_=pa[:, 0:24], func=AF.Sigmoid), [pak], ["gn"])

                    fw.barrier()
                with contextlib.ExitStack() as SB:
                    cbA = [sb(SB, f"cbA{i}", [128, 512], BF16) for i in range(2)]
                    cbB = [sb(SB, f"cbB{i}", [128, 2, 512], BF16) for i in range(2)]
                    addb = [sb(SB, f"addb{i}", [128, 128], F32) for i in range(2)]
                    futb = [sb(SB, f"futb{i}", [128, 128], F32) for i in range(2)]
                    Pa = [sb(SB, f"Pa{i}", [128, 512], F32) for i in range(2)]
                    PTb = [sb(SB, f"PTb{i}", [128, 512], BF16) for i in range(4)]
                    imp = sb(SB, "imp", [128, 512], F32)
                    chk = sb(SB, "chk", [128, 512], F32)
                    sc2 = sb(SB, "sc2", [128, 128], F32)
                    sc3 = sb(SB, "sc3", [128, 128], F32)
                    m8a = sb(SB, "m8a", [128, 8], F32)
                    m8b = sb(SB, "m8b", [128, 8], F32)
                    selb = sb(SB, "selb", [128, 128], F32)
                    selsw = sb(SB, "selsw", [128, 128], F32)
                    QaA = [sb(SB, f"QaA{i}", [128, 4, 128], BF16) for i in range(2)]
                    QaB = [sb(SB, f"QaB{i}", [128, 4, 128], BF16) for i in range(2)]
                    rsum = sb(SB, "rsum", [128, 4], F32)
                    rinv = sb(SB, "rinv", [128, 4], F32)
                    coef = sb(SB, "coef", [128, 4], F32)
                    oacc = sb(SB, "oacc", [128, 512], F32)
                    otmp = sb(SB, "otmp", [128, 256], F32)
                    oaccb = sb(SB, "oaccb", [128, 512], BF16)
                    omacc = sb(SB, "omacc", [128, 512], F32)
                    omb = sb(SB, "omb", [128, 512], BF16)
                    for tl in range(4):
                        j = B * 4 + tl
                        qt = OWN0 + j
                        c2 = j % 2
                        fw.dma("pool", cbA[c2][:], cba_d[j, :, :], writes=[f"cbA{c2}"])
                        fw.dma("pool", cbB[c2][:], cbb_d[j, :, :, :].rearrange("a p c -> p a c"), writes=[f"cbB{c2}"])
                        fw.dma("sp", addb[c2][:], addb_d[j, :, :], writes=[f"addb{c2}"])
                        fw.dma("sp", futb[c2][:], futb_d[j, :, :], writes=[f"futb{c2}"])
                        for g in range(2):
                            r0 = g * 64
                            Qg = QT[r0:r0 + 64, tl, :, :].rearrange("p h q -> p (h q)")
                            for hh in range(4):
                                pS, pSk = nxt(PS_S, "s", "PS_S")
                                mm(pS[:], QT[r0:r0 + 64, tl, hh, :], CkT[r0:r0 + 64, :], True, False, ["QT", "CkT"], [pSk])
                                mm(pS[:], identb[:], cbA[c2][:], False, True, ["identb", f"cbA{c2}"], [pSk])
                                pa_ = Pa[hh % 2]
                                pak_ = f"Pa{hh % 2}"
                                act(lambda: A.activation(out=pa_[:], in_=pS[:], func=AF.Exp, scale=0.125, accum_out=rsum[:, hh:hh + 1]), [pSk], [pak_, "rsum"])
                                dve(lambda: V.tensor_scalar(out=rinv[:, hh:hh + 1], in0=rsum[:, hh:hh + 1], scalar1=1e-30, scalar2=None, op0=ALU.max), ["rsum"], ["rinv"])
                                dve(lambda: V.reciprocal(out=rinv[:, hh:hh + 1], in_=rinv[:, hh:hh + 1]), ["rinv"], ["rinv"])
                                if hh == 0:
                                    dve(lambda: V.tensor_scalar(out=imp[:], in0=pa_[:], scalar1=rinv[:, 0:1], scalar2=None, op0=ALU.mult), [pak_, "rinv"], ["imp"])
                                else:
                                    dve(lambda: V.scalar_tensor_tensor(out=imp[:], in0=pa_[:], scalar=rinv[:, hh:hh + 1], in1=imp[:], op0=ALU.mult, op1=ALU.add),
                                        [pak_, "rinv", "imp"], ["imp"])
                            dve(lambda: V.tensor_tensor(out=chk[:, 1:512], in0=imp[:, 1:512], in1=imp[:, 0:511], op=ALU.add), ["imp"], ["chk"])
                            dve(lambda: V.tensor_copy(out=chk[:, 0:1], in_=imp[:, 0:1]), ["imp", "chk"], ["chk"])
                            dve(lambda: V.tensor_reduce(out=sc2[:], in_=chk[:].rearrange("p (a b) -> p a b", b=4), axis=AX.X, op=ALU.add), ["chk"], ["sc2"])
                            dve(lambda: V.tensor_tensor(out=sc2[:], in0=sc2[:], in1=addb[c2][:], op=ALU.add), ["sc2", f"addb{c2}"], ["sc2"])
                            dve(lambda: V.max(out=m8a[:], in_=sc2[:]), ["sc2"], ["m8a"])
                            dve(lambda: V.match_replace(out=sc3[:], in_to_replace=m8a[:], in_values=sc2[:], imm_value=-3e9), ["sc2", "m8a"], ["sc3"])
                            dve(lambda: V.max(out=m8b[:], in_=sc3[:]), ["sc3"], ["m8b"])
                            dve(lambda: V.tensor_scalar(out=selb[:], in0=sc2[:], scalar1=m8b[:, 7:8], scalar2=NEG, op0=ALU.is_lt, op1=ALU.mult), ["sc2", "m8b"], ["selb"])
                            dve(lambda: V.tensor_tensor(out=selb[:], in0=selb[:], in1=futb[c2][:], op=ALU.min), ["selb", f"futb{c2}"], ["selb"])
                            dve(lambda: V.tensor_copy(out=selsw[:, 0:64], in_=selb[:, 64:128]), ["selb"], ["selsw"])
                            dve(lambda: V.tensor_copy(out=selsw[:, 64:128], in_=selb[:, 0:64]), ["selb", "selsw"], ["selsw"])
                            tr(PX[:, 0:128], selb[:], identf[:], ["selb", "identf"], ["PX"], signal=False)
                            tr(PX[:, 128:256], selsw[:], identf[:], ["selsw", "identf"], ["PX"])
                            qa, qb = QaA[g], QaB[g]
                            qr = slice(0, 64) if g == 0 else slice(64, 128)
                            br_ = slice(64, 128) if g == 0 else slice(0, 64)
                            srcA = PX[br_, 128:256] if g == 0 else PX[br_, 0:128]
                            srcB = PX[br_, 0:128] if g == 0 else PX[br_, 128:256]
                            act(lambda: A.copy(out=qa[br_, :, :], in_=srcA.unsqueeze(1).broadcast_to([64, 4, 128])), ["PX"], [f"QaA{g}"])
                            act(lambda: A.copy(out=qb[br_, :, :], in_=srcB.unsqueeze(1).broadcast_to([64, 4, 128])), ["PX"], [f"QaB{g}"])
                            pool(lambda: G.tensor_copy(out=qa[qr, :, :], in_=QT[qr, tl, :, :]), ["QT", f"QaA{g}"], [f"QaA{g}"])
                            pool(lambda: G.tensor_copy(out=qb[qr, :, :], in_=QT[qr, tl, :, :]), ["QT", f"QaB{g}"], [f"QaB{g}"])

                            def branch(klist, gate_idx, first):
                                po, pok = nxt(PO, "o", "PO")
                                n = len(klist)
                                scores = []

                                def emit_score(i):
                                    kap, kkey, extra, vap, vkey = klist[i][:5]
                                    rhs_, rhsk_ = (klist[i][5], klist[i][6]) if len(klist[i]) > 5 else (Qg, "QT")
                                    pS, pSk = nxt(PS_S, "s", "PS_S")
                                    mm(pS[:], kap, rhs_, True, len(extra) == 0, kkey if isinstance(kkey, list) else [kkey, rhsk_], [pSk])
                                    for ei, (el, er, ek) in enumerate(extra):
                                        mm(pS[:], el, er, False, ei == len(extra) - 1, ek, [pSk])
                                    return pS, pSk

                                mm(po[:, 0:260], zerob[:], VsA[:, 0:2, :, :].rearrange("p a b c -> p (a b c)"), True, False, ["zerob", "VsA"], [pok])
                                pend = [emit_score(0)]
                                if n > 1:
                                    pend.append(emit_score(1))
                                for i in range(n):
                                    pS, pSk = pend.pop(0)
                                    if i + 2 < n:
                                        pend.append(emit_score(i + 2))
                                    pt_, ptk = nxt(PTb, "pt", "PTb")
                                    act(lambda: A.activation(out=pt_[:], in_=pS[:], func=AF.Exp, scale=0.125), [pSk], [ptk])
                                    vap, vkey = klist[i][3], klist[i][4]
                                    for hh in range(4):
                                        mm(po[:, hh * 65:(hh + 1) * 65], pt_[:, hh * 128:(hh + 1) * 128], vap, False, (i == n - 1 and hh == 3), [ptk, vkey], [pok])
                                pov = po[:, 0:260].rearrange("p (h e) -> p h e", h=4)
                                dve(lambda: V.tensor_scalar(out=coef[:], in0=pov[:, :, 64], scalar1=1e-30, scalar2=None, op0=ALU.max), [pok], ["coef"])
                                dve(lambda: V.reciprocal(out=coef[:], in_=coef[:]), ["coef"], ["coef"])
                                dve(lambda: V.tensor_tensor(out=coef[:], in0=coef[:], in1=gn[:, tl, 12 * g + gate_idx:12 * g + 12:3], op=ALU.mult), ["coef", "gn"], ["coef"])
                                oslice = oacc[:, g * 256:(g + 1) * 256].rearrange("p (h d) -> p h d", h=4)
                                cb_ = coef[:].unsqueeze(2).broadcast_to([128, 4, 64])
                                if first:
                                    dve(lambda: V.tensor_tensor(out=oslice, in0=pov[:, :, 0:64], in1=cb_, op=ALU.mult), [pok, "coef", "oacc"], ["oacc"])
                                else:
                                    dve(lambda: V.tensor_tensor(out=otmp[:].rearrange("p (h d) -> p h d", h=4), in0=pov[:, :, 0:64], in1=cb_, op=ALU.mult),
                                        [pok, "coef"], ["otmp"])
                                    dve(lambda: V.tensor_tensor(out=oslice, in0=oslice, in1=otmp[:].rearrange("p (h d) -> p h d", h=4), op=ALU.add),
                                        ["oacc", "otmp"], ["oacc"])

                            kl = []
                            for cc in range(4):
                                extra = []
                                if cc >= 2:
                                    extra = [(identb[:], cbB[c2][:, cc - 2, :], ["identb", f"cbB{c2}"])]
                                kl.append((CkT[r0:r0 + 64, cc * 128:(cc + 1) * 128], "CkT", extra, CvA[:, cc, g, :], "CvA"))
                            branch(kl, 0, True)
                            kl = []
                            for kt in range(qt + 1):
                                extra = []
                                if kt == qt:
                                    extra.append((identb[:], tri[:], ["identb", "tri"]))
                                qx, qxk = (QaA[g], f"QaA{g}") if kt < 32 else (QaB[g], f"QaB{g}")
                                kl.append((KA[g][:, kt * 128:(kt + 1) * 128], [f"KA{g}", f"KA{g}e", qxk], extra, VsA[:, kt, g, :], "VsA",
                                           qx[:].rearrange("p h q -> p (h q)"), qxk))
                            branch(kl, 1, False)
                            kl = []
                            for kt in range(qt - 4, qt + 1):
                                extra = []
                                if kt == qt:
                                    extra = [(identb[:], tri[:], ["identb", "tri"])]
                                elif kt == qt - 4:
                                    extra = [(identb[:], atri[:], ["identb", "atri"])]
                                kl.append((KwT[r0:r0 + 64, (kt - 44) * 128:(kt - 43) * 128], "KwT", extra, VwA[:, kt - 44, g, :], "VwA"))
                            branch(kl, 2, False)
                        pos_ = [nxt(PO, "o", "PO"), nxt(PO, "o", "PO")]
                        for po, pok in pos_:
                            mm(po[:, 0:258], zerob[:], VsA[:, 0:2, :, :].rearrange("p a b c -> p (a b c)")[:, 0:258], True, False, ["zerob", "VsA"], [pok])
                        for mc in range(2):
                            pS, pSk = nxt(PS_S, "s", "PS_S")
                            for h in range(4):
                                mm(pS[:, h * 128:(h + 1) * 128], KmT[:, h, mc * 128:(mc + 1) * 128], qmT[:, tl, h, :], True, True, ["KmT", "qmT"], [pSk])
                            pt_, ptk = nxt(PTb, "pt", "PTb")
                            act(lambda: A.activation(out=pt_[:], in_=pS[:], func=AF.Exp, scale=128.0 ** -0.5), [pSk], [ptk])
                            for h in range(4):
                                po, pok = pos_[h // 2]
                                mm(po[:, (h % 2) * 129:(h % 2 + 1) * 129], pt_[:, h * 128:(h + 1) * 128], VmA[:, mc, h, :], False, (mc == 1 and h % 2 == 1), [ptk, "VmA"], [pok])
                        for hp in range(2):
                            po, pok = pos_[hp]
                            pov = po[:, 0:258].rearrange("p (h e) -> p h e", h=2)
                            dve(lambda: V.reciprocal(out=coef[:, 0:2], in_=pov[:, :, 128]), [pok], ["coef"])
                            dve(lambda: V.tensor_tensor(out=omacc[:, hp * 256:(hp + 1) * 256].rearrange("p (h d) -> p h d", h=2), in0=pov[:, :, 0:128],
                                                        in1=coef[:, 0:2].unsqueeze(2).broadcast_to([128, 2, 128]), op=ALU.mult), [pok, "coef"], ["omacc"])
                        dve(lambda: V.tensor_copy(out=oaccb[:], in_=oacc[:]), ["oacc"], ["oaccb"])
                        dve(lambda: V.tensor_copy(out=omb[:], in_=omacc[:]), ["omacc"], ["omb"])
                        for src_, sk_, dstT, dk_ in ((oaccb, "oaccb", oT, "oT"), (omb, "omb", omT, "omT")):
                            for kc in range(4):
                                tr(PT4[:, kc * 128:(kc + 1) * 128], src_[:, kc * 128:(kc + 1) * 128], identb[:], [sk_, "identb"], ["PT4"], signal=(kc == 3))
                            act(lambda: A.copy(out=dstT[:, :, tl * 128:(tl + 1) * 128], in_=PT4[:, 0:512].rearrange("p (a b) -> p a b", a=4)), ["PT4"], [dk_])

                    fw.barrier()
                if "p4o" in dbg and B == 0:
                    fw.dma("sp", ddbg("d_QT", [128, 2048], BF16)[:, :], QT[:].rearrange("p a b c -> p (a b c)"), reads=["QT"], writes=["d_QT"])
                    fw.dma("sp", ddbg("d_qmT", [128, 2048], BF16)[:, :], qmT[:].rearrange("p a b c -> p (a b c)"), reads=["qmT"], writes=["d_qmT"])
                    fw.dma("sp", ddbg("d_gn", [128, 96], F32)[:, :], gn[:].rearrange("p a b -> p (a b)"), reads=["gn"], writes=["d_gn"])
                    fw.dma("sp", ddbg("d_oT", [128, 4 * 512], BF16)[:, :], oT[:].rearrange("p a b -> p (a b)"), reads=["oT"], writes=["d_oT"])
                    fw.dma("sp", ddbg("d_omT", [128, 4 * 512], BF16)[:, :], omT[:].rearrange("p a b -> p (a b)"), reads=["omT"], writes=["d_omT"])

                with contextlib.ExitStack() as SC:
                    xq = [sb(SC, f"xq{i}", [128, 1024], F32) for i in range(2)]
                    h0t = sb(SC, "h0t", [128, 1024], F32)
                    sgt = [sb(SC, f"sgt{i}", [128, 512], F32) for i in range(2)]
                    mrg2 = [sb(SC, f"mrg{i}", [128, 512], F32) for i in range(2)]
                    ytm2 = [sb(SC, f"ytm{i}", [128, 512], F32) for i in range(2)]
                    vres = sb(SC, "vres", [128, 1024], F32)
                    wp = [sb(SC, f"wp{i}", [128, 8, 128], BF16) for i in range(6)]
                    for ft in range(8):
                        fs = slice(ft * 128, (ft + 1) * 128)
                        mrg = mrg2[ft % 2]
                        ytm = ytm2[ft % 2]
                        mk_ = f"mrg{ft % 2}"
                        yk_ = f"ytm{ft % 2}"

                        def proj(wsrc, nk, rhsT, rk):
                            w_, wk_ = wpiece(wsrc)
                            pf, pfk = nxt(PF, "f", "PF4_")
                            for kc in range(nk):
                                mm(pf[:], w_[:, kc, :], (rhsT[:, kc, B * 512:(B + 1) * 512] if rhsT is gyT else rhsT[:, kc, :]), kc == 0, kc == nk - 1, [wk_, rk], [pfk])
                            return pf, pfk

                        def gate(i):
                            pf, pfk = proj(wv[:, :, C_GM + i * 1024 + ft * 128:C_GM + i * 1024 + (ft + 1) * 128], 8, hTq, "hTq")
                            sg, sgk = nxt(sgt, "wp", "sgt")
                            act(lambda: A.activation(out=sg[:], in_=pf[:], func=AF.Sigmoid), [pfk], [sgk])
                            return sg, sgk

                        py, pyk = proj(wno_v[:, :, fs], 4, oT, "oT")
                        sg, sgk = gate(0)
                        dve(lambda: V.tensor_tensor(out=mrg[:], in0=py[:], in1=sg[:], op=ALU.mult), [pyk, sgk], [mk_])
                        pga, pgak = proj(wgl_v[:, :, fs], 4, gyT, "gyT")
                        pgb, pgbk = proj(wgl_v[:, :, 1024 + ft * 128:1024 + (ft + 1) * 128], 4, gyT, "gyT")
                        sg2, sg2k = nxt(sgt, "wp", "sgt")
                        act(lambda: A.activation(out=sg2[:], in_=pgb[:], func=AF.Sigmoid), [pgbk], [sg2k])
                        dve(lambda: V.tensor_tensor(out=ytm[:], in0=pga[:], in1=sg2[:], op=ALU.mult), [pgak, sg2k], [yk_])
                        sg, sgk = gate(1)
                        dve(lambda: V.tensor_tensor(out=ytm[:], in0=ytm[:], in1=sg[:], op=ALU.mult), [yk_, sgk], [yk_])
                        dve(lambda: V.tensor_tensor(out=mrg[:], in0=mrg[:], in1=ytm[:], op=ALU.add), [yk_, mk_], [mk_])
                        py, pyk = proj(wmo_v[:, :, fs], 4, omT, "omT")
                        sg, sgk = gate(2)
                        dve(lambda: V.tensor_tensor(out=ytm[:], in0=py[:], in1=sg[:], op=ALU.mult), [pyk, sgk], [yk_])
                        dve(lambda: V.tensor_tensor(out=mrgT[:, ft, :], in0=mrg[:], in1=ytm[:], op=ALU.add), [yk_, mk_], ["mrgT"])

                    if "p4o" in dbg and B == 0:
                        fw.dma("sp", ddbg("d_mrgT", [128, 4096], BF16)[:, :], mrgT[:].rearrange("p a b -> p (a b)"), reads=["mrgT"], writes=["d_mrgT"])
                    for tl in range(4):
                        j = B * 4 + tl
                        Tt = OWN0 + j
                        xt, xk = nxt(xq, "pt", "xq")
                        fw.dma("sp", xt[:], xr[Tt * 128:(Tt + 1) * 128, :], writes=[xk])
                        ln_tok(xt, xk, h0t, "h0t", grow, brow, "grow", "brow")
                        for hf in range(2):
                            pf, pfk = nxt(PF, "f", "PF4_")
                            for ft in range(8):
                                mm(pf[:], mrgT[:, ft, tl * 128:(tl + 1) * 128], Wo[:, ft, hf * 512:(hf + 1) * 512], ft == 0, ft == 7, ["mrgT", "Wo"], [pfk])
                            dve(lambda: V.scalar_tensor_tensor(out=vres[:, hf * 512:(hf + 1) * 512], in0=h0t[:, hf * 512:(hf + 1) * 512], scalar=ALPHA,
                                                               in1=pf[:], op0=ALU.mult, op1=ALU.add), ["h0t", pfk], ["vres"])
                        ln_tok(vres, "vres", h0t, "h0t", g1row, b1row, "g1row", "b1row")
                        fw.dma("sp", h1scr[j * 128:(j + 1) * 128, :], h0t[:], reads=["h0t"], writes=["h1scr"])
                    fw.barrier()
            if "p4" in dbg:
                fw.dma("sp", ddbg("d_h1", [2048, 1024], F32)[:, :], h1scr[:, :], reads=["h1scr"], writes=["d_h1"])
            fw.barrier()
        if upto <= 4:
            fw.finish("sp")
            print("instr", fw.ninstr, "waits", fw.nwait)
            return nc, dbg_out
        SKV.close()

        with contextlib.ExitStack() as S5:
            acc = sb(S5, "acc", [128, 16, 1024], F32)
            h1T = sb(S5, "h1T", [128, 8, 2048], BF16)
            gates = sb(S5, "gates", [128, 16, 32], F32)
            bguT = sb(S5, "bguT", [128, 512], F32)
            g2row = sb(S5, "g2row", [128, 1024], F32)
            b2row = sb(S5, "b2row", [128, 1024], F32)
            st5 = sb(S5, "st5", [128, 12], F32)
            mv5 = sb(S5, "mv5", [128, 2], F32)
            rs5 = sb(S5, "rs5", [128, 1], F32)
            fw.dma("sp", g2row[:], ln2_d[0:1, :].partition_broadcast(128), writes=["g2row"])
            fw.dma("sp", b2row[:], ln2_d[1:2, :].partition_broadcast(128), writes=["b2row"])
            with contextlib.ExitStack() as S5i:
                Wr = sb(S5i, "Wr", [128, 8, 32], F32)
                brr = sb(S5i, "brr", [128, 32], F32)
                bdn = sb(S5i, "bdn", [32, 1024], F32)
                bgn = sb(S5i, "bgn", [128, 4, 128], F32)
                fw.dma("sp", Wr[:], w_router.rearrange("(dc p) e -> p dc e", p=128), writes=["Wr"])
                fw.dma("sp", brr[:], b_router[0:1, :].partition_broadcast(128), writes=["brr"])
                fw.dma("sp", bdn[:], b_down[:, :], writes=["bdn"])
                fw.dma("sp", bgn[:], b_gate_up.rearrange("(a p) c -> p a c", p=128), writes=["bgn"])
                h1t = [sb(S5i, f"h1t{i}", [128, 1024], F32) for i in range(2)]
                h1b = sb(S5i, "h1b", [128, 1024], BF16)
                h1Tf = sb(S5i, "h1Tf", [128, 8, 128], F32)
                lg = sb(S5i, "lg", [128, 32], F32)
                ex = sb(S5i, "ex", [128, 32], F32)
                msk = sb(S5i, "msk", [128, 32], F32)
                m85 = sb(S5i, "m85", [128, 8], F32)
                nmx = sb(S5i, "nmx", [128, 1], F32)
                ssm = sb(S5i, "ssm", [128, 1], F32)
                gT = sb(S5i, "gT", [32, 128], F32)
                PT5 = ps(S5i, "PT5", [128, 1024], BF16)
                PTf = [ps(S5i, f"PTf{i}", [128, 512], F32) for i in range(2)]
                PLg = ps(S5i, "PLg", [128, 128], F32)
                PBd = [ps(S5i, f"PBd{i}", [128, 512], F32) for i in range(2)]
                for a in range(4):
                    tr(PTf[a % 2][:, 0:128], bgn[:, a, :], identf[:], ["bgn", "identf"], [f"PTf{a % 2}"])
                    act(lambda: A.copy(out=bguT[:, a * 128:(a + 1) * 128], in_=PTf[a % 2][:, 0:128]), [f"PTf{a % 2}"], ["bguT"])
                for j in range(16):
                    ht = h1t[j % 2]
                    hk_ = f"h1t{j % 2}"
                    fw.dma("sp", ht[:], h1scr[j * 128:(j + 1) * 128, :], reads=["h1scr"], writes=[hk_])
                    act(lambda: A.mul(out=acc[:, j, :], in_=ht[:], mul=ALPHA), [hk_], [f"acc{j}"])
                    dve(lambda: V.tensor_copy(out=h1b[:], in_=ht[:]), [hk_], ["h1b"])
                    for dc in range(8):
                        tr(PT5[:, dc * 128:(dc + 1) * 128], h1b[:, dc * 128:(dc + 1) * 128], identb[:], ["h1b", "identb"], ["PT5"], signal=(dc == 7))
                    act(lambda: A.copy(out=h1T[:, :, j * 128:(j + 1) * 128], in_=PT5[:].rearrange("p (a b) -> p a b", a=8)), ["PT5"], ["h1T"])
                    for hf in range(2):
                        for d4 in range(4):
                            dc = hf * 4 + d4
                            tr(PTf[hf][:, d4 * 128:(d4 + 1) * 128], ht[:, dc * 128:(dc + 1) * 128], identf[:], [hk_, "identf"], [f"PTf{hf}"], signal=(d4 == 3))
                        dve(lambda: V.tensor_copy(out=h1Tf[:, hf * 4:(hf + 1) * 4, :], in_=PTf[hf][:].rearrange("p (a b) -> p a b", a=4)), [f"PTf{hf}"], ["h1Tf"])
                    for dc in range(8):
                        mm(PLg[:, 0:32], h1Tf[:, dc, :], Wr[:, dc, :], dc == 0, dc == 7, ["h1Tf", "Wr"], ["PLg"])
                    dve(lambda: V.tensor_tensor(out=lg[:], in0=PLg[:, 0:32], in1=brr[:], op=ALU.add), ["PLg", "brr"], ["lg"])
                    dve(lambda: V.max(out=m85[:], in_=lg[:]), ["lg"], ["m85"])
                    dve(lambda: V.tensor_scalar(out=nmx[:], in0=m85[:, 0:1], scalar1=-1.0, scalar2=None, op0=ALU.mult), ["m85"], ["nmx"])
                    act(lambda: A.activation(out=ex[:], in_=lg[:], func=AF.Exp, bias=nmx[:, 0:1], scale=1.0), ["lg", "nmx"], ["ex"])
                    dve(lambda: V.tensor_scalar(out=msk[:], in0=lg[:], scalar1=m85[:, 3:4], scalar2=None, op0=ALU.is_ge), ["lg", "m85"], ["msk"])
                    dve(lambda: V.tensor_tensor(out=ex[:], in0=ex[:], in1=msk[:], op=ALU.mult), ["ex", "msk"], ["ex"])
                    dve(lambda: V.reduce_sum(out=ssm[:], in_=ex[:], axis=AX.X), ["ex"], ["ssm"])
                    dve(lambda: V.reciprocal(out=ssm[:], in_=ssm[:]), ["ssm"], ["ssm"])
                    dve(lambda: V.tensor_scalar(out=gates[:, j, :], in0=ex[:], scalar1=ssm[:, 0:1], scalar2=None, op0=ALU.mult), ["ex", "ssm"], ["gates"])
                    tr(PLg[0:32, :], gates[:, j, :], identf[:], ["gates", "identf"], ["PLg"])
                    act(lambda: A.copy(out=gT[:], in_=PLg[0:32, :]), ["PLg"], ["gT"])
                    for hf in range(2):
                        mm(PBd[hf][:], gT[:], bdn[:, hf * 512:(hf + 1) * 512], True, True, ["gT", "bdn"], [f"PBd{hf}"])
                        dve(lambda: V.tensor_tensor(out=acc[:, j, hf * 512:(hf + 1) * 512], in0=PBd[hf][:], in1=acc[:, j, hf * 512:(hf + 1) * 512], op=ALU.add),
                            [f"PBd{hf}", f"acc{j}"], [f"acc{j}"])
                if "p5g" in dbg:
                    fw.dma("sp", ddbg("d_gates", [128, 512], F32)[:, :], gates[:].rearrange("p a b -> p (a b)"), reads=["gates"], writes=["d_gates"])
                fw.barrier()
            actT = sb(S5, "actT", [128, 8, 2048], BF16)
            Wgu = [sb(S5, f"Wgu{i}", [128, 8, 2, 256], BF16) for i in range(2)]
            Wd = [sb(S5, f"Wd{i}", [128, 8, 512], BF16) for i in range(2)]
            gs = [sb(S5, f"gs{i}", [128, 512], F32) for i in range(3)]
            sg = [sb(S5, f"sg{i}", [128, 512], F32) for i in range(3)]
            ls = [sb(S5, f"ls{i}", [128, 512], F32) for i in range(3)]
            PG = [ps(S5, f"PG{i}", [128, 512], F32) for i in range(3)]
            PL = [ps(S5, f"PL{i}", [128, 512], F32) for i in range(3)]
            PD = [ps(S5, f"PD{i}", [128, 512], F32) for i in range(2)]
            ui = 0
            di = 0
            NE = 32 if "moe_e" not in dbg else 2
            NU = 3

            def load_wgu(e, pg):
                par = (e * 4 + pg) % 2
                wgu_v = w_gate_up[e].rearrange("(dc p) c -> p dc c", p=128)
                for gl_ in range(2):
                    c0 = gl_ * 1024 + pg * 256
                    for d0 in range(0, 8, 4):
                        fw.dma("pool", Wgu[par][:, d0:d0 + 4, gl_, :], wgu_v[:, d0:d0 + 4, c0:c0 + 256], writes=[f"Wgu{par}_{gl_}_{d0 // 4}"])

            def load_wd(e, h2):
                wd_v = w_down[e].rearrange("(fc p) c -> p fc c", p=128)
                for d0 in range(0, 8, 4):
                    fw.dma("pool", Wd[h2][:, d0:d0 + 4, :], wd_v[:, d0:d0 + 4, h2 * 512:(h2 + 1) * 512], writes=[f"Wd{h2}_{d0 // 4}"])

            load_wgu(0, 0)
            load_wd(0, 0)
            load_wd(0, 1)
            for e in range(NE):
                for pg in range(4):
                    par = (e * 4 + pg) % 2
                    wb = Wgu[par]
                    if pg < 3:
                        load_wgu(e, pg + 1)
                    elif e + 1 < NE:
                        load_wgu(e + 1, 0)
                    for pi in range(2):
                        i = pg * 2 + pi
                        bg_ = bguT[:, e * 16 + i:e * 16 + i + 1]
                        bl_ = bguT[:, e * 16 + 8 + i:e * 16 + 8 + i + 1]
                        for tb in range(4):
                            u2 = ui % NU
                            ui += 1
                            for dc in range(8):
                                mm(PG[u2][:], wb[:, dc, 0, pi * 128:(pi + 1) * 128], h1T[:, dc, tb * 512:(tb + 1) * 512], dc == 0, dc == 7,
                                   [f"Wgu{par}_0_{dc // 4}", "h1T"], [f"PG{u2}"])
                            for dc in range(8):
                                mm(PL[u2][:], wb[:, dc, 1, pi * 128:(pi + 1) * 128], h1T[:, dc, tb * 512:(tb + 1) * 512], dc == 0, dc == 7,
                                   [f"Wgu{par}_1_{dc // 4}", "h1T"], [f"PL{u2}"])
                            dve(lambda: V.tensor_scalar(out=gs[u2][:], in0=PG[u2][:], scalar1=bg_, scalar2=7.0, op0=ALU.add, op1=ALU.min), [f"PG{u2}", "bguT"], [f"gs{u2}"])
                            act(lambda: A.activation(out=sg[u2][:], in_=gs[u2][:], func=AF.Sigmoid, scale=1.702), [f"gs{u2}"], [f"sg{u2}"])
                            dve(lambda: V.tensor_scalar(out=ls[u2][:], in0=PL[u2][:], scalar1=bl_, scalar2=7.0, op0=ALU.add, op1=ALU.min), [f"PL{u2}", "bguT"], [f"ls{u2}"])
                            dve(lambda: V.tensor_scalar(out=ls[u2][:], in0=ls[u2][:], scalar1=-7.0, scalar2=1.0, op0=ALU.max, op1=ALU.add), [f"ls{u2}"], [f"ls{u2}"])
                            pool(lambda: G.tensor_tensor(out=gs[u2][:], in0=gs[u2][:], in1=sg[u2][:], op=ALU.mult), [f"gs{u2}", f"sg{u2}"], [f"gs{u2}"])
                            pool(lambda: G.tensor_tensor(out=actT[:, i, tb * 512:(tb + 1) * 512], in0=gs[u2][:], in1=ls[u2][:], op=ALU.mult),
                                 [f"gs{u2}", f"ls{u2}"], [f"actT{tb}"])
                for h2 in range(2):
                    wdb = Wd[h2]
                    for j in range(16):
                        d2 = di % 2
                        di += 1
                        for fc in range(8):
                            mm(PD[d2][:], actT[:, fc, j * 128:(j + 1) * 128], wdb[:, fc, :], fc == 0, fc == 7, [f"actT{j // 4}", f"Wd{h2}_{fc // 4}"], [f"PD{d2}"])
                        dve(lambda: V.scalar_tensor_tensor(out=acc[:, j, h2 * 512:(h2 + 1) * 512], in0=PD[d2][:], scalar=gates[:, j, e:e + 1],
                                                           in1=acc[:, j, h2 * 512:(h2 + 1) * 512], op0=ALU.mult, op1=ALU.add),
                            [f"PD{d2}", "gates", f"acc{j}"], [f"acc{j}"])
                    if e + 1 < NE:
                        load_wd(e + 1, h2)
            ot = [sb(S5, f"ot{i}", [128, 1024], F32) for i in range(2)]
            for j in range(16):
                o_ = ot[j % 2]
                ok_ = f"ot{j % 2}"
                xt = acc[:, j, :]
                xk = f"acc{j}"
                dve(lambda: V.bn_stats(out=st5[:, 0:6], in_=xt[:, 0:512]), [xk], ["st5"])
                dve(lambda: V.bn_stats(out=st5[:, 6:12], in_=xt[:, 512:1024]), [xk], ["st5"])
                dve(lambda: V.bn_aggr(out=mv5[:, 0:2], in_=st5[:, 0:12]), ["st5"], ["mv5"])
                act(lambda: A.activation(out=rs5[:], in_=mv5[:, 1:2], func=AF.Sqrt, bias=LN_EPS, scale=1.0), ["mv5"], ["rs5"])
                dve(lambda: V.reciprocal(out=rs5[:], in_=rs5[:]), ["rs5"], ["rs5"])
                dve(lambda: V.tensor_scalar(out=o_[:], in0=xt, scalar1=mv5[:, 0:1], scalar2=rs5[:, 0:1], op0=ALU.subtract, op1=ALU.mult),
                    [xk, "mv5", "rs5"], [ok_])
                pool(lambda: G.tensor_tensor(out=o_[:], in0=o_[:], in1=g2row[:], op=ALU.mult), [ok_, "g2row"], [ok_])
                pool(lambda: G.tensor_tensor(out=o_[:], in0=o_[:], in1=b2row[:], op=ALU.add), [ok_, "b2row"], [ok_])
                fw.dma("sp", out_d[j * 128:(j + 1) * 128, :], o_[:], reads=[ok_], writes=[f"out{j}"])
            fw.finish("sp")
            fw.barrier()
        print("instr", fw.ninstr, "waits", fw.nwait)
        return nc, dbg_out

        fw.finish("sp")
    return nc, dbg_out


def host_prep(inputs):
    x = np.asarray(inputs["x"], np.float32)
    pos = np.asarray(inputs["positions"], np.int32)
    maps = []
    inv = (np.float32(500000.0) ** (-(np.arange(0, 16, 2, dtype=np.float32)) / np.float32(16))).astype(np.float32)
    invf = np.zeros((128, 1), np.float32)
    for base in (0, 64):
        invf[base:base + 8, 0] = inv
        invf[base + 8:base + 16, 0] = inv
    identf = np.eye(128, dtype=np.float32)
    ln_emb = np.stack([inputs["ln_emb_g"], inputs["ln_emb_b"]]).astype(np.float32)
    w_in = np.ascontiguousarray(np.asarray(inputs["w_in"], np.float32)[0])
    g0 = lambda n: np.ascontiguousarray(np.asarray(inputs[n], np.float32)[0])
    sgn = np.ones((128, 1), np.float32); sgn[64:] = -1.0
    psw = np.zeros((128, 128), np.float32)
    psw[np.arange(128), (np.arange(128) + 64) % 128] = 1.0
    shared = {
        "w_kcmp1": g0("w_kcmp1"), "w_kcmp2": g0("w_kcmp2"), "w_vcmp1": g0("w_vcmp1"), "w_vcmp2": g0("w_vcmp2"),
        "pe_k": g0("pe_k_cmp"), "pe_v": g0("pe_v_cmp"),
        "s5_a_re": g0("s5_a_re"), "s5_a_im": g0("s5_a_im"), "s5_log_dt": np.asarray(inputs["s5_log_dt"], np.float32).reshape(1, 32),
        "s5_b_re": g0("s5_b_re"), "s5_b_im": g0("s5_b_im"), "s5_c_re": g0("s5_c_re"), "s5_c_im": g0("s5_c_im"), "s5_d": g0("s5_d"),
        "sgn": sgn, "psw": psw,
        "w_mem_kv": g0("w_mem_kv"), "w_nsa_out": g0("w_nsa_out"), "w_mem_out": g0("w_mem_out"), "w_s5_glu": g0("w_s5_glu"),
        "w_o": g0("w_o"), "ln1": np.stack([g0("ln1_g"), g0("ln1_b")]),
        "ln2": np.stack([g0("ln2_g"), g0("ln2_b")]), "w_router": g0("w_router"), "b_router": g0("b_router").reshape(1, 32),
        "w_gate_up": g0("w_gate_up"), "b_gate_up": g0("b_gate_up").reshape(512, 128), "w_down": g0("w_down"), "b_down": g0("b_down"),
    }
    kk = np.arange(8192)
    shared["epat"] = (((kk[None, :] // 64) % 64) == np.arange(64)[:, None]).astype(np.float32)
    ii = np.arange(128)
    tri = np.where(ii[:, None] <= ii[None, :], 0.0, NEG).astype(np.float32)
    atri = np.where(ii[:, None] > ii[None, :], 0.0, NEG).astype(np.float32)
    shared["tri"] = np.tile(tri, (1, 4))
    shared["atri"] = np.tile(atri, (1, 4))
    for k in range(8):
        b, r = k // 4, k % 4
        pad = 2048 * (3 - r)
        xr = np.zeros((8192, 1024), np.float32)
        xr[pad:] = x[b, :8192 - pad]
        posr = np.zeros((1, 8192), np.int32)
        posr[0, pad:] = pos[b, :8192 - pad]
        valid = (np.arange(8192) >= pad).astype(np.float32)
        cidx = np.arange(512)
        vc = ((cidx <= 510) & (16 * cidx >= pad)).astype(np.float32)
        rho = 6144 + np.arange(2048)
        cend = 16 * cidx + 31
        okc = (vc[None, :] > 0) & (cend[None, :] <= rho[:, None])
        cba = np.where(okc, 0.0, NEG).astype(np.float32).reshape(16, 128, 512)
        cbb = np.where(okc, 0.0, NEG).astype(np.float32).reshape(16, 128, 512)[:, :, 256:512]
        cbb = cbb.reshape(16, 128, 2, 128).transpose(0, 2, 3, 1)
        cbb = np.ascontiguousarray(np.tile(cbb, (1, 1, 1, 4)))
        blk = np.arange(128)
        tb = rho // 64
        blk0 = pad // 64
        forced = (blk[None, :] == tb[:, None]) | (blk[None, :] == tb[:, None] - 1) | (blk[None, :] == blk0)
        invalid = (blk[None, :] > tb[:, None]) | (blk[None, :] < blk0)
        addb = (forced * 1e4 + invalid * (-1e9)).astype(np.float32).reshape(16, 128, 128)
        futb = np.where(invalid, NEG, 0.0).astype(np.float32).reshape(16, 128, 128)
        m = {
            "cba": cba, "cbb": cbb, "addb": addb, "futb": futb, "mem": np.ascontiguousarray(np.asarray(inputs["mem"], np.float32)[b]),
            "vcc": np.ascontiguousarray(vc.reshape(4, 128).T),
            "xr": xr, "posr": posr, "vrow": valid[None, :].copy(),
            "vcol": np.ascontiguousarray(valid.reshape(64, 128).T),
            "ln_emb": ln_emb, "w_in": w_in, "invf": invf, "identf": identf,
        }
        m.update(shared)
        maps.append(m)
    return maps


def kernel(**inputs):
    nc, _ = build_program()
    maps = host_prep(inputs)
    res = run_bass_kernel_spmd(nc, maps, core_ids=list(range(8)))
    out = np.zeros((2, 8192, 1024), np.float32)
    for k in range(8):
        b, r = k // 4, k % 4
        out[b, 2048 * r:2048 * (r + 1)] = res.results[k]["out"]
    return out
```

```python
import contextlib
import math
import numpy as np
import ml_dtypes
import concourse.bass as bass
import concourse.mybir as mybir
from concourse.bass_utils import run_bass_kernel_spmd

F32 = mybir.dt.float32
BF16 = mybir.dt.bfloat16
I32 = mybir.dt.int32
ALU = mybir.AluOpType
AF = mybir.ActivationFunctionType
AX = mybir.AxisListType

EPOCH = 20000
NDMA_SLOTS = 6
NEG = -30000.0
LN_EPS = 1e-5
ALPHA = 2.0 ** 0.25
TWO_PI = 2.0 * math.pi
CW1 = 6.28125
CW2 = TWO_PI - 6.28125
PI_LO = 3.1415925


class _Eng:
    def __init__(self, fw, name, handle):
        self.fw = fw
        self.name = name
        self.h = handle
        self.sems = []
        self.epoch = -1
        self.count = 0
        self.pending = False
        self.seen = {}
        self.dma_slots = None
        self.dma_next = 0
        self.new_epoch()

    def new_epoch(self):
        self.epoch += 1
        self.count = 0
        s = self.fw.new_sem(f"{self.name}_e{self.epoch}")
        self.sems.append(s)
        self.fw.semobjs[(self.name, self.epoch)] = s

    def cur_key(self):
        return (self.name, self.epoch)


class FW:
    def __init__(self, nc, stack):
        self.nc = nc
        self.stack = stack
        self.semobjs = {}
        self.eng = {}
        for name, h in (("pe", nc.tensor), ("dve", nc.vector), ("act", nc.scalar),
                        ("pool", nc.gpsimd), ("sp", nc.sync)):
            self.eng[name] = _Eng(self, name, h)
        self.last_w = {}
        self.readers = {}
        self.ninstr = 0
        self.nwait = 0

    def new_sem(self, name):
        return self.stack.enter_context(self.nc.semaphore(name))

    def _wait(self, e, tok):
        semkey, val = tok
        if e.seen.get(semkey, 0) >= val:
            return
        if not semkey[0].endswith("_dma"):
            for (n2, ep2), v2 in e.seen.items():
                if n2 == semkey[0] and ep2 > semkey[1]:
                    return
        e.h.wait_ge(self.semobjs[semkey], val)
        self.nwait += 1
        e.seen[semkey] = val

    def _deps(self, reads, writes):
        deps = []
        for r in reads:
            t = self.last_w.get(r)
            if t is not None:
                deps.append(t)
        for w in writes:
            t = self.last_w.get(w)
            if t is not None:
                deps.append(t)
            deps.extend(self.readers.get(w, ()))
        return deps

    def _record(self, tok, reads, writes):
        for r in reads:
            lst = self.readers.setdefault(r, [])
            lst.append(tok)
            if len(lst) > 64:
                best = {}
                for sk, v in lst:
                    if best.get(sk, 0) < v:
                        best[sk] = v
                self.readers[r] = list(best.items())
        for w in writes:
            self.last_w[w] = tok
            self.readers[w] = []

    def op(self, engname, fn, reads=(), writes=(), signal=True):
        e = self.eng[engname]
        if e.count >= EPOCH and not e.pending:
            e.new_epoch()
        semkey = e.cur_key()
        for tok in self._deps(reads, writes):
            if engname == "pe" and tok[0][0] == "pe":
                continue
            self._wait(e, tok)
        ins = fn()
        self.ninstr += 1
        if signal:
            ins.then_inc(e.sems[e.epoch], 1)
            e.count += 1
            e.pending = False
            tok = (semkey, e.count)
        else:
            e.pending = True
            tok = (semkey, e.count + 1)
        self._record(tok, reads, writes)
        return ins

    def dma(self, qname, out, in_, reads=(), writes=(), **kw):
        e = self.eng[qname]
        if e.dma_slots is None:
            e.dma_slots = []
            for i in range(NDMA_SLOTS):
                key = (qname + "_dma", i)
                self.semobjs[key] = self.new_sem(f"{qname}_d{i}")
                e.dma_slots.append([key, 0])
        slot = e.dma_slots[e.dma_next % NDMA_SLOTS]
        e.dma_next += 1
        key, uses = slot
        if uses > 0:
            self._wait(e, (key, 16 * uses))
        for tok in self._deps(reads, writes):
            self._wait(e, tok)
        ins = e.h.dma_start(out=out, in_=in_, **kw)
        ins.then_inc(self.semobjs[key], 16)
        self.ninstr += 1
        slot[1] = uses + 1
        tok = (key, 16 * (uses + 1))
        self._record(tok, reads, writes)
        return tok

    def barrier(self):
        toks = []
        for e in self.eng.values():
            assert not e.pending, e.name
            if e.count > 0:
                toks.append((e.cur_key(), e.count))
            if e.dma_slots:
                for key, uses in e.dma_slots:
                    if uses > 0:
                        toks.append((key, 16 * uses))
        for e in self.eng.values():
            for t in toks:
                self._wait(e, t)
        self.last_w = {}
        self.readers = {}

    def finish(self, qname="sp"):
        e = self.eng[qname]
        for k, tok in list(self.last_w.items()):
            self._wait(e, tok)


C_Q, C_KC, C_VC, C_KS, C_VS, C_KW, C_VW, C_GN, C_U, C_QM, C_GM = 0, 512, 640, 768, 896, 1024, 1152, 1280, 1304, 1816, 2328
NT = 64
OWN0 = 48


def build_program(upto=99, dbg=()):
    nc = bass.Bass("TRN2", target_bir_lowering=False)
    V, A, G, T = nc.vector, nc.scalar, nc.gpsimd, nc.tensor

    def din(name, shape, dt=F32):
        return nc.dram_tensor(name, list(shape), dt, kind="ExternalInput").ap()

    def dscr(name, shape, dt):
        return nc.dram_tensor(name, list(shape), dt, kind="Internal").ap()

    dbg_out = {}

    def ddbg(name, shape, dt):
        dbg_out[name] = nc.dram_tensor(name, list(shape), dt, kind="ExternalOutput").ap()
        return dbg_out[name]

    xr = din("xr", [8192, 1024])
    posr = din("posr", [1, 8192], I32)
    vrow_d = din("vrow", [1, 8192])
    vcol_d = din("vcol", [128, 64])
    ln_emb = din("ln_emb", [2, 1024])
    w_in = din("w_in", [1024, 5400])
    invf_d = din("invf", [128, 1])
    identf_d = din("identf", [128, 128])
    w_kcmp1 = din("w_kcmp1", [32, 64, 128]); w_kcmp2 = din("w_kcmp2", [128, 64])
    w_vcmp1 = din("w_vcmp1", [32, 64, 128]); w_vcmp2 = din("w_vcmp2", [128, 64])
    pe_k = din("pe_k", [32, 64]); pe_v = din("pe_v", [32, 64])
    vcc_d = din("vcc", [128, 4])
    s5_a_re = din("s5_a_re", [32, 64]); s5_a_im = din("s5_a_im", [32, 64]); s5_log_dt = din("s5_log_dt", [1, 32])
    s5_b_re = din("s5_b_re", [32, 64, 16]); s5_b_im = din("s5_b_im", [32, 64, 16])
    s5_c_re = din("s5_c_re", [32, 16, 64]); s5_c_im = din("s5_c_im", [32, 16, 64]); s5_d = din("s5_d", [512])
    sgn_d = din("sgn", [128, 1]); psw_d = din("psw", [128, 128])
    mem_d = din("mem", [256, 1024]); w_mem_kv = din("w_mem_kv", [1024, 1024])
    w_nsa_out = din("w_nsa_out", [512, 1024]); w_mem_out = din("w_mem_out", [512, 1024])
    w_s5_glu = din("w_s5_glu", [512, 2048]); w_o = din("w_o", [1024, 1024]); ln1_d = din("ln1", [2, 1024])
    epat_d = din("epat", [64, 8192]); tri_d = din("tri", [128, 512]); atri_d = din("atri", [128, 512])
    cba_d = din("cba", [16, 128, 512]); cbb_d = din("cbb", [16, 2, 128, 512])
    addb_d = din("addb", [16, 128, 128]); futb_d = din("futb", [16, 128, 128])
    h1scr = dscr("h1scr", [2048, 1024], F32)
    wscr = dscr("wscr", [60, 128, 1024], BF16)
    wqscr = dscr("wqscr", [3, 128, 4096], BF16)
    wgnscr = dscr("wgnscr", [128, 192], BF16)
    ln2_d = din("ln2", [2, 1024]); w_router = din("w_router", [1024, 32]); b_router = din("b_router", [1, 32])
    w_gate_up = din("w_gate_up", [32, 1024, 2048]); b_gate_up = din("b_gate_up", [512, 128])
    w_down = din("w_down", [32, 1024, 1024]); b_down = din("b_down", [32, 1024])
    out_d = nc.dram_tensor("out", [2048, 1024], F32, kind="ExternalOutput").ap()

    uscr = dscr("uscr", [4, 128, 8192], BF16)

    with contextlib.ExitStack() as S0:
        fw = FW(nc, S0)

        _names = {}

        def _uniq(n):
            c = _names.get(n, 0)
            _names[n] = c + 1
            return n if c == 0 else f"{n}__{c}"

        def sb(st, name, shape, dt):
            return st.enter_context(nc.sbuf_tensor(_uniq("s_" + name), list(shape), dt))

        def ps(st, name, shape, dt=F32):
            return st.enter_context(nc.psum_tensor(_uniq("p_" + name), list(shape), dt))

        def dve(fn, r, w):
            return fw.op("dve", fn, r, w)

        def act(fn, r, w):
            return fw.op("act", fn, r, w)

        def pool(fn, r, w):
            return fw.op("pool", fn, r, w)

        def mm(out, lhsT, rhs, start, stop, r, w):
            return fw.op("pe", lambda: T.matmul(out, lhsT=lhsT, rhs=rhs, start=start, stop=stop), r, w, signal=stop)

        def tr(out, in_, ident, r, w, signal=True):
            return fw.op("pe", lambda: T.transpose(out=out, in_=in_, identity=ident), r, w, signal=signal)

        identf = sb(S0, "identf", [128, 128], F32)
        identb = sb(S0, "identb", [128, 128], BF16)
        zerob = sb(S0, "zerob", [128, 128], BF16)
        invf = sb(S0, "invf", [128, 1], F32)
        vcol = sb(S0, "vcol", [128, 64], F32)
        gcol = sb(S0, "gcol", [128, 8], F32)
        bcol = sb(S0, "bcol", [128, 8], F32)
        fw.dma("sp", identf[:], identf_d[:, :], writes=["identf"])
        fw.dma("pool", identb[:], identf_d[:, :], writes=["identb"])
        fw.dma("sp", invf[:], invf_d[:, :], writes=["invf"])
        fw.op("pool", lambda: G.memset(zerob[:], 0.0), [], ["zerob"])
        fw.dma("sp", vcol[:], vcol_d[:, :], writes=["vcol"])
        fw.dma("sp", gcol[:], ln_emb[0, :].rearrange("(dc p) -> p dc", p=128), writes=["gcol"], allow_slow_non_contiguous=True)
        fw.dma("sp", bcol[:], ln_emb[1, :].rearrange("(dc p) -> p dc", p=128), writes=["bcol"], allow_slow_non_contiguous=True)

        SKV = S0.enter_context(contextlib.ExitStack())
        KA = [sb(SKV, f"KA{i}", [128, 8192], BF16) for i in range(2)]
        VsA = sb(SKV, "VsA", [128, NT, 2, 65], BF16)
        KwT = sb(SKV, "KwT", [128, 20 * 128], BF16)
        VwA = sb(SKV, "VwA", [128, 20, 2, 65], BF16)
        CkT = sb(SKV, "CkT", [128, 512], BF16)
        CvA = sb(SKV, "CvA", [128, 4, 2, 65], BF16)
        gyT = sb(SKV, "gyT", [128, 4, 2048], BF16)
        vcc = sb(SKV, "vcc", [128, 4], F32)
        fw.dma("sp", vcc[:], vcc_d[:, :], writes=["vcc"])
        for q4 in range(4):
            fw.dma("pool", KA[0][64:128, q4 * 2048:(q4 + 1) * 2048], epat_d[:, q4 * 2048:(q4 + 1) * 2048], writes=["KA0e"])
            fw.dma("pool", KA[1][0:64, q4 * 2048:(q4 + 1) * 2048], epat_d[:, q4 * 2048:(q4 + 1) * 2048], writes=["KA1e"])

        def layer_norm_T(st_keys, xt_ap, hT_dst, PT, xn, stt, mv, rs, tmpf, gexp, bexp, tag):
            kx = st_keys
            dve(lambda: V.bn_stats(out=stt[:, 0:6], in_=xt_ap[:, 0:512]), [kx], ["stt" + tag])
            dve(lambda: V.bn_stats(out=stt[:, 6:12], in_=xt_ap[:, 512:1024]), [kx], ["stt" + tag])
            dve(lambda: V.bn_aggr(out=mv[:, 0:2], in_=stt[:, 0:12]), ["stt" + tag], ["mv" + tag])
            act(lambda: A.activation(out=rs[:], in_=mv[:, 1:2], func=AF.Sqrt, bias=LN_EPS, scale=1.0), ["mv" + tag], ["rs" + tag])
            dve(lambda: V.reciprocal(out=rs[:], in_=rs[:]), ["rs" + tag], ["rs" + tag])
            dve(lambda: V.tensor_scalar(out=xn[:], in0=xt_ap, scalar1=mv[:, 0:1], scalar2=rs[:, 0:1],
                                        op0=ALU.subtract, op1=ALU.mult), [kx, "mv" + tag, "rs" + tag], ["xn" + tag])
            for dc in range(8):
                tr(PT[:, dc * 128:(dc + 1) * 128], xn[:, dc * 128:(dc + 1) * 128], identb[:],
                   ["xn" + tag, "identb"], ["PT" + tag], signal=(dc == 7))
            dve(lambda: V.tensor_tensor(out=tmpf[:], in0=PT[:], in1=gexp[:], op=ALU.mult), ["PT" + tag, "gexp"], ["tmpf"])
            return tmpf

        with contextlib.ExitStack() as S1:
            kcT = sb(S1, "kcT", [128, 8192], BF16)
            vcT = sb(S1, "vcT", [128, 8192], BF16)
            S1t = S1.enter_context(contextlib.ExitStack())
            Wk = sb(S1t, "Wk", [128, 8, 1280], BF16)
            Wsw = sb(S1t, "Wsw", [128, 8, 3, 128], BF16)
            wv = w_in.rearrange("(dc p) c -> p dc c", p=128)
            for i, (c0, n) in enumerate(((C_KC, 128), (C_KS, 128), (C_KW, 128), (C_VC, 128), (C_VS, 128), (C_VW, 128), (C_U, 512))):
                dst0 = i * 128
                for dc in range(0, 8, 4):
                    fw.dma("pool", Wk[:, dc:dc + 4, dst0:dst0 + n], wv[:, dc:dc + 4, c0:c0 + n], writes=["Wk"])
            pool(lambda: G.memset(Wsw[:], 0.0), [], ["Wsw"])
            for i in range(3):
                for g in range(2):
                    b0 = i * 128 + g * 64
                    dve(lambda: V.tensor_scalar(out=Wsw[:, :, i, g * 64:g * 64 + 8], in0=Wk[:, :, b0 + 8:b0 + 16], scalar1=-1.0,
                                                scalar2=None, op0=ALU.mult), ["Wk", "Wsw"], ["Wsw"])
                    dve(lambda: V.tensor_copy(out=Wsw[:, :, i, g * 64 + 8:g * 64 + 16], in_=Wk[:, :, b0:b0 + 8]), ["Wk", "Wsw"], ["Wsw"])
            gexp = sb(S1t, "gexp", [128, 1024], F32)
            bexp = sb(S1t, "bexp", [128, 1024], F32)
            pool(lambda: G.memset(gexp[:], 1.0), [], ["gexp"])
            pool(lambda: G.memset(bexp[:], 1.0), [], ["bexp"])
            for dc in range(8):
                dve(lambda: V.tensor_scalar(out=gexp[:, dc * 128:(dc + 1) * 128], in0=gexp[:, dc * 128:(dc + 1) * 128],
                                            scalar1=gcol[:, dc:dc + 1], scalar2=None, op0=ALU.mult), ["gexp", "gcol"], ["gexp"])
                dve(lambda: V.tensor_scalar(out=bexp[:, dc * 128:(dc + 1) * 128], in0=bexp[:, dc * 128:(dc + 1) * 128],
                                            scalar1=bcol[:, dc:dc + 1], scalar2=None, op0=ALU.mult), ["bexp", "bcol"], ["bexp"])
            xbuf = [sb(S1t, f"xb{i}", [128, 1024], F32) for i in range(2)]
            xn = [sb(S1t, f"xn{i}", [128, 1024], BF16) for i in range(2)]
            tmpf = [sb(S1t, f"tmpf{i}", [128, 1024], F32) for i in range(1)] * 2
            stt = [sb(S1t, f"stt{i}", [128, 12], F32) for i in range(2)]
            mv = [sb(S1t, f"mv{i}", [128, 2], F32) for i in range(2)]
            rs = [sb(S1t, f"rs{i}", [128, 1], F32) for i in range(2)]
            hTb = [sb(S1t, f"hTb{i}", [128, 8, 512], BF16) for i in range(2)]
            posi = sb(S1t, "posi", [128, 512], I32)
            posf = sb(S1t, "posf", [128, 512], F32)
            angs = sb(S1t, "angs", [128, 512], F32)
            ki = sb(S1t, "ki", [128, 512], I32)
            kf = sb(S1t, "kf", [128, 512], F32)
            r1 = sb(S1t, "r1", [128, 512], F32)
            cosT = [sb(S1t, f"cosT{i}", [128, 512], F32) for i in range(1)] * 2
            sinT = [sb(S1t, f"sinT{i}", [128, 512], F32) for i in range(1)] * 2
            vrow = [sb(S1t, f"vrow{i}", [128, 512], F32) for i in range(2)]
            t1 = [sb(S1t, f"t1_{i}", [128, 512], F32) for i in range(1)] * 2
            t2 = [sb(S1t, f"t2_{i}", [128, 512], F32) for i in range(1)] * 2
            ust = [sb(S1t, f"ust{i}", [128, 512], BF16) for i in range(2)]
            S1p = S1.enter_context(contextlib.ExitStack())
            PT = [ps(S1p, f"PT{i}", [128, 1024], BF16) for i in range(2)]
            PF = [ps(S1p, f"PF{i}", [128, 512], F32) for i in range(4)]
            PV = [ps(S1p, f"PV{i}", [128, 256], F32) for i in range(2)]
            pfi = [0]
            ropei = [0]

            def next_pf():
                i = pfi[0] % 4
                pfi[0] += 1
                return PF[i], f"PF{i}"

            for bi in range(16):
                p2 = bi % 2
                hT = hTb[p2]
                hk = f"hTb{p2}"
                fw.dma("sp", posi[:], posr[:, bi * 512:(bi + 1) * 512].partition_broadcast(128), writes=["posi"])
                fw.dma("sp", vrow[p2][:], vrow_d[:, bi * 512:(bi + 1) * 512].partition_broadcast(128), writes=[f"vrow{p2}"])
                dve(lambda: V.tensor_copy(out=posf[:], in_=posi[:]), ["posi"], ["posf"])
                for which, shift, dst, dk in (("s", 0.0, sinT[p2], "sinT"), ("c", math.pi / 2, cosT[p2], "cosT")):
                    dve(lambda: V.tensor_scalar(out=angs[:], in0=posf[:], scalar1=invf[:, 0:1], scalar2=shift, op0=ALU.mult, op1=ALU.add),
                        ["posf", "invf"], ["angs"])
                    dve(lambda: V.tensor_scalar(out=ki[:], in0=angs[:], scalar1=1.0 / TWO_PI, scalar2=None, op0=ALU.mult), ["angs"], ["ki"])
                    dve(lambda: V.tensor_copy(out=kf[:], in_=ki[:]), ["ki"], ["kf"])
                    dve(lambda: V.scalar_tensor_tensor(out=r1[:], in0=kf[:], scalar=-CW1, in1=angs[:], op0=ALU.mult, op1=ALU.add),
                        ["kf", "angs"], ["r1"])
                    dve(lambda: V.scalar_tensor_tensor(out=r1[:], in0=kf[:], scalar=-CW2, in1=r1[:], op0=ALU.mult, op1=ALU.add),
                        ["kf", "r1"], ["r1"])
                    dve(lambda: V.tensor_scalar(out=r1[:], in0=r1[:], scalar1=-PI_LO, scalar2=PI_LO, op0=ALU.max, op1=ALU.min), ["r1"], ["r1"])
                    act(lambda: A.activation(out=dst[:], in_=r1[:], func=AF.Sin), ["r1"], [dk])
                for tl in range(4):
                    Tt = bi * 4 + tl
                    q2 = Tt % 2
                    tag = str(q2)
                    fw.dma("sp", xbuf[q2][:], xr[Tt * 128:(Tt + 1) * 128, :], writes=[f"xb{q2}"])
                    tf = layer_norm_T(f"xb{q2}", xbuf[q2][:], None, PT[q2], xn[q2], stt[q2], mv[q2], rs[q2], tmpf[q2], gexp, bexp, tag)
                    pool(lambda: G.tensor_tensor(out=hT[:, :, tl * 128:(tl + 1) * 128], in0=tf[:].rearrange("p (a b) -> p a b", a=8),
                                                 in1=bexp[:].rearrange("p (a b) -> p a b", a=8), op=ALU.add),
                         ["tmpf", "bexp"], [hk])
                for i, dstT, dkey, col0 in ((0, kcT, "kcT", bi * 512), (1, None, "KA", bi * 512), (2, KwT, "KwT", (bi - 11) * 512)):
                    if i == 2 and bi < 11:
                        continue
                    pa, pak = next_pf()
                    for dc in range(8):
                        mm(pa[:], Wk[:, dc, i * 128:(i + 1) * 128], hT[:, dc, :], dc == 0, dc == 7, ["Wk", hk], [pak])
                    pb, pbk = next_pf()
                    for dc in range(8):
                        mm(pb[:], Wsw[:, dc, i, :], hT[:, dc, :], dc == 0, dc == 7, ["Wsw", hk], [pbk])
                    ri = ropei[0] % 2
                    ropei[0] += 1
                    dve(lambda: V.tensor_tensor(out=t1[ri][:], in0=pa[:], in1=cosT[p2][:], op=ALU.mult), [pak, "cosT"], ["t1_"])
                    dve(lambda: V.tensor_tensor(out=t2[ri][:], in0=pb[:], in1=sinT[p2][:], op=ALU.mult), [pbk, "sinT"], ["t2_"])
                    if dstT is None:
                        pool(lambda: G.tensor_tensor(out=KA[0][0:64, col0:col0 + 512], in0=t1[ri][0:64, :], in1=t2[ri][0:64, :], op=ALU.add),
                             ["t1_", "t2_"], ["KA0"])
                        pool(lambda: G.tensor_tensor(out=KA[1][64:128, col0:col0 + 512], in0=t1[ri][64:128, :], in1=t2[ri][64:128, :], op=ALU.add),
                             ["t1_", "t2_"], ["KA1"])
                    else:
                        pool(lambda: G.tensor_tensor(out=dstT[:, col0:col0 + 512], in0=t1[ri][:], in1=t2[ri][:], op=ALU.add),
                             ["t1_", "t2_"], [dkey])
                pa, pak = next_pf()
                for dc in range(8):
                    mm(pa[:], Wk[:, dc, 384:512], hT[:, dc, :], dc == 0, dc == 7, ["Wk", hk], [pak])
                act(lambda: A.copy(out=vcT[:, bi * 512:(bi + 1) * 512], in_=pa[:]), [pak], ["vcT"])
                for ct in range(4):
                    pa, pak = next_pf()
                    for dc in range(8):
                        mm(pa[:], Wk[:, dc, 768 + ct * 128:768 + (ct + 1) * 128], hT[:, dc, :], dc == 0, dc == 7, ["Wk", hk], [pak])
                    u2 = ct % 2
                    dve(lambda: V.tensor_tensor(out=ust[u2][:], in0=pa[:], in1=vrow[p2][:], op=ALU.mult), [pak, f"vrow{p2}"], [f"ust{u2}"])
                    fw.dma("sp", uscr[ct, :, bi * 512:(bi + 1) * 512], ust[u2][:], reads=[f"ust{u2}"], writes=["uscr"])
                for tl in range(4):
                    Tt = bi * 4 + tl
                    pv = PV[tl % 2]
                    pvk = f"PV{tl % 2}"
                    for dc in range(8):
                        mm(pv[:], hT[:, dc, tl * 128:(tl + 1) * 128], Wk[:, dc, 512:768], dc == 0, dc == 7, ["Wk", hk], [pvk])
                    act(lambda: A.activation(out=VsA[:, Tt, :, 0:64], in_=pv[:, 0:128].rearrange("p (g d) -> p g d", g=2),
                                             func=AF.Copy, scale=vcol[:, Tt:Tt + 1]), [pvk, "vcol"], ["VsA"])
                    if Tt >= 44:
                        act(lambda: A.activation(out=VwA[:, Tt - 44, :, 0:64], in_=pv[:, 128:256].rearrange("p (g d) -> p g d", g=2),
                                                 func=AF.Copy, scale=vcol[:, Tt:Tt + 1]), [pvk, "vcol"], ["VwA"])
            for g in range(2):
                dve(lambda: V.tensor_copy(out=VsA[:, :, g, 64], in_=vcol[:, :]), ["vcol", "VsA"], ["VsA"])
                dve(lambda: V.tensor_copy(out=VwA[:, :, g, 64], in_=vcol[:, 44:64]), ["vcol", "VwA"], ["VwA"])

            if "p1" in dbg:
                fw.dma("sp", ddbg("d_KA0", [128, 8192], BF16)[:, :], KA[0][:], reads=["KA0", "KA0e"], writes=["d_KA0"])
                fw.dma("sp", ddbg("d_KA1", [128, 8192], BF16)[:, :], KA[1][:], reads=["KA1", "KA1e"], writes=["d_KA1"])
                fw.dma("sp", ddbg("d_kcT", [128, 8192], BF16)[:, :], kcT[:], reads=["kcT"], writes=["d_kcT"])
                fw.dma("sp", ddbg("d_vcT", [128, 8192], BF16)[:, :], vcT[:], reads=["vcT"], writes=["d_vcT"])
                fw.dma("sp", ddbg("d_KwT", [128, 2560], BF16)[:, :], KwT[:], reads=["KwT"], writes=["d_KwT"])
                fw.dma("sp", ddbg("d_VsA", [128, NT * 130], BF16)[:, :], VsA[:].rearrange("p a b c -> p (a b c)"), reads=["VsA"], writes=["d_VsA"])
                fw.dma("sp", ddbg("d_VwA", [128, 20 * 130], BF16)[:, :], VwA[:].rearrange("p a b c -> p (a b c)"), reads=["VwA"], writes=["d_VwA"])
                fw.dma("sp", ddbg("d_u", [4, 128, 8192], BF16)[:, :, :], uscr[:, :, :], reads=["uscr"], writes=["d_u"])
            if upto <= 1:
                fw.finish("sp")
                print("instr", fw.ninstr, "waits", fw.nwait)
                return nc, dbg_out
            fw.barrier()
            S1p.close()
            S1t.close()

            with contextlib.ExitStack() as S2:
                W1 = {}
                for kind, wd in (("k", w_kcmp1), ("v", w_vcmp1)):
                    W1[kind] = sb(S2, "W1" + kind, [128, 32, 128], BF16)
                    src = wd.rearrange("s d f -> d s f")
                    for half in (0, 64):
                        for s0 in range(0, 32, 8):
                            fw.dma("pool", W1[kind][half:half + 64, s0:s0 + 8, :], src[:, s0:s0 + 8, :], writes=["W1" + kind])
                W2p = sb(S2, "W2p", [128, 2, 128], BF16)
                W2v = sb(S2, "W2v", [128, 64], BF16)
                pool(lambda: G.memset(W2p[:], 0.0), [], ["W2p"])
                for g in range(2):
                    fw.dma("pool", W2p[:, g, g * 64:(g + 1) * 64], w_kcmp2[:, :], writes=["W2p"])
                fw.dma("pool", W2v[:], w_vcmp2[:, :], writes=["W2v"])
                peT = {}
                constc = {}
                PSc = ps(S2, "PSc", [128, 8], F32)
                PSh = [ps(S2, f"PSh{i}", [128, 512], F32) for i in range(2)]
                PSck = ps(S2, "PSck", [128, 512], F32)
                PScv = [ps(S2, f"PScv{i}", [128, 64], F32) for i in range(2)]
                for kind, ped in (("k", pe_k), ("v", pe_v)):
                    peT[kind] = sb(S2, "peT" + kind, [64, 32], BF16)
                    fw.dma("pool", peT[kind][:], ped.rearrange("s d -> d s"), writes=["peT" + kind], allow_slow_non_contiguous=True)
                    constc[kind] = sb(S2, "constc" + kind, [128, 1], F32)
                    for s_ in range(32):
                        mm(PSc[:, 0:1], W1[kind][0:64, s_, :], peT[kind][:, s_:s_ + 1], s_ == 0, s_ == 31, ["W1" + kind, "peT" + kind], ["PSc"])
                    act(lambda: A.copy(out=constc[kind][:], in_=PSc[:, 0:1]), ["PSc"], ["constc" + kind])
                hid = {}
                hi = 0
                for kind, srcT, sk in (("k", kcT, "kcT"), ("v", vcT, "vcT")):
                    for g in range(2):
                        hd = sb(S2, f"hid{kind}{g}", [128, 512], BF16)
                        hid[(kind, g)] = hd
                        hk2 = f"hid{kind}{g}"
                        pool(lambda: G.memset(hd[:], 0.0), [], [hk2])
                        ph = PSh[hi % 2]
                        phk = f"PSh{hi % 2}"
                        hi += 1
                        for s_ in range(32):
                            mm(ph[:, 0:511], W1[kind][g * 64:(g + 1) * 64, s_, :], srcT[g * 64:(g + 1) * 64, s_:s_ + 16 * 510 + 1:16],
                               s_ == 0, s_ == 31, ["W1" + kind, sk], [phk])
                        act(lambda: A.activation(out=hd[:, 0:511], in_=ph[:, 0:511], func=AF.Gelu_apprx_tanh, bias=constc[kind][:, 0:1]),
                            [phk, "constc" + kind], [hk2])
                for g in range(2):
                    mm(PSck[:], W2p[:, g, :], hid[("k", g)][:], g == 0, g == 1, ["W2p", f"hidk{g}"], ["PSck"])
                act(lambda: A.copy(out=CkT[:], in_=PSck[:]), ["PSck"], ["CkT"])
                ci = 0
                for g in range(2):
                    for cc in range(4):
                        pc = PScv[ci % 2]
                        pck = f"PScv{ci % 2}"
                        ci += 1
                        mm(pc[:], hid[("v", g)][:, cc * 128:(cc + 1) * 128], W2v[:], True, True, [f"hidv{g}", "W2v"], [pck])
                        act(lambda: A.activation(out=CvA[:, cc, g, 0:64], in_=pc[:], func=AF.Copy, scale=vcc[:, cc:cc + 1]), [pck, "vcc"], ["CvA"])
                    dve(lambda: V.tensor_copy(out=CvA[:, :, g, 64], in_=vcc[:, :]), ["vcc", "CvA"], ["CvA"])
                if "p2" in dbg:
                    fw.dma("sp", ddbg("d_CkT", [128, 512], BF16)[:, :], CkT[:], reads=["CkT"], writes=["d_CkT"])
                    fw.dma("sp", ddbg("d_CvA", [128, 4 * 130], BF16)[:, :], CvA[:].rearrange("p a b c -> p (a b c)"), reads=["CvA"], writes=["d_CvA"])
                fw.barrier()
            if upto <= 2:
                fw.finish("sp")
                print("instr", fw.ninstr, "waits", fw.nwait)
                return nc, dbg_out
        with contextlib.ExitStack() as S3:
            def sincos(st, angt, n, sdst, cdst, tagk):
                a2 = sb(st, "sc_a2" + tagk, [128, n], F32)
                ki_ = sb(st, "sc_ki" + tagk, [128, n], I32)
                kf_ = sb(st, "sc_kf" + tagk, [128, n], F32)
                r_ = sb(st, "sc_r" + tagk, [128, n], F32)
                for shift, dst, dk in ((0.0, sdst, "sc_s" + tagk), (math.pi / 2, cdst, "sc_c" + tagk)):
                    dve(lambda: V.tensor_scalar(out=a2[:], in0=angt[:], scalar1=shift, scalar2=None, op0=ALU.add), ["sc_ang" + tagk], ["sc_a2" + tagk])
                    dve(lambda: V.tensor_scalar(out=ki_[:], in0=a2[:], scalar1=1.0 / TWO_PI, scalar2=None, op0=ALU.mult), ["sc_a2" + tagk], ["sc_ki" + tagk])
                    dve(lambda: V.tensor_copy(out=kf_[:], in_=ki_[:]), ["sc_ki" + tagk], ["sc_kf" + tagk])
                    dve(lambda: V.scalar_tensor_tensor(out=r_[:], in0=kf_[:], scalar=-CW1, in1=a2[:], op0=ALU.mult, op1=ALU.add),
                        ["sc_kf" + tagk, "sc_a2" + tagk], ["sc_r" + tagk])
                    dve(lambda: V.scalar_tensor_tensor(out=r_[:], in0=kf_[:], scalar=-CW2, in1=r_[:], op0=ALU.mult, op1=ALU.add),
                        ["sc_kf" + tagk, "sc_r" + tagk], ["sc_r" + tagk])
                    dve(lambda: V.tensor_scalar(out=r_[:], in0=r_[:], scalar1=-PI_LO, scalar2=PI_LO, op0=ALU.max, op1=ALU.min), ["sc_r" + tagk], ["sc_r" + tagk])
                    act(lambda: A.activation(out=dst[:], in_=r_[:], func=AF.Sin), ["sc_r" + tagk], [dk])

            def tt(out, a, b, op, r, w):
                dve(lambda: V.tensor_tensor(out=out, in0=a, in1=b, op=op), r, w)

            LR = sb(S3, "LR", [128, 32], F32)
            LI = sb(S3, "LI", [128, 32], F32)
            LDT = sb(S3, "LDT", [128, 32], F32)
            for half in (0, 64):
                fw.dma("sp", LR[half:half + 64, :], s5_a_re.rearrange("g n -> n g"), writes=["LR"], allow_slow_non_contiguous=True)
                fw.dma("sp", LI[half:half + 64, :], s5_a_im.rearrange("g n -> n g"), writes=["LI"], allow_slow_non_contiguous=True)
            fw.dma("sp", LDT[:], s5_log_dt[0:1, :].partition_broadcast(128), writes=["LDT"])
            sgn = sb(S3, "sgn", [128, 1], F32)
            Psw = sb(S3, "Psw", [128, 128], F32)
            dcol = sb(S3, "dcol", [128, 4], F32)
            fw.dma("sp", sgn[:], sgn_d[:, :], writes=["sgn"])
            fw.dma("sp", Psw[:], psw_d[:, :], writes=["Psw"])
            fw.dma("sp", dcol[:], s5_d.rearrange("(ct p) -> p ct", p=128), writes=["dcol"], allow_slow_non_contiguous=True)
            stp = sb(S3, "stp", [128, 32], F32)
            mag = sb(S3, "mag", [128, 32], F32)
            ang = sb(S3, "ang", [128, 32], F32)
            sn = sb(S3, "sn", [128, 32], F32)
            cs = sb(S3, "cs", [128, 32], F32)
            abr = sb(S3, "abr", [128, 32], F32)
            abi = sb(S3, "abi", [128, 32], F32)
            den = sb(S3, "den", [128, 32], F32)
            tq = sb(S3, "tq", [128, 32], F32)
            nr = sb(S3, "nr", [128, 32], F32)
            cre = sb(S3, "cre", [128, 32], F32)
            cim = sb(S3, "cim", [128, 32], F32)
            act(lambda: A.activation(out=stp[:], in_=LDT[:], func=AF.Exp), ["LDT"], ["stp"])
            tt(mag[:], LR[:], stp[:], ALU.mult, ["LR", "stp"], ["mag"])
            act(lambda: A.activation(out=mag[:], in_=mag[:], func=AF.Exp), ["mag"], ["mag"])
            tt(ang[:], LI[:], stp[:], ALU.mult, ["LI", "stp"], ["sc_angp"])
            sincos(S3, ang, 32, sn, cs, "p")
            tt(abr[:], mag[:], cs[:], ALU.mult, ["mag", "sc_cp"], ["abr"])
            tt(abi[:], mag[:], sn[:], ALU.mult, ["mag", "sc_sp"], ["abi"])
            tt(den[:], LR[:], LR[:], ALU.mult, ["LR"], ["den"])
            tt(tq[:], LI[:], LI[:], ALU.mult, ["LI"], ["tq"])
            tt(den[:], den[:], tq[:], ALU.add, ["den", "tq"], ["den"])
            dve(lambda: V.reciprocal(out=den[:], in_=den[:]), ["den"], ["den"])
            dve(lambda: V.tensor_scalar(out=nr[:], in0=abr[:], scalar1=-1.0, scalar2=None, op0=ALU.add), ["abr"], ["nr"])
            tt(cre[:], nr[:], LR[:], ALU.mult, ["nr", "LR"], ["cre"])
            tt(tq[:], abi[:], LI[:], ALU.mult, ["abi", "LI"], ["tq"])
            tt(cre[:], cre[:], tq[:], ALU.add, ["cre", "tq"], ["cre"])
            tt(cre[:], cre[:], den[:], ALU.mult, ["cre", "den"], ["cre"])
            tt(cim[:], abi[:], LR[:], ALU.mult, ["abi", "LR"], ["cim"])
            tt(tq[:], nr[:], LI[:], ALU.mult, ["nr", "LI"], ["tq"])
            tt(cim[:], cim[:], tq[:], ALU.subtract, ["cim", "tq"], ["cim"])
            tt(cim[:], cim[:], den[:], ALU.mult, ["cim", "den"], ["cim"])
            NL = 12
            NR = 18
            WR = sb(S3, "WR", [128, NR, 32], F32)
            WIS = sb(S3, "WIS", [128, NR, 32], F32)
            dve(lambda: V.tensor_copy(out=WR[:, 0, :], in_=abr[:]), ["abr"], ["WR"])
            dve(lambda: V.tensor_scalar(out=WIS[:, 0, :], in0=abi[:], scalar1=sgn[:, 0:1], scalar2=None, op0=ALU.mult), ["abi", "sgn"], ["WIS"])
            for l in range(1, NL):
                tt(tq[:], WR[:, l - 1, :], WR[:, l - 1, :], ALU.mult, ["WR"], ["tq"])
                tt(nr[:], WIS[:, l - 1, :], WIS[:, l - 1, :], ALU.mult, ["WIS"], ["nr"])
                tt(WR[:, l, :], tq[:], nr[:], ALU.subtract, ["tq", "nr", "WR"], ["WR"])
                dve(lambda: V.scalar_tensor_tensor(out=WIS[:, l, :], in0=WR[:, l - 1, :], scalar=2.0, in1=WIS[:, l - 1, :], op0=ALU.mult, op1=ALU.mult),
                    ["WR", "WIS"], ["WIS"])
            for l in range(6):
                a_, c_ = 2 * l, 2 * l + 1
                tt(tq[:], WR[:, a_, :], WR[:, c_, :], ALU.mult, ["WR"], ["tq"])
                tt(nr[:], WIS[:, a_, :], WIS[:, c_, :], ALU.mult, ["WIS"], ["nr"])
                tt(WR[:, 12 + l, :], tq[:], nr[:], ALU.subtract, ["tq", "nr", "WR"], ["WR"])
                tt(tq[:], WR[:, a_, :], WIS[:, c_, :], ALU.mult, ["WR", "WIS"], ["tq"])
                tt(nr[:], WIS[:, a_, :], WR[:, c_, :], ALU.mult, ["WR", "WIS"], ["nr"])
                tt(WIS[:, 12 + l, :], tq[:], nr[:], ALU.add, ["tq", "nr", "WIS"], ["WIS"])
            Bpad = sb(S3, "Bpad", [128, 32, 128], BF16)
            Cpad = sb(S3, "Cpad", [128, 32, 128], BF16)
            with contextlib.ExitStack() as S3a:
                PSs = [ps(S3a, f"PSs{i}", [128, 128], F32) for i in range(2)]
                BR = sb(S3a, "BR", [64, 32, 16], F32)
                BI = sb(S3a, "BI", [64, 32, 16], F32)
                fw.dma("sp", BR[:], s5_b_re.rearrange("g n p -> n g p"), writes=["BR"])
                fw.dma("sp", BI[:], s5_b_im.rearrange("g n p -> n g p"), writes=["BI"])
                bbr = sb(S3a, "bbr", [64, 32, 16], F32)
                bbi = sb(S3a, "bbi", [64, 32, 16], F32)
                tb_ = sb(S3a, "tb_", [64, 32, 16], F32)
                creb = cre[0:64, :].unsqueeze(2).broadcast_to([64, 32, 16])
                cimb = cim[0:64, :].unsqueeze(2).broadcast_to([64, 32, 16])
                tt(bbr[:], BR[:], creb, ALU.mult, ["BR", "cre"], ["bbr"])
                tt(tb_[:], BI[:], cimb, ALU.mult, ["BI", "cim"], ["tb_"])
                tt(bbr[:], bbr[:], tb_[:], ALU.subtract, ["bbr", "tb_"], ["bbr"])
                tt(bbi[:], BI[:], creb, ALU.mult, ["BI", "cre"], ["bbi"])
                tt(tb_[:], BR[:], cimb, ALU.mult, ["BR", "cim", "bbr"], ["tb_"])
                tt(bbi[:], bbi[:], tb_[:], ALU.add, ["bbi", "tb_"], ["bbi"])
                ZpR = sb(S3a, "ZpR", [64, 32, 128], F32)
                ZpI = sb(S3a, "ZpI", [64, 32, 128], F32)
                pool(lambda: G.memset(ZpR[:], 0.0), [], ["ZpR"])
                pool(lambda: G.memset(ZpI[:], 0.0), [], ["ZpI"])
                for gl in range(8):
                    for Z, bb, zk, bk in ((ZpR, bbr, "ZpR", "bbr"), (ZpI, bbi, "ZpI", "bbi")):
                        dve(lambda: V.tensor_copy(out=Z[:].rearrange("n (ct gl) c -> n ct gl c", gl=8)[:, :, gl, 16 * gl:16 * gl + 16],
                                                  in_=bb[:].rearrange("n (ct gl) p -> n ct gl p", gl=8)[:, :, gl, :]), [bk, zk], [zk])
                for g in range(32):
                    pz = PSs[g % 2]
                    pzk = f"PSs{g % 2}"
                    tr(pz[:, 0:64], ZpR[:, g, :], identf[0:64, 0:64], ["ZpR", "identf"], [pzk], signal=False)
                    tr(pz[:, 64:128], ZpI[:, g, :], identf[0:64, 0:64], ["ZpI", "identf"], [pzk])
                    act(lambda: A.copy(out=Bpad[:, g, :], in_=pz[:]), [pzk], ["Bpad"])
                Cnat = sb(S3a, "Cnat", [128, 4, 128], F32)
                fw.dma("sp", Cnat[:, :, 0:64], s5_c_re.rearrange("(ct gl) p n -> (gl p) ct n", gl=8), writes=["Cnat"])
                fw.dma("sp", Cnat[:, :, 64:128], s5_c_im.rearrange("(ct gl) p n -> (gl p) ct n", gl=8), writes=["Cnat"])
                dve(lambda: V.tensor_scalar(out=Cnat[:, :, 64:128], in0=Cnat[:, :, 64:128], scalar1=-1.0, scalar2=None, op0=ALU.mult), ["Cnat"], ["Cnat"])
                pool(lambda: G.memset(Cpad[:], 0.0), [], ["Cpad"])
                for ct in range(4):
                    pz = PSs[ct % 2]
                    pzk = f"PSs{ct % 2}"
                    tr(pz[:, :], Cnat[:, ct, :], identf[:], ["Cnat", "identf"], [pzk])
                    for gl in range(8):
                        act(lambda: A.copy(out=Cpad[:, ct * 8 + gl, 16 * gl:16 * gl + 16], in_=pz[:, 16 * gl:16 * gl + 16]), [pzk, "Cpad"], ["Cpad"])
                fw.barrier()
            uT = [sb(S3, f"uT{i}", [128, 8192], BF16) for i in range(2)]
            X0 = [sb(S3, f"X0_{i}", [128, 8192], BF16) for i in range(2)]
            Ya = [sb(S3, f"Ya{i}", [128, 1536], BF16) for i in range(2)]
            Yb = [sb(S3, f"Yb{i}", [128, 384], BF16) for i in range(2)]
            Wa = [sb(S3, f"Wa{i}", [128, 2048], BF16) for i in range(2)]
            Wb = [sb(S3, f"Wb{i}", [128, 2048], BF16) for i in range(2)]
            Rm = [sb(S3, f"Rm{i}", [128, NR, 128], BF16) for i in range(2)]
            rtmp = sb(S3, "rtmp", [128, 128], F32)
            rtmp2 = sb(S3, "rtmp2", [128, 128], F32)
            xin = [sb(S3, f"xin{i}", [128, 2], BF16) for i in range(2)]
            ytmp = sb(S3, "ytmp", [128, 512], F32)
            yps = [ps(S3, f"yps{i}", [128, 512], F32) for i in range(4)]
            PB = [ps(S3, f"PB{i}", [128, 512], F32) for i in range(4)]
            evi = [0]
            pbi = [0, 0]

            def evac(out, in_, r, w):
                evi[0] += 1
                if evi[0] % 2:
                    act(lambda: A.copy(out=out, in_=in_), r, w)
                else:
                    dve(lambda: V.tensor_copy(out=out, in_=in_), r, w)

            def next_pb(gp):
                i = 2 * gp + pbi[gp] % 2
                pbi[gp] += 1
                return PB[i], f"PB{i}"

            for ct in range(4):
                u = uT[ct % 2]
                uk = f"uT{ct % 2}"
                for q4 in range(4):
                    fw.dma("sp", u[:, q4 * 2048:(q4 + 1) * 2048], uscr[ct, :, q4 * 2048:(q4 + 1) * 2048], reads=["uscr"], writes=[uk])
                def s5_group(ct, gl, u, uk):
                    g = ct * 8 + gl
                    gp = g % 2
                    R = Rm[gp]
                    rk = f"Rm{gp}"
                    for l in range(NR):
                        dve(lambda: V.tensor_scalar(out=rtmp[:], in0=identf[:], scalar1=WR[:, l, g:g + 1], scalar2=None, op0=ALU.mult),
                            ["identf", "WR"], ["rtmp"])
                        dve(lambda: V.scalar_tensor_tensor(out=R[:, l, :], in0=Psw[:], scalar=WIS[:, l, g:g + 1], in1=rtmp[:], op0=ALU.mult, op1=ALU.add),
                            ["Psw", "WIS", "rtmp"], [rk])
                    x0 = X0[gp]
                    x0k = f"X0_{gp}"
                    for tb in range(16):
                        pb, pbk = next_pb(gp)
                        mm(pb[:], Bpad[:, g, :], u[:, tb * 512:(tb + 1) * 512], True, True, ["Bpad", uk], [pbk])
                        evac(x0[:, tb * 512:(tb + 1) * 512], pb[:], [pbk], [x0k])
                        yield
                    src, srck = x0, x0k
                    bufs = [(Ya[gp], f"Ya{gp}"), (Yb[gp], f"Yb{gp}")]
                    n = 6144
                    bsel = 0
                    for l in range(5):
                        quarter = n // 4
                        dst, dstk = bufs[bsel]
                        for c0 in range(0, quarter, 512):
                            w_ = min(512, quarter - c0)
                            pb, pbk = next_pb(gp)
                            mm(pb[:, 0:w_], R[:, 12 + l, :], src[:, 4 * c0:4 * c0 + 4 * w_:4], True, False, [rk, srck], [pbk])
                            mm(pb[:, 0:w_], R[:, 2 * l + 1, :], src[:, 4 * c0 + 1:4 * c0 + 4 * w_:4], False, False, [rk, srck], [pbk])
                            mm(pb[:, 0:w_], R[:, 2 * l, :], src[:, 4 * c0 + 2:4 * c0 + 4 * w_:4], False, True, [rk, srck], [pbk])
                            dve(lambda: V.tensor_tensor(out=dst[:, c0:c0 + w_], in0=pb[:, 0:w_], in1=src[:, 4 * c0 + 3:4 * c0 + 4 * w_:4], op=ALU.add),
                                [pbk, srck], [dstk])
                            yield
                        src, srck = dst, dstk
                        bsel ^= 1
                        n = quarter
                    assert n == 6
                    dst, dstk = bufs[bsel]
                    pb, pbk = next_pb(gp)
                    mm(pb[:, 0:3], R[:, 10, :], src[:, 0:6:2], True, False, [rk, srck], [pbk])
                    mm(pb[:, 0:3], identb[:], src[:, 1:6:2], False, True, ["identb", srck], [pbk])
                    evac(dst[:, 0:3], pb[:, 0:3], [pbk], [dstk])
                    yield
                    src, srck = dst, dstk
                    xi = xin[gp]
                    xik = f"xin{gp}"
                    pb, pbk = next_pb(gp)
                    mm(pb[:, 0:1], R[:, 11, :], src[:, 0:1], True, False, [rk, srck], [pbk])
                    mm(pb[:, 0:1], identb[:], src[:, 1:2], False, True, ["identb", srck], [pbk])
                    evac(xi[:, 0:1], pb[:, 0:1], [pbk], [xik])
                    yield
                    pb, pbk = next_pb(gp)
                    mm(pb[:, 0:1], R[:, 11, :], xi[:, 0:1], True, False, [rk, xik], [pbk])
                    mm(pb[:, 0:1], identb[:], src[:, 2:3], False, True, ["identb", srck], [pbk])
                    evac(xi[:, 1:2], pb[:, 0:1], [pbk], [xik])
                    yield
                    pb, pbk = next_pb(gp)
                    mm(pb[:, 0:1], R[:, 0, :], xi[:, 1:2], True, False, [rk, xik], [pbk])
                    mm(pb[:, 0:1], identb[:], x0[:, 6144:6145], False, True, ["identb", x0k], [pbk])
                    evac(x0[:, 6144:6145], pb[:, 0:1], [pbk], [x0k])
                    yield
                    cur, curk, off = x0, x0k, 6144
                    wb = [(Wa[gp], f"Wa{gp}"), (Wb[gp], f"Wb{gp}")]
                    for l in range(6):
                        k_ = 4 ** l
                        dst, dstk = wb[l % 2]
                        for blk in range(4):
                            c0 = blk * 512
                            terms = []
                            for j_, ridx in ((1, 2 * l), (2, 2 * l + 1), (3, 12 + l)):
                                sh = j_ * k_
                                if c0 + 512 > sh:
                                    terms.append((R[:, ridx, :], rk, sh, max(c0, sh)))
                            if not terms:
                                act(lambda: A.copy(out=dst[:, c0:c0 + 512], in_=cur[:, off + c0:off + c0 + 512]), [curk], [dstk])
                                yield
                                continue
                            pb, pbk = next_pb(gp)
                            for ti_, (lh, lhk, sh, lo) in enumerate(terms):
                                mm(pb[:, lo - c0:512], lh, cur[:, off + lo - sh:off + c0 + 512 - sh], ti_ == 0, ti_ == len(terms) - 1, [lhk, curk], [pbk])
                            lo1 = terms[0][3]
                            if lo1 > c0:
                                act(lambda: A.copy(out=dst[:, c0:lo1], in_=cur[:, off + c0:off + lo1]), [curk], [dstk])
                            dve(lambda: V.tensor_tensor(out=dst[:, lo1:c0 + 512], in0=pb[:, lo1 - c0:512], in1=cur[:, off + lo1:off + c0 + 512], op=ALU.add),
                                [pbk, curk], [dstk])
                            yield
                        cur, curk, off = dst, dstk, 0
                    for blk in range(4):
                        mm(yps[blk][:], Cpad[:, g, :], cur[:, blk * 512:(blk + 1) * 512], gl == 0, gl == 7, ["Cpad", curk], [f"yps{blk}"])

                for gpair in range(4):
                    gens = [s5_group(ct, 2 * gpair, u, uk), s5_group(ct, 2 * gpair + 1, u, uk)]
                    alive = [True, True]
                    while any(alive):
                        for gi_ in range(2):
                            if alive[gi_]:
                                try:
                                    next(gens[gi_])
                                except StopIteration:
                                    alive[gi_] = False
                for blk in range(4):
                    dve(lambda: V.scalar_tensor_tensor(out=ytmp[:], in0=u[:, 6144 + blk * 512:6144 + (blk + 1) * 512], scalar=dcol[:, ct:ct + 1],
                                                       in1=yps[blk][:], op0=ALU.mult, op1=ALU.add), [uk, "dcol", f"yps{blk}"], ["ytmp"])
                    act(lambda: A.activation(out=gyT[:, ct, blk * 512:(blk + 1) * 512], in_=ytmp[:], func=AF.Gelu_apprx_tanh), ["ytmp"], ["gyT"])
            if "p3" in dbg:
                fw.dma("sp", ddbg("d_gyT", [128, 4 * 2048], BF16)[:, :], gyT[:].rearrange("p a b -> p (a b)"), reads=["gyT"], writes=["d_gyT"])
            fw.barrier()
        if upto <= 3:
            fw.finish("sp")
            print("instr", fw.ninstr, "waits", fw.nwait)
            return nc, dbg_out

        with contextlib.ExitStack() as S4:
            tri = sb(S4, "tri", [128, 512], BF16)
            atri = sb(S4, "atri", [128, 512], BF16)
            fw.dma("pool", tri[:], tri_d[:, :], writes=["tri"])
            fw.dma("pool", atri[:], atri_d[:, :], writes=["atri"])
            grow = sb(S4, "grow", [128, 1024], F32)
            brow = sb(S4, "brow", [128, 1024], F32)
            g1row = sb(S4, "g1row", [128, 1024], F32)
            b1row = sb(S4, "b1row", [128, 1024], F32)
            fw.dma("sp", grow[:], ln_emb[0:1, :].partition_broadcast(128), writes=["grow"])
            fw.dma("sp", brow[:], ln_emb[1:2, :].partition_broadcast(128), writes=["brow"])
            fw.dma("sp", g1row[:], ln1_d[0:1, :].partition_broadcast(128), writes=["g1row"])
            fw.dma("sp", b1row[:], ln1_d[1:2, :].partition_broadcast(128), writes=["b1row"])
            Wo = sb(S4, "Wo", [128, 8, 1024], BF16)
            wov = w_o.rearrange("(kc p) c -> p kc c", p=128)
            for kc in range(0, 8, 2):
                fw.dma("pool", Wo[:, kc:kc + 2, :], wov[:, kc:kc + 2, :], writes=["Wo"])
            KmT = sb(S4, "KmT", [128, 4, 256], BF16)
            VmA = sb(S4, "VmA", [128, 2, 4, 129], BF16)
            wv = w_in.rearrange("(dc p) c -> p dc c", p=128)
            PS_S = [ps(S4, f"PS_S{i}", [128, 512], F32) for i in range(2)]
            PO = [ps(S4, f"PO{i}", [128, 512], F32) for i in range(2)]
            PF = [ps(S4, f"PF4_{i}", [128, 512], F32) for i in range(2)]
            PT4 = ps(S4, "PT4", [128, 1024], BF16)
            PX = ps(S4, "PX", [128, 512], F32)
            PS_S = PS_S + [PF[1]]
            cnt = {"s": 0, "o": 0, "f": 0, "pt": 0, "wp": 0}

            def nxt(lst, key, name):
                i = cnt[key] % len(lst)
                cnt[key] += 1
                if name == "PS_S" and i == 2:
                    return lst[i], "PF4_1"
                return lst[i], f"{name}{i}"

            with contextlib.ExitStack() as S4m:
                memf = sb(S4m, "memf", [128, 2, 1024], F32)
                memb = sb(S4m, "memb", [128, 2, 1024], BF16)
                memT = sb(S4m, "memT", [128, 8, 256], BF16)
                Wmk = sb(S4m, "Wmk", [128, 8, 512], BF16)
                Wmv = sb(S4m, "Wmv", [128, 8, 512], BF16)
                fw.dma("sp", memf[:], mem_d.rearrange("(t p) c -> p t c", p=128), writes=["memf"])
                wmv_ = w_mem_kv.rearrange("(dc p) c -> p dc c", p=128)
                for dc in range(0, 8, 4):
                    fw.dma("pool", Wmk[:, dc:dc + 4, :], wmv_[:, dc:dc + 4, 0:512], writes=["Wmk"])
                    fw.dma("pool", Wmv[:, dc:dc + 4, :], wmv_[:, dc:dc + 4, 512:1024], writes=["Wmv"])
                dve(lambda: V.tensor_copy(out=memb[:], in_=memf[:]), ["memf"], ["memb"])
                for mt in range(2):
                    for dc in range(8):
                        tr(PT4[:, dc * 128:(dc + 1) * 128], memb[:, mt, dc * 128:(dc + 1) * 128], identb[:], ["memb", "identb"], ["PT4"], signal=(dc == 7))
                    act(lambda: A.copy(out=memT[:, :, mt * 128:(mt + 1) * 128], in_=PT4[:].rearrange("p (a b) -> p a b", a=8)), ["PT4"], ["memT"])
                for h in range(4):
                    pf, pfk = nxt(PF, "f", "PF4_")
                    for dc in range(8):
                        mm(pf[:, 0:256], Wmk[:, dc, h * 128:(h + 1) * 128], memT[:, dc, :], dc == 0, dc == 7, ["Wmk", "memT"], [pfk])
                    act(lambda: A.copy(out=KmT[:, h, :], in_=pf[:, 0:256]), [pfk], ["KmT"])
                for mc in range(2):
                    pf, pfk = nxt(PF, "f", "PF4_")
                    for dc in range(8):
                        mm(pf[:], memT[:, dc, mc * 128:(mc + 1) * 128], Wmv[:, dc, :], dc == 0, dc == 7, ["Wmv", "memT"], [pfk])
                    act(lambda: A.copy(out=VmA[:, mc, :, 0:128], in_=pf[:].rearrange("p (h d) -> p h d", h=4)), [pfk], ["VmA"])
                pool(lambda: G.memset(VmA[:, :, :, 128:129], 1.0), ["VmA"], ["VmA"])
                fw.barrier()

            hTq = sb(S4, "hTq", [128, 8, 512], BF16)
            QT = sb(S4, "QT", [128, 4, 4, 128], BF16)
            qmT = sb(S4, "qmT", [128, 4, 4, 128], BF16)
            gn = sb(S4, "gn", [128, 4, 24], F32)
            oT = sb(S4, "oT", [128, 4, 512], BF16)
            omT = sb(S4, "omT", [128, 4, 512], BF16)
            mrgT = sb(S4, "mrgT", [128, 8, 512], BF16)
            st4 = sb(S4, "st4", [128, 12], F32)
            mv4 = sb(S4, "mv4", [128, 2], F32)
            rs4 = sb(S4, "rs4", [128, 1], F32)
            def ln_tok(xt, xk, dst, dstk, grow_, brow_, gk, bk):
                dve(lambda: V.bn_stats(out=st4[:, 0:6], in_=xt[:, 0:512]), [xk], ["st4"])
                dve(lambda: V.bn_stats(out=st4[:, 6:12], in_=xt[:, 512:1024]), [xk], ["st4"])
                dve(lambda: V.bn_aggr(out=mv4[:, 0:2], in_=st4[:, 0:12]), ["st4"], ["mv4"])
                act(lambda: A.activation(out=rs4[:], in_=mv4[:, 1:2], func=AF.Sqrt, bias=LN_EPS, scale=1.0), ["mv4"], ["rs4"])
                dve(lambda: V.reciprocal(out=rs4[:], in_=rs4[:]), ["rs4"], ["rs4"])
                dve(lambda: V.tensor_scalar(out=dst[:], in0=xt[:], scalar1=mv4[:, 0:1], scalar2=rs4[:, 0:1], op0=ALU.subtract, op1=ALU.mult),
                    [xk, "mv4", "rs4"], [dstk])
                pool(lambda: G.tensor_tensor(out=dst[:], in0=dst[:], in1=grow_[:], op=ALU.mult), [dstk, gk], [dstk])
                pool(lambda: G.tensor_tensor(out=dst[:], in0=dst[:], in1=brow_[:], op=ALU.add), [dstk, bk], [dstk])

            wpid = [0]
            curB = [0]

            def wpiece(src_ap):
                w_, wk_ = nxt(wp, "wp", "wp")
                nk_ = src_ap.shape[1]
                pid = wpid[0]
                wpid[0] += 1
                sc_ = wscr[pid, :, 0:nk_ * 128].rearrange("p (k c) -> p k c", k=nk_)
                if curB[0] == 0:
                    fw.dma("pool", w_[:, 0:nk_, :], src_ap, writes=[wk_])
                    fw.dma("sp", sc_, w_[:, 0:nk_, :], reads=[wk_], writes=[f"wscr{pid}"])
                else:
                    fw.dma("sp", w_[:, 0:nk_, :], sc_, reads=[f"wscr{pid}"], writes=[wk_])
                return w_, wk_

            wno_v = w_nsa_out.rearrange("(kc p) c -> p kc c", p=128)
            wmo_v = w_mem_out.rearrange("(kc p) c -> p kc c", p=128)
            wgl_v = w_s5_glu.rearrange("(kc p) c -> p kc c", p=128)

            for B in range(4):
                wpid[0] = 0
                curB[0] = B
                with contextlib.ExitStack() as SA:
                    xq = [sb(SA, f"xq{i}", [128, 1024], F32) for i in range(1)]
                    h0t = sb(SA, "h0t", [128, 1024], F32)
                    h0b = sb(SA, "h0b", [128, 1024], BF16)
                    posi4 = sb(SA, "posi4", [128, 512], I32)
                    posf4 = sb(SA, "posf4", [128, 512], F32)
                    angs4 = sb(SA, "sc_angq", [128, 512], F32)
                    cos4 = sb(SA, "cos4", [128, 512], F32)
                    sin4 = sb(SA, "sin4", [128, 512], F32)
                    t14 = sb(SA, "t14", [128, 512], F32)
                    t24 = sb(SA, "t24", [128, 512], F32)
                    Wq4 = sb(SA, "Wq4", [128, 8, 4, 128], BF16)
                    Wqsw = sb(SA, "Wqsw", [128, 8, 4, 128], BF16)
                    Wgn = sb(SA, "Wgn", [128, 8, 24], BF16)
                    if B == 0:
                        for half in range(2):
                            for dc in range(8):
                                fw.dma("pool", Wq4[:, dc, :, half * 64:(half + 1) * 64],
                                       wv[:, dc, half * 256:(half + 1) * 256].rearrange("p (h d) -> p h d", h=4), writes=["Wq4"])
                        fw.dma("pool", Wgn[:], wv[:, :, C_GN:C_GN + 24], writes=["Wgn"])
                        pool(lambda: G.memset(Wqsw[:], 0.0), [], ["Wqsw"])
                        for half in range(2):
                            b0 = half * 64
                            dve(lambda: V.tensor_scalar(out=Wqsw[:, :, :, b0:b0 + 8], in0=Wq4[:, :, :, b0 + 8:b0 + 16], scalar1=-1.0, scalar2=None, op0=ALU.mult),
                                ["Wq4", "Wqsw"], ["Wqsw"])
                            dve(lambda: V.tensor_copy(out=Wqsw[:, :, :, b0 + 8:b0 + 16], in_=Wq4[:, :, :, b0:b0 + 8]), ["Wq4", "Wqsw"], ["Wqsw"])
                        fw.dma("sp", wqscr[0, :, :], Wq4[:].rearrange("p a b c -> p (a b c)"), reads=["Wq4"], writes=["wqscr0"])
                        fw.dma("sp", wqscr[1, :, :], Wqsw[:].rearrange("p a b c -> p (a b c)"), reads=["Wqsw"], writes=["wqscr1"])
                        fw.dma("sp", wgnscr[:, :], Wgn[:].rearrange("p a b -> p (a b)"), reads=["Wgn"], writes=["wgnscr"])
                    else:
                        fw.dma("sp", Wq4[:].rearrange("p a b c -> p (a b c)"), wqscr[0, :, :], reads=["wqscr0"], writes=["Wq4"])
                        fw.dma("sp", Wqsw[:].rearrange("p a b c -> p (a b c)"), wqscr[1, :, :], reads=["wqscr1"], writes=["Wqsw"])
                        fw.dma("sp", Wgn[:].rearrange("p a b -> p (a b)"), wgnscr[:, :], reads=["wgnscr"], writes=["Wgn"])

                    fw.dma("sp", posi4[:], posr[:, 6144 + B * 512:6144 + (B + 1) * 512].partition_broadcast(128), writes=["posi4"])
                    dve(lambda: V.tensor_copy(out=posf4[:], in_=posi4[:]), ["posi4"], ["posf4"])
                    dve(lambda: V.tensor_scalar(out=angs4[:], in0=posf4[:], scalar1=invf[:, 0:1], scalar2=None, op0=ALU.mult), ["posf4", "invf"], ["sc_angq"])
                    with contextlib.ExitStack() as Ssc:
                        sincos(Ssc, angs4, 512, sin4, cos4, "q")
                        fw.barrier()
                    Wqm = sb(SA, "Wqm", [128, 8, 512], BF16)
                    if B == 0:
                        for dc in range(0, 8, 4):
                            fw.dma("pool", Wqm[:, dc:dc + 4, :], wv[:, dc:dc + 4, C_QM:C_QM + 512], writes=["Wqm"])
                        fw.dma("sp", wqscr[2, :, :], Wqm[:].rearrange("p a b -> p (a b)"), reads=["Wqm"], writes=["wqscr2"])
                    else:
                        fw.dma("sp", Wqm[:].rearrange("p a b -> p (a b)"), wqscr[2, :, :], reads=["wqscr2"], writes=["Wqm"])
                    for tl in range(4):
                        Tt = OWN0 + B * 4 + tl
                        xt, xk = nxt(xq, "pt", "xq")
                        fw.dma("sp", xt[:], xr[Tt * 128:(Tt + 1) * 128, :], writes=[xk])
                        ln_tok(xt, xk, h0t, "h0t", grow, brow, "grow", "brow")
                        dve(lambda: V.tensor_copy(out=h0b[:], in_=h0t[:]), ["h0t"], ["h0b"])
                        for dc in range(8):
                            tr(PT4[:, dc * 128:(dc + 1) * 128], h0b[:, dc * 128:(dc + 1) * 128], identb[:], ["h0b", "identb"], ["PT4"], signal=(dc == 7))
                        act(lambda: A.copy(out=hTq[:, :, tl * 128:(tl + 1) * 128], in_=PT4[:].rearrange("p (a b) -> p a b", a=8)), ["PT4"], ["hTq"])
                    if "p4o" in dbg and B == 0:
                        fw.dma("sp", ddbg("d_Wq4", [128, 4096], BF16)[:, :], Wq4[:].rearrange("p a b c -> p (a b c)"), reads=["Wq4"], writes=["d_Wq4"])
                        fw.dma("sp", ddbg("d_Wqsw", [128, 4096], BF16)[:, :], Wqsw[:].rearrange("p a b c -> p (a b c)"), reads=["Wqsw"], writes=["d_Wqsw"])
                        fw.dma("sp", ddbg("d_hTq", [128, 4096], BF16)[:, :], hTq[:].rearrange("p a b -> p (a b)"), reads=["hTq"], writes=["d_hTq"])
                        fw.dma("sp", ddbg("d_cos4", [128, 512], F32)[:, :], cos4[:], reads=["sc_cq"], writes=["d_cos4"])
                        fw.dma("sp", ddbg("d_sin4", [128, 512], F32)[:, :], sin4[:], reads=["sc_sq"], writes=["d_sin4"])
                    for hh in range(4):
                        pa, pak = nxt(PF, "f", "PF4_")
                        for dc in range(8):
                            mm(pa[:], Wq4[:, dc, hh, :], hTq[:, dc, :], dc == 0, dc == 7, ["Wq4", "hTq"], [pak])
                        pb, pbk = nxt(PF, "f", "PF4_")
                        for dc in range(8):
                            mm(pb[:], Wqsw[:, dc, hh, :], hTq[:, dc, :], dc == 0, dc == 7, ["Wqsw", "hTq"], [pbk])
                        dve(lambda: V.tensor_tensor(out=t14[:], in0=pa[:], in1=cos4[:], op=ALU.mult), [pak, "sc_cq"], ["t14"])
                        dve(lambda: V.tensor_tensor(out=t24[:], in0=pb[:], in1=sin4[:], op=ALU.mult), [pbk, "sc_sq"], ["t24"])
                        pool(lambda: G.tensor_tensor(out=QT[:, :, hh, :], in0=t14[:].rearrange("p (t q) -> p t q", t=4),
                                                     in1=t24[:].rearrange("p (t q) -> p t q", t=4), op=ALU.add), ["t14", "t24"], ["QT"])
                    for h in range(4):
                        pa, pak = nxt(PF, "f", "PF4_")
                        for dc in range(8):
                            mm(pa[:], Wqm[:, dc, h * 128:(h + 1) * 128], hTq[:, dc, :], dc == 0, dc == 7, ["Wqm", "hTq"], [pak])
                        act(lambda: A.copy(out=qmT[:, :, h, :], in_=pa[:].rearrange("p (t q) -> p t q", t=4)), [pak], ["qmT"])
                    for tl in range(4):
                        pa, pak = nxt(PF, "f", "PF4_")
                        for dc in range(8):
                            mm(pa[:, 0:24], hTq[:, dc, tl * 128:(tl + 1) * 128], Wgn[:, dc, :], dc == 0, dc == 7, ["Wgn", "hTq"], [pak])
                        act(lambda: A.activation(out=gn[:, tl, :], in_=pa[:, 0:24], func=AF.Sigmoid), [pak], ["gn"])

                    fw.barrier()
                with contextlib.ExitStack() as SB:
                    cbA = [sb(SB, f"cbA{i}", [128, 512], BF16) for i in range(2)]
                    cbB = [sb(SB, f"cbB{i}", [128, 2, 512], BF16) for i in range(2)]
                    addb = [sb(SB, f"addb{i}", [128, 128], F32) for i in range(2)]
                    futb = [sb(SB, f"futb{i}", [128, 128], F32) for i in range(2)]
                    Pa = [sb(SB, f"Pa{i}", [128, 512], F32) for i in range(2)]
                    PTb = [sb(SB, f"PTb{i}", [128, 512], BF16) for i in range(4)]
                    imp = sb(SB, "imp", [128, 512], F32)
                    chk = sb(SB, "chk", [128, 512], F32)
                    sc2 = sb(SB, "sc2", [128, 128], F32)
                    sc3 = sb(SB, "sc3", [128, 128], F32)
                    m8a = sb(SB, "m8a", [128, 8], F32)
                    m8b = sb(SB, "m8b", [128, 8], F32)
                    selb = sb(SB, "selb", [128, 128], F32)
                    selsw = sb(SB, "selsw", [128, 128], F32)
                    QaA = [sb(SB, f"QaA{i}", [128, 4, 128], BF16) for i in range(2)]
                    QaB = [sb(SB, f"QaB{i}", [128, 4, 128], BF16) for i in range(2)]
                    rsum = sb(SB, "rsum", [128, 4], F32)
                    rinv = sb(SB, "rinv", [128, 4], F32)
                    coef = sb(SB, "coef", [128, 4], F32)
                    oacc = sb(SB, "oacc", [128, 512], F32)
                    otmp = sb(SB, "otmp", [128, 256], F32)
                    oaccb = sb(SB, "oaccb", [128, 512], BF16)
                    omacc = sb(SB, "omacc", [128, 512], F32)
                    omb = sb(SB, "omb", [128, 512], BF16)
                    for tl in range(4):
                        j = B * 4 + tl
                        qt = OWN0 + j
                        c2 = j % 2
                        fw.dma("pool", cbA[c2][:], cba_d[j, :, :], writes=[f"cbA{c2}"])
                        fw.dma("pool", cbB[c2][:], cbb_d[j, :, :, :].rearrange("a p c -> p a c"), writes=[f"cbB{c2}"])
                        fw.dma("sp", addb[c2][:], addb_d[j, :, :], writes=[f"addb{c2}"])
                        fw.dma("sp", futb[c2][:], futb_d[j, :, :], writes=[f"futb{c2}"])
                        for g in range(2):
                            r0 = g * 64
                            Qg = QT[r0:r0 + 64, tl, :, :].rearrange("p h q -> p (h q)")
                            for hh in range(4):
                                pS, pSk = nxt(PS_S, "s", "PS_S")
                                mm(pS[:], QT[r0:r0 + 64, tl, hh, :], CkT[r0:r0 + 64, :], True, False, ["QT", "CkT"], [pSk])
                                mm(pS[:], identb[:], cbA[c2][:], False, True, ["identb", f"cbA{c2}"], [pSk])
                                pa_ = Pa[hh % 2]
                                pak_ = f"Pa{hh % 2}"
                                act(lambda: A.activation(out=pa_[:], in_=pS[:], func=AF.Exp, scale=0.125, accum_out=rsum[:, hh:hh + 1]), [pSk], [pak_, "rsum"])
                                dve(lambda: V.tensor_scalar(out=rinv[:, hh:hh + 1], in0=rsum[:, hh:hh + 1], scalar1=1e-30, scalar2=None, op0=ALU.max), ["rsum"], ["rinv"])
                                dve(lambda: V.reciprocal(out=rinv[:, hh:hh + 1], in_=rinv[:, hh:hh + 1]), ["rinv"], ["rinv"])
                                if hh == 0:
                                    dve(lambda: V.tensor_scalar(out=imp[:], in0=pa_[:], scalar1=rinv[:, 0:1], scalar2=None, op0=ALU.mult), [pak_, "rinv"], ["imp"])
                                else:
                                    dve(lambda: V.scalar_tensor_tensor(out=imp[:], in0=pa_[:], scalar=rinv[:, hh:hh + 1], in1=imp[:], op0=ALU.mult, op1=ALU.add),
                                        [pak_, "rinv", "imp"], ["imp"])
                            dve(lambda: V.tensor_tensor(out=chk[:, 1:512], in0=imp[:, 1:512], in1=imp[:, 0:511], op=ALU.add), ["imp"], ["chk"])
                            dve(lambda: V.tensor_copy(out=chk[:, 0:1], in_=imp[:, 0:1]), ["imp", "chk"], ["chk"])
                            dve(lambda: V.tensor_reduce(out=sc2[:], in_=chk[:].rearrange("p (a b) -> p a b", b=4), axis=AX.X, op=ALU.add), ["chk"], ["sc2"])
                            dve(lambda: V.tensor_tensor(out=sc2[:], in0=sc2[:], in1=addb[c2][:], op=ALU.add), ["sc2", f"addb{c2}"], ["sc2"])
                            dve(lambda: V.max(out=m8a[:], in_=sc2[:]), ["sc2"], ["m8a"])
                            dve(lambda: V.match_replace(out=sc3[:], in_to_replace=m8a[:], in_values=sc2[:], imm_value=-3e9), ["sc2", "m8a"], ["sc3"])
                            dve(lambda: V.max(out=m8b[:], in_=sc3[:]), ["sc3"], ["m8b"])
                            dve(lambda: V.tensor_scalar(out=selb[:], in0=sc2[:], scalar1=m8b[:, 7:8], scalar2=NEG, op0=ALU.is_lt, op1=ALU.mult), ["sc2", "m8b"], ["selb"])
                            dve(lambda: V.tensor_tensor(out=selb[:], in0=selb[:], in1=futb[c2][:], op=ALU.min), ["selb", f"futb{c2}"], ["selb"])
                            dve(lambda: V.tensor_copy(out=selsw[:, 0:64], in_=selb[:, 64:128]), ["selb"], ["selsw"])
                            dve(lambda: V.tensor_copy(out=selsw[:, 64:128], in_=selb[:, 0:64]), ["selb", "selsw"], ["selsw"])
                            tr(PX[:, 0:128], selb[:], identf[:], ["selb", "identf"], ["PX"], signal=False)
                            tr(PX[:, 128:256], selsw[:], identf[:], ["selsw", "identf"], ["PX"])
                            qa, qb = QaA[g], QaB[g]
                            qr = slice(0, 64) if g == 0 else slice(64, 128)
                            br_ = slice(64, 128) if g == 0 else slice(0, 64)
                            srcA = PX[br_, 128:256] if g == 0 else PX[br_, 0:128]
                            srcB = PX[br_, 0:128] if g == 0 else PX[br_, 128:256]
                            act(lambda: A.copy(out=qa[br_, :, :], in_=srcA.unsqueeze(1).broadcast_to([64, 4, 128])), ["PX"], [f"QaA{g}"])
                            act(lambda: A.copy(out=qb[br_, :, :], in_=srcB.unsqueeze(1).broadcast_to([64, 4, 128])), ["PX"], [f"QaB{g}"])
                            pool(lambda: G.tensor_copy(out=qa[qr, :, :], in_=QT[qr, tl, :, :]), ["QT", f"QaA{g}"], [f"QaA{g}"])
                            pool(lambda: G.tensor_copy(out=qb[qr, :, :], in_=QT[qr, tl, :, :]), ["QT", f"QaB{g}"], [f"QaB{g}"])

                            def branch(klist, gate_idx, first):
                                po, pok = nxt(PO, "o", "PO")
                                n = len(klist)
                                scores = []

                                def emit_score(i):
                                    kap, kkey, extra, vap, vkey = klist[i][:5]
                                    rhs_, rhsk_ = (klist[i][5], klist[i][6]) if len(klist[i]) > 5 else (Qg, "QT")
                                    pS, pSk = nxt(PS_S, "s", "PS_S")
                                    mm(pS[:], kap, rhs_, True, len(extra) == 0, kkey if isinstance(kkey, list) else [kkey, rhsk_], [pSk])
                                    for ei, (el, er, ek) in enumerate(extra):
                                        mm(pS[:], el, er, False, ei == len(extra) - 1, ek, [pSk])
                                    return pS, pSk

                                mm(po[:, 0:260], zerob[:], VsA[:, 0:2, :, :].rearrange("p a b c -> p (a b c)"), True, False, ["zerob", "VsA"], [pok])
                                pend = [emit_score(0)]
                                if n > 1:
                                    pend.append(emit_score(1))
                                for i in range(n):
                                    pS, pSk = pend.pop(0)
                                    if i + 2 < n:
                                        pend.append(emit_score(i + 2))
                                    pt_, ptk = nxt(PTb, "pt", "PTb")
                                    act(lambda: A.activation(out=pt_[:], in_=pS[:], func=AF.Exp, scale=0.125), [pSk], [ptk])
                                    vap, vkey = klist[i][3], klist[i][4]
                                    for hh in range(4):
                                        mm(po[:, hh * 65:(hh + 1) * 65], pt_[:, hh * 128:(hh + 1) * 128], vap, False, (i == n - 1 and hh == 3), [ptk, vkey], [pok])
                                pov = po[:, 0:260].rearrange("p (h e) -> p h e", h=4)
                                dve(lambda: V.tensor_scalar(out=coef[:], in0=pov[:, :, 64], scalar1=1e-30, scalar2=None, op0=ALU.max), [pok], ["coef"])
                                dve(lambda: V.reciprocal(out=coef[:], in_=coef[:]), ["coef"], ["coef"])
                                dve(lambda: V.tensor_tensor(out=coef[:], in0=coef[:], in1=gn[:, tl, 12 * g + gate_idx:12 * g + 12:3], op=ALU.mult), ["coef", "gn"], ["coef"])
                                oslice = oacc[:, g * 256:(g + 1) * 256].rearrange("p (h d) -> p h d", h=4)
                                cb_ = coef[:].unsqueeze(2).broadcast_to([128, 4, 64])
                                if first:
                                    dve(lambda: V.tensor_tensor(out=oslice, in0=pov[:, :, 0:64], in1=cb_, op=ALU.mult), [pok, "coef", "oacc"], ["oacc"])
                                else:
                                    dve(lambda: V.tensor_tensor(out=otmp[:].rearrange("p (h d) -> p h d", h=4), in0=pov[:, :, 0:64], in1=cb_, op=ALU.mult),
                                        [pok, "coef"], ["otmp"])
                                    dve(lambda: V.tensor_tensor(out=oslice, in0=oslice, in1=otmp[:].rearrange("p (h d) -> p h d", h=4), op=ALU.add),
                                        ["oacc", "otmp"], ["oacc"])

                            kl = []
                            for cc in range(4):
                                extra = []
                                if cc >= 2:
                                    extra = [(identb[:], cbB[c2][:, cc - 2, :], ["identb", f"cbB{c2}"])]
                                kl.append((CkT[r0:r0 + 64, cc * 128:(cc + 1) * 128], "CkT", extra, CvA[:, cc, g, :], "CvA"))
                            branch(kl, 0, True)
                            kl = []
                            for kt in range(qt + 1):
                                extra = []
                                if kt == qt:
                                    extra.append((identb[:], tri[:], ["identb", "tri"]))
                                qx, qxk = (QaA[g], f"QaA{g}") if kt < 32 else (QaB[g], f"QaB{g}")
                                kl.append((KA[g][:, kt * 128:(kt + 1) * 128], [f"KA{g}", f"KA{g}e", qxk], extra, VsA[:, kt, g, :], "VsA",
                                           qx[:].rearrange("p h q -> p (h q)"), qxk))
                            branch(kl, 1, False)
                            kl = []
                            for kt in range(qt - 4, qt + 1):
                                extra = []
                                if kt == qt:
                                    extra = [(identb[:], tri[:], ["identb", "tri"])]
                                elif kt == qt - 4:
                                    extra = [(identb[:], atri[:], ["identb", "atri"])]
                                kl.append((KwT[r0:r0 + 64, (kt - 44) * 128:(kt - 43) * 128], "KwT", extra, VwA[:, kt - 44, g, :], "VwA"))
                            branch(kl, 2, False)
                        pos_ = [nxt(PO, "o", "PO"), nxt(PO, "o", "PO")]
                        for po, pok in pos_:
                            mm(po[:, 0:258], zerob[:], VsA[:, 0:2, :, :].rearrange("p a b c -> p (a b c)")[:, 0:258], True, False, ["zerob", "VsA"], [pok])
                        for mc in range(2):
                            pS, pSk = nxt(PS_S, "s", "PS_S")
                            for h in range(4):
                                mm(pS[:, h * 128:(h + 1) * 128], KmT[:, h, mc * 128:(mc + 1) * 128], qmT[:, tl, h, :], True, True, ["KmT", "qmT"], [pSk])
                            pt_, ptk = nxt(PTb, "pt", "PTb")
                            act(lambda: A.activation(out=pt_[:], in_=pS[:], func=AF.Exp, scale=128.0 ** -0.5), [pSk], [ptk])
                            for h in range(4):
                                po, pok = pos_[h // 2]
                                mm(po[:, (h % 2) * 129:(h % 2 + 1) * 129], pt_[:, h * 128:(h + 1) * 128], VmA[:, mc, h, :], False, (mc == 1 and h % 2 == 1), [ptk, "VmA"], [pok])
                        for hp in range(2):
                            po, pok = pos_[hp]
                            pov = po[:, 0:258].rearrange("p (h e) -> p h e", h=2)
                            dve(lambda: V.reciprocal(out=coef[:, 0:2], in_=pov[:, :, 128]), [pok], ["coef"])
                            dve(lambda: V.tensor_tensor(out=omacc[:, hp * 256:(hp + 1) * 256].rearrange("p (h d) -> p h d", h=2), in0=pov[:, :, 0:128],
                                                        in1=coef[:, 0:2].unsqueeze(2).broadcast_to([128, 2, 128]), op=ALU.mult), [pok, "coef"], ["omacc"])
                        dve(lambda: V.tensor_copy(out=oaccb[:], in_=oacc[:]), ["oacc"], ["oaccb"])
                        dve(lambda: V.tensor_copy(out=omb[:], in_=omacc[:]), ["omacc"], ["omb"])
                        for src_, sk_, dstT, dk_ in ((oaccb, "oaccb", oT, "oT"), (omb, "omb", omT, "omT")):
                            for kc in range(4):
                                tr(PT4[:, kc * 128:(kc + 1) * 128], src_[:, kc * 128:(kc + 1) * 128], identb[:], [sk_, "identb"], ["PT4"], signal=(kc == 3))
                            act(lambda: A.copy(out=dstT[:, :, tl * 128:(tl + 1) * 128], in_=PT4[:, 0:512].rearrange("p (a b) -> p a b", a=4)), ["PT4"], [dk_])

                    fw.barrier()
                if "p4o" in dbg and B == 0:
                    fw.dma("sp", ddbg("d_QT", [128, 2048], BF16)[:, :], QT[:].rearrange("p a b c -> p (a b c)"), reads=["QT"], writes=["d_QT"])
                    fw.dma("sp", ddbg("d_qmT", [128, 2048], BF16)[:, :], qmT[:].rearrange("p a b c -> p (a b c)"), reads=["qmT"], writes=["d_qmT"])
                    fw.dma("sp", ddbg("d_gn", [128, 96], F32)[:, :], gn[:].rearrange("p a b -> p (a b)"), reads=["gn"], writes=["d_gn"])
                    fw.dma("sp", ddbg("d_oT", [128, 4 * 512], BF16)[:, :], oT[:].rearrange("p a b -> p (a b)"), reads=["oT"], writes=["d_oT"])
                    fw.dma("sp", ddbg("d_omT", [128, 4 * 512], BF16)[:, :], omT[:].rearrange("p a b -> p (a b)"), reads=["omT"], writes=["d_omT"])

                with contextlib.ExitStack() as SC:
                    xq = [sb(SC, f"xq{i}", [128, 1024], F32) for i in range(2)]
                    h0t = sb(SC, "h0t", [128, 1024], F32)
                    sgt = [sb(SC, f"sgt{i}", [128, 512], F32) for i in range(2)]
                    mrg2 = [sb(SC, f"mrg{i}", [128, 512], F32) for i in range(2)]
                    ytm2 = [sb(SC, f"ytm{i}", [128, 512], F32) for i in range(2)]
                    vres = sb(SC, "vres", [128, 1024], F32)
                    wp = [sb(SC, f"wp{i}", [128, 8, 128], BF16) for i in range(6)]
                    for ft in range(8):
                        fs = slice(ft * 128, (ft + 1) * 128)
                        mrg = mrg2[ft % 2]
                        ytm = ytm2[ft % 2]
                        mk_ = f"mrg{ft % 2}"
                        yk_ = f"ytm{ft % 2}"

                        def proj(wsrc, nk, rhsT, rk):
                            w_, wk_ = wpiece(wsrc)
                            pf, pfk = nxt(PF, "f", "PF4_")
                            for kc in range(nk):
                                mm(pf[:], w_[:, kc, :], (rhsT[:, kc, B * 512:(B + 1) * 512] if rhsT is gyT else rhsT[:, kc, :]), kc == 0, kc == nk - 1, [wk_, rk], [pfk])
                            return pf, pfk

                        def gate(i):
                            pf, pfk = proj(wv[:, :, C_GM + i * 1024 + ft * 128:C_GM + i * 1024 + (ft + 1) * 128], 8, hTq, "hTq")
                            sg, sgk = nxt(sgt, "wp", "sgt")
                            act(lambda: A.activation(out=sg[:], in_=pf[:], func=AF.Sigmoid), [pfk], [sgk])
                            return sg, sgk

                        py, pyk = proj(wno_v[:, :, fs], 4, oT, "oT")
                        sg, sgk = gate(0)
                        dve(lambda: V.tensor_tensor(out=mrg[:], in0=py[:], in1=sg[:], op=ALU.mult), [pyk, sgk], [mk_])
                        pga, pgak = proj(wgl_v[:, :, fs], 4, gyT, "gyT")
                        pgb, pgbk = proj(wgl_v[:, :, 1024 + ft * 128:1024 + (ft + 1) * 128], 4, gyT, "gyT")
                        sg2, sg2k = nxt(sgt, "wp", "sgt")
                        act(lambda: A.activation(out=sg2[:], in_=pgb[:], func=AF.Sigmoid), [pgbk], [sg2k])
                        dve(lambda: V.tensor_tensor(out=ytm[:], in0=pga[:], in1=sg2[:], op=ALU.mult), [pgak, sg2k], [yk_])
                        sg, sgk = gate(1)
                        dve(lambda: V.tensor_tensor(out=ytm[:], in0=ytm[:], in1=sg[:], op=ALU.mult), [yk_, sgk], [yk_])
                        dve(lambda: V.tensor_tensor(out=mrg[:], in0=mrg[:], in1=ytm[:], op=ALU.add), [yk_, mk_], [mk_])
                        py, pyk = proj(wmo_v[:, :, fs], 4, omT, "omT")
                        sg, sgk = gate(2)
                        dve(lambda: V.tensor_tensor(out=ytm[:], in0=py[:], in1=sg[:], op=ALU.mult), [pyk, sgk], [yk_])
                        dve(lambda: V.tensor_tensor(out=mrgT[:, ft, :], in0=mrg[:], in1=ytm[:], op=ALU.add), [yk_, mk_], ["mrgT"])

                    if "p4o" in dbg and B == 0:
                        fw.dma("sp", ddbg("d_mrgT", [128, 4096], BF16)[:, :], mrgT[:].rearrange("p a b -> p (a b)"), reads=["mrgT"], writes=["d_mrgT"])
                    for tl in range(4):
                        j = B * 4 + tl
                        Tt = OWN0 + j
                        xt, xk = nxt(xq, "pt", "xq")
                        fw.dma("sp", xt[:], xr[Tt * 128:(Tt + 1) * 128, :], writes=[xk])
                        ln_tok(xt, xk, h0t, "h0t", grow, brow, "grow", "brow")
                        for hf in range(2):
                            pf, pfk = nxt(PF, "f", "PF4_")
                            for ft in range(8):
                                mm(pf[:], mrgT[:, ft, tl * 128:(tl + 1) * 128], Wo[:, ft, hf * 512:(hf + 1) * 512], ft == 0, ft == 7, ["mrgT", "Wo"], [pfk])
                            dve(lambda: V.scalar_tensor_tensor(out=vres[:, hf * 512:(hf + 1) * 512], in0=h0t[:, hf * 512:(hf + 1) * 512], scalar=ALPHA,
                                                               in1=pf[:], op0=ALU.mult, op1=ALU.add), ["h0t", pfk], ["vres"])
                        ln_tok(vres, "vres", h0t, "h0t", g1row, b1row, "g1row", "b1row")
                        fw.dma("sp", h1scr[j * 128:(j + 1) * 128, :], h0t[:], reads=["h0t"], writes=["h1scr"])
                    fw.barrier()
            if "p4" in dbg:
                fw.dma("sp", ddbg("d_h1", [2048, 1024], F32)[:, :], h1scr[:, :], reads=["h1scr"], writes=["d_h1"])
            fw.barrier()
        if upto <= 4:
            fw.finish("sp")
            print("instr", fw.ninstr, "waits", fw.nwait)
            return nc, dbg_out
        SKV.close()

        with contextlib.ExitStack() as S5:
            acc = sb(S5, "acc", [128, 16, 1024], F32)
            h1T = sb(S5, "h1T", [128, 8, 2048], BF16)
            gates = sb(S5, "gates", [128, 16, 32], F32)
            bguT = sb(S5, "bguT", [128, 512], F32)
            g2row = sb(S5, "g2row", [128, 1024], F32)
            b2row = sb(S5, "b2row", [128, 1024], F32)
            st5 = sb(S5, "st5", [128, 12], F32)
            mv5 = sb(S5, "mv5", [128, 2], F32)
            rs5 = sb(S5, "rs5", [128, 1], F32)
            fw.dma("sp", g2row[:], ln2_d[0:1, :].partition_broadcast(128), writes=["g2row"])
            fw.dma("sp", b2row[:], ln2_d[1:2, :].partition_broadcast(128), writes=["b2row"])
            with contextlib.ExitStack() as S5i:
                Wr = sb(S5i, "Wr", [128, 8, 32], F32)
                brr = sb(S5i, "brr", [128, 32], F32)
                bdn = sb(S5i, "bdn", [32, 1024], F32)
                bgn = sb(S5i, "bgn", [128, 4, 128], F32)
                fw.dma("sp", Wr[:], w_router.rearrange("(dc p) e -> p dc e", p=128), writes=["Wr"])
                fw.dma("sp", brr[:], b_router[0:1, :].partition_broadcast(128), writes=["brr"])
                fw.dma("sp", bdn[:], b_down[:, :], writes=["bdn"])
                fw.dma("sp", bgn[:], b_gate_up.rearrange("(a p) c -> p a c", p=128), writes=["bgn"])
                h1t = [sb(S5i, f"h1t{i}", [128, 1024], F32) for i in range(2)]
                h1b = sb(S5i, "h1b", [128, 1024], BF16)
                h1Tf = sb(S5i, "h1Tf", [128, 8, 128], F32)
                lg = sb(S5i, "lg", [128, 32], F32)
                ex = sb(S5i, "ex", [128, 32], F32)
                msk = sb(S5i, "msk", [128, 32], F32)
                m85 = sb(S5i, "m85", [128, 8], F32)
                nmx = sb(S5i, "nmx", [128, 1], F32)
                ssm = sb(S5i, "ssm", [128, 1], F32)
                gT = sb(S5i, "gT", [32, 128], F32)
                PT5 = ps(S5i, "PT5", [128, 1024], BF16)
                PTf = [ps(S5i, f"PTf{i}", [128, 512], F32) for i in range(2)]
                PLg = ps(S5i, "PLg", [128, 128], F32)
                PBd = [ps(S5i, f"PBd{i}", [128, 512], F32) for i in range(2)]
                for a in range(4):
                    tr(PTf[a % 2][:, 0:128], bgn[:, a, :], identf[:], ["bgn", "identf"], [f"PTf{a % 2}"])
                    act(lambda: A.copy(out=bguT[:, a * 128:(a + 1) * 128], in_=PTf[a % 2][:, 0:128]), [f"PTf{a % 2}"], ["bguT"])
                for j in range(16):
                    ht = h1t[j % 2]
                    hk_ = f"h1t{j % 2}"
                    fw.dma("sp", ht[:], h1scr[j * 128:(j + 1) * 128, :], reads=["h1scr"], writes=[hk_])
                    act(lambda: A.mul(out=acc[:, j, :], in_=ht[:], mul=ALPHA), [hk_], [f"acc{j}"])
                    dve(lambda: V.tensor_copy(out=h1b[:], in_=ht[:]), [hk_], ["h1b"])
                    for dc in range(8):
                        tr(PT5[:, dc * 128:(dc + 1) * 128], h1b[:, dc * 128:(dc + 1) * 128], identb[:], ["h1b", "identb"], ["PT5"], signal=(dc == 7))
                    act(lambda: A.copy(out=h1T[:, :, j * 128:(j + 1) * 128], in_=PT5[:].rearrange("p (a b) -> p a b", a=8)), ["PT5"], ["h1T"])
                    for hf in range(2):
                        for d4 in range(4):
                            dc = hf * 4 + d4
                            tr(PTf[hf][:, d4 * 128:(d4 + 1) * 128], ht[:, dc * 128:(dc + 1) * 128], identf[:], [hk_, "identf"], [f"PTf{hf}"], signal=(d4 == 3))
                        dve(lambda: V.tensor_copy(out=h1Tf[:, hf * 4:(hf + 1) * 4, :], in_=PTf[hf][:].rearrange("p (a b) -> p a b", a=4)), [f"PTf{hf}"], ["h1Tf"])
                    for dc in range(8):
                        mm(PLg[:, 0:32], h1Tf[:, dc, :], Wr[:, dc, :], dc == 0, dc == 7, ["h1Tf", "Wr"], ["PLg"])
                    dve(lambda: V.tensor_tensor(out=lg[:], in0=PLg[:, 0:32], in1=brr[:], op=ALU.add), ["PLg", "brr"], ["lg"])
                    dve(lambda: V.max(out=m85[:], in_=lg[:]), ["lg"], ["m85"])
                    dve(lambda: V.tensor_scalar(out=nmx[:], in0=m85[:, 0:1], scalar1=-1.0, scalar2=None, op0=ALU.mult), ["m85"], ["nmx"])
                    act(lambda: A.activation(out=ex[:], in_=lg[:], func=AF.Exp, bias=nmx[:, 0:1], scale=1.0), ["lg", "nmx"], ["ex"])
                    dve(lambda: V.tensor_scalar(out=msk[:], in0=lg[:], scalar1=m85[:, 3:4], scalar2=None, op0=ALU.is_ge), ["lg", "m85"], ["msk"])
                    dve(lambda: V.tensor_tensor(out=ex[:], in0=ex[:], in1=msk[:], op=ALU.mult), ["ex", "msk"], ["ex"])
                    dve(lambda: V.reduce_sum(out=ssm[:], in_=ex[:], axis=AX.X), ["ex"], ["ssm"])
                    dve(lambda: V.reciprocal(out=ssm[:], in_=ssm[:]), ["ssm"], ["ssm"])
                    dve(lambda: V.tensor_scalar(out=gates[:, j, :], in0=ex[:], scalar1=ssm[:, 0:1], scalar2=None, op0=ALU.mult), ["ex", "ssm"], ["gates"])
                    tr(PLg[0:32, :], gates[:, j, :], identf[:], ["gates", "identf"], ["PLg"])
                    act(lambda: A.copy(out=gT[:], in_=PLg[0:32, :]), ["PLg"], ["gT"])
                    for hf in range(2):
                        mm(PBd[hf][:], gT[:], bdn[:, hf * 512:(hf + 1) * 512], True, True, ["gT", "bdn"], [f"PBd{hf}"])
                        dve(lambda: V.tensor_tensor(out=acc[:, j, hf * 512:(hf + 1) * 512], in0=PBd[hf][:], in1=acc[:, j, hf * 512:(hf + 1) * 512], op=ALU.add),
                            [f"PBd{hf}", f"acc{j}"], [f"acc{j}"])
                if "p5g" in dbg:
                    fw.dma("sp", ddbg("d_gates", [128, 512], F32)[:, :], gates[:].rearrange("p a b -> p (a b)"), reads=["gates"], writes=["d_gates"])
                fw.barrier()
            actT = sb(S5, "actT", [128, 8, 2048], BF16)
            Wgu = [sb(S5, f"Wgu{i}", [128, 8, 2, 256], BF16) for i in range(2)]
            Wd = [sb(S5, f"Wd{i}", [128, 8, 512], BF16) for i in range(2)]
            gs = [sb(S5, f"gs{i}", [128, 512], F32) for i in range(3)]
            sg = [sb(S5, f"sg{i}", [128, 512], F32) for i in range(3)]
            ls = [sb(S5, f"ls{i}", [128, 512], F32) for i in range(3)]
            PG = [ps(S5, f"PG{i}", [128, 512], F32) for i in range(3)]
            PL = [ps(S5, f"PL{i}", [128, 512], F32) for i in range(3)]
            PD = [ps(S5, f"PD{i}", [128, 512], F32) for i in range(2)]
            ui = 0
            di = 0
            NE = 32 if "moe_e" not in dbg else 2
            NU = 3

            def load_wgu(e, pg):
                par = (e * 4 + pg) % 2
                wgu_v = w_gate_up[e].rearrange("(dc p) c -> p dc c", p=128)
                for gl_ in range(2):
                    c0 = gl_ * 1024 + pg * 256
                    for d0 in range(0, 8, 4):
                        fw.dma("pool", Wgu[par][:, d0:d0 + 4, gl_, :], wgu_v[:, d0:d0 + 4, c0:c0 + 256], writes=[f"Wgu{par}_{gl_}_{d0 // 4}"])

            def load_wd(e, h2):
                wd_v = w_down[e].rearrange("(fc p) c -> p fc c", p=128)
                for d0 in range(0, 8, 4):
                    fw.dma("pool", Wd[h2][:, d0:d0 + 4, :], wd_v[:, d0:d0 + 4, h2 * 512:(h2 + 1) * 512], writes=[f"Wd{h2}_{d0 // 4}"])

            load_wgu(0, 0)
            load_wd(0, 0)
            load_wd(0, 1)
            for e in range(NE):
                for pg in range(4):
                    par = (e * 4 + pg) % 2
                    wb = Wgu[par]
                    if pg < 3:
                        load_wgu(e, pg + 1)
                    elif e + 1 < NE:
                        load_wgu(e + 1, 0)
                    for pi in range(2):
                        i = pg * 2 + pi
                        bg_ = bguT[:, e * 16 + i:e * 16 + i + 1]
                        bl_ = bguT[:, e * 16 + 8 + i:e * 16 + 8 + i + 1]
                        for tb in range(4):
                            u2 = ui % NU
                            ui += 1
                            for dc in range(8):
                                mm(PG[u2][:], wb[:, dc, 0, pi * 128:(pi + 1) * 128], h1T[:, dc, tb * 512:(tb + 1) * 512], dc == 0, dc == 7,
                                   [f"Wgu{par}_0_{dc // 4}", "h1T"], [f"PG{u2}"])
                            for dc in range(8):
                                mm(PL[u2][:], wb[:, dc, 1, pi * 128:(pi + 1) * 128], h1T[:, dc, tb * 512:(tb + 1) * 512], dc == 0, dc == 7,
                                   [f"Wgu{par}_1_{dc // 4}", "h1T"], [f"PL{u2}"])
                            dve(lambda: V.tensor_scalar(out=gs[u2][:], in0=PG[u2][:], scalar1=bg_, scalar2=7.0, op0=ALU.add, op1=ALU.min), [f"PG{u2}", "bguT"], [f"gs{u2}"])
                            act(lambda: A.activation(out=sg[u2][:], in_=gs[u2][:], func=AF.Sigmoid, scale=1.702), [f"gs{u2}"], [f"sg{u2}"])
                            dve(lambda: V.tensor_scalar(out=ls[u2][:], in0=PL[u2][:], scalar1=bl_, scalar2=7.0, op0=ALU.add, op1=ALU.min), [f"PL{u2}", "bguT"], [f"ls{u2}"])
                            dve(lambda: V.tensor_scalar(out=ls[u2][:], in0=ls[u2][:], scalar1=-7.0, scalar2=1.0, op0=ALU.max, op1=ALU.add), [f"ls{u2}"], [f"ls{u2}"])
                            pool(lambda: G.tensor_tensor(out=gs[u2][:], in0=gs[u2][:], in1=sg[u2][:], op=ALU.mult), [f"gs{u2}", f"sg{u2}"], [f"gs{u2}"])
                            pool(lambda: G.tensor_tensor(out=actT[:, i, tb * 512:(tb + 1) * 512], in0=gs[u2][:], in1=ls[u2][:], op=ALU.mult),
                                 [f"gs{u2}", f"ls{u2}"], [f"actT{tb}"])
                for h2 in range(2):
                    wdb = Wd[h2]
                    for j in range(16):
                        d2 = di % 2
                        di += 1
                        for fc in range(8):
                            mm(PD[d2][:], actT[:, fc, j * 128:(j + 1) * 128], wdb[:, fc, :], fc == 0, fc == 7, [f"actT{j // 4}", f"Wd{h2}_{fc // 4}"], [f"PD{d2}"])
                        dve(lambda: V.scalar_tensor_tensor(out=acc[:, j, h2 * 512:(h2 + 1) * 512], in0=PD[d2][:], scalar=gates[:, j, e:e + 1],
                                                           in1=acc[:, j, h2 * 512:(h2 + 1) * 512], op0=ALU.mult, op1=ALU.add),
                            [f"PD{d2}", "gates", f"acc{j}"], [f"acc{j}"])
                    if e + 1 < NE:
                        load_wd(e + 1, h2)
            ot = [sb(S5, f"ot{i}", [128, 1024], F32) for i in range(2)]
            for j in range(16):
                o_ = ot[j % 2]
                ok_ = f"ot{j % 2}"
                xt = acc[:, j, :]
                xk = f"acc{j}"
                dve(lambda: V.bn_stats(out=st5[:, 0:6], in_=xt[:, 0:512]), [xk], ["st5"])
                dve(lambda: V.bn_stats(out=st5[:, 6:12], in_=xt[:, 512:1024]), [xk], ["st5"])
                dve(lambda: V.bn_aggr(out=mv5[:, 0:2], in_=st5[:, 0:12]), ["st5"], ["mv5"])
                act(lambda: A.activation(out=rs5[:], in_=mv5[:, 1:2], func=AF.Sqrt, bias=LN_EPS, scale=1.0), ["mv5"], ["rs5"])
                dve(lambda: V.reciprocal(out=rs5[:], in_=rs5[:]), ["rs5"], ["rs5"])
                dve(lambda: V.tensor_scalar(out=o_[:], in0=xt, scalar1=mv5[:, 0:1], scalar2=rs5[:, 0:1], op0=ALU.subtract, op1=ALU.mult),
                    [xk, "mv5", "rs5"], [ok_])
                pool(lambda: G.tensor_tensor(out=o_[:], in0=o_[:], in1=g2row[:], op=ALU.mult), [ok_, "g2row"], [ok_])
                pool(lambda: G.tensor_tensor(out=o_[:], in0=o_[:], in1=b2row[:], op=ALU.add), [ok_, "b2row"], [ok_])
                fw.dma("sp", out_d[j * 128:(j + 1) * 128, :], o_[:], reads=[ok_], writes=[f"out{j}"])
            fw.finish("sp")
            fw.barrier()
        print("instr", fw.ninstr, "waits", fw.nwait)
        return nc, dbg_out

        fw.finish("sp")
    return nc, dbg_out


def host_prep(inputs):
    x = np.asarray(inputs["x"], np.float32)
    pos = np.asarray(inputs["positions"], np.int32)
    maps = []
    inv = (np.float32(500000.0) ** (-(np.arange(0, 16, 2, dtype=np.float32)) / np.float32(16))).astype(np.float32)
    invf = np.zeros((128, 1), np.float32)
    for base in (0, 64):
        invf[base:base + 8, 0] = inv
        invf[base + 8:base + 16, 0] = inv
    identf = np.eye(128, dtype=np.float32)
    ln_emb = np.stack([inputs["ln_emb_g"], inputs["ln_emb_b"]]).astype(np.float32)
    w_in = np.ascontiguousarray(np.asarray(inputs["w_in"], np.float32)[0])
    g0 = lambda n: np.ascontiguousarray(np.asarray(inputs[n], np.float32)[0])
    sgn = np.ones((128, 1), np.float32); sgn[64:] = -1.0
    psw = np.zeros((128, 128), np.float32)
    psw[np.arange(128), (np.arange(128) + 64) % 128] = 1.0
    shared = {
        "w_kcmp1": g0("w_kcmp1"), "w_kcmp2": g0("w_kcmp2"), "w_vcmp1": g0("w_vcmp1"), "w_vcmp2": g0("w_vcmp2"),
        "pe_k": g0("pe_k_cmp"), "pe_v": g0("pe_v_cmp"),
        "s5_a_re": g0("s5_a_re"), "s5_a_im": g0("s5_a_im"), "s5_log_dt": np.asarray(inputs["s5_log_dt"], np.float32).reshape(1, 32),
        "s5_b_re": g0("s5_b_re"), "s5_b_im": g0("s5_b_im"), "s5_c_re": g0("s5_c_re"), "s5_c_im": g0("s5_c_im"), "s5_d": g0("s5_d"),
        "sgn": sgn, "psw": psw,
        "w_mem_kv": g0("w_mem_kv"), "w_nsa_out": g0("w_nsa_out"), "w_mem_out": g0("w_mem_out"), "w_s5_glu": g0("w_s5_glu"),
        "w_o": g0("w_o"), "ln1": np.stack([g0("ln1_g"), g0("ln1_b")]),
        "ln2": np.stack([g0("ln2_g"), g0("ln2_b")]), "w_router": g0("w_router"), "b_router": g0("b_router").reshape(1, 32),
        "w_gate_up": g0("w_gate_up"), "b_gate_up": g0("b_gate_up").reshape(512, 128), "w_down": g0("w_down"), "b_down": g0("b_down"),
    }
    kk = np.arange(8192)
    shared["epat"] = (((kk[None, :] // 64) % 64) == np.arange(64)[:, None]).astype(np.float32)
    ii = np.arange(128)
    tri = np.where(ii[:, None] <= ii[None, :], 0.0, NEG).astype(np.float32)
    atri = np.where(ii[:, None] > ii[None, :], 0.0, NEG).astype(np.float32)
    shared["tri"] = np.tile(tri, (1, 4))
    shared["atri"] = np.tile(atri, (1, 4))
    for k in range(8):
        b, r = k // 4, k % 4
        pad = 2048 * (3 - r)
        xr = np.zeros((8192, 1024), np.float32)
        xr[pad:] = x[b, :8192 - pad]
        posr = np.zeros((1, 8192), np.int32)
        posr[0, pad:] = pos[b, :8192 - pad]
        valid = (np.arange(8192) >= pad).astype(np.float32)
        cidx = np.arange(512)
        vc = ((cidx <= 510) & (16 * cidx >= pad)).astype(np.float32)
        rho = 6144 + np.arange(2048)
        cend = 16 * cidx + 31
        okc = (vc[None, :] > 0) & (cend[None, :] <= rho[:, None])
        cba = np.where(okc, 0.0, NEG).astype(np.float32).reshape(16, 128, 512)
        cbb = np.where(okc, 0.0, NEG).astype(np.float32).reshape(16, 128, 512)[:, :, 256:512]
        cbb = cbb.reshape(16, 128, 2, 128).transpose(0, 2, 3, 1)
        cbb = np.ascontiguousarray(np.tile(cbb, (1, 1, 1, 4)))
        blk = np.arange(128)
        tb = rho // 64
        blk0 = pad // 64
        forced = (blk[None, :] == tb[:, None]) | (blk[None, :] == tb[:, None] - 1) | (blk[None, :] == blk0)
        invalid = (blk[None, :] > tb[:, None]) | (blk[None, :] < blk0)
        addb = (forced * 1e4 + invalid * (-1e9)).astype(np.float32).reshape(16, 128, 128)
        futb = np.where(invalid, NEG, 0.0).astype(np.float32).reshape(16, 128, 128)
        m = {
            "cba": cba, "cbb": cbb, "addb": addb, "futb": futb, "mem": np.ascontiguousarray(np.asarray(inputs["mem"], np.float32)[b]),
            "vcc": np.ascontiguousarray(vc.reshape(4, 128).T),
            "xr": xr, "posr": posr, "vrow": valid[None, :].copy(),
            "vcol": np.ascontiguousarray(valid.reshape(64, 128).T),
            "ln_emb": ln_emb, "w_in": w_in, "invf": invf, "identf": identf,
        }
        m.update(shared)
        maps.append(m)
    return maps


def kernel(**inputs):
    nc, _ = build_program()
    maps = host_prep(inputs)
    res = run_bass_kernel_spmd(nc, maps, core_ids=list(range(8)))
    out = np.zeros((2, 8192, 1024), np.float32)
    for k in range(8):
        b, r = k // 4, k % 4
        out[b, 2048 * r:2048 * (r + 1)] = res.results[k]["out"]
    return out
```

```python
import contextlib
import math
import numpy as np
import ml_dtypes
import concourse.bass as bass
import concourse.mybir as mybir
from concourse.bass_utils import run_bass_kernel_spmd

F32 = mybir.dt.float32
BF16 = mybir.dt.bfloat16
I32 = mybir.dt.int32
ALU = mybir.AluOpType
AF = mybir.ActivationFunctionType
AX = mybir.AxisListType

EPOCH = 20000
NDMA_SLOTS = 6
NEG = -30000.0
LN_EPS = 1e-5
ALPHA = 2.0 ** 0.25
TWO_PI = 2.0 * math.pi
CW1 = 6.28125
CW2 = TWO_PI - 6.28125
PI_LO = 3.1415925


class _Eng:
    def __init__(self, fw, name, handle):
        self.fw = fw
        self.name = name
        self.h = handle
        self.sems = []
        self.epoch = -1
        self.count = 0
        self.pending = False
        self.seen = {}
        self.dma_slots = None
        self.dma_next = 0
        self.new_epoch()

    def new_epoch(self):
        self.epoch += 1
        self.count = 0
        s = self.fw.new_sem(f"{self.name}_e{self.epoch}")
        self.sems.append(s)
        self.fw.semobjs[(self.name, self.epoch)] = s

    def cur_key(self):
        return (self.name, self.epoch)


class FW:
    def __init__(self, nc, stack):
        self.nc = nc
        self.stack = stack
        self.semobjs = {}
        self.eng = {}
        for name, h in (("pe", nc.tensor), ("dve", nc.vector), ("act", nc.scalar),
                        ("pool", nc.gpsimd), ("sp", nc.sync)):
            self.eng[name] = _Eng(self, name, h)
        self.last_w = {}
        self.readers = {}
        self.ninstr = 0
        self.nwait = 0

    def new_sem(self, name):
        return self.stack.enter_context(self.nc.semaphore(name))

    def _wait(self, e, tok):
        semkey, val = tok
        if e.seen.get(semkey, 0) >= val:
            return
        if not semkey[0].endswith("_dma"):
            for (n2, ep2), v2 in e.seen.items():
                if n2 == semkey[0] and ep2 > semkey[1]:
                    return
        e.h.wait_ge(self.semobjs[semkey], val)
        self.nwait += 1
        e.seen[semkey] = val

    def _deps(self, reads, writes):
        deps = []
        for r in reads:
            t = self.last_w.get(r)
            if t is not None:
                deps.append(t)
        for w in writes:
            t = self.last_w.get(w)
            if t is not None:
                deps.append(t)
            deps.extend(self.readers.get(w, ()))
        return deps

    def _record(self, tok, reads, writes):
        for r in reads:
            lst = self.readers.setdefault(r, [])
            lst.append(tok)
            if len(lst) > 64:
                best = {}
                for sk, v in lst:
                    if best.get(sk, 0) < v:
                        best[sk] = v
                self.readers[r] = list(best.items())
        for w in writes:
            self.last_w[w] = tok
            self.readers[w] = []

    def op(self, engname, fn, reads=(), writes=(), signal=True):
        e = self.eng[engname]
        if e.count >= EPOCH and not e.pending:
            e.new_epoch()
        semkey = e.cur_key()
        for tok in self._deps(reads, writes):
            if engname == "pe" and tok[0][0] == "pe":
                continue
            self._wait(e, tok)
        ins = fn()
        self.ninstr += 1
        if signal:
            ins.then_inc(e.sems[e.epoch], 1)
            e.count += 1
            e.pending = False
            tok = (semkey, e.count)
        else:
            e.pending = True
            tok = (semkey, e.count + 1)
        self._record(tok, reads, writes)
        return ins

    def dma(self, qname, out, in_, reads=(), writes=(), **kw):
        e = self.eng[qname]
        if e.dma_slots is None:
            e.dma_slots = []
            for i in range(NDMA_SLOTS):
                key = (qname + "_dma", i)
                self.semobjs[key] = self.new_sem(f"{qname}_d{i}")
                e.dma_slots.append([key, 0])
        slot = e.dma_slots[e.dma_next % NDMA_SLOTS]
        e.dma_next += 1
        key, uses = slot
        if uses > 0:
            self._wait(e, (key, 16 * uses))
        for tok in self._deps(reads, writes):
            self._wait(e, tok)
        ins = e.h.dma_start(out=out, in_=in_, **kw)
        ins.then_inc(self.semobjs[key], 16)
        self.ninstr += 1
        slot[1] = uses + 1
        tok = (key, 16 * (uses + 1))
        self._record(tok, reads, writes)
        return tok

    def barrier(self):
        toks = []
        for e in self.eng.values():
            assert not e.pending, e.name
            if e.count > 0:
                toks.append((e.cur_key(), e.count))
            if e.dma_slots:
                for key, uses in e.dma_slots:
                    if uses > 0:
                        toks.append((key, 16 * uses))
        for e in self.eng.values():
            for t in toks:
                self._wait(e, t)
        self.last_w = {}
        self.readers = {}

    def finish(self, qname="sp"):
        e = self.eng[qname]
        for k, tok in list(self.last_w.items()):
            self._wait(e, tok)


C_Q, C_KC, C_VC, C_KS, C_VS, C_KW, C_VW, C_GN, C_U, C_QM, C_GM = 0, 512, 640, 768, 896, 1024, 1152, 1280, 1304, 1816, 2328
NT = 64
OWN0 = 48


def build_program(upto=99, dbg=()):
    nc = bass.Bass("TRN2", target_bir_lowering=False)
    V, A, G, T = nc.vector, nc.scalar, nc.gpsimd, nc.tensor

    def din(name, shape, dt=F32):
        return nc.dram_tensor(name, list(shape), dt, kind="ExternalInput").ap()

    def dscr(name, shape, dt):
        return nc.dram_tensor(name, list(shape), dt, kind="Internal").ap()

    dbg_out = {}

    def ddbg(name, shape, dt):
        dbg_out[name] = nc.dram_tensor(name, list(shape), dt, kind="ExternalOutput").ap()
        return dbg_out[name]

    xr = din("xr", [8192, 1024])
    posr = din("posr", [1, 8192], I32)
    vrow_d = din("vrow", [1, 8192])
    vcol_d = din("vcol", [128, 64])
    ln_emb = din("ln_emb", [2, 1024])
    w_in = din("w_in", [1024, 5400])
    invf_d = din("invf", [128, 1])
    identf_d = din("identf", [128, 128])
    w_kcmp1 = din("w_kcmp1", [32, 64, 128]); w_kcmp2 = din("w_kcmp2", [128, 64])
    w_vcmp1 = din("w_vcmp1", [32, 64, 128]); w_vcmp2 = din("w_vcmp2", [128, 64])
    pe_k = din("pe_k", [32, 64]); pe_v = din("pe_v", [32, 64])
    vcc_d = din("vcc", [128, 4])
    s5_a_re = din("s5_a_re", [32, 64]); s5_a_im = din("s5_a_im", [32, 64]); s5_log_dt = din("s5_log_dt", [1, 32])
    s5_b_re = din("s5_b_re", [32, 64, 16]); s5_b_im = din("s5_b_im", [32, 64, 16])
    s5_c_re = din("s5_c_re", [32, 16, 64]); s5_c_im = din("s5_c_im", [32, 16, 64]); s5_d = din("s5_d", [512])
    sgn_d = din("sgn", [128, 1]); psw_d = din("psw", [128, 128])
    mem_d = din("mem", [256, 1024]); w_mem_kv = din("w_mem_kv", [1024, 1024])
    w_nsa_out = din("w_nsa_out", [512, 1024]); w_mem_out = din("w_mem_out", [512, 1024])
    w_s5_glu = din("w_s5_glu", [512, 2048]); w_o = din("w_o", [1024, 1024]); ln1_d = din("ln1", [2, 1024])
    epat_d = din("epat", [64, 8192]); tri_d = din("tri", [128, 512]); atri_d = din("atri", [128, 512])
    cba_d = din("cba", [16, 128, 512]); cbb_d = din("cbb", [16, 2, 128, 512])
    addb_d = din("addb", [16, 128, 128]); futb_d = din("futb", [16, 128, 128])
    h1scr = dscr("h1scr", [2048, 1024], F32)
    wscr = dscr("wscr", [60, 128, 1024], BF16)
    wqscr = dscr("wqscr", [3, 128, 4096], BF16)
    wgnscr = dscr("wgnscr", [128, 192], BF16)
    ln2_d = din("ln2", [2, 1024]); w_router = din("w_router", [1024, 32]); b_router = din("b_router", [1, 32])
    w_gate_up = din("w_gate_up", [32, 1024, 2048]); b_gate_up = din("b_gate_up", [512, 128])
    w_down = din("w_down", [32, 1024, 1024]); b_down = din("b_down", [32, 1024])
    out_d = nc.dram_tensor("out", [2048, 1024], F32, kind="ExternalOutput").ap()

    uscr = dscr("uscr", [4, 128, 8192], BF16)

    with contextlib.ExitStack() as S0:
        fw = FW(nc, S0)

        _names = {}

        def _uniq(n):
            c = _names.get(n, 0)
            _names[n] = c + 1
            return n if c == 0 else f"{n}__{c}"

        def sb(st, name, shape, dt):
            return st.enter_context(nc.sbuf_tensor(_uniq("s_" + name), list(shape), dt))

        def ps(st, name, shape, dt=F32):
            return st.enter_context(nc.psum_tensor(_uniq("p_" + name), list(shape), dt))

        def dve(fn, r, w):
            return fw.op("dve", fn, r, w)

        def act(fn, r, w):
            return fw.op("act", fn, r, w)

        def pool(fn, r, w):
            return fw.op("pool", fn, r, w)

        def mm(out, lhsT, rhs, start, stop, r, w):
            return fw.op("pe", lambda: T.matmul(out, lhsT=lhsT, rhs=rhs, start=start, stop=stop), r, w, signal=stop)

        def tr(out, in_, ident, r, w, signal=True):
            return fw.op("pe", lambda: T.transpose(out=out, in_=in_, identity=ident), r, w, signal=signal)

        identf = sb(S0, "identf", [128, 128], F32)
        identb = sb(S0, "identb", [128, 128], BF16)
        zerob = sb(S0, "zerob", [128, 128], BF16)
        invf = sb(S0, "invf", [128, 1], F32)
        vcol = sb(S0, "vcol", [128, 64], F32)
        gcol = sb(S0, "gcol", [128, 8], F32)
        bcol = sb(S0, "bcol", [128, 8], F32)
        fw.dma("sp", identf[:], identf_d[:, :], writes=["identf"])
        fw.dma("pool", identb[:], identf_d[:, :], writes=["identb"])
        fw.dma("sp", invf[:], invf_d[:, :], writes=["invf"])
        fw.op("pool", lambda: G.memset(zerob[:], 0.0), [], ["zerob"])
        fw.dma("sp", vcol[:], vcol_d[:, :], writes=["vcol"])
        fw.dma("sp", gcol[:], ln_emb[0, :].rearrange("(dc p) -> p dc", p=128), writes=["gcol"], allow_slow_non_contiguous=True)
        fw.dma("sp", bcol[:], ln_emb[1, :].rearrange("(dc p) -> p dc", p=128), writes=["bcol"], allow_slow_non_contiguous=True)

        SKV = S0.enter_context(contextlib.ExitStack())
        KA = [sb(SKV, f"KA{i}", [128, 8192], BF16) for i in range(2)]
        VsA = sb(SKV, "VsA", [128, NT, 2, 65], BF16)
        KwT = sb(SKV, "KwT", [128, 20 * 128], BF16)
        VwA = sb(SKV, "VwA", [128, 20, 2, 65], BF16)
        CkT = sb(SKV, "CkT", [128, 512], BF16)
        CvA = sb(SKV, "CvA", [128, 4, 2, 65], BF16)
        gyT = sb(SKV, "gyT", [128, 4, 2048], BF16)
        vcc = sb(SKV, "vcc", [128, 4], F32)
        fw.dma("sp", vcc[:], vcc_d[:, :], writes=["vcc"])
        for q4 in range(4):
            fw.dma("pool", KA[0][64:128, q4 * 2048:(q4 + 1) * 2048], epat_d[:, q4 * 2048:(q4 + 1) * 2048], writes=["KA0e"])
            fw.dma("pool", KA[1][0:64, q4 * 2048:(q4 + 1) * 2048], epat_d[:, q4 * 2048:(q4 + 1) * 2048], writes=["KA1e"])

        def layer_norm_T(st_keys, xt_ap, hT_dst, PT, xn, stt, mv, rs, tmpf, gexp, bexp, tag):
            kx = st_keys
            dve(lambda: V.bn_stats(out=stt[:, 0:6], in_=xt_ap[:, 0:512]), [kx], ["stt" + tag])
            dve(lambda: V.bn_stats(out=stt[:, 6:12], in_=xt_ap[:, 512:1024]), [kx], ["stt" + tag])
            dve(lambda: V.bn_aggr(out=mv[:, 0:2], in_=stt[:, 0:12]), ["stt" + tag], ["mv" + tag])
            act(lambda: A.activation(out=rs[:], in_=mv[:, 1:2], func=AF.Sqrt, bias=LN_EPS, scale=1.0), ["mv" + tag], ["rs" + tag])
            dve(lambda: V.reciprocal(out=rs[:], in_=rs[:]), ["rs" + tag], ["rs" + tag])
            dve(lambda: V.tensor_scalar(out=xn[:], in0=xt_ap, scalar1=mv[:, 0:1], scalar2=rs[:, 0:1],
                                        op0=ALU.subtract, op1=ALU.mult), [kx, "mv" + tag, "rs" + tag], ["xn" + tag])
            for dc in range(8):
                tr(PT[:, dc * 128:(dc + 1) * 128], xn[:, dc * 128:(dc + 1) * 128], identb[:],
                   ["xn" + tag, "identb"], ["PT" + tag], signal=(dc == 7))
            dve(lambda: V.tensor_tensor(out=tmpf[:], in0=PT[:], in1=gexp[:], op=ALU.mult), ["PT" + tag, "gexp"], ["tmpf"])
            return tmpf

        with contextlib.ExitStack() as S1:
            kcT = sb(S1, "kcT", [128, 8192], BF16)
            vcT = sb(S1, "vcT", [128, 8192], BF16)
            S1t = S1.enter_context(contextlib.ExitStack())
            Wk = sb(S1t, "Wk", [128, 8, 1280], BF16)
            Wsw = sb(S1t, "Wsw", [128, 8, 3, 128], BF16)
            wv = w_in.rearrange("(dc p) c -> p dc c", p=128)
            for i, (c0, n) in enumerate(((C_KC, 128), (C_KS, 128), (C_KW, 128), (C_VC, 128), (C_VS, 128), (C_VW, 128), (C_U, 512))):
                dst0 = i * 128
                for dc in range(0, 8, 4):
                    fw.dma("pool", Wk[:, dc:dc + 4, dst0:dst0 + n], wv[:, dc:dc + 4, c0:c0 + n], writes=["Wk"])
            pool(lambda: G.memset(Wsw[:], 0.0), [], ["Wsw"])
            for i in range(3):
                for g in range(2):
                    b0 = i * 128 + g * 64
                    dve(lambda: V.tensor_scalar(out=Wsw[:, :, i, g * 64:g * 64 + 8], in0=Wk[:, :, b0 + 8:b0 + 16], scalar1=-1.0,
                                                scalar2=None, op0=ALU.mult), ["Wk", "Wsw"], ["Wsw"])
                    dve(lambda: V.tensor_copy(out=Wsw[:, :, i, g * 64 + 8:g * 64 + 16], in_=Wk[:, :, b0:b0 + 8]), ["Wk", "Wsw"], ["Wsw"])
            gexp = sb(S1t, "gexp", [128, 1024], F32)
            bexp = sb(S1t, "bexp", [128, 1024], F32)
            pool(lambda: G.memset(gexp[:], 1.0), [], ["gexp"])
            pool(lambda: G.memset(bexp[:], 1.0), [], ["bexp"])
            for dc in range(8):
                dve(lambda: V.tensor_scalar(out=gexp[:, dc * 128:(dc + 1) * 128], in0=gexp[:, dc * 128:(dc + 1) * 128],
                                            scalar1=gcol[:, dc:dc + 1], scalar2=None, op0=ALU.mult), ["gexp", "gcol"], ["gexp"])
                dve(lambda: V.tensor_scalar(out=bexp[:, dc * 128:(dc + 1) * 128], in0=bexp[:, dc * 128:(dc + 1) * 128],
                                            scalar1=bcol[:, dc:dc + 1], scalar2=None, op0=ALU.mult), ["bexp", "bcol"], ["bexp"])
            xbuf = [sb(S1t, f"xb{i}", [128, 1024], F32) for i in range(2)]
            xn = [sb(S1t, f"xn{i}", [128, 1024], BF16) for i in range(2)]
            tmpf = [sb(S1t, f"tmpf{i}", [128, 1024], F32) for i in range(1)] * 2
            stt = [sb(S1t, f"stt{i}", [128, 12], F32) for i in range(2)]
            mv = [sb(S1t, f"mv{i}", [128, 2], F32) for i in range(2)]
            rs = [sb(S1t, f"rs{i}", [128, 1], F32) for i in range(2)]
            hTb = [sb(S1t, f"hTb{i}", [128, 8, 512], BF16) for i in range(2)]
            posi = sb(S1t, "posi", [128, 512], I32)
            posf = sb(S1t, "posf", [128, 512], F32)
            angs = sb(S1t, "angs", [128, 512], F32)
            ki = sb(S1t, "ki", [128, 512], I32)
            kf = sb(S1t, "kf", [128, 512], F32)
            r1 = sb(S1t, "r1", [128, 512], F32)
            cosT = [sb(S1t, f"cosT{i}", [128, 512], F32) for i in range(1)] * 2
            sinT = [sb(S1t, f"sinT{i}", [128, 512], F32) for i in range(1)] * 2
            vrow = [sb(S1t, f"vrow{i}", [128, 512], F32) for i in range(2)]
            t1 = [sb(S1t, f"t1_{i}", [128, 512], F32) for i in range(1)] * 2
            t2 = [sb(S1t, f"t2_{i}", [128, 512], F32) for i in range(1)] * 2
            ust = [sb(S1t, f"ust{i}", [128, 512], BF16) for i in range(2)]
            S1p = S1.enter_context(contextlib.ExitStack())
            PT = [ps(S1p, f"PT{i}", [128, 1024], BF16) for i in range(2)]
            PF = [ps(S1p, f"PF{i}", [128, 512], F32) for i in range(4)]
            PV = [ps(S1p, f"PV{i}", [128, 256], F32) for i in range(2)]
            pfi = [0]
            ropei = [0]

            def next_pf():
                i = pfi[0] % 4
                pfi[0] += 1
                return PF[i], f"PF{i}"

            for bi in range(16):
                p2 = bi % 2
                hT = hTb[p2]
                hk = f"hTb{p2}"
                fw.dma("sp", posi[:], posr[:, bi * 512:(bi + 1) * 512].partition_broadcast(128), writes=["posi"])
                fw.dma("sp", vrow[p2][:], vrow_d[:, bi * 512:(bi + 1) * 512].partition_broadcast(128), writes=[f"vrow{p2}"])
                dve(lambda: V.tensor_copy(out=posf[:], in_=posi[:]), ["posi"], ["posf"])
                for which, shift, dst, dk in (("s", 0.0, sinT[p2], "sinT"), ("c", math.pi / 2, cosT[p2], "cosT")):
                    dve(lambda: V.tensor_scalar(out=angs[:], in0=posf[:], scalar1=invf[:, 0:1], scalar2=shift, op0=ALU.mult, op1=ALU.add),
                        ["posf", "invf"], ["angs"])
                    dve(lambda: V.tensor_scalar(out=ki[:], in0=angs[:], scalar1=1.0 / TWO_PI, scalar2=None, op0=ALU.mult), ["angs"], ["ki"])
                    dve(lambda: V.tensor_copy(out=kf[:], in_=ki[:]), ["ki"], ["kf"])
                    dve(lambda: V.scalar_tensor_tensor(out=r1[:], in0=kf[:], scalar=-CW1, in1=angs[:], op0=ALU.mult, op1=ALU.add),
                        ["kf", "angs"], ["r1"])
                    dve(lambda: V.scalar_tensor_tensor(out=r1[:], in0=kf[:], scalar=-CW2, in1=r1[:], op0=ALU.mult, op1=ALU.add),
                        ["kf", "r1"], ["r1"])
                    dve(lambda: V.tensor_scalar(out=r1[:], in0=r1[:], scalar1=-PI_LO, scalar2=PI_LO, op0=ALU.max, op1=ALU.min), ["r1"], ["r1"])
                    act(lambda: A.activation(out=dst[:], in_=r1[:], func=AF.Sin), ["r1"], [dk])
                for tl in range(4):
                    Tt = bi * 4 + tl
                    q2 = Tt % 2
                    tag = str(q2)
                    fw.dma("sp", xbuf[q2][:], xr[Tt * 128:(Tt + 1) * 128, :], writes=[f"xb{q2}"])
                    tf = layer_norm_T(f"xb{q2}", xbuf[q2][:], None, PT[q2], xn[q2], stt[q2], mv[q2], rs[q2], tmpf[q2], gexp, bexp, tag)
                    pool(lambda: G.tensor_tensor(out=hT[:, :, tl * 128:(tl + 1) * 128], in0=tf[:].rearrange("p (a b) -> p a b", a=8),
                                                 in1=bexp[:].rearrange("p (a b) -> p a b", a=8), op=ALU.add),
                         ["tmpf", "bexp"], [hk])
                for i, dstT, dkey, col0 in ((0, kcT, "kcT", bi * 512), (1, None, "KA", bi * 512), (2, KwT, "KwT", (bi - 11) * 512)):
                    if i == 2 and bi < 11:
                        continue
                    pa, pak = next_pf()
                    for dc in range(8):
                        mm(pa[:], Wk[:, dc, i * 128:(i + 1) * 128], hT[:, dc, :], dc == 0, dc == 7, ["Wk", hk], [pak])
                    pb, pbk = next_pf()
                    for dc in range(8):
                        mm(pb[:], Wsw[:, dc, i, :], hT[:, dc, :], dc == 0, dc == 7, ["Wsw", hk], [pbk])
                    ri = ropei[0] % 2
                    ropei[0] += 1
                    dve(lambda: V.tensor_tensor(out=t1[ri][:], in0=pa[:], in1=cosT[p2][:], op=ALU.mult), [pak, "cosT"], ["t1_"])
                    dve(lambda: V.tensor_tensor(out=t2[ri][:], in0=pb[:], in1=sinT[p2][:], op=ALU.mult), [pbk, "sinT"], ["t2_"])
                    if dstT is None:
                        pool(lambda: G.tensor_tensor(out=KA[0][0:64, col0:col0 + 512], in0=t1[ri][0:64, :], in1=t2[ri][0:64, :], op=ALU.add),
                             ["t1_", "t2_"], ["KA0"])
                        pool(lambda: G.tensor_tensor(out=KA[1][64:128, col0:col0 + 512], in0=t1[ri][64:128, :], in1=t2[ri][64:128, :], op=ALU.add),
                             ["t1_", "t2_"], ["KA1"])
                    else:
                        pool(lambda: G.tensor_tensor(out=dstT[:, col0:col0 + 512], in0=t1[ri][:], in1=t2[ri][:], op=ALU.add),
                             ["t1_", "t2_"], [dkey])
                pa, pak = next_pf()
                for dc in range(8):
                    mm(pa[:], Wk[:, dc, 384:512], hT[:, dc, :], dc == 0, dc == 7, ["Wk", hk], [pak])
                act(lambda: A.copy(out=vcT[:, bi * 512:(bi + 1) * 512], in_=pa[:]), [pak], ["vcT"])
                for ct in range(4):
                    pa, pak = next_pf()
                    for dc in range(8):
                        mm(pa[:], Wk[:, dc, 768 + ct * 128:768 + (ct + 1) * 128], hT[:, dc, :], dc == 0, dc == 7, ["Wk", hk], [pak])
                    u2 = ct % 2
                    dve(lambda: V.tensor_tensor(out=ust[u2][:], in0=pa[:], in1=vrow[p2][:], op=ALU.mult), [pak, f"vrow{p2}"], [f"ust{u2}"])
                    fw.dma("sp", uscr[ct, :, bi * 512:(bi + 1) * 512], ust[u2][:], reads=[f"ust{u2}"], writes=["uscr"])
                for tl in range(4):
                    Tt = bi * 4 + tl
                    pv = PV[tl % 2]
                    pvk = f"PV{tl % 2}"
                    for dc in range(8):
                        mm(pv[:], hT[:, dc, tl * 128:(tl + 1) * 128], Wk[:, dc, 512:768], dc == 0, dc == 7, ["Wk", hk], [pvk])
                    act(lambda: A.activation(out=VsA[:, Tt, :, 0:64], in_=pv[:, 0:128].rearrange("p (g d) -> p g d", g=2),
                                             func=AF.Copy, scale=vcol[:, Tt:Tt + 1]), [pvk, "vcol"], ["VsA"])
                    if Tt >= 44:
                        act(lambda: A.activation(out=VwA[:, Tt - 44, :, 0:64], in_=pv[:, 128:256].rearrange("p (g d) -> p g d", g=2),
                                                 func=AF.Copy, scale=vcol[:, Tt:Tt + 1]), [pvk, "vcol"], ["VwA"])
            for g in range(2):
                dve(lambda: V.tensor_copy(out=VsA[:, :, g, 64], in_=vcol[:, :]), ["vcol", "VsA"], ["VsA"])
                dve(lambda: V.tensor_copy(out=VwA[:, :, g, 64], in_=vcol[:, 44:64]), ["vcol", "VwA"], ["VwA"])

            if "p1" in dbg:
                fw.dma("sp", ddbg("d_KA0", [128, 8192], BF16)[:, :], KA[0][:], reads=["KA0", "KA0e"], writes=["d_KA0"])
                fw.dma("sp", ddbg("d_KA1", [128, 8192], BF16)[:, :], KA[1][:], reads=["KA1", "KA1e"], writes=["d_KA1"])
                fw.dma("sp", ddbg("d_kcT", [128, 8192], BF16)[:, :], kcT[:], reads=["kcT"], writes=["d_kcT"])
                fw.dma("sp", ddbg("d_vcT", [128, 8192], BF16)[:, :], vcT[:], reads=["vcT"], writes=["d_vcT"])
                fw.dma("sp", ddbg("d_KwT", [128, 2560], BF16)[:, :], KwT[:], reads=["KwT"], writes=["d_KwT"])
                fw.dma("sp", ddbg("d_VsA", [128, NT * 130], BF16)[:, :], VsA[:].rearrange("p a b c -> p (a b c)"), reads=["VsA"], writes=["d_VsA"])
                fw.dma("sp", ddbg("d_VwA", [128, 20 * 130], BF16)[:, :], VwA[:].rearrange("p a b c -> p (a b c)"), reads=["VwA"], writes=["d_VwA"])
                fw.dma("sp", ddbg("d_u", [4, 128, 8192], BF16)[:, :, :], uscr[:, :, :], reads=["uscr"], writes=["d_u"])
            if upto <= 1:
                fw.finish("sp")
                print("instr", fw.ninstr, "waits", fw.nwait)
                return nc, dbg_out
            fw.barrier()
            S1p.close()
            S1t.close()

            with contextlib.ExitStack() as S2:
                W1 = {}
                for kind, wd in (("k", w_kcmp1), ("v", w_vcmp1)):
                    W1[kind] = sb(S2, "W1" + kind, [128, 32, 128], BF16)
                    src = wd.rearrange("s d f -> d s f")
                    for half in (0, 64):
                        for s0 in range(0, 32, 8):
                            fw.dma("pool", W1[kind][half:half + 64, s0:s0 + 8, :], src[:, s0:s0 + 8, :], writes=["W1" + kind])
                W2p = sb(S2, "W2p", [128, 2, 128], BF16)
                W2v = sb(S2, "W2v", [128, 64], BF16)
                pool(lambda: G.memset(W2p[:], 0.0), [], ["W2p"])
                for g in range(2):
                    fw.dma("pool", W2p[:, g, g * 64:(g + 1) * 64], w_kcmp2[:, :], writes=["W2p"])
                fw.dma("pool", W2v[:], w_vcmp2[:, :], writes=["W2v"])
                peT = {}
                constc = {}
                PSc = ps(S2, "PSc", [128, 8], F32)
                PSh = [ps(S2, f"PSh{i}", [128, 512], F32) for i in range(2)]
                PSck = ps(S2, "PSck", [128, 512], F32)
                PScv = [ps(S2, f"PScv{i}", [128, 64], F32) for i in range(2)]
                for kind, ped in (("k", pe_k), ("v", pe_v)):
                    peT[kind] = sb(S2, "peT" + kind, [64, 32], BF16)
                    fw.dma("pool", peT[kind][:], ped.rearrange("s d -> d s"), writes=["peT" + kind], allow_slow_non_contiguous=True)
                    constc[kind] = sb(S2, "constc" + kind, [128, 1], F32)
                    for s_ in range(32):
                        mm(PSc[:, 0:1], W1[kind][0:64, s_, :], peT[kind][:, s_:s_ + 1], s_ == 0, s_ == 31, ["W1" + kind, "peT" + kind], ["PSc"])
                    act(lambda: A.copy(out=constc[kind][:], in_=PSc[:, 0:1]), ["PSc"], ["constc" + kind])
                hid = {}
                hi = 0
                for kind, srcT, sk in (("k", kcT, "kcT"), ("v", vcT, "vcT")):
                    for g in range(2):
                        hd = sb(S2, f"hid{kind}{g}", [128, 512], BF16)
                        hid[(kind, g)] = hd
                        hk2 = f"hid{kind}{g}"
                        pool(lambda: G.memset(hd[:], 0.0), [], [hk2])
                        ph = PSh[hi % 2]
                        phk = f"PSh{hi % 2}"
                        hi += 1
                        for s_ in range(32):
                            mm(ph[:, 0:511], W1[kind][g * 64:(g + 1) * 64, s_, :], srcT[g * 64:(g + 1) * 64, s_:s_ + 16 * 510 + 1:16],
                               s_ == 0, s_ == 31, ["W1" + kind, sk], [phk])
                        act(lambda: A.activation(out=hd[:, 0:511], in_=ph[:, 0:511], func=AF.Gelu_apprx_tanh, bias=constc[kind][:, 0:1]),
                            [phk, "constc" + kind], [hk2])
                for g in range(2):
                    mm(PSck[:], W2p[:, g, :], hid[("k", g)][:], g == 0, g == 1, ["W2p", f"hidk{g}"], ["PSck"])
                act(lambda: A.copy(out=CkT[:], in_=PSck[:]), ["PSck"], ["CkT"])
                ci = 0
                for g in range(2):
                    for cc in range(4):
                        pc = PScv[ci % 2]
                        pck = f"PScv{ci % 2}"
                        ci += 1
                        mm(pc[:], hid[("v", g)][:, cc * 128:(cc + 1) * 128], W2v[:], True, True, [f"hidv{g}", "W2v"], [pck])
                        act(lambda: A.activation(out=CvA[:, cc, g, 0:64], in_=pc[:], func=AF.Copy, scale=vcc[:, cc:cc + 1]), [pck, "vcc"], ["CvA"])
                    dve(lambda: V.tensor_copy(out=CvA[:, :, g, 64], in_=vcc[:, :]), ["vcc", "CvA"], ["CvA"])
                if "p2" in dbg:
                    fw.dma("sp", ddbg("d_CkT", [128, 512], BF16)[:, :], CkT[:], reads=["CkT"], writes=["d_CkT"])
                    fw.dma("sp", ddbg("d_CvA", [128, 4 * 130], BF16)[:, :], CvA[:].rearrange("p a b c -> p (a b c)"), reads=["CvA"], writes=["d_CvA"])
                fw.barrier()
            if upto <= 2:
                fw.finish("sp")
                print("instr", fw.ninstr, "waits", fw.nwait)
                return nc, dbg_out
        with contextlib.ExitStack() as S3:
            def sincos(st, angt, n, sdst, cdst, tagk):
                a2 = sb(st, "sc_a2" + tagk, [128, n], F32)
                ki_ = sb(st, "sc_ki" + tagk, [128, n], I32)
                kf_ = sb(st, "sc_kf" + tagk, [128, n], F32)
                r_ = sb(st, "sc_r" + tagk, [128, n], F32)
                for shift, dst, dk in ((0.0, sdst, "sc_s" + tagk), (math.pi / 2, cdst, "sc_c" + tagk)):
                    dve(lambda: V.tensor_scalar(out=a2[:], in0=angt[:], scalar1=shift, scalar2=None, op0=ALU.add), ["sc_ang" + tagk], ["sc_a2" + tagk])
                    dve(lambda: V.tensor_scalar(out=ki_[:], in0=a2[:], scalar1=1.0 / TWO_PI, scalar2=None, op0=ALU.mult), ["sc_a2" + tagk], ["sc_ki" + tagk])
                    dve(lambda: V.tensor_copy(out=kf_[:], in_=ki_[:]), ["sc_ki" + tagk], ["sc_kf" + tagk])
                    dve(lambda: V.scalar_tensor_tensor(out=r_[:], in0=kf_[:], scalar=-CW1, in1=a2[:], op0=ALU.mult, op1=ALU.add),
                        ["sc_kf" + tagk, "sc_a2" + tagk], ["sc_r" + tagk])
                    dve(lambda: V.scalar_tensor_tensor(out=r_[:], in0=kf_[:], scalar=-CW2, in1=r_[:], op0=ALU.mult, op1=ALU.add),
                        ["sc_kf" + tagk, "sc_r" + tagk], ["sc_r" + tagk])
                    dve(lambda: V.tensor_scalar(out=r_[:], in0=r_[:], scalar1=-PI_LO, scalar2=PI_LO, op0=ALU.max, op1=ALU.min), ["sc_r" + tagk], ["sc_r" + tagk])
                    act(lambda: A.activation(out=dst[:], in_=r_[:], func=AF.Sin), ["sc_r" + tagk], [dk])

            def tt(out, a, b, op, r, w):
                dve(lambda: V.tensor_tensor(out=out, in0=a, in1=b, op=op), r, w)

            LR = sb(S3, "LR", [128, 32], F32)
            LI = sb(S3, "LI", [128, 32], F32)
            LDT = sb(S3, "LDT", [128, 32], F32)
            for half in (0, 64):
                fw.dma("sp", LR[half:half + 64, :], s5_a_re.rearrange("g n -> n g"), writes=["LR"], allow_slow_non_contiguous=True)
                fw.dma("sp", LI[half:half + 64, :], s5_a_im.rearrange("g n -> n g"), writes=["LI"], allow_slow_non_contiguous=True)
            fw.dma("sp", LDT[:], s5_log_dt[0:1, :].partition_broadcast(128), writes=["LDT"])
            sgn = sb(S3, "sgn", [128, 1], F32)
            Psw = sb(S3, "Psw", [128, 128], F32)
            dcol = sb(S3, "dcol", [128, 4], F32)
            fw.dma("sp", sgn[:], sgn_d[:, :], writes=["sgn"])
            fw.dma("sp", Psw[:], psw_d[:, :], writes=["Psw"])
            fw.dma("sp", dcol[:], s5_d.rearrange("(ct p) -> p ct", p=128), writes=["dcol"], allow_slow_non_contiguous=True)
            stp = sb(S3, "stp", [128, 32], F32)
            mag = sb(S3, "mag", [128, 32], F32)
            ang = sb(S3, "ang", [128, 32], F32)
            sn = sb(S3, "sn", [128, 32], F32)
            cs = sb(S3, "cs", [128, 32], F32)
            abr = sb(S3, "abr", [128, 32], F32)
            abi = sb(S3, "abi", [128, 32], F32)
            den = sb(S3, "den", [128, 32], F32)
            tq = sb(S3, "tq", [128, 32], F32)
            nr = sb(S3, "nr", [128, 32], F32)
            cre = sb(S3, "cre", [128, 32], F32)
            cim = sb(S3, "cim", [128, 32], F32)
            act(lambda: A.activation(out=stp[:], in_=LDT[:], func=AF.Exp), ["LDT"], ["stp"])
            tt(mag[:], LR[:], stp[:], ALU.mult, ["LR", "stp"], ["mag"])
            act(lambda: A.activation(out=mag[:], in_=mag[:], func=AF.Exp), ["mag"], ["mag"])
            tt(ang[:], LI[:], stp[:], ALU.mult, ["LI", "stp"], ["sc_angp"])
            sincos(S3, ang, 32, sn, cs, "p")
            tt(abr[:], mag[:], cs[:], ALU.mult, ["mag", "sc_cp"], ["abr"])
            tt(abi[:], mag[:], sn[:], ALU.mult, ["mag", "sc_sp"], ["abi"])
            tt(den[:], LR[:], LR[:], ALU.mult, ["LR"], ["den"])
            tt(tq[:], LI[:], LI[:], ALU.mult, ["LI"], ["tq"])
            tt(den[:], den[:], tq[:], ALU.add, ["den", "tq"], ["den"])
            dve(lambda: V.reciprocal(out=den[:], in_=den[:]), ["den"], ["den"])
            dve(lambda: V.tensor_scalar(out=nr[:], in0=abr[:], scalar1=-1.0, scalar2=None, op0=ALU.add), ["abr"], ["nr"])
            tt(cre[:], nr[:], LR[:], ALU.mult, ["nr", "LR"], ["cre"])
            tt(tq[:], abi[:], LI[:], ALU.mult, ["abi", "LI"], ["tq"])
            tt(cre[:], cre[:], tq[:], ALU.add, ["cre", "tq"], ["cre"])
            tt(cre[:], cre[:], den[:], ALU.mult, ["cre", "den"], ["cre"])
            tt(cim[:], abi[:], LR[:], ALU.mult, ["abi", "LR"], ["cim"])
            tt(tq[:], nr[:], LI[:], ALU.mult, ["nr", "LI"], ["tq"])
            tt(cim[:], cim[:], tq[:], ALU.subtract, ["cim", "tq"], ["cim"])
            tt(cim[:], cim[:], den[:], ALU.mult, ["cim", "den"], ["cim"])
            NL = 12
            NR = 18
            WR = sb(S3, "WR", [128, NR, 32], F32)
            WIS = sb(S3, "WIS", [128, NR, 32], F32)
            dve(lambda: V.tensor_copy(out=WR[:, 0, :], in_=abr[:]), ["abr"], ["WR"])
            dve(lambda: V.tensor_scalar(out=WIS[:, 0, :], in0=abi[:], scalar1=sgn[:, 0:1], scalar2=None, op0=ALU.mult), ["abi", "sgn"], ["WIS"])
            for l in range(1, NL):
                tt(tq[:], WR[:, l - 1, :], WR[:, l - 1, :], ALU.mult, ["WR"], ["tq"])
                tt(nr[:], WIS[:, l - 1, :], WIS[:, l - 1, :], ALU.mult, ["WIS"], ["nr"])
                tt(WR[:, l, :], tq[:], nr[:], ALU.subtract, ["tq", "nr", "WR"], ["WR"])
                dve(lambda: V.scalar_tensor_tensor(out=WIS[:, l, :], in0=WR[:, l - 1, :], scalar=2.0, in1=WIS[:, l - 1, :], op0=ALU.mult, op1=ALU.mult),
                    ["WR", "WIS"], ["WIS"])
            for l in range(6):
                a_, c_ = 2 * l, 2 * l + 1
                tt(tq[:], WR[:, a_, :], WR[:, c_, :], ALU.mult, ["WR"], ["tq"])
                tt(nr[:], WIS[:, a_, :], WIS[:, c_, :], ALU.mult, ["WIS"], ["nr"])
                tt(WR[:, 12 + l, :], tq[:], nr[:], ALU.subtract, ["tq", "nr", "WR"], ["WR"])
                tt(tq[:], WR[:, a_, :], WIS[:, c_, :], ALU.mult, ["WR", "WIS"], ["tq"])
                tt(nr[:], WIS[:, a_, :], WR[:, c_, :], ALU.mult, ["WR", "WIS"], ["nr"])
                tt(WIS[:, 12 + l, :], tq[:], nr[:], ALU.add, ["tq", "nr", "WIS"], ["WIS"])
            Bpad = sb(S3, "Bpad", [128, 32, 128], BF16)
            Cpad = sb(S3, "Cpad", [128, 32, 128], BF16)
            with contextlib.ExitStack() as S3a:
                PSs = [ps(S3a, f"PSs{i}", [128, 128], F32) for i in range(2)]
                BR = sb(S3a, "BR", [64, 32, 16], F32)
                BI = sb(S3a, "BI", [64, 32, 16], F32)
                fw.dma("sp", BR[:], s5_b_re.rearrange("g n p -> n g p"), writes=["BR"])
                fw.dma("sp", BI[:], s5_b_im.rearrange("g n p -> n g p"), writes=["BI"])
                bbr = sb(S3a, "bbr", [64, 32, 16], F32)
                bbi = sb(S3a, "bbi", [64, 32, 16], F32)
                tb_ = sb(S3a, "tb_", [64, 32, 16], F32)
                creb = cre[0:64, :].unsqueeze(2).broadcast_to([64, 32, 16])
                cimb = cim[0:64, :].unsqueeze(2).broadcast_to([64, 32, 16])
                tt(bbr[:], BR[:], creb, ALU.mult, ["BR", "cre"], ["bbr"])
                tt(tb_[:], BI[:], cimb, ALU.mult, ["BI", "cim"], ["tb_"])
                tt(bbr[:], bbr[:], tb_[:], ALU.subtract, ["bbr", "tb_"], ["bbr"])
                tt(bbi[:], BI[:], creb, ALU.mult, ["BI", "cre"], ["bbi"])
                tt(tb_[:], BR[:], cimb, ALU.mult, ["BR", "cim", "bbr"], ["tb_"])
                tt(bbi[:], bbi[:], tb_[:], ALU.add, ["bbi", "tb_"], ["bbi"])
                ZpR = sb(S3a, "ZpR", [64, 32, 128], F32)
                ZpI = sb(S3a, "ZpI", [64, 32, 128], F32)
                pool(lambda: G.memset(ZpR[:], 0.0), [], ["ZpR"])
                pool(lambda: G.memset(ZpI[:], 0.0), [], ["ZpI"])
                for gl in range(8):
                    for Z, bb, zk, bk in ((ZpR, bbr, "ZpR", "bbr"), (ZpI, bbi, "ZpI", "bbi")):
                        dve(lambda: V.tensor_copy(out=Z[:].rearrange("n (ct gl) c -> n ct gl c", gl=8)[:, :, gl, 16 * gl:16 * gl + 16],
                                                  in_=bb[:].rearrange("n (ct gl) p -> n ct gl p", gl=8)[:, :, gl, :]), [bk, zk], [zk])
                for g in range(32):
                    pz = PSs[g % 2]
                    pzk = f"PSs{g % 2}"
                    tr(pz[:, 0:64], ZpR[:, g, :], identf[0:64, 0:64], ["ZpR", "identf"], [pzk], signal=False)
                    tr(pz[:, 64:128], ZpI[:, g, :], identf[0:64, 0:64], ["ZpI", "identf"], [pzk])
                    act(lambda: A.copy(out=Bpad[:, g, :], in_=pz[:]), [pzk], ["Bpad"])
                Cnat = sb(S3a, "Cnat", [128, 4, 128], F32)
                fw.dma("sp", Cnat[:, :, 0:64], s5_c_re.rearrange("(ct gl) p n -> (gl p) ct n", gl=8), writes=["Cnat"])
                fw.dma("sp", Cnat[:, :, 64:128], s5_c_im.rearrange("(ct gl) p n -> (gl p) ct n", gl=8), writes=["Cnat"])
                dve(lambda: V.tensor_scalar(out=Cnat[:, :, 64:128], in0=Cnat[:, :, 64:128], scalar1=-1.0, scalar2=None, op0=ALU.mult), ["Cnat"], ["Cnat"])
                pool(lambda: G.memset(Cpad[:], 0.0), [], ["Cpad"])
                for ct in range(4):
                    pz = PSs[ct % 2]
                    pzk = f"PSs{ct % 2}"
                    tr(pz[:, :], Cnat[:, ct, :], identf[:], ["Cnat", "identf"], [pzk])
                    for gl in range(8):
                        act(lambda: A.copy(out=Cpad[:, ct * 8 + gl, 16 * gl:16 * gl + 16], in_=pz[:, 16 * gl:16 * gl + 16]), [pzk, "Cpad"], ["Cpad"])
                fw.barrier()
            uT = [sb(S3, f"uT{i}", [128, 8192], BF16) for i in range(2)]
            X0 = [sb(S3, f"X0_{i}", [128, 8192], BF16) for i in range(2)]
            Ya = [sb(S3, f"Ya{i}", [128, 1536], BF16) for i in range(2)]
            Yb = [sb(S3, f"Yb{i}", [128, 384], BF16) for i in range(2)]
            Wa = [sb(S3, f"Wa{i}", [128, 2048], BF16) for i in range(2)]
            Wb = [sb(S3, f"Wb{i}", [128, 2048], BF16) for i in range(2)]
            Rm = [sb(S3, f"Rm{i}", [128, NR, 128], BF16) for i in range(2)]
            rtmp = sb(S3, "rtmp", [128, 128], F32)
            rtmp2 = sb(S3, "rtmp2", [128, 128], F32)
            xin = [sb(S3, f"xin{i}", [128, 2], BF16) for i in range(2)]
            ytmp = sb(S3, "ytmp", [128, 512], F32)
            yps = [ps(S3, f"yps{i}", [128, 512], F32) for i in range(4)]
            PB = [ps(S3, f"PB{i}", [128, 512], F32) for i in range(4)]
            evi = [0]
            pbi = [0, 0]

            def evac(out, in_, r, w):
                evi[0] += 1
                if evi[0] % 2:
                    act(lambda: A.copy(out=out, in_=in_), r, w)
                else:
                    dve(lambda: V.tensor_copy(out=out, in_=in_), r, w)

            def next_pb(gp):
                i = 2 * gp + pbi[gp] % 2
                pbi[gp] += 1
                return PB[i], f"PB{i}"

            for ct in range(4):
                u = uT[ct % 2]
                uk = f"uT{ct % 2}"
                for q4 in range(4):
                    fw.dma("sp", u[:, q4 * 2048:(q4 + 1) * 2048], uscr[ct, :, q4 * 2048:(q4 + 1) * 2048], reads=["uscr"], writes=[uk])
                def s5_group(ct, gl, u, uk):
                    g = ct * 8 + gl
                    gp = g % 2
                    R = Rm[gp]
                    rk = f"Rm{gp}"
                    for l in range(NR):
                        dve(lambda: V.tensor_scalar(out=rtmp[:], in0=identf[:], scalar1=WR[:, l, g:g + 1], scalar2=None, op0=ALU.mult),
                            ["identf", "WR"], ["rtmp"])
                        dve(lambda: V.scalar_tensor_tensor(out=R[:, l, :], in0=Psw[:], scalar=WIS[:, l, g:g + 1], in1=rtmp[:], op0=ALU.mult, op1=ALU.add),
                            ["Psw", "WIS", "rtmp"], [rk])
                    x0 = X0[gp]
                    x0k = f"X0_{gp}"
                    for tb in range(16):
                        pb, pbk = next_pb(gp)
                        mm(pb[:], Bpad[:, g, :], u[:, tb * 512:(tb + 1) * 512], True, True, ["Bpad", uk], [pbk])
                        evac(x0[:, tb * 512:(tb + 1) * 512], pb[:], [pbk], [x0k])
                        yield
                    src, srck = x0, x0k
                    bufs = [(Ya[gp], f"Ya{gp}"), (Yb[gp], f"Yb{gp}")]
                    n = 6144
                    bsel = 0
                    for l in range(5):
                        quarter = n // 4
                        dst, dstk = bufs[bsel]
                        for c0 in range(0, quarter, 512):
                            w_ = min(512, quarter - c0)
                            pb, pbk = next_pb(gp)
                            mm(pb[:, 0:w_], R[:, 12 + l, :], src[:, 4 * c0:4 * c0 + 4 * w_:4], True, False, [rk, srck], [pbk])
                            mm(pb[:, 0:w_], R[:, 2 * l + 1, :], src[:, 4 * c0 + 1:4 * c0 + 4 * w_:4], False, False, [rk, srck], [pbk])
                            mm(pb[:, 0:w_], R[:, 2 * l, :], src[:, 4 * c0 + 2:4 * c0 + 4 * w_:4], False, True, [rk, srck], [pbk])
                            dve(lambda: V.tensor_tensor(out=dst[:, c0:c0 + w_], in0=pb[:, 0:w_], in1=src[:, 4 * c0 + 3:4 * c0 + 4 * w_:4], op=ALU.add),
                                [pbk, srck], [dstk])
                            yield
                        src, srck = dst, dstk
                        bsel ^= 1
                        n = quarter
                    assert n == 6
                    dst, dstk = bufs[bsel]
                    pb, pbk = next_pb(gp)
                    mm(pb[:, 0:3], R[:, 10, :], src[:, 0:6:2], True, False, [rk, srck], [pbk])
                    mm(pb[:, 0:3], identb[:], src[:, 1:6:2], False, True, ["identb", srck], [pbk])
                    evac(dst[:, 0:3], pb[:, 0:3], [pbk], [dstk])
                    yield
                    src, srck = dst, dstk
                    xi = xin[gp]
                    xik = f"xin{gp}"
                    pb, pbk = next_pb(gp)
                    mm(pb[:, 0:1], R[:, 11, :], src[:, 0:1], True, False, [rk, srck], [pbk])
                    mm(pb[:, 0:1], identb[:], src[:, 1:2], False, True, ["identb", srck], [pbk])
                    evac(xi[:, 0:1], pb[:, 0:1], [pbk], [xik])
                    yield
                    pb, pbk = next_pb(gp)
                    mm(pb[:, 0:1], R[:, 11, :], xi[:, 0:1], True, False, [rk, xik], [pbk])
                    mm(pb[:, 0:1], identb[:], src[:, 2:3], False, True, ["identb", srck], [pbk])
                    evac(xi[:, 1:2], pb[:, 0:1], [pbk], [xik])
                    yield
                    pb, pbk = next_pb(gp)
                    mm(pb[:, 0:1], R[:, 0, :], xi[:, 1:2], True, False, [rk, xik], [pbk])
                    mm(pb[:, 0:1], identb[:], x0[:, 6144:6145], False, True, ["identb", x0k], [pbk])
                    evac(x0[:, 6144:6145], pb[:, 0:1], [pbk], [x0k])
                    yield
                    cur, curk, off = x0, x0k, 6144
                    wb = [(Wa[gp], f"Wa{gp}"), (Wb[gp], f"Wb{gp}")]
                    for l in range(6):
                        k_ = 4 ** l
                        dst, dstk = wb[l % 2]
                        for blk in range(4):
                            c0 = blk * 512
                            terms = []
                            for j_, ridx in ((1, 2 * l), (2, 2 * l + 1), (3, 12 + l)):
                                sh = j_ * k_
                                if c0 + 512 > sh:
                                    terms.append((R[:, ridx, :], rk, sh, max(c0, sh)))
                            if not terms:
                                act(lambda: A.copy(out=dst[:, c0:c0 + 512], in_=cur[:, off + c0:off + c0 + 512]), [curk], [dstk])
                                yield
                                continue
                            pb, pbk = next_pb(gp)
                            for ti_, (lh, lhk, sh, lo) in enumerate(terms):
                                mm(pb[:, lo - c0:512], lh, cur[:, off + lo - sh:off + c0 + 512 - sh], ti_ == 0, ti_ == len(terms) - 1, [lhk, curk], [pbk])
                            lo1 = terms[0][3]
                            if lo1 > c0:
                                act(lambda: A.copy(out=dst[:, c0:lo1], in_=cur[:, off + c0:off + lo1]), [curk], [dstk])
                            dve(lambda: V.tensor_tensor(out=dst[:, lo1:c0 + 512], in0=pb[:, lo1 - c0:512], in1=cur[:, off + lo1:off + c0 + 512], op=ALU.add),
                                [pbk, curk], [dstk])
                            yield
                        cur, curk, off = dst, dstk, 0
                    for blk in range(4):
                        mm(yps[blk][:], Cpad[:, g, :], cur[:, blk * 512:(blk + 1) * 512], gl == 0, gl == 7, ["Cpad", curk], [f"yps{blk}"])

                for gpair in range(4):
                    gens = [s5_group(ct, 2 * gpair, u, uk), s5_group(ct, 2 * gpair + 1, u, uk)]
                    alive = [True, True]
                    while any(alive):
                        for gi_ in range(2):
                            if alive[gi_]:
                                try:
                                    next(gens[gi_])
                                except StopIteration:
                                    alive[gi_] = False
                for blk in range(4):
                    dve(lambda: V.scalar_tensor_tensor(out=ytmp[:], in0=u[:, 6144 + blk * 512:6144 + (blk + 1) * 512], scalar=dcol[:, ct:ct + 1],
                                                       in1=yps[blk][:], op0=ALU.mult, op1=ALU.add), [uk, "dcol", f"yps{blk}"], ["ytmp"])
                    act(lambda: A.activation(out=gyT[:, ct, blk * 512:(blk + 1) * 512], in_=ytmp[:], func=AF.Gelu_apprx_tanh), ["ytmp"], ["gyT"])
            if "p3" in dbg:
                fw.dma("sp", ddbg("d_gyT", [128, 4 * 2048], BF16)[:, :], gyT[:].rearrange("p a b -> p (a b)"), reads=["gyT"], writes=["d_gyT"])
            fw.barrier()
        if upto <= 3:
            fw.finish("sp")
            print("instr", fw.ninstr, "waits", fw.nwait)
            return nc, dbg_out

        with contextlib.ExitStack() as S4:
            tri = sb(S4, "tri", [128, 512], BF16)
            atri = sb(S4, "atri", [128, 512], BF16)
            fw.dma("pool", tri[:], tri_d[:, :], writes=["tri"])
            fw.dma("pool", atri[:], atri_d[:, :], writes=["atri"])
            grow = sb(S4, "grow", [128, 1024], F32)
            brow = sb(S4, "brow", [128, 1024], F32)
            g1row = sb(S4, "g1row", [128, 1024], F32)
            b1row = sb(S4, "b1row", [128, 1024], F32)
            fw.dma("sp", grow[:], ln_emb[0:1, :].partition_broadcast(128), writes=["grow"])
            fw.dma("sp", brow[:], ln_emb[1:2, :].partition_broadcast(128), writes=["brow"])
            fw.dma("sp", g1row[:], ln1_d[0:1, :].partition_broadcast(128), writes=["g1row"])
            fw.dma("sp", b1row[:], ln1_d[1:2, :].partition_broadcast(128), writes=["b1row"])
            Wo = sb(S4, "Wo", [128, 8, 1024], BF16)
            wov = w_o.rearrange("(kc p) c -> p kc c", p=128)
            for kc in range(0, 8, 2):
                fw.dma("pool", Wo[:, kc:kc + 2, :], wov[:, kc:kc + 2, :], writes=["Wo"])
            KmT = sb(S4, "KmT", [128, 4, 256], BF16)
            VmA = sb(S4, "VmA", [128, 2, 4, 129], BF16)
            wv = w_in.rearrange("(dc p) c -> p dc c", p=128)
            PS_S = [ps(S4, f"PS_S{i}", [128, 512], F32) for i in range(2)]
            PO = [ps(S4, f"PO{i}", [128, 512], F32) for i in range(2)]
            PF = [ps(S4, f"PF4_{i}", [128, 512], F32) for i in range(2)]
            PT4 = ps(S4, "PT4", [128, 1024], BF16)
            PX = ps(S4, "PX", [128, 512], F32)
            PS_S = PS_S + [PF[1]]
            cnt = {"s": 0, "o": 0, "f": 0, "pt": 0, "wp": 0}

            def nxt(lst, key, name):
                i = cnt[key] % len(lst)
                cnt[key] += 1
                if name == "PS_S" and i == 2:
                    return lst[i], "PF4_1"
                return lst[i], f"{name}{i}"

            with contextlib.ExitStack() as S4m:
                memf = sb(S4m, "memf", [128, 2, 1024], F32)
                memb = sb(S4m, "memb", [128, 2, 1024], BF16)
                memT = sb(S4m, "memT", [128, 8, 256], BF16)
                Wmk = sb(S4m, "Wmk", [128, 8, 512], BF16)
                Wmv = sb(S4m, "Wmv", [128, 8, 512], BF16)
                fw.dma("sp", memf[:], mem_d.rearrange("(t p) c -> p t c", p=128), writes=["memf"])
                wmv_ = w_mem_kv.rearrange("(dc p) c -> p dc c", p=128)
                for dc in range(0, 8, 4):
                    fw.dma("pool", Wmk[:, dc:dc + 4, :], wmv_[:, dc:dc + 4, 0:512], writes=["Wmk"])
                    fw.dma("pool", Wmv[:, dc:dc + 4, :], wmv_[:, dc:dc + 4, 512:1024], writes=["Wmv"])
                dve(lambda: V.tensor_copy(out=memb[:], in_=memf[:]), ["memf"], ["memb"])
                for mt in range(2):
                    for dc in range(8):
                        tr(PT4[:, dc * 128:(dc + 1) * 128], memb[:, mt, dc * 128:(dc + 1) * 128], identb[:], ["memb", "identb"], ["PT4"], signal=(dc == 7))
                    act(lambda: A.copy(out=memT[:, :, mt * 128:(mt + 1) * 128], in_=PT4[:].rearrange("p (a b) -> p a b", a=8)), ["PT4"], ["memT"])
                for h in range(4):
                    pf, pfk = nxt(PF, "f", "PF4_")
                    for dc in range(8):
                        mm(pf[:, 0:256], Wmk[:, dc, h * 128:(h + 1) * 128], memT[:, dc, :], dc == 0, dc == 7, ["Wmk", "memT"], [pfk])
                    act(lambda: A.copy(out=KmT[:, h, :], in_=pf[:, 0:256]), [pfk], ["KmT"])
                for mc in range(2):
                    pf, pfk = nxt(PF, "f", "PF4_")
                    for dc in range(8):
                        mm(pf[:], memT[:, dc, mc * 128:(mc + 1) * 128], Wmv[:, dc, :], dc == 0, dc == 7, ["Wmv", "memT"], [pfk])
                    act(lambda: A.copy(out=VmA[:, mc, :, 0:128], in_=pf[:].rearrange("p (h d) -> p h d", h=4)), [pfk], ["VmA"])
                pool(lambda: G.memset(VmA[:, :, :, 128:129], 1.0), ["VmA"], ["VmA"])
                fw.barrier()

            hTq = sb(S4, "hTq", [128, 8, 512], BF16)
            QT = sb(S4, "QT", [128, 4, 4, 128], BF16)
            qmT = sb(S4, "qmT", [128, 4, 4, 128], BF16)
            gn = sb(S4, "gn", [128, 4, 24], F32)
            oT = sb(S4, "oT", [128, 4, 512], BF16)
            omT = sb(S4, "omT", [128, 4, 512], BF16)
            mrgT = sb(S4, "mrgT", [128, 8, 512], BF16)
            st4 = sb(S4, "st4", [128, 12], F32)
            mv4 = sb(S4, "mv4", [128, 2], F32)
            rs4 = sb(S4, "rs4", [128, 1], F32)
            st4b = sb(S4, "st4b", [128, 12], F32)
            mv4b = sb(S4, "mv4b", [128, 2], F32)
            rs4b = sb(S4, "rs4b", [128, 1], F32)

            def ln_tok(xt, xk, dst, dstk, grow_, brow_, gk, bk, par=0):
                st_, mv_, rs_ = (st4, mv4, rs4) if par == 0 else (st4b, mv4b, rs4b)
                sk_, mk2_, rk_ = f"st4_{par}", f"mv4_{par}", f"rs4_{par}"
                dve(lambda: V.bn_stats(out=st_[:, 0:6], in_=xt[:, 0:512]), [xk], [sk_])
                dve(lambda: V.bn_stats(out=st_[:, 6:12], in_=xt[:, 512:1024]), [xk], [sk_])
                dve(lambda: V.bn_aggr(out=mv_[:, 0:2], in_=st_[:, 0:12]), [sk_], [mk2_])
                act(lambda: A.activation(out=rs_[:], in_=mv_[:, 1:2], func=AF.Sqrt, bias=LN_EPS, scale=1.0), [mk2_], [rk_])
                dve(lambda: V.reciprocal(out=rs_[:], in_=rs_[:]), [rk_], [rk_])
                dve(lambda: V.tensor_scalar(out=dst[:], in0=xt[:], scalar1=mv_[:, 0:1], scalar2=rs_[:, 0:1], op0=ALU.subtract, op1=ALU.mult),
                    [xk, mk2_, rk_], [dstk])
                dve(lambda: V.tensor_tensor(out=dst[:], in0=dst[:], in1=grow_[:], op=ALU.mult), [dstk, gk], [dstk])
                dve(lambda: V.tensor_tensor(out=dst[:], in0=dst[:], in1=brow_[:], op=ALU.add), [dstk, bk], [dstk])

            wpid = [0]
            curB = [0]

            def wpiece(src_ap):
                w_, wk_ = nxt(wp, "wp", "wp")
                nk_ = src_ap.shape[1]
                pid = wpid[0]
                wpid[0] += 1
                sc_ = wscr[pid, :, 0:nk_ * 128].rearrange("p (k c) -> p k c", k=nk_)
                if curB[0] == 0:
                    fw.dma("pool", w_[:, 0:nk_, :], src_ap, writes=[wk_])
                    fw.dma("sp", sc_, w_[:, 0:nk_, :], reads=[wk_], writes=[f"wscr{pid}"])
                else:
                    fw.dma("sp", w_[:, 0:nk_, :], sc_, reads=[f"wscr{pid}"], writes=[wk_])
                return w_, wk_

            wno_v = w_nsa_out.rearrange("(kc p) c -> p kc c", p=128)
            wmo_v = w_mem_out.rearrange("(kc p) c -> p kc c", p=128)
            wgl_v = w_s5_glu.rearrange("(kc p) c -> p kc c", p=128)

            for B in range(4):
                wpid[0] = 0
                curB[0] = B
                with contextlib.ExitStack() as SA:
                    xq = [sb(SA, f"xq{i}", [128, 1024], F32) for i in range(1)]
                    h0t = sb(SA, "h0t", [128, 1024], F32)
                    h0b = sb(SA, "h0b", [128, 1024], BF16)
                    posi4 = sb(SA, "posi4", [128, 512], I32)
                    posf4 = sb(SA, "posf4", [128, 512], F32)
                    angs4 = sb(SA, "sc_angq", [128, 512], F32)
                    cos4 = sb(SA, "cos4", [128, 512], F32)
                    sin4 = sb(SA, "sin4", [128, 512], F32)
                    t14 = sb(SA, "t14", [128, 512], F32)
                    t24 = sb(SA, "t24", [128, 512], F32)
                    Wq4 = sb(SA, "Wq4", [128, 8, 4, 128], BF16)
                    Wqsw = sb(SA, "Wqsw", [128, 8, 4, 128], BF16)
                    Wgn = sb(SA, "Wgn", [128, 8, 24], BF16)
                    if B == 0:
                        for half in range(2):
                            for dc in range(8):
                                fw.dma("pool", Wq4[:, dc, :, half * 64:(half + 1) * 64],
                                       wv[:, dc, half * 256:(half + 1) * 256].rearrange("p (h d) -> p h d", h=4), writes=["Wq4"])
                        fw.dma("pool", Wgn[:], wv[:, :, C_GN:C_GN + 24], writes=["Wgn"])
                        pool(lambda: G.memset(Wqsw[:], 0.0), [], ["Wqsw"])
                        for half in range(2):
                            b0 = half * 64
                            dve(lambda: V.tensor_scalar(out=Wqsw[:, :, :, b0:b0 + 8], in0=Wq4[:, :, :, b0 + 8:b0 + 16], scalar1=-1.0, scalar2=None, op0=ALU.mult),
                                ["Wq4", "Wqsw"], ["Wqsw"])
                            dve(lambda: V.tensor_copy(out=Wqsw[:, :, :, b0 + 8:b0 + 16], in_=Wq4[:, :, :, b0:b0 + 8]), ["Wq4", "Wqsw"], ["Wqsw"])
                        fw.dma("sp", wqscr[0, :, :], Wq4[:].rearrange("p a b c -> p (a b c)"), reads=["Wq4"], writes=["wqscr0"])
                        fw.dma("sp", wqscr[1, :, :], Wqsw[:].rearrange("p a b c -> p (a b c)"), reads=["Wqsw"], writes=["wqscr1"])
                        fw.dma("sp", wgnscr[:, :], Wgn[:].rearrange("p a b -> p (a b)"), reads=["Wgn"], writes=["wgnscr"])
                    else:
                        fw.dma("sp", Wq4[:].rearrange("p a b c -> p (a b c)"), wqscr[0, :, :], reads=["wqscr0"], writes=["Wq4"])
                        fw.dma("sp", Wqsw[:].rearrange("p a b c -> p (a b c)"), wqscr[1, :, :], reads=["wqscr1"], writes=["Wqsw"])
                        fw.dma("sp", Wgn[:].rearrange("p a b -> p (a b)"), wgnscr[:, :], reads=["wgnscr"], writes=["Wgn"])

                    fw.dma("sp", posi4[:], posr[:, 6144 + B * 512:6144 + (B + 1) * 512].partition_broadcast(128), writes=["posi4"])
                    dve(lambda: V.tensor_copy(out=posf4[:], in_=posi4[:]), ["posi4"], ["posf4"])
                    dve(lambda: V.tensor_scalar(out=angs4[:], in0=posf4[:], scalar1=invf[:, 0:1], scalar2=None, op0=ALU.mult), ["posf4", "invf"], ["sc_angq"])
                    with contextlib.ExitStack() as Ssc:
                        sincos(Ssc, angs4, 512, sin4, cos4, "q")
                        fw.barrier()
                    Wqm = sb(SA, "Wqm", [128, 8, 512], BF16)
                    if B == 0:
                        for dc in range(0, 8, 4):
                            fw.dma("pool", Wqm[:, dc:dc + 4, :], wv[:, dc:dc + 4, C_QM:C_QM + 512], writes=["Wqm"])
                        fw.dma("sp", wqscr[2, :, :], Wqm[:].rearrange("p a b -> p (a b)"), reads=["Wqm"], writes=["wqscr2"])
                    else:
                        fw.dma("sp", Wqm[:].rearrange("p a b -> p (a b)"), wqscr[2, :, :], reads=["wqscr2"], writes=["Wqm"])
                    for tl in range(4):
                        Tt = OWN0 + B * 4 + tl
                        xt, xk = nxt(xq, "pt", "xq")
                        fw.dma("sp", xt[:], xr[Tt * 128:(Tt + 1) * 128, :], writes=[xk])
                        ln_tok(xt, xk, h0t, "h0t", grow, brow, "grow", "brow")
                        dve(lambda: V.tensor_copy(out=h0b[:], in_=h0t[:]), ["h0t"], ["h0b"])
                        for dc in range(8):
                            tr(PT4[:, dc * 128:(dc + 1) * 128], h0b[:, dc * 128:(dc + 1) * 128], identb[:], ["h0b", "identb"], ["PT4"], signal=(dc == 7))
                        act(lambda: A.copy(out=hTq[:, :, tl * 128:(tl + 1) * 128], in_=PT4[:].rearrange("p (a b) -> p a b", a=8)), ["PT4"], ["hTq"])
                    if "p4o" in dbg and B == 0:
                        fw.dma("sp", ddbg("d_Wq4", [128, 4096], BF16)[:, :], Wq4[:].rearrange("p a b c -> p (a b c)"), reads=["Wq4"], writes=["d_Wq4"])
                        fw.dma("sp", ddbg("d_Wqsw", [128, 4096], BF16)[:, :], Wqsw[:].rearrange("p a b c -> p (a b c)"), reads=["Wqsw"], writes=["d_Wqsw"])
                        fw.dma("sp", ddbg("d_hTq", [128, 4096], BF16)[:, :], hTq[:].rearrange("p a b -> p (a b)"), reads=["hTq"], writes=["d_hTq"])
                        fw.dma("sp", ddbg("d_cos4", [128, 512], F32)[:, :], cos4[:], reads=["sc_cq"], writes=["d_cos4"])
                        fw.dma("sp", ddbg("d_sin4", [128, 512], F32)[:, :], sin4[:], reads=["sc_sq"], writes=["d_sin4"])
                    for hh in range(4):
                        pa, pak = nxt(PF, "f", "PF4_")
                        for dc in range(8):
                            mm(pa[:], Wq4[:, dc, hh, :], hTq[:, dc, :], dc == 0, dc == 7, ["Wq4", "hTq"], [pak])
                        pb, pbk = nxt(PF, "f", "PF4_")
                        for dc in range(8):
                            mm(pb[:], Wqsw[:, dc, hh, :], hTq[:, dc, :], dc == 0, dc == 7, ["Wqsw", "hTq"], [pbk])
                        dve(lambda: V.tensor_tensor(out=t14[:], in0=pa[:], in1=cos4[:], op=ALU.mult), [pak, "sc_cq"], ["t14"])
                        dve(lambda: V.tensor_tensor(out=t24[:], in0=pb[:], in1=sin4[:], op=ALU.mult), [pbk, "sc_sq"], ["t24"])
                        pool(lambda: G.tensor_tensor(out=QT[:, :, hh, :], in0=t14[:].rearrange("p (t q) -> p t q", t=4),
                                                     in1=t24[:].rearrange("p (t q) -> p t q", t=4), op=ALU.add), ["t14", "t24"], ["QT"])
                    for h in range(4):
                        pa, pak = nxt(PF, "f", "PF4_")
                        for dc in range(8):
                            mm(pa[:], Wqm[:, dc, h * 128:(h + 1) * 128], hTq[:, dc, :], dc == 0, dc == 7, ["Wqm", "hTq"], [pak])
                        act(lambda: A.copy(out=qmT[:, :, h, :], in_=pa[:].rearrange("p (t q) -> p t q", t=4)), [pak], ["qmT"])
                    for tl in range(4):
                        pa, pak = nxt(PF, "f", "PF4_")
                        for dc in range(8):
                            mm(pa[:, 0:24], hTq[:, dc, tl * 128:(tl + 1) * 128], Wgn[:, dc, :], dc == 0, dc == 7, ["Wgn", "hTq"], [pak])
                        act(lambda: A.activation(out=gn[:, tl, :], in_=pa[:, 0:24], func=AF.Sigmoid), [pak], ["gn"])

                    fw.barrier()
                with contextlib.ExitStack() as SB:
                    cbA = [sb(SB, f"cbA{i}", [128, 512], BF16) for i in range(2)]
                    cbB = [sb(SB, f"cbB{i}", [128, 2, 512], BF16) for i in range(2)]
                    addb = [sb(SB, f"addb{i}", [128, 128], F32) for i in range(2)]
                    futb = [sb(SB, f"futb{i}", [128, 128], F32) for i in range(2)]
                    Pa = [sb(SB, f"Pa{i}", [128, 512], F32) for i in range(2)]
                    PTb = [sb(SB, f"PTb{i}", [128, 512], BF16) for i in range(4)]
                    imp = sb(SB, "imp", [128, 512], F32)
                    chk = sb(SB, "chk", [128, 512], F32)
                    sc2 = sb(SB, "sc2", [128, 128], F32)
                    sc3 = sb(SB, "sc3", [128, 128], F32)
                    m8a = sb(SB, "m8a", [128, 8], F32)
                    m8b = sb(SB, "m8b", [128, 8], F32)
                    selb = sb(SB, "selb", [128, 128], F32)
                    selsw = sb(SB, "selsw", [128, 128], F32)
                    QaA = [sb(SB, f"QaA{i}", [128, 4, 128], BF16) for i in range(2)]
                    QaB = [sb(SB, f"QaB{i}", [128, 4, 128], BF16) for i in range(2)]
                    rsum = sb(SB, "rsum", [128, 4], F32)
                    rinv = sb(SB, "rinv", [128, 4], F32)
                    coef = sb(SB, "coef", [128, 4], F32)
                    oacc = sb(SB, "oacc", [128, 512], F32)
                    otmp = sb(SB, "otmp", [128, 256], F32)
                    oaccb = sb(SB, "oaccb", [128, 512], BF16)
                    omacc = sb(SB, "omacc", [128, 512], F32)
                    omb = sb(SB, "omb", [128, 512], BF16)
                    for tl in range(4):
                        j = B * 4 + tl
                        qt = OWN0 + j
                        c2 = j % 2
                        fw.dma("pool", cbA[c2][:], cba_d[j, :, :], writes=[f"cbA{c2}"])
                        fw.dma("pool", cbB[c2][:], cbb_d[j, :, :, :].rearrange("a p c -> p a c"), writes=[f"cbB{c2}"])
                        fw.dma("sp", addb[c2][:], addb_d[j, :, :], writes=[f"addb{c2}"])
                        fw.dma("sp", futb[c2][:], futb_d[j, :, :], writes=[f"futb{c2}"])
                        for g in range(2):
                            r0 = g * 64
                            Qg = QT[r0:r0 + 64, tl, :, :].rearrange("p h q -> p (h q)")
                            for hh in range(4):
                                pS, pSk = nxt(PS_S, "s", "PS_S")
                                mm(pS[:], QT[r0:r0 + 64, tl, hh, :], CkT[r0:r0 + 64, :], True, False, ["QT", "CkT"], [pSk])
                                mm(pS[:], identb[:], cbA[c2][:], False, True, ["identb", f"cbA{c2}"], [pSk])
                                pa_ = Pa[hh % 2]
                                pak_ = f"Pa{hh % 2}"
                                act(lambda: A.activation(out=pa_[:], in_=pS[:], func=AF.Exp, scale=0.125, accum_out=rsum[:, hh:hh + 1]), [pSk], [pak_, "rsum"])
                                dve(lambda: V.tensor_scalar(out=rinv[:, hh:hh + 1], in0=rsum[:, hh:hh + 1], scalar1=1e-30, scalar2=None, op0=ALU.max), ["rsum"], ["rinv"])
                                dve(lambda: V.reciprocal(out=rinv[:, hh:hh + 1], in_=rinv[:, hh:hh + 1]), ["rinv"], ["rinv"])
                                if hh == 0:
                                    dve(lambda: V.tensor_scalar(out=imp[:], in0=pa_[:], scalar1=rinv[:, 0:1], scalar2=None, op0=ALU.mult), [pak_, "rinv"], ["imp"])
                                else:
                                    dve(lambda: V.scalar_tensor_tensor(out=imp[:], in0=pa_[:], scalar=rinv[:, hh:hh + 1], in1=imp[:], op0=ALU.mult, op1=ALU.add),
                                        [pak_, "rinv", "imp"], ["imp"])
                            dve(lambda: V.tensor_tensor(out=chk[:, 1:512], in0=imp[:, 1:512], in1=imp[:, 0:511], op=ALU.add), ["imp"], ["chk"])
                            dve(lambda: V.tensor_copy(out=chk[:, 0:1], in_=imp[:, 0:1]), ["imp", "chk"], ["chk"])
                            dve(lambda: V.tensor_reduce(out=sc2[:], in_=chk[:].rearrange("p (a b) -> p a b", b=4), axis=AX.X, op=ALU.add), ["chk"], ["sc2"])
                            dve(lambda: V.tensor_tensor(out=sc2[:], in0=sc2[:], in1=addb[c2][:], op=ALU.add), ["sc2", f"addb{c2}"], ["sc2"])
                            dve(lambda: V.max(out=m8a[:], in_=sc2[:]), ["sc2"], ["m8a"])
                            dve(lambda: V.match_replace(out=sc3[:], in_to_replace=m8a[:], in_values=sc2[:], imm_value=-3e9), ["sc2", "m8a"], ["sc3"])
                            dve(lambda: V.max(out=m8b[:], in_=sc3[:]), ["sc3"], ["m8b"])
                            dve(lambda: V.tensor_scalar(out=selb[:], in0=sc2[:], scalar1=m8b[:, 7:8], scalar2=NEG, op0=ALU.is_lt, op1=ALU.mult), ["sc2", "m8b"], ["selb"])
                            dve(lambda: V.tensor_tensor(out=selb[:], in0=selb[:], in1=futb[c2][:], op=ALU.min), ["selb", f"futb{c2}"], ["selb"])
                            dve(lambda: V.tensor_copy(out=selsw[:, 0:64], in_=selb[:, 64:128]), ["selb"], ["selsw"])
                            dve(lambda: V.tensor_copy(out=selsw[:, 64:128], in_=selb[:, 0:64]), ["selb", "selsw"], ["selsw"])
                            def branch(klist, gate_idx, first):
                                po, pok = nxt(PO, "o", "PO")
                                n = len(klist)
                                scores = []

                                def emit_score(i):
                                    kap, kkey, extra, vap, vkey = klist[i][:5]
                                    rhs_, rhsk_ = (klist[i][5], klist[i][6]) if len(klist[i]) > 5 else (Qg, "QT")
                                    pS, pSk = nxt(PS_S, "s", "PS_S")
                                    mm(pS[:], kap, rhs_, True, len(extra) == 0, kkey if isinstance(kkey, list) else [kkey, rhsk_], [pSk])
                                    for ei, (el, er, ek) in enumerate(extra):
                                        mm(pS[:], el, er, False, ei == len(extra) - 1, ek, [pSk])
                                    return pS, pSk

                                mm(po[:, 0:260], zerob[:], VsA[:, 0:2, :, :].rearrange("p a b c -> p (a b c)"), True, False, ["zerob", "VsA"], [pok])
                                pend = [emit_score(0)]
                                if n > 1:
                                    pend.append(emit_score(1))
                                for i in range(n):
                                    pS, pSk = pend.pop(0)
                                    if i + 2 < n:
                                        pend.append(emit_score(i + 2))
                                    pt_, ptk = nxt(PTb, "pt", "PTb")
                                    act(lambda: A.activation(out=pt_[:], in_=pS[:], func=AF.Exp, scale=0.125), [pSk], [ptk])
                                    vap, vkey = klist[i][3], klist[i][4]
                                    for hh in range(4):
                                        mm(po[:, hh * 65:(hh + 1) * 65], pt_[:, hh * 128:(hh + 1) * 128], vap, False, (i == n - 1 and hh == 3), [ptk, vkey], [pok])
                                pov = po[:, 0:260].rearrange("p (h e) -> p h e", h=4)
                                dve(lambda: V.tensor_scalar(out=coef[:], in0=pov[:, :, 64], scalar1=1e-30, scalar2=None, op0=ALU.max), [pok], ["coef"])
                                dve(lambda: V.reciprocal(out=coef[:], in_=coef[:]), ["coef"], ["coef"])
                                dve(lambda: V.tensor_tensor(out=coef[:], in0=coef[:], in1=gn[:, tl, 12 * g + gate_idx:12 * g + 12:3], op=ALU.mult), ["coef", "gn"], ["coef"])
                                oslice = oacc[:, g * 256:(g + 1) * 256].rearrange("p (h d) -> p h d", h=4)
                                cb_ = coef[:].unsqueeze(2).broadcast_to([128, 4, 64])
                                if first:
                                    dve(lambda: V.tensor_tensor(out=oslice, in0=pov[:, :, 0:64], in1=cb_, op=ALU.mult), [pok, "coef", "oacc"], ["oacc"])
                                else:
                                    dve(lambda: V.tensor_tensor(out=otmp[:].rearrange("p (h d) -> p h d", h=4), in0=pov[:, :, 0:64], in1=cb_, op=ALU.mult),
                                        [pok, "coef"], ["otmp"])
                                    dve(lambda: V.tensor_tensor(out=oslice, in0=oslice, in1=otmp[:].rearrange("p (h d) -> p h d", h=4), op=ALU.add),
                                        ["oacc", "otmp"], ["oacc"])

                            kl = []
                            for cc in range(4):
                                extra = []
                                if cc >= 2:
                                    extra = [(identb[:], cbB[c2][:, cc - 2, :], ["identb", f"cbB{c2}"])]
                                kl.append((CkT[r0:r0 + 64, cc * 128:(cc + 1) * 128], "CkT", extra, CvA[:, cc, g, :], "CvA"))
                            branch(kl, 0, True)
                            kl = []
                            for kt in range(qt - 4, qt + 1):
                                extra = []
                                if kt == qt:
                                    extra = [(identb[:], tri[:], ["identb", "tri"])]
                                elif kt == qt - 4:
                                    extra = [(identb[:], atri[:], ["identb", "atri"])]
                                kl.append((KwT[r0:r0 + 64, (kt - 44) * 128:(kt - 43) * 128], "KwT", extra, VwA[:, kt - 44, g, :], "VwA"))
                            branch(kl, 2, False)
                            tr(PX[:, 0:128], selb[:], identf[:], ["selb", "identf"], ["PX"], signal=False)
                            tr(PX[:, 128:256], selsw[:], identf[:], ["selsw", "identf"], ["PX"])
                            qa, qb = QaA[g], QaB[g]
                            qr = slice(0, 64) if g == 0 else slice(64, 128)
                            br_ = slice(64, 128) if g == 0 else slice(0, 64)
                            srcA = PX[br_, 128:256] if g == 0 else PX[br_, 0:128]
                            srcB = PX[br_, 0:128] if g == 0 else PX[br_, 128:256]
                            act(lambda: A.copy(out=qa[br_, :, :], in_=srcA.unsqueeze(1).broadcast_to([64, 4, 128])), ["PX"], [f"QaA{g}"])
                            act(lambda: A.copy(out=qb[br_, :, :], in_=srcB.unsqueeze(1).broadcast_to([64, 4, 128])), ["PX"], [f"QaB{g}"])
                            pool(lambda: G.tensor_copy(out=qa[qr, :, :], in_=QT[qr, tl, :, :]), ["QT", f"QaA{g}"], [f"QaA{g}"])
                            pool(lambda: G.tensor_copy(out=qb[qr, :, :], in_=QT[qr, tl, :, :]), ["QT", f"QaB{g}"], [f"QaB{g}"])

                            kl = []
                            for kt in range(qt + 1):
                                extra = []
                                if kt == qt:
                                    extra.append((identb[:], tri[:], ["identb", "tri"]))
                                qx, qxk = (QaA[g], f"QaA{g}") if kt < 32 else (QaB[g], f"QaB{g}")
                                kl.append((KA[g][:, kt * 128:(kt + 1) * 128], [f"KA{g}", f"KA{g}e", qxk], extra, VsA[:, kt, g, :], "VsA",
                                           qx[:].rearrange("p h q -> p (h q)"), qxk))
                            branch(kl, 1, False)
                        pos_ = [nxt(PO, "o", "PO"), nxt(PO, "o", "PO")]
                        for po, pok in pos_:
                            mm(po[:, 0:258], zerob[:], VsA[:, 0:2, :, :].rearrange("p a b c -> p (a b c)")[:, 0:258], True, False, ["zerob", "VsA"], [pok])
                        for mc in range(2):
                            pS, pSk = nxt(PS_S, "s", "PS_S")
                            for h in range(4):
                                mm(pS[:, h * 128:(h + 1) * 128], KmT[:, h, mc * 128:(mc + 1) * 128], qmT[:, tl, h, :], True, True, ["KmT", "qmT"], [pSk])
                            pt_, ptk = nxt(PTb, "pt", "PTb")
                            act(lambda: A.activation(out=pt_[:], in_=pS[:], func=AF.Exp, scale=128.0 ** -0.5), [pSk], [ptk])
                            for h in range(4):
                                po, pok = pos_[h // 2]
                                mm(po[:, (h % 2) * 129:(h % 2 + 1) * 129], pt_[:, h * 128:(h + 1) * 128], VmA[:, mc, h, :], False, (mc == 1 and h % 2 == 1), [ptk, "VmA"], [pok])
                        for hp in range(2):
                            po, pok = pos_[hp]
                            pov = po[:, 0:258].rearrange("p (h e) -> p h e", h=2)
                            dve(lambda: V.reciprocal(out=coef[:, 0:2], in_=pov[:, :, 128]), [pok], ["coef"])
                            dve(lambda: V.tensor_tensor(out=omacc[:, hp * 256:(hp + 1) * 256].rearrange("p (h d) -> p h d", h=2), in0=pov[:, :, 0:128],
                                                        in1=coef[:, 0:2].unsqueeze(2).broadcast_to([128, 2, 128]), op=ALU.mult), [pok, "coef"], ["omacc"])
                        dve(lambda: V.tensor_copy(out=oaccb[:], in_=oacc[:]), ["oacc"], ["oaccb"])
                        dve(lambda: V.tensor_copy(out=omb[:], in_=omacc[:]), ["omacc"], ["omb"])
                        for src_, sk_, dstT, dk_ in ((oaccb, "oaccb", oT, "oT"), (omb, "omb", omT, "omT")):
                            for kc in range(4):
                                tr(PT4[:, kc * 128:(kc + 1) * 128], src_[:, kc * 128:(kc + 1) * 128], identb[:], [sk_, "identb"], ["PT4"], signal=(kc == 3))
                            act(lambda: A.copy(out=dstT[:, :, tl * 128:(tl + 1) * 128], in_=PT4[:, 0:512].rearrange("p (a b) -> p a b", a=4)), ["PT4"], [dk_])

                    fw.barrier()
                if "p4o" in dbg and B == 0:
                    fw.dma("sp", ddbg("d_QT", [128, 2048], BF16)[:, :], QT[:].rearrange("p a b c -> p (a b c)"), reads=["QT"], writes=["d_QT"])
                    fw.dma("sp", ddbg("d_qmT", [128, 2048], BF16)[:, :], qmT[:].rearrange("p a b c -> p (a b c)"), reads=["qmT"], writes=["d_qmT"])
                    fw.dma("sp", ddbg("d_gn", [128, 96], F32)[:, :], gn[:].rearrange("p a b -> p (a b)"), reads=["gn"], writes=["d_gn"])
                    fw.dma("sp", ddbg("d_oT", [128, 4 * 512], BF16)[:, :], oT[:].rearrange("p a b -> p (a b)"), reads=["oT"], writes=["d_oT"])
                    fw.dma("sp", ddbg("d_omT", [128, 4 * 512], BF16)[:, :], omT[:].rearrange("p a b -> p (a b)"), reads=["omT"], writes=["d_omT"])

                with contextlib.ExitStack() as SC:
                    xq = [sb(SC, f"xq{i}", [128, 1024], F32) for i in range(2)]
                    h0t2 = [sb(SC, f"h0c{i}", [128, 1024], F32) for i in range(2)]
                    sgt = [sb(SC, f"sgt{i}", [128, 512], F32) for i in range(2)]
                    mrg2 = [sb(SC, f"mrg{i}", [128, 512], F32) for i in range(2)]
                    ytm2 = [sb(SC, f"ytm{i}", [128, 512], F32) for i in range(2)]
                    vres2 = [sb(SC, f"vres{i}", [128, 1024], F32) for i in range(2)]
                    wp = [sb(SC, f"wp{i}", [128, 8, 128], BF16) for i in range(6)]
                    for ft in range(8):
                        fs = slice(ft * 128, (ft + 1) * 128)
                        mrg = mrg2[ft % 2]
                        ytm = ytm2[ft % 2]
                        mk_ = f"mrg{ft % 2}"
                        yk_ = f"ytm{ft % 2}"

                        def proj(wsrc, nk, rhsT, rk):
                            w_, wk_ = wpiece(wsrc)
                            pf, pfk = nxt(PF, "f", "PF4_")
                            for kc in range(nk):
                                mm(pf[:], w_[:, kc, :], (rhsT[:, kc, B * 512:(B + 1) * 512] if rhsT is gyT else rhsT[:, kc, :]), kc == 0, kc == nk - 1, [wk_, rk], [pfk])
                            return pf, pfk

                        def gate(i):
                            pf, pfk = proj(wv[:, :, C_GM + i * 1024 + ft * 128:C_GM + i * 1024 + (ft + 1) * 128], 8, hTq, "hTq")
                            sg, sgk = nxt(sgt, "wp", "sgt")
                            act(lambda: A.activation(out=sg[:], in_=pf[:], func=AF.Sigmoid), [pfk], [sgk])
                            return sg, sgk

                        py, pyk = proj(wno_v[:, :, fs], 4, oT, "oT")
                        sg, sgk = gate(0)
                        dve(lambda: V.tensor_tensor(out=mrg[:], in0=py[:], in1=sg[:], op=ALU.mult), [pyk, sgk], [mk_])
                        pga, pgak = proj(wgl_v[:, :, fs], 4, gyT, "gyT")
                        pgb, pgbk = proj(wgl_v[:, :, 1024 + ft * 128:1024 + (ft + 1) * 128], 4, gyT, "gyT")
                        sg2, sg2k = nxt(sgt, "wp", "sgt")
                        act(lambda: A.activation(out=sg2[:], in_=pgb[:], func=AF.Sigmoid), [pgbk], [sg2k])
                        dve(lambda: V.tensor_tensor(out=ytm[:], in0=pga[:], in1=sg2[:], op=ALU.mult), [pgak, sg2k], [yk_])
                        sg, sgk = gate(1)
                        dve(lambda: V.tensor_tensor(out=ytm[:], in0=ytm[:], in1=sg[:], op=ALU.mult), [yk_, sgk], [yk_])
                        dve(lambda: V.tensor_tensor(out=mrg[:], in0=mrg[:], in1=ytm[:], op=ALU.add), [yk_, mk_], [mk_])
                        py, pyk = proj(wmo_v[:, :, fs], 4, omT, "omT")
                        sg, sgk = gate(2)
                        dve(lambda: V.tensor_tensor(out=ytm[:], in0=py[:], in1=sg[:], op=ALU.mult), [pyk, sgk], [yk_])
                        dve(lambda: V.tensor_tensor(out=mrgT[:, ft, :], in0=mrg[:], in1=ytm[:], op=ALU.add), [yk_, mk_], ["mrgT"])

                    if "p4o" in dbg and B == 0:
                        fw.dma("sp", ddbg("d_mrgT", [128, 4096], BF16)[:, :], mrgT[:].rearrange("p a b -> p (a b)"), reads=["mrgT"], writes=["d_mrgT"])
                    for tl in range(4):
                        j = B * 4 + tl
                        Tt = OWN0 + j
                        p_ = tl % 2
                        xt, xk = nxt(xq, "pt", "xq")
                        fw.dma("sp", xt[:], xr[Tt * 128:(Tt + 1) * 128, :], writes=[xk])
                        h0c, h0k = h0t2[p_], f"h0c{p_}"
                        vr, vrk = vres2[p_], f"vres{p_}"
                        ln_tok(xt, xk, h0c, h0k, grow, brow, "grow", "brow", par=p_)
                        for hf in range(2):
                            pf, pfk = nxt(PF, "f", "PF4_")
                            for ft in range(8):
                                mm(pf[:], mrgT[:, ft, tl * 128:(tl + 1) * 128], Wo[:, ft, hf * 512:(hf + 1) * 512], ft == 0, ft == 7, ["mrgT", "Wo"], [pfk])
                            dve(lambda: V.scalar_tensor_tensor(out=vr[:, hf * 512:(hf + 1) * 512], in0=h0c[:, hf * 512:(hf + 1) * 512], scalar=ALPHA,
                                                               in1=pf[:], op0=ALU.mult, op1=ALU.add), [h0k, pfk], [vrk])
                        ln_tok(vr, vrk, h0c, h0k, g1row, b1row, "g1row", "b1row", par=p_)
                        fw.dma("sp", h1scr[j * 128:(j + 1) * 128, :], h0c[:], reads=[h0k], writes=["h1scr"])
                    fw.barrier()
            if "p4" in dbg:
                fw.dma("sp", ddbg("d_h1", [2048, 1024], F32)[:, :], h1scr[:, :], reads=["h1scr"], writes=["d_h1"])
            fw.barrier()
        if upto <= 4:
            fw.finish("sp")
            print("instr", fw.ninstr, "waits", fw.nwait)
            return nc, dbg_out
        SKV.close()

        with contextlib.ExitStack() as S5:
            acc = sb(S5, "acc", [128, 16, 1024], F32)
            h1T = sb(S5, "h1T", [128, 8, 2048], BF16)
            gates = sb(S5, "gates", [128, 16, 32], F32)
            bguT = sb(S5, "bguT", [128, 512], F32)
            g2row = sb(S5, "g2row", [128, 1024], F32)
            b2row = sb(S5, "b2row", [128, 1024], F32)
            st5 = sb(S5, "st5", [128, 12], F32)
            mv5 = sb(S5, "mv5", [128, 2], F32)
            rs5 = sb(S5, "rs5", [128, 1], F32)
            fw.dma("sp", g2row[:], ln2_d[0:1, :].partition_broadcast(128), writes=["g2row"])
            fw.dma("sp", b2row[:], ln2_d[1:2, :].partition_broadcast(128), writes=["b2row"])
            with contextlib.ExitStack() as S5i:
                Wr = sb(S5i, "Wr", [128, 8, 32], F32)
                brr = sb(S5i, "brr", [128, 32], F32)
                bdn = sb(S5i, "bdn", [32, 1024], F32)
                bgn = sb(S5i, "bgn", [128, 4, 128], F32)
                fw.dma("sp", Wr[:], w_router.rearrange("(dc p) e -> p dc e", p=128), writes=["Wr"])
                fw.dma("sp", brr[:], b_router[0:1, :].partition_broadcast(128), writes=["brr"])
                fw.dma("sp", bdn[:], b_down[:, :], writes=["bdn"])
                fw.dma("sp", bgn[:], b_gate_up.rearrange("(a p) c -> p a c", p=128), writes=["bgn"])
                h1t = [sb(S5i, f"h1t{i}", [128, 1024], F32) for i in range(2)]
                h1b = sb(S5i, "h1b", [128, 1024], BF16)
                h1Tf = sb(S5i, "h1Tf", [128, 8, 128], F32)
                lg = sb(S5i, "lg", [128, 32], F32)
                ex = sb(S5i, "ex", [128, 32], F32)
                msk = sb(S5i, "msk", [128, 32], F32)
                m85 = sb(S5i, "m85", [128, 8], F32)
                nmx = sb(S5i, "nmx", [128, 1], F32)
                ssm = sb(S5i, "ssm", [128, 1], F32)
                gT = sb(S5i, "gT", [32, 128], F32)
                PT5 = ps(S5i, "PT5", [128, 1024], BF16)
                PTf = [ps(S5i, f"PTf{i}", [128, 512], F32) for i in range(2)]
                PLg = ps(S5i, "PLg", [128, 128], F32)
                PBd = [ps(S5i, f"PBd{i}", [128, 512], F32) for i in range(2)]
                for a in range(4):
                    tr(PTf[a % 2][:, 0:128], bgn[:, a, :], identf[:], ["bgn", "identf"], [f"PTf{a % 2}"])
                    act(lambda: A.copy(out=bguT[:, a * 128:(a + 1) * 128], in_=PTf[a % 2][:, 0:128]), [f"PTf{a % 2}"], ["bguT"])
                for j in range(16):
                    ht = h1t[j % 2]
                    hk_ = f"h1t{j % 2}"
                    fw.dma("sp", ht[:], h1scr[j * 128:(j + 1) * 128, :], reads=["h1scr"], writes=[hk_])
                    act(lambda: A.mul(out=acc[:, j, :], in_=ht[:], mul=ALPHA), [hk_], [f"acc{j}"])
                    dve(lambda: V.tensor_copy(out=h1b[:], in_=ht[:]), [hk_], ["h1b"])
                    for dc in range(8):
                        tr(PT5[:, dc * 128:(dc + 1) * 128], h1b[:, dc * 128:(dc + 1) * 128], identb[:], ["h1b", "identb"], ["PT5"], signal=(dc == 7))
                    act(lambda: A.copy(out=h1T[:, :, j * 128:(j + 1) * 128], in_=PT5[:].rearrange("p (a b) -> p a b", a=8)), ["PT5"], ["h1T"])
                    for hf in range(2):
                        for d4 in range(4):
                            dc = hf * 4 + d4
                            tr(PTf[hf][:, d4 * 128:(d4 + 1) * 128], ht[:, dc * 128:(dc + 1) * 128], identf[:], [hk_, "identf"], [f"PTf{hf}"], signal=(d4 == 3))
                        dve(lambda: V.tensor_copy(out=h1Tf[:, hf * 4:(hf + 1) * 4, :], in_=PTf[hf][:].rearrange("p (a b) -> p a b", a=4)), [f"PTf{hf}"], ["h1Tf"])
                    for dc in range(8):
                        mm(PLg[:, 0:32], h1Tf[:, dc, :], Wr[:, dc, :], dc == 0, dc == 7, ["h1Tf", "Wr"], ["PLg"])
                    dve(lambda: V.tensor_tensor(out=lg[:], in0=PLg[:, 0:32], in1=brr[:], op=ALU.add), ["PLg", "brr"], ["lg"])
                    dve(lambda: V.max(out=m85[:], in_=lg[:]), ["lg"], ["m85"])
                    dve(lambda: V.tensor_scalar(out=nmx[:], in0=m85[:, 0:1], scalar1=-1.0, scalar2=None, op0=ALU.mult), ["m85"], ["nmx"])
                    act(lambda: A.activation(out=ex[:], in_=lg[:], func=AF.Exp, bias=nmx[:, 0:1], scale=1.0), ["lg", "nmx"], ["ex"])
                    dve(lambda: V.tensor_scalar(out=msk[:], in0=lg[:], scalar1=m85[:, 3:4], scalar2=None, op0=ALU.is_ge), ["lg", "m85"], ["msk"])
                    dve(lambda: V.tensor_tensor(out=ex[:], in0=ex[:], in1=msk[:], op=ALU.mult), ["ex", "msk"], ["ex"])
                    dve(lambda: V.reduce_sum(out=ssm[:], in_=ex[:], axis=AX.X), ["ex"], ["ssm"])
                    dve(lambda: V.reciprocal(out=ssm[:], in_=ssm[:]), ["ssm"], ["ssm"])
                    dve(lambda: V.tensor_scalar(out=gates[:, j, :], in0=ex[:], scalar1=ssm[:, 0:1], scalar2=None, op0=ALU.mult), ["ex", "ssm"], ["gates"])
                    tr(PLg[0:32, :], gates[:, j, :], identf[:], ["gates", "identf"], ["PLg"])
                    act(lambda: A.copy(out=gT[:], in_=PLg[0:32, :]), ["PLg"], ["gT"])
                    for hf in range(2):
                        mm(PBd[hf][:], gT[:], bdn[:, hf * 512:(hf + 1) * 512], True, True, ["gT", "bdn"], [f"PBd{hf}"])
                        dve(lambda: V.tensor_tensor(out=acc[:, j, hf * 512:(hf + 1) * 512], in0=PBd[hf][:], in1=acc[:, j, hf * 512:(hf + 1) * 512], op=ALU.add),
                            [f"PBd{hf}", f"acc{j}"], [f"acc{j}"])
                if "p5g" in dbg:
                    fw.dma("sp", ddbg("d_gates", [128, 512], F32)[:, :], gates[:].rearrange("p a b -> p (a b)"), reads=["gates"], writes=["d_gates"])
                fw.barrier()
            actT = sb(S5, "actT", [128, 8, 2048], BF16)
            Wgu = [sb(S5, f"Wgu{i}", [128, 8, 2, 256], BF16) for i in range(2)]
            Wd = [sb(S5, f"Wd{i}", [128, 8, 512], BF16) for i in range(2)]
            gs = [sb(S5, f"gs{i}", [128, 512], F32) for i in range(3)]
            sg = [sb(S5, f"sg{i}", [128, 512], F32) for i in range(3)]
            ls = [sb(S5, f"ls{i}", [128, 512], F32) for i in range(3)]
            PG = [ps(S5, f"PG{i}", [128, 512], F32) for i in range(3)]
            PL = [ps(S5, f"PL{i}", [128, 512], F32) for i in range(3)]
            PD = [ps(S5, f"PD{i}", [128, 512], F32) for i in range(2)]
            ui = 0
            di = 0
            NE = 32 if "moe_e" not in dbg else 2
            NU = 3

            def load_wgu(e, pg):
                par = (e * 4 + pg) % 2
                wgu_v = w_gate_up[e].rearrange("(dc p) c -> p dc c", p=128)
                for gl_ in range(2):
                    c0 = gl_ * 1024 + pg * 256
                    for d0 in range(0, 8, 4):
                        fw.dma("pool", Wgu[par][:, d0:d0 + 4, gl_, :], wgu_v[:, d0:d0 + 4, c0:c0 + 256], writes=[f"Wgu{par}_{gl_}_{d0 // 4}"])

            def load_wd(e, h2):
                wd_v = w_down[e].rearrange("(fc p) c -> p fc c", p=128)
                for d0 in range(0, 8, 4):
                    fw.dma("pool", Wd[h2][:, d0:d0 + 4, :], wd_v[:, d0:d0 + 4, h2 * 512:(h2 + 1) * 512], writes=[f"Wd{h2}_{d0 // 4}"])

            load_wgu(0, 0)
            load_wd(0, 0)
            load_wd(0, 1)
            for e in range(NE):
                for pg in range(4):
                    par = (e * 4 + pg) % 2
                    wb = Wgu[par]
                    if pg < 3:
                        load_wgu(e, pg + 1)
                    elif e + 1 < NE:
                        load_wgu(e + 1, 0)
                    for pi in range(2):
                        i = pg * 2 + pi
                        bg_ = bguT[:, e * 16 + i:e * 16 + i + 1]
                        bl_ = bguT[:, e * 16 + 8 + i:e * 16 + 8 + i + 1]
                        for tb in range(4):
                            u2 = ui % NU
                            ui += 1
                            for dc in range(8):
                                mm(PG[u2][:], wb[:, dc, 0, pi * 128:(pi + 1) * 128], h1T[:, dc, tb * 512:(tb + 1) * 512], dc == 0, dc == 7,
                                   [f"Wgu{par}_0_{dc // 4}", "h1T"], [f"PG{u2}"])
                            for dc in range(8):
                                mm(PL[u2][:], wb[:, dc, 1, pi * 128:(pi + 1) * 128], h1T[:, dc, tb * 512:(tb + 1) * 512], dc == 0, dc == 7,
                                   [f"Wgu{par}_1_{dc // 4}", "h1T"], [f"PL{u2}"])
                            dve(lambda: V.tensor_scalar(out=gs[u2][:], in0=PG[u2][:], scalar1=bg_, scalar2=7.0, op0=ALU.add, op1=ALU.min), [f"PG{u2}", "bguT"], [f"gs{u2}"])
                            act(lambda: A.activation(out=sg[u2][:], in_=gs[u2][:], func=AF.Sigmoid, scale=1.702), [f"gs{u2}"], [f"sg{u2}"])
                            dve(lambda: V.tensor_scalar(out=ls[u2][:], in0=PL[u2][:], scalar1=bl_, scalar2=7.0, op0=ALU.add, op1=ALU.min), [f"PL{u2}", "bguT"], [f"ls{u2}"])
                            dve(lambda: V.tensor_scalar(out=ls[u2][:], in0=ls[u2][:], scalar1=-7.0, scalar2=1.0, op0=ALU.max, op1=ALU.add), [f"ls{u2}"], [f"ls{u2}"])
                            pool(lambda: G.tensor_tensor(out=gs[u2][:], in0=gs[u2][:], in1=sg[u2][:], op=ALU.mult), [f"gs{u2}", f"sg{u2}"], [f"gs{u2}"])
                            pool(lambda: G.tensor_tensor(out=actT[:, i, tb * 512:(tb + 1) * 512], in0=gs[u2][:], in1=ls[u2][:], op=ALU.mult),
                                 [f"gs{u2}", f"ls{u2}"], [f"actT{tb}"])
                for h2 in range(2):
                    wdb = Wd[h2]
                    for j in range(16):
                        d2 = di % 2
                        di += 1
                        for fc in range(8):
                            mm(PD[d2][:], actT[:, fc, j * 128:(j + 1) * 128], wdb[:, fc, :], fc == 0, fc == 7, [f"actT{j // 4}", f"Wd{h2}_{fc // 4}"], [f"PD{d2}"])
                        dve(lambda: V.scalar_tensor_tensor(out=acc[:, j, h2 * 512:(h2 + 1) * 512], in0=PD[d2][:], scalar=gates[:, j, e:e + 1],
                                                           in1=acc[:, j, h2 * 512:(h2 + 1) * 512], op0=ALU.mult, op1=ALU.add),
                            [f"PD{d2}", "gates", f"acc{j}"], [f"acc{j}"])
                    if e + 1 < NE:
                        load_wd(e + 1, h2)
            ot = [sb(S5, f"ot{i}", [128, 1024], F32) for i in range(2)]
            for j in range(16):
                o_ = ot[j % 2]
                ok_ = f"ot{j % 2}"
                xt = acc[:, j, :]
                xk = f"acc{j}"
                dve(lambda: V.bn_stats(out=st5[:, 0:6], in_=xt[:, 0:512]), [xk], ["st5"])
                dve(lambda: V.bn_stats(out=st5[:, 6:12], in_=xt[:, 512:1024]), [xk], ["st5"])
                dve(lambda: V.bn_aggr(out=mv5[:, 0:2], in_=st5[:, 0:12]), ["st5"], ["mv5"])
                act(lambda: A.activation(out=rs5[:], in_=mv5[:, 1:2], func=AF.Sqrt, bias=LN_EPS, scale=1.0), ["mv5"], ["rs5"])
                dve(lambda: V.reciprocal(out=rs5[:], in_=rs5[:]), ["rs5"], ["rs5"])
                dve(lambda: V.tensor_scalar(out=o_[:], in0=xt, scalar1=mv5[:, 0:1], scalar2=rs5[:, 0:1], op0=ALU.subtract, op1=ALU.mult),
                    [xk, "mv5", "rs5"], [ok_])
                pool(lambda: G.tensor_tensor(out=o_[:], in0=o_[:], in1=g2row[:], op=ALU.mult), [ok_, "g2row"], [ok_])
                pool(lambda: G.tensor_tensor(out=o_[:], in0=o_[:], in1=b2row[:], op=ALU.add), [ok_, "b2row"], [ok_])
                fw.dma("sp", out_d[j * 128:(j + 1) * 128, :], o_[:], reads=[ok_], writes=[f"out{j}"])
            fw.finish("sp")
            fw.barrier()
        print("instr", fw.ninstr, "waits", fw.nwait)
        return nc, dbg_out

        fw.finish("sp")
    return nc, dbg_out


def host_prep(inputs):
    x = np.asarray(inputs["x"], np.float32)
    pos = np.asarray(inputs["positions"], np.int32)
    maps = []
    inv = (np.float32(500000.0) ** (-(np.arange(0, 16, 2, dtype=np.float32)) / np.float32(16))).astype(np.float32)
    invf = np.zeros((128, 1), np.float32)
    for base in (0, 64):
        invf[base:base + 8, 0] = inv
        invf[base + 8:base + 16, 0] = inv
    identf = np.eye(128, dtype=np.float32)
    ln_emb = np.stack([inputs["ln_emb_g"], inputs["ln_emb_b"]]).astype(np.float32)
    w_in = np.ascontiguousarray(np.asarray(inputs["w_in"], np.float32)[0])
    g0 = lambda n: np.ascontiguousarray(np.asarray(inputs[n], np.float32)[0])
    sgn = np.ones((128, 1), np.float32); sgn[64:] = -1.0
    psw = np.zeros((128, 128), np.float32)
    psw[np.arange(128), (np.arange(128) + 64) % 128] = 1.0
    shared = {
        "w_kcmp1": g0("w_kcmp1"), "w_kcmp2": g0("w_kcmp2"), "w_vcmp1": g0("w_vcmp1"), "w_vcmp2": g0("w_vcmp2"),
        "pe_k": g0("pe_k_cmp"), "pe_v": g0("pe_v_cmp"),
        "s5_a_re": g0("s5_a_re"), "s5_a_im": g0("s5_a_im"), "s5_log_dt": np.asarray(inputs["s5_log_dt"], np.float32).reshape(1, 32),
        "s5_b_re": g0("s5_b_re"), "s5_b_im": g0("s5_b_im"), "s5_c_re": g0("s5_c_re"), "s5_c_im": g0("s5_c_im"), "s5_d": g0("s5_d"),
        "sgn": sgn, "psw": psw,
        "w_mem_kv": g0("w_mem_kv"), "w_nsa_out": g0("w_nsa_out"), "w_mem_out": g0("w_mem_out"), "w_s5_glu": g0("w_s5_glu"),
        "w_o": g0("w_o"), "ln1": np.stack([g0("ln1_g"), g0("ln1_b")]),
        "ln2": np.stack([g0("ln2_g"), g0("ln2_b")]), "w_router": g0("w_router"), "b_router": g0("b_router").reshape(1, 32),
        "w_gate_up": g0("w_gate_up"), "b_gate_up": g0("b_gate_up").reshape(512, 128), "w_down": g0("w_down"), "b_down": g0("b_down"),
    }
    kk = np.arange(8192)
    shared["epat"] = (((kk[None, :] // 64) % 64) == np.arange(64)[:, None]).astype(np.float32)
    ii = np.arange(128)
    tri = np.where(ii[:, None] <= ii[None, :], 0.0, NEG).astype(np.float32)
    atri = np.where(ii[:, None] > ii[None, :], 0.0, NEG).astype(np.float32)
    shared["tri"] = np.tile(tri, (1, 4))
    shared["atri"] = np.tile(atri, (1, 4))
    for k in range(8):
        b, r = k // 4, k % 4
        pad = 2048 * (3 - r)
        xr = np.zeros((8192, 1024), np.float32)
        xr[pad:] = x[b, :8192 - pad]
        posr = np.zeros((1, 8192), np.int32)
        posr[0, pad:] = pos[b, :8192 - pad]
        valid = (np.arange(8192) >= pad).astype(np.float32)
        cidx = np.arange(512)
        vc = ((cidx <= 510) & (16 * cidx >= pad)).astype(np.float32)
        rho = 6144 + np.arange(2048)
        cend = 16 * cidx + 31
        okc = (vc[None, :] > 0) & (cend[None, :] <= rho[:, None])
        cba = np.where(okc, 0.0, NEG).astype(np.float32).reshape(16, 128, 512)
        cbb = np.where(okc, 0.0, NEG).astype(np.float32).reshape(16, 128, 512)[:, :, 256:512]
        cbb = cbb.reshape(16, 128, 2, 128).transpose(0, 2, 3, 1)
        cbb = np.ascontiguousarray(np.tile(cbb, (1, 1, 1, 4)))
        blk = np.arange(128)
        tb = rho // 64
        blk0 = pad // 64
        forced = (blk[None, :] == tb[:, None]) | (blk[None, :] == tb[:, None] - 1) | (blk[None, :] == blk0)
        invalid = (blk[None, :] > tb[:, None]) | (blk[None, :] < blk0)
        addb = (forced * 1e4 + invalid * (-1e9)).astype(np.float32).reshape(16, 128, 128)
        futb = np.where(invalid, NEG, 0.0).astype(np.float32).reshape(16, 128, 128)
        m = {
            "cba": cba, "cbb": cbb, "addb": addb, "futb": futb, "mem": np.ascontiguousarray(np.asarray(inputs["mem"], np.float32)[b]),
            "vcc": np.ascontiguousarray(vc.reshape(4, 128).T),
            "xr": xr, "posr": posr, "vrow": valid[None, :].copy(),
            "vcol": np.ascontiguousarray(valid.reshape(64, 128).T),
            "ln_emb": ln_emb, "w_in": w_in, "invf": invf, "identf": identf,
        }
        m.update(shared)
        maps.append(m)
    return maps


def kernel(**inputs):
    nc, _ = build_program()
    maps = host_prep(inputs)
    res = run_bass_kernel_spmd(nc, maps, core_ids=list(range(8)))
    out = np.zeros((2, 8192, 1024), np.float32)
    for k in range(8):
        b, r = k // 4, k % 4
        out[b, 2048 * r:2048 * (r + 1)] = res.results[k]["out"]
    return out
```
